# Optimizing a Trainium2 kernel written in Bass

```python
import math
import jax, jax.numpy as jnp
from jax import lax
import numpy as np

D_MODEL = 1024
BATCH = 8
SEQ = 2048
DEPTH = 2
DEC_BATCH = 128
DEC_SEQ = 4
PAST_LEN = 2048
PAGE_SIZE = 128

N_HEADS_A = 4
HEAD_DIM_A = D_MODEL // 8
RET_CHUNK = 128
N_HEADS_B = 4
HEAD_DIM_B = D_MODEL // 8
MOBA_BLOCK = 256
MOBA_TOPK = 3
MOBA_QCHUNK = 64
ROPE_THETA = 10000.0
GROUP_W = N_HEADS_A * HEAD_DIM_A
N_IN_COLS = 7
CONV_W = 31
D_CONF = D_MODEL
D_FF = 11 * D_MODEL // 4
FFN_CONV_W = 3
N_EVEN = (DEPTH + 1) // 2
N_ODD = DEPTH // 2
ALPHA = (2 * DEPTH) ** 0.25
BETA = (8 * DEPTH) ** -0.25
LN_EPS = 1e-5
GN_EPS = 1e-6

kernel_name = 'retnet_moba_conformer_convffn_decode_step'


def layer_norm(x, g, b):
    xf = x.astype(jnp.float32)
    mu = jnp.mean(xf, -1, keepdims=True)
    var = jnp.mean(jnp.square(xf - mu), -1, keepdims=True)
    y = (xf - mu) * lax.rsqrt(var + LN_EPS) * g.astype(jnp.float32) + b.astype(jnp.float32)
    return y.astype(x.dtype)


def adaln(c, w, b):
    return (jax.nn.silu(c) @ w + b).reshape(c.shape[0], 6, D_MODEL)


def modulate(x, shift, scale):
    return x * (1.0 + scale[:, None, :]) + shift[:, None, :]


def post_norm(x, y, gate, g, b):
    return layer_norm(ALPHA * x + (1.0 + gate[:, None, :]) * y, g, b)


def rope_rotate_half(x, pos):
    d = x.shape[-1]
    inv = ROPE_THETA ** (-jnp.arange(0, d, 2, dtype=jnp.float32) / d)
    ang = pos.astype(jnp.float32)[:, None] * inv[None, :]
    cos = jnp.cos(ang)[None, :, None, :]
    sin = jnp.sin(ang)[None, :, None, :]
    xf = x.astype(jnp.float32)
    x1, x2 = xf[..., : d // 2], xf[..., d // 2:]
    return jnp.concatenate([x1 * cos - x2 * sin, x2 * cos + x1 * sin], -1).astype(x.dtype)


def retnet_rotate(x, pos):
    d = x.shape[-1]
    inv = 1.0 / (ROPE_THETA ** jnp.linspace(0.0, 1.0, d // 2, dtype=jnp.float32))
    ang = pos.astype(jnp.float32)[:, None] * inv[None, :]
    cos = jnp.cos(ang)[None, :, None, :]
    sin = jnp.sin(ang)[None, :, None, :]
    xf = x.astype(jnp.float32).reshape(x.shape[:-1] + (d // 2, 2))
    x0, x1 = xf[..., 0], xf[..., 1]
    out = jnp.stack([x0 * cos - x1 * sin, x1 * cos + x0 * sin], -1).reshape(x.shape)
    return out.astype(x.dtype)


def chunk_retention(q, k, v, s0, chunk):
    B, H, T, dk = q.shape
    dv = v.shape[-1]
    n = T // chunk
    log_g = jnp.log1p(-jnp.exp2(-5.0 - jnp.arange(H, dtype=jnp.float32)))
    idx = jnp.arange(chunk, dtype=jnp.float32)
    diff = idx[:, None] - idx[None, :]
    decay_in = jnp.where(diff[None] >= 0, jnp.exp(jnp.maximum(diff, 0.0)[None] * log_g[:, None, None]), 0.0)
    q_dec = jnp.exp((idx + 1.0)[None, :] * log_g[:, None])
    k_dec = jnp.exp((chunk - 1.0 - idx)[None, :] * log_g[:, None])
    c_dec = jnp.exp(chunk * log_g)

    def step(s, inp):
        qc, kc, vc = inp
        att = jnp.einsum('bhtd,bhsd->bhts', qc, kc) * decay_in
        o = jnp.einsum('bhts,bhsv->bhtv', att, vc) + jnp.einsum('bhtd,bhdv->bhtv', qc * q_dec[:, :, None], s)
        s = c_dec[:, None, None] * s + jnp.einsum('bhsd,bhsv->bhdv', kc * k_dec[:, :, None], vc)
        return s, o

    split = lambda t: t.reshape(B, H, n, chunk, t.shape[-1]).transpose(2, 0, 1, 3, 4)
    s_final, o = lax.scan(step, s0, (split(q), split(k), split(v)))
    o = o.transpose(1, 2, 0, 3, 4).reshape(B, H, T, dv)
    return o, s_final


def moba_attend(q, k_all, v_all, q_pos, q_chunk):
    B, Tq, H, Dh = q.shape
    L = k_all.shape[1]
    nblk = -(-L // MOBA_BLOCK)
    pad = nblk * MOBA_BLOCK - L
    to_blocks = lambda t: jnp.pad(t, ((0, 0), (0, pad), (0, 0), (0, 0))).reshape(B, nblk, MOBA_BLOCK, H, Dh).transpose(0, 3, 1, 2, 4)
    kb = to_blocks(k_all)
    vb = to_blocks(v_all)
    kmean = jnp.mean(kb.astype(jnp.float32), axis=3)
    n_top = min(MOBA_TOPK, nblk)
    bi = jnp.arange(B)[:, None, None, None]
    hi = jnp.arange(H)[None, :, None, None]
    blk_ids = jnp.arange(nblk)
    scale = Dh ** -0.5

    def attend(args):
        qc, pc = args
        qcn = qc.shape[1]
        own = pc // MOBA_BLOCK
        gate = jnp.einsum('bthd,bhnd->bhtn', qc.astype(jnp.float32), kmean)
        gate = jnp.where(blk_ids[None, None, None, :] < own[None, None, :, None], gate, -jnp.inf)
        _, top = lax.top_k(gate, n_top)
        sel = jnp.concatenate([top.astype(jnp.int32), jnp.broadcast_to(own[None, None, :, None], (B, H, qcn, 1)).astype(jnp.int32)], -1)
        top_ok = jnp.broadcast_to((jnp.arange(n_top)[None, :] < own[:, None])[None, None], (B, H, qcn, n_top))
        sel_ok = jnp.concatenate([top_ok, jnp.ones((B, H, qcn, 1), bool)], -1)
        kg = kb[bi, hi, sel]
        vg = vb[bi, hi, sel]
        kpos = sel[..., None] * MOBA_BLOCK + jnp.arange(MOBA_BLOCK, dtype=jnp.int32)
        mask = sel_ok[..., None] & (kpos <= pc[None, None, :, None, None])
        logits = jnp.einsum('bthd,bhtskd->bhtsk', qc, kg, preferred_element_type=jnp.float32) * scale
        logits = jnp.where(mask, logits, -jnp.inf)
        p = jax.nn.softmax(logits.reshape(B, H, qcn, -1), axis=-1).reshape(logits.shape)
        o = jnp.einsum('bhtsk,bhtskd->bthd', p.astype(vg.dtype), vg, preferred_element_type=jnp.float32)
        return o.astype(q.dtype)

    nq = Tq // q_chunk
    qs = q.reshape(B, nq, q_chunk, H, Dh).transpose(1, 0, 2, 3, 4)
    ps = q_pos.reshape(nq, q_chunk)
    out = lax.map(attend, (qs, ps))
    return out.transpose(1, 0, 2, 3, 4).reshape(B, Tq, H, Dh)


def ab_mixer(h, pos, k_past, v_past, ret_state, w_in, w_out, q_chunk):
    B, T, _ = h.shape
    z = h @ w_in
    rq, rk, rv, rg, mq, mk, mv = jnp.split(z, N_IN_COLS, axis=-1)
    rq = retnet_rotate(rq.reshape(B, T, N_HEADS_A, HEAD_DIM_A), pos)
    rk = retnet_rotate(rk.reshape(B, T, N_HEADS_A, HEAD_DIM_A), pos) * (HEAD_DIM_A ** -0.5)
    rv = rv.reshape(B, T, N_HEADS_A, HEAD_DIM_A)
    to_bhtd = lambda t: t.astype(jnp.float32).transpose(0, 2, 1, 3)
    o_r, s_new = chunk_retention(to_bhtd(rq), to_bhtd(rk), to_bhtd(rv), ret_state.astype(jnp.float32), min(RET_CHUNK, T))
    o_r = o_r.transpose(0, 2, 1, 3)
    o_r = o_r * lax.rsqrt(jnp.mean(jnp.square(o_r), -1, keepdims=True) + GN_EPS)
    o_r = o_r.reshape(B, T, GROUP_W).astype(h.dtype) * jax.nn.silu(rg)
    mq = rope_rotate_half(mq.reshape(B, T, N_HEADS_B, HEAD_DIM_B), pos)
    mk = rope_rotate_half(mk.reshape(B, T, N_HEADS_B, HEAD_DIM_B), pos)
    mv = mv.reshape(B, T, N_HEADS_B, HEAD_DIM_B)
    if k_past is None:
        k_all, v_all = mk, mv
    else:
        k_all = jnp.concatenate([k_past.astype(mk.dtype), mk], axis=1)
        v_all = jnp.concatenate([v_past.astype(mv.dtype), mv], axis=1)
    o_m = moba_attend(mq, k_all, v_all, pos, q_chunk).reshape(B, T, GROUP_W)
    y = jnp.concatenate([o_r, o_m], axis=-1) @ w_out
    return y, mk, mv, s_new.astype(h.dtype)


def causal_dwconv(x, prev, w, b):
    width = w.shape[0]
    xp = jnp.concatenate([prev.astype(x.dtype), x], axis=1)
    y = lax.conv_general_dilated(xp, w[:, None, :].astype(x.dtype), window_strides=(1,), padding='VALID',
                                 dimension_numbers=('NWC', 'WIO', 'NWC'), feature_group_count=x.shape[-1])
    return y + b, xp[:, xp.shape[1] - (width - 1):]


def conformer_conv(h, prev, w1, b1, w_dw, b_dw, g_ln, b_ln, w2, b2):
    a, g = jnp.split(h @ w1 + b1, 2, axis=-1)
    glu = a * jax.nn.sigmoid(g)
    y, new_prev = causal_dwconv(glu, prev, w_dw, b_dw)
    y = jax.nn.silu(layer_norm(y, g_ln, b_ln))
    return y @ w2 + b2, new_prev


def conv_ffn(h, prev, w_up, w_dw, b_dw, w_down):
    u, v = jnp.split(h @ w_up, 2, axis=-1)
    uc, new_prev = causal_dwconv(u, prev, w_dw, b_dw)
    return (jax.nn.gelu(uc, approximate=False) * v) @ w_down, new_prev


def setup_inputs(seed: int = 0) -> dict:
    key = jax.random.key(seed)
    ks = jax.random.split(key, 32)
    n_pages = PAST_LEN // PAGE_SIZE
    n_used = DEC_BATCH * n_pages
    n_phys = n_used + n_used // 4
    nrm = lambda k, shape, s: jax.random.normal(k, shape, jnp.float32) * s
    page_table = jax.random.permutation(ks[7], n_phys)[:n_used].reshape(DEC_BATCH, n_pages).astype(jnp.int32)
    return {
        'x_prompt': nrm(ks[0], (BATCH, SEQ, D_MODEL), 1.0),
        'x_sample': nrm(ks[1], (DEC_BATCH, DEC_SEQ, D_MODEL), 1.0),
        'cache_k': nrm(ks[2], (N_EVEN, n_phys, PAGE_SIZE, N_HEADS_B, HEAD_DIM_B), 1.0),
        'cache_v': nrm(ks[3], (N_EVEN, n_phys, PAGE_SIZE, N_HEADS_B, HEAD_DIM_B), 1.0),
        'state_ret': nrm(ks[4], (N_EVEN, DEC_BATCH, N_HEADS_A, HEAD_DIM_A, HEAD_DIM_A), 0.5),
        'state_conv': nrm(ks[5], (N_ODD, DEC_BATCH, CONV_W - 1, D_CONF), 0.5),
        'state_ffn': nrm(ks[6], (DEPTH, DEC_BATCH, FFN_CONV_W - 1, D_FF), 1.0),
        'page_table': page_table,
        'c_prompt': nrm(ks[8], (BATCH, D_MODEL), 1.0),
        'c_sample': nrm(ks[9], (DEC_BATCH, D_MODEL), 1.0),
        'ab_w_in': nrm(ks[10], (N_EVEN, D_MODEL, N_IN_COLS * GROUP_W), D_MODEL ** -0.5),
        'ab_w_out': nrm(ks[11], (N_EVEN, 2 * GROUP_W, D_MODEL), BETA * (2 * GROUP_W) ** -0.5),
        'cf_w_pw1': nrm(ks[12], (N_ODD, D_MODEL, 2 * D_CONF), D_MODEL ** -0.5),
        'cf_b_pw1': nrm(ks[13], (N_ODD, 2 * D_CONF), 0.01),
        'cf_w_dw': nrm(ks[14], (N_ODD, CONV_W, D_CONF), CONV_W ** -0.5),
        'cf_b_dw': nrm(ks[15], (N_ODD, D_CONF), 0.01),
        'cf_ln_g': 1.0 + nrm(ks[16], (N_ODD, D_CONF), 0.01),
        'cf_ln_b': nrm(ks[17], (N_ODD, D_CONF), 0.01),
        'cf_w_pw2': nrm(ks[18], (N_ODD, D_CONF, D_MODEL), BETA * D_CONF ** -0.5),
        'cf_b_pw2': nrm(ks[19], (N_ODD, D_MODEL), 0.01),
        'ffn_w_up': nrm(ks[20], (DEPTH, D_MODEL, 2 * D_FF), D_MODEL ** -0.5),
        'ffn_w_dw': nrm(ks[21], (DEPTH, FFN_CONV_W, D_FF), FFN_CONV_W ** -0.5),
        'ffn_b_dw': nrm(ks[22], (DEPTH, D_FF), 0.01),
        'ffn_w_down': nrm(ks[23], (DEPTH, D_FF, D_MODEL), BETA * D_FF ** -0.5),
        'ada_w': nrm(ks[24], (DEPTH, D_MODEL, 6 * D_MODEL), 0.1 * D_MODEL ** -0.5),
        'ada_b': nrm(ks[25], (DEPTH, 6 * D_MODEL), 0.01),
        'ln_g': 1.0 + nrm(ks[26], (DEPTH, 2, D_MODEL), 0.01),
        'ln_b': nrm(ks[27], (DEPTH, 2, D_MODEL), 0.01),
    }


def reference(x_prompt, x_sample, cache_k, cache_v, state_ret, state_conv, state_ffn, page_table, c_prompt, c_sample,
              ab_w_in, ab_w_out, cf_w_pw1, cf_b_pw1, cf_w_dw, cf_b_dw, cf_ln_g, cf_ln_b, cf_w_pw2, cf_b_pw2,
              ffn_w_up, ffn_w_dw, ffn_b_dw, ffn_w_down, ada_w, ada_b, ln_g, ln_b):
    n_pages = PAST_LEN // PAGE_SIZE
    pos_p = jnp.arange(SEQ, dtype=jnp.int32)
    pos_s = PAST_LEN + jnp.arange(DEC_SEQ, dtype=jnp.int32)
    xp, xs = x_prompt, x_sample
    bp = x_prompt.shape[0]
    kp_l, vp_l, ks_l, vs_l, rp_l, rs_l, cp_l, cs_l, fp_l, fs_l = ([] for _ in range(10))
    for l in range(DEPTH):
        mp = adaln(c_prompt, ada_w[l], ada_b[l])
        ms = adaln(c_sample, ada_w[l], ada_b[l])
        hp = modulate(xp, mp[:, 0], mp[:, 1])
        hs = modulate(xs, ms[:, 0], ms[:, 1])
        i = l // 2
        if l % 2 == 0:
            k_past = cache_k[i][page_table].reshape(DEC_BATCH, n_pages * PAGE_SIZE, N_HEADS_B, HEAD_DIM_B)
            v_past = cache_v[i][page_table].reshape(DEC_BATCH, n_pages * PAGE_SIZE, N_HEADS_B, HEAD_DIM_B)
            s0 = jnp.zeros((bp, N_HEADS_A, HEAD_DIM_A, HEAD_DIM_A), jnp.float32)
            yp, kp, vp, rp = ab_mixer(hp, pos_p, None, None, s0, ab_w_in[i], ab_w_out[i], min(MOBA_QCHUNK, SEQ))
            ys, ksn, vsn, rs = ab_mixer(hs, pos_s, k_past, v_past, state_ret[i], ab_w_in[i], ab_w_out[i], 1)
            kp_l.append(kp); vp_l.append(vp); ks_l.append(ksn); vs_l.append(vsn)
            rp_l.append(rp); rs_l.append(rs)
        else:
            c0 = jnp.zeros((bp, CONV_W - 1, D_CONF), xp.dtype)
            yp, cp = conformer_conv(hp, c0, cf_w_pw1[i], cf_b_pw1[i], cf_w_dw[i], cf_b_dw[i], cf_ln_g[i], cf_ln_b[i], cf_w_pw2[i], cf_b_pw2[i])
            ys, cs = conformer_conv(hs, state_conv[i], cf_w_pw1[i], cf_b_pw1[i], cf_w_dw[i], cf_b_dw[i], cf_ln_g[i], cf_ln_b[i], cf_w_pw2[i], cf_b_pw2[i])
            cp_l.append(cp); cs_l.append(cs)
        xp = post_norm(xp, yp, mp[:, 2], ln_g[l, 0], ln_b[l, 0])
        xs = post_norm(xs, ys, ms[:, 2], ln_g[l, 0], ln_b[l, 0])
        hp = modulate(xp, mp[:, 3], mp[:, 4])
        hs = modulate(xs, ms[:, 3], ms[:, 4])
        f0 = jnp.zeros((bp, FFN_CONV_W - 1, D_FF), xp.dtype)
        fyp, fp = conv_ffn(hp, f0, ffn_w_up[l], ffn_w_dw[l], ffn_b_dw[l], ffn_w_down[l])
        fys, fs = conv_ffn(hs, state_ffn[l], ffn_w_up[l], ffn_w_dw[l], ffn_b_dw[l], ffn_w_down[l])
        fp_l.append(fp); fs_l.append(fs)
        xp = post_norm(xp, fyp, mp[:, 5], ln_g[l, 1], ln_b[l, 1])
        xs = post_norm(xs, fys, ms[:, 5], ln_g[l, 1], ln_b[l, 1])
    return (xp, xs, jnp.stack(kp_l), jnp.stack(vp_l), jnp.stack(ks_l), jnp.stack(vs_l),
            jnp.stack(rp_l), jnp.stack(rs_l), jnp.stack(cp_l), jnp.stack(cs_l), jnp.stack(fp_l), jnp.stack(fs_l))
```

```python
import math
import numpy as np
import ml_dtypes
import concourse.bass as bass
import concourse.mybir as mybir
from concourse.bass_utils import run_bass_kernel_spmd

F32 = mybir.dt.float32
BF16 = mybir.dt.bfloat16
I32 = mybir.dt.int32
ALU = mybir.AluOpType
AF = mybir.ActivationFunctionType
AX = mybir.AxisListType

NCORES = 8
D = 1024
T = 2048
NT = 16
NS = 16
SR = 64
DFF = 2816
NFC = 22
ALPHA = 4.0 ** 0.25
LN_EPS = 1e-5
GN_EPS = 1e-6
SCALE = 128.0 ** -0.5
NEG = -1.0e30
LOGG = [math.log1p(-2.0 ** (-5.0 - h)) for h in range(4)]


class Reg:
    __slots__ = ("w", "r", "p", "dsem", "dcnt", "psum")

    def __init__(self, psum=False):
        self.psum = psum
        self.w = {}
        self.r = {}
        self.p = {}
        self.dsem = None
        self.dcnt = 0


class Buf:
    __slots__ = ("t", "r")

    def __init__(self, t):
        self.t = t
        self.r = Reg()


class Pool:
    def __init__(self, bufs):
        self.bufs = bufs
        self.i = 0

    def next(self):
        b = self.bufs[self.i % len(self.bufs)]
        self.i += 1
        return b


def _merge(dst, src):
    for s, v in src.items():
        if dst.get(s, 0) < v:
            dst[s] = v


class KB:
    def __init__(self, nc):
        self.nc = nc
        self.eng = {"pe": nc.tensor, "act": nc.scalar, "dve": nc.vector, "pool": nc.gpsimd, "sp": nc.sync}
        self.esem = {e: nc.alloc_semaphore("es_" + e) for e in ("pe", "act", "dve", "pool")}
        self.ecnt = {e: 0 for e in self.esem}
        self.seen = {e: {} for e in self.eng}
        self.nsem = 4
        self.out_toks = {}
        self.sb_off = (nc.sbuf_base + 63) // 64 * 64
        self.sb_top = nc.sbuf_top
        self.sb_marks = []
        self.uid = 0
        self.ninst = 0
        self.anchors = []
        self.limit = self.sb_top

    def barrier(self):
        for e, E in self.eng.items():
            seen = self.seen[e]
            for e2, s in self.esem.items():
                v = self.ecnt[e2]
                if v > seen.get(s, 0) and not (e == "pe" and e2 == "pe"):
                    E.wait_ge(s, v)
                    seen[s] = v
                    self.ninst += 1
            for a in self.anchors:
                if a.dcnt > seen.get(a.dsem, 0):
                    E.wait_ge(a.dsem, a.dcnt)
                    seen[a.dsem] = a.dcnt
                    self.ninst += 1

    def sb(self, shape, dtype, name=None):
        self.uid += 1
        nm = (name or "t") + "_%d" % self.uid
        esz = 2 if dtype == BF16 else 4
        n = 1
        for s in shape[1:]:
            n *= s
        nbytes = (n * esz + 63) // 64 * 64
        off = self.sb_off
        self.sb_off += nbytes
        assert self.sb_off <= self.limit, "SBUF overflow %d > %d (%s)" % (self.sb_off, self.limit, nm)
        return self.nc.alloc_sbuf_tensor_at(nm, list(shape), dtype, offset=off)

    def buf(self, shape, dtype, name=None):
        return Buf(self.sb(shape, dtype, name))

    def pool(self, n, shape, dtype, name=None):
        return Pool([self.buf(shape, dtype, name) for _ in range(n)])

    def mark(self):
        self.sb_marks.append(self.sb_off)

    def release(self):
        self.sb_off = self.sb_marks.pop()
        self.barrier()

    def _waits(self, eng, reads, writes, accum):
        need = {}
        mysem0 = self.esem.get(eng)
        for r in reads:
            _merge(need, r.w)
            if r.psum:
                for s_, v_ in r.r.items():
                    if s_ is not mysem0 and need.get(s_, 0) < v_:
                        need[s_] = v_
        for w in writes:
            if accum and not w.r:
                _merge(need, w.p)
            else:
                _merge(need, w.r)
                _merge(need, w.w)
        E = self.eng[eng]
        mysem = self.esem.get(eng)
        seen = self.seen[eng]
        for s, v in need.items():
            if eng == "pe" and s is mysem:
                continue
            if seen.get(s, 0) >= v:
                continue
            E.wait_ge(s, v)
            self.ninst += 1
            seen[s] = v

    def _update(self, tok, reads, writes, accum):
        s, v = tok
        for r in reads:
            if r.r.get(s, 0) < v:
                r.r[s] = v
        for w in writes:
            if accum and not w.r:
                if w.w.get(s, 0) < v:
                    w.w[s] = v
            else:
                p = dict(w.w)
                _merge(p, w.r)
                w.p = p
                w.w = {s: v}
                w.r = {}

    def op(self, eng, fn, reads=(), writes=(), accum=False, inc=True):
        self._waits(eng, reads, writes, accum)
        ins = fn(self.eng[eng])
        self.ninst += 1
        if inc:
            self.ecnt[eng] += 1
            ins.then_inc(self.esem[eng], 1)
        tok = (self.esem[eng], self.ecnt[eng] + (0 if inc else 1))
        self._update(tok, reads, writes, accum)
        return ins

    def _dma_any(self, q, mk, reads, writes, anchor, accum, is_output):
        if anchor is None:
            anchor = writes[0] if writes else reads[0]
        if anchor.dsem is None:
            anchor.dsem = self.nc.alloc_semaphore("ds_%d" % self.nsem)
            self.nsem += 1
            self.anchors.append(anchor)
        self._waits(q, reads, writes, accum)
        ins = mk(self.eng[q])
        self.ninst += 1
        anchor.dcnt += 16
        ins.then_inc(anchor.dsem, 16)
        tok = (anchor.dsem, anchor.dcnt)
        self._update(tok, reads, writes, accum)
        if is_output:
            self.out_toks[anchor.dsem] = anchor.dcnt
        return ins

    def dma(self, q, out, in_, reads=(), writes=(), anchor=None, accum=False, is_output=False, **kw):
        return self._dma_any(q, lambda e: e.dma_start(out=out, in_=in_, **kw), reads, writes, anchor, accum, is_output)

    def gather(self, out, table, idx_ap, reads=(), writes=(), accum=False):
        return self._dma_any(
            "pool",
            lambda e: e.indirect_dma_start(out=out, out_offset=None, in_=table,
                                           in_offset=bass.IndirectOffsetOnAxis(ap=idx_ap, axis=0)),
            reads, writes, None, accum, False)

    def finish(self):
        sp = self.eng["sp"]
        for s, v in self.out_toks.items():
            sp.wait_ge(s, v)
        for e, s in self.esem.items():
            if self.ecnt[e] > 0:
                sp.wait_ge(s, self.ecnt[e])

    def mm(self, out, lhsT, rhs, start, stop, reads=(), writes=(), inc=None):
        return self.op("pe", lambda e: e.matmul(out, lhsT, rhs, start=start, stop=stop),
                       reads, writes, accum=not start, inc=(stop if inc is None else inc))

    def tr(self, out, in_, ident, reads=(), writes=(), accum=False):
        return self.op("pe", lambda e: e.transpose(out, in_, ident), reads, writes, accum=accum)

    def act(self, out, in_, func, reads=(), writes=(), accum=False, **kw):
        return self.op("act", lambda e: e.activation(out, in_, func, **kw), reads, writes, accum=accum)

    def tt(self, eng, out, in0, in1, op, reads=(), writes=(), accum=False):
        return self.op(eng, lambda e: e.tensor_tensor(out, in0, in1, op), reads, writes, accum=accum)

    def ts(self, eng, out, in0, s1, s2, op0, op1=None, reads=(), writes=(), accum=False):
        if op1 is None:
            return self.op(eng, lambda e: e.tensor_scalar(out, in0, s1, None, op0), reads, writes, accum=accum)
        return self.op(eng, lambda e: e.tensor_scalar(out, in0, s1, s2, op0, op1), reads, writes, accum=accum)

    def stt(self, out, in0, scalar, in1, op0, op1, reads=(), writes=(), accum=False):
        return self.op("dve", lambda e: e.scalar_tensor_tensor(out, in0, scalar, in1, op0, op1), reads, writes, accum=accum)

    def cp(self, eng, out, in_, reads=(), writes=(), accum=False):
        if eng == "act":
            return self.op("act", lambda e: e.copy(out, in_), reads, writes, accum=accum)
        return self.op(eng, lambda e: e.tensor_copy(out, in_), reads, writes, accum=accum)

    def memset(self, eng, ap, val, writes=(), accum=False):
        return self.op(eng, lambda e: e.memset(ap, val), (), writes, accum=accum)


def bc(ap, axis, n):
    a = ap.unsqueeze(axis)
    shp = list(a.shape)
    shp[axis] = n
    return a.broadcast_to(shp)


def build(stage=99, nphys=2560, skip_ms=False):
    nc = bass.Bass("TRN2", target_bir_lowering=False)
    k = KB(nc)

    def din(name, shape, dt=F32):
        return nc.dram_tensor(name, list(shape), dt, kind="ExternalInput").ap()

    def dout(name, shape):
        return nc.dram_tensor(name, list(shape), F32, kind="ExternalOutput").ap()

    xp = din("xp", [T, D]); xs_d = din("xs", [SR, D]); call = din("call", [17, D])
    ck = din("ck", [nphys * 128, 512]); cv = din("cv", [nphys * 128, 512])
    ptd = din("pt", [1, NS * 16], I32)
    sret = din("sret", [NS * 4 * 128, 128]); sconv = din("sconv", [NS * 30, D]); sffn = din("sffn", [2, NS * 2, DFF])
    w_in = din("w_in", [D, 3584]); w_out = din("w_out", [D, D]); pw1 = din("pw1", [D, 2048])
    cfp_d = din("cfp", [36, D]); pw2 = din("pw2", [D, D]); b_pw2 = din("b_pw2", [1, D])
    ffn_up = din("ffn_up", [2, D, 2 * DFF]); ffp_d = din("ffp", [2, 4, DFF]); ffn_down = din("ffn_down", [2, DFF, D])
    ada_w = din("ada_w", [2, D, 6 * D]); ada_b = din("ada_b", [2, 6 * D])
    ln_g = din("ln_g", [4, D]); ln_b = din("ln_b", [4, D])
    ident_d = din("ident", [128, 128]); iota_d = din("iota", [128, 1])
    rot_d = din("rot", [17, 128, 4, 128])
    rdm_d = din("rdm", [128, 4, 128]); rqd_d = din("rqd", [128, 4, 128]); rkd_d = din("rkd", [128, 4])
    rdms_d = din("rdms", [64, 4, 64]); rqds_d = din("rqds", [128, 4, 64]); rkds_d = din("rkds", [64, 4])
    blk_d = din("blk", [64, 16]); tri_d = din("tri", [128, 128]); nmask_d = din("nmask", [64, 16, 16])
    Ep_d = din("Ep", [18, 128]); Es_d = din("Es", [18, 64]); ep_d = din("ep", [18, 1])

    yp = dout("yp", [T, D]); ys = dout("ys", [SR, D])
    kp = dout("kp", [T, 512]); vp = dout("vp", [T, 512]); ks = dout("ks", [SR, 512]); vs = dout("vs", [SR, 512])
    rpo = dout("rpo", [512, 128]); rso = dout("rso", [NS * 512, 128])
    cpo = dout("cpo", [30, D]); cso = dout("cso", [NS * 30, D])
    fpo = dout("fpo", [2, 2, DFF]); fso = dout("fso", [2, NS * 2, DFF])

    sc_mods = [nc.dram_tensor("sc_mods%d" % l, [SR, 3 * D], F32).ap() for l in range(2)]
    sc_g2p = [nc.dram_tensor("sc_g2p%d" % l, [1, D], F32).ap() for l in range(2)]
    r_scm = [Reg(), Reg()]
    r_scg = [Reg(), Reg()]

    ps = nc.alloc_psum_tensor("ps", [128, 4096], F32)
    psb = ps[:].bitcast(BF16)
    rb = [Reg(psum=True) for _ in range(8)]

    def bank(i, rows=128, c0=0, c1=512):
        return ps[0:rows, i * 512 + c0:i * 512 + c1]

    def bankb(i, rows=128, c0=0, c1=1024):
        return psb[0:rows, i * 1024 + c0:i * 1024 + c1]

    rconst = Reg()

    def cload(shape, src, dt=F32, q="sp"):
        t = k.sb(shape, dt)
        k.dma(q, t[:], src, writes=[rconst], anchor=rconst, accum=True)
        return t

    ident = cload([128, 128], ident_d)
    iota = cload([128, 1], iota_d)
    Ep = cload([18, 128], Ep_d); Es = cload([18, 64], Es_d); ep = cload([18, 1], ep_d)
    identb_t = k.sb([128, 128], BF16)
    onesb = k.sb([128, 128], BF16)
    onesf = k.sb([128, 128], F32)
    k.memset("pool", onesf[:], 1.0, writes=[rconst], accum=True)
    k.cp("dve", identb_t[:], ident[:], reads=[rconst], writes=[rconst], accum=True)
    k.memset("pool", onesb[:], 1.0, writes=[rconst], accum=True)
    XOFF = (k.sb_top - NT * D * 4) // 64 * 64
    x_all = nc.alloc_sbuf_tensor_at("x_all", [128, NT, D], F32, offset=XOFF)
    rx = [Reg() for _ in range(NT)]
    x_s = k.buf([SR, D], F32)
    k.dma("sp", x_s.t[:], xs_d, writes=[x_s.r])
    modT = k.buf([128, 2, 6, 8], F32)
    scT = k.buf([128, 8, 32], BF16)
    k.mark()
    callsb = cload([17, D], call)
    scs = k.buf([17, D], F32)
    k.act(scs.t[:], callsb[:], AF.Silu, reads=[rconst], writes=[scs.r])
    for c in range(8):
        k.tr(bank(0, 128, c * 32, c * 32 + 17), scs.t[0:17, c * 128:(c + 1) * 128], ident[0:17, 0:17],
             reads=[scs.r, rconst], writes=[rb[0]], accum=(c > 0))
    k.cp("dve", scT.t[:, :, 0:17], bank(0, 128, 0, 256).rearrange("p (c j) -> p c j", j=32)[:, :, 0:17],
         reads=[rb[0]], writes=[scT.r])
    k.release()

    def load_w_bf16(dst_ap, dst_reg, src_ap, stg, eng="pool", first=True, mul=None, mul_reg=None):
        shp = list(src_ap.shape)
        if len(shp) == 3:
            st = stg.t[:, 0:shp[1] * shp[2]].rearrange("p (a b) -> p a b", b=shp[2])
        else:
            st = stg.t[:, 0:shp[1]]
        k.dma("sp", st, src_ap, writes=[stg.r])
        if mul is None:
            k.cp(eng, dst_ap, st, reads=[stg.r], writes=[dst_reg], accum=not first)
        else:
            k.tt(eng, dst_ap, st, mul, ALU.mult, reads=[stg.r, mul_reg], writes=[dst_reg], accum=not first)

    def layer_norm(xin, xin_reg, rows, gam, bet, gb_reg, out_ap, out_reg, wk):
        st = wk["st"].next(); mv = wk["mv"].next()
        xv = xin.rearrange("p (c f) -> p c f", f=512)
        for c in range(2):
            k.op("dve", lambda e, c=c: e.bn_stats(st.t[0:rows, c, :], xv[:, c, :]), reads=[xin_reg], writes=[st.r], accum=(c > 0))
        k.op("dve", lambda e: e.bn_aggr(mv.t[0:rows, 0:2], st.t[0:rows, :, :]), reads=[st.r], writes=[mv.r])
        k.ts("dve", mv.t[0:rows, 2:3], mv.t[0:rows, 1:2], LN_EPS, None, ALU.add, reads=[mv.r], writes=[mv.r])
        k.act(mv.t[0:rows, 2:3], mv.t[0:rows, 2:3], AF.Sqrt, reads=[mv.r], writes=[mv.r])
        k.op("dve", lambda e: e.reciprocal(mv.t[0:rows, 2:3], mv.t[0:rows, 2:3]), reads=[mv.r], writes=[mv.r])
        k.stt(mv.t[0:rows, 3:4], mv.t[0:rows, 0:1], -1.0, mv.t[0:rows, 2:3], ALU.mult, ALU.mult, reads=[mv.r], writes=[mv.r])
        xn = wk["xn"].next()
        k.act(xn.t[0:rows, :], xin, AF.Identity, reads=[xin_reg, mv.r], writes=[xn.r], scale=mv.t[0:rows, 2:3], bias=mv.t[0:rows, 3:4])
        k.tt("pool", xn.t[0:rows, :], xn.t[0:rows, :], gam[0:rows, :], ALU.mult, reads=[xn.r, gb_reg], writes=[xn.r])
        k.tt("pool", out_ap, xn.t[0:rows, :], bet[0:rows, :], ALU.add, reads=[xn.r, gb_reg], writes=[out_reg])

    def make_hT(xin, xin_reg, rows, hT_ap, hT_reg, layer, which, mods_s=None, mods_reg=None, wk=None, pbanks=(0, 1), first=True):
        if rows == 128:
            src, src_reg = xin, xin_reg
        else:
            hs = wk["hs"].next()
            k.tt("dve", hs.t[0:rows], xin, mods_s[:, D:2 * D], ALU.mult, reads=[xin_reg, mods_reg], writes=[hs.r])
            k.tt("dve", hs.t[0:rows], hs.t[0:rows], mods_s[:, 0:D], ALU.add, reads=[hs.r, mods_reg], writes=[hs.r])
            src, src_reg = hs.t[0:rows], hs.r
        for c in range(8):
            b = pbanks[c // 4]
            k.tr(bank(b, 128, (c % 4) * 128, (c % 4) * 128 + rows), src[:, c * 128:(c + 1) * 128], ident[0:rows, 0:rows],
                 reads=[src_reg, rconst], writes=[rb[b]], accum=(c % 4 > 0))
        for c in range(8):
            b = pbanks[c // 4]
            pin = bank(b, 128, (c % 4) * 128, (c % 4) * 128 + rows)
            if rows == 128:
                k.act(hT_ap[:, c, :], pin, AF.Identity, reads=[rb[b], modT.r], writes=[hT_reg], accum=not (first and c == 0),
                      scale=modT.t[:, layer, which + 1, c:c + 1], bias=modT.t[:, layer, which, c:c + 1])
            else:
                k.cp("act", hT_ap[:, c, :], pin, reads=[rb[b]], writes=[hT_reg], accum=not (first and c == 0))

    def rotate(xv, xregs, rows, G, Ct, St, tab_reg, out3, out_reg, tmp3, tmp_reg, mode):
        if mode == "half":
            x4 = xv.rearrange("p g (two j) -> p g two j", two=2)
            t4 = tmp3.rearrange("p g (two j) -> p g two j", two=2)
            a0, a1 = x4[:, :, 1, :], x4[:, :, 0, :]
            d0, d1 = t4[:, :, 0, :], t4[:, :, 1, :]
            s0, s1 = St[:, 0:64], St[:, 64:128]
        else:
            x4 = xv.rearrange("p g (j two) -> p g j two", two=2)
            t4 = tmp3.rearrange("p g (j two) -> p g j two", two=2)
            a0, a1 = x4[:, :, :, 1], x4[:, :, :, 0]
            d0, d1 = t4[:, :, :, 0], t4[:, :, :, 1]
            Sv = St.rearrange("p (j two) -> p j two", two=2)
            s0, s1 = Sv[:, :, 0], Sv[:, :, 1]
        k.tt("dve", d0, a0, bc(s0, 1, G), ALU.mult, reads=list(xregs) + [tab_reg], writes=[tmp_reg])
        k.tt("dve", d1, a1, bc(s1, 1, G), ALU.mult, reads=list(xregs) + [tab_reg], writes=[tmp_reg], accum=True)
        k.tt("dve", out3, xv, bc(Ct, 1, G), ALU.mult, reads=list(xregs) + [tab_reg], writes=[out_reg])
        k.tt("pool", out3, out3, tmp3, ALU.add, reads=[out_reg, tmp_reg], writes=[out_reg])

    def adaln(l, mods_mix, g1p):
        k.mark()
        stg = k.pool(2, [128, 8 * 512], F32, "ada_stg")
        wbp = k.pool(2, [128, 8, 512], BF16, "ada_wb")
        m17p = k.pool(2, [18, 512], F32, "m17")
        outp = k.pool(2, [128, 512], F32, "ada_out")
        for j in range(12):
            which, half = divmod(j, 2)
            wb = wbp.next()
            load_w_bf16(wb.t[:], wb.r, ada_w[l, :, j * 512:(j + 1) * 512].rearrange("(kc p) n -> p kc n", p=128), stg.next())
            m17 = m17p.next()
            k.dma("sp", m17.t[17:18, :], ada_b[l:l + 1, j * 512:(j + 1) * 512], writes=[m17.r])
            for kc in range(8):
                k.mm(bank(0, 17), scT.t[:, kc, 0:17], wb.t[:, kc, :], kc == 0, kc == 7, reads=[scT.r, wb.r], writes=[rb[0]])
            k.cp("act", m17.t[0:17, :], bank(0, 17), reads=[rb[0]], writes=[m17.r], accum=True)
            plus1 = 1.0 if which in (1, 2, 4, 5) else 0.0
            k.mm(bank(1, 64), Es[:], m17.t[:], True, True, reads=[rconst, m17.r], writes=[rb[1]])
            if which < 3:
                k.ts("dve", mods_mix.t[:, j * 512:(j + 1) * 512], bank(1, 64), plus1, None, ALU.add,
                     reads=[rb[1]], writes=[mods_mix.r], accum=(j > 0))
            else:
                o = outp.next()
                k.ts("dve", o.t[0:64, :], bank(1, 64), plus1, None, ALU.add, reads=[rb[1]], writes=[o.r])
                k.dma("sp", sc_mods[l][:, (j - 6) * 512:(j - 5) * 512], o.t[0:64, :], reads=[o.r], writes=[r_scm[l]], accum=(j > 6))
            if which in (2, 5):
                k.mm(bank(2), Ep[:], m17.t[:], True, True, reads=[rconst, m17.r], writes=[rb[2]])
                if which == 2:
                    k.ts("dve", g1p.t[:, half * 512:(half + 1) * 512], bank(2), 1.0, None, ALU.add,
                         reads=[rb[2]], writes=[g1p.r], accum=(half > 0))
                else:
                    o = outp.next()
                    k.ts("dve", o.t[:, :], bank(2), 1.0, None, ALU.add, reads=[rb[2]], writes=[o.r])
                    k.dma("sp", sc_g2p[l][:, half * 512:(half + 1) * 512], o.t[0:1, :], reads=[o.r], writes=[r_scg[l]], accum=(half > 0))
            for cc in range(4):
                k.mm(bank(3, 128, cc, cc + 1), m17.t[:, cc * 128:(cc + 1) * 128], ep[:], True, True,
                     reads=[m17.r, rconst], writes=[rb[3]])
            k.ts("dve", modT.t[:, l, which, half * 4:(half + 1) * 4], bank(3, 128, 0, 4), plus1, None, ALU.add,
                 reads=[rb[3]], writes=[modT.r], accum=True)
        k.release()

    def pass_moba(mods_mix, omT):
        k.mark()
        wm = k.buf([128, 8, 1536], BF16, "wm")
        qTs_b = k.buf([128, 4, SR], BF16, "qTs_b"); qTs_f = k.buf([128, 4, SR], F32, "qTs_f")
        kTs_b = k.buf([128, 4, SR], BF16, "kTs_b"); v_s = k.buf([SR, 4, 132], BF16, "v_s")
        k.mark()
        kT_hist = k.buf([128, 4, T], BF16, "kT_hist")
        v_hist = k.buf([128, NT, 512], BF16, "v_hist")
        kmT = k.buf([128, 4, 8], F32, "kmT")
        k.mark()
        stg = k.pool(2, [128, 8 * 512], F32, "stg")
        for g in range(3):
            load_w_bf16(wm.t[:, :, g * 512:(g + 1) * 512], wm.r,
                        w_in[:, 2048 + g * 512:2048 + (g + 1) * 512].rearrange("(kc p) n -> p kc n", p=128),
                        stg.next(), first=(g == 0))
        k.release()
        wk = {"hs": k.pool(1, [SR, D], F32, "hs")}
        xt_p = k.pool(2, [128, D], F32, "xt")
        hT_p = k.pool(2, [128, 8, 128], BF16, "hT")
        rot_p = k.pool(2, [128, 4, 128], F32, "rot")
        qk_p = k.pool(2, [128, 8, 128], F32, "qkrot")
        tmp_p = k.pool(1, [128, 8, 128], F32, "rtmp")
        vf_p = k.pool(2, [128, 512], F32, "vf")
        qTb_p = k.pool(2, [128, 4, 128], BF16, "qTb")
        qTf_p = k.pool(2, [128, 4, 128], F32, "qTf")
        ksum_p = k.pool(2, [128, 4], F32, "ksum")
        gate_p = k.pool(1, [128, 4, 8], F32, "gate")
        cmp_p = k.pool(1, [128, 4, 8, 8], F32, "cmp")
        bias_p = k.pool(2, [128, 4, 8], F32, "bias")
        S_p = k.pool(1, [128, T], F32, "S")
        P_p = k.pool(2, [128, T], BF16, "P")
        PT_p = k.pool(2, [128, NT, 128], BF16, "PT")
        sm_p = k.pool(4, [128, 4], F32, "sm")
        om_p = k.pool(2, [128, 512], BF16, "om")

        for t in range(NT + 1):
            rows = 128 if t < NT else SR
            if t < NT:
                xt = xt_p.next()
                k.dma("sp", xt.t[:], xp[t * 128:(t + 1) * 128, :], writes=[xt.r])
                xin, xin_reg = xt.t[:], xt.r
            else:
                xin, xin_reg = x_s.t[:], x_s.r
            hT = hT_p.next()
            make_hT(xin, xin_reg, rows, hT.t[:, :, 0:rows], hT.r, 0, 0, mods_s=mods_mix.t, mods_reg=mods_mix.r, wk=wk, pbanks=(0, 1))
            rot = rot_p.next()
            k.dma("sp", rot.t[0:rows], rot_d[t, 0:rows], writes=[rot.r])
            for g in range(3):
                for kc in range(8):
                    k.mm(bank(4 + g, rows), hT.t[:, kc, 0:rows], wm.t[:, kc, g * 512:(g + 1) * 512], kc == 0, kc == 7,
                         reads=[hT.r, wm.r], writes=[rb[4 + g]])
            qk = qk_p.next(); tmp = tmp_p.next()
            zqk = ps[0:rows, 4 * 512:6 * 512].rearrange("p (g d) -> p g d", d=128)
            rotate(zqk, [rb[4], rb[5]], rows, 8, rot.t[0:rows, 2, :], rot.t[0:rows, 3, :], rot.r,
                   qk.t[0:rows], qk.r, tmp.t[0:rows], tmp.r, "half")
            vf = vf_p.next()
            k.cp("act", vf.t[0:rows], bank(6, rows), reads=[rb[6]], writes=[vf.r])
            kdst = kp[t * 128:(t + 1) * 128, :] if t < NT else ks
            vdst = vp[t * 128:(t + 1) * 128, :] if t < NT else vs
            k.dma("sp", kdst, qk.t[0:rows, 4:8, :].rearrange("p g d -> p (g d)"), reads=[qk.r], is_output=True)
            k.dma("sp", vdst, vf.t[0:rows], reads=[vf.r], is_output=True)
            if stage < 2:
                continue
            if t < NT:
                k.cp("pool", v_hist.t[:, t, :], vf.t[:], reads=[vf.r], writes=[v_hist.r], accum=(t > 0))
            else:
                k.memset("pool", v_s.t[:], 1.0, writes=[v_s.r])
                k.cp("pool", v_s.t[:, :, 0:128], vf.t[0:SR].rearrange("p (h d) -> p h d", d=128), reads=[vf.r], writes=[v_s.r])
            for g in range(8):
                b = 2 + g // 4
                k.tr(bank(b, 128, (g % 4) * 128, (g % 4) * 128 + rows), qk.t[0:rows, g, :], ident[0:rows, 0:rows],
                     reads=[qk.r, rconst], writes=[rb[b]], accum=(g % 4 > 0))
            qv = bank(2).rearrange("p (h s) -> p h s", s=128)[:, :, 0:rows]
            kv = bank(3).rearrange("p (h s) -> p h s", s=128)[:, :, 0:rows]
            if t < NT:
                qTb = qTb_p.next(); qTf = qTf_p.next()
                k.cp("act", qTb.t[:], qv, reads=[rb[2]], writes=[qTb.r])
                k.cp("dve", qTf.t[:], qv, reads=[rb[2]], writes=[qTf.r])
                k.cp("act", kT_hist.t[:, :, t * 128:(t + 1) * 128], kv, reads=[rb[3]], writes=[kT_hist.r], accum=(t > 0))
                ksum = ksum_p.next()
                k.op("dve", lambda e, ksum=ksum, kv=kv: e.tensor_reduce(ksum.t[:], kv, axis=AX.X, op=ALU.add), reads=[rb[3]], writes=[ksum.r])
                if t % 2 == 0:
                    k.cp("pool", kmT.t[:, :, t // 2], ksum.t[:], reads=[ksum.r], writes=[kmT.r], accum=True)
                else:
                    k.tt("pool", kmT.t[:, :, t // 2], kmT.t[:, :, t // 2], ksum.t[:], ALU.add, reads=[ksum.r, kmT.r], writes=[kmT.r])
            else:
                k.cp("act", qTs_b.t[:], qv, reads=[rb[2]], writes=[qTs_b.r])
                k.cp("dve", qTs_f.t[:], qv, reads=[rb[2]], writes=[qTs_f.r])
                k.cp("act", kTs_b.t[:], kv, reads=[rb[3]], writes=[kTs_b.r])
                continue
            own = t // 2
            bias = None
            if own >= 4:
                for h in range(4):
                    k.mm(bank(7, 128, 448 + h * 8, 448 + h * 8 + own), qTf.t[:, h, :], kmT.t[:, h, 0:own], True, True,
                         reads=[qTf.r, kmT.r], writes=[rb[7]])
                gate = gate_p.next(); cmpb = cmp_p.next(); bias = bias_p.next()
                gpv = bank(7, 128, 448, 480).rearrange("p (h n) -> p h n", n=8)[:, :, 0:own]
                k.cp("act", gate.t[:, :, 0:own], gpv, reads=[rb[7]], writes=[gate.r])
                gv = gate.t[:, :, 0:own]
                k.tt("dve", cmpb.t[:, :, 0:own, 0:own], bc(gv, 2, own), bc(gv, 3, own), ALU.is_gt, reads=[gate.r], writes=[cmpb.r])
                k.op("dve", lambda e, gate=gate, cmpb=cmpb, own=own: e.tensor_reduce(gate.t[:, :, 0:own], cmpb.t[:, :, 0:own, 0:own], axis=AX.X, op=ALU.add),
                     reads=[cmpb.r], writes=[gate.r])
                k.ts("dve", bias.t[:, :, 0:own], gate.t[:, :, 0:own], 2.5, NEG, ALU.is_gt, ALU.mult, reads=[gate.r], writes=[bias.r])
            nk = (t + 1) * 128
            om = om_p.next()
            for h in range(4):
                for c0 in range(0, nk, 512):
                    c1 = min(nk, c0 + 512)
                    b = c0 // 512
                    k.mm(bank(b, 128, 0, c1 - c0), qTb.t[:, h, :], kT_hist.t[:, h, c0:c1], True, True,
                         reads=[qTb.r, kT_hist.r], writes=[rb[b]])
                nb_used = (nk + 511) // 512
                sregs = [rb[i] for i in range(nb_used)]
                S = S_p.next()
                npast = own * 256
                first = True
                if npast > 0:
                    if bias is not None:
                        k.tt("dve", S.t[:, 0:npast].rearrange("p (n s) -> p n s", s=256),
                             ps[:, 0:npast].rearrange("p (n s) -> p n s", s=256), bc(bias.t[:, h, 0:own], 2, 256), ALU.add,
                             reads=sregs + [bias.r], writes=[S.r])
                    else:
                        k.cp("act", S.t[:, 0:npast], ps[:, 0:npast], reads=sregs, writes=[S.r])
                    first = False
                if t % 2 == 1:
                    k.cp("act", S.t[:, npast:npast + 128], ps[:, npast:npast + 128], reads=sregs, writes=[S.r], accum=not first)
                    first = False
                k.tt("dve", S.t[:, nk - 128:nk], ps[:, nk - 128:nk], tri[:], ALU.add, reads=sregs + [rconst], writes=[S.r], accum=not first)
                sm = sm_p.next()
                k.op("dve", lambda e, sm=sm, S=S, nk=nk: e.reduce_max(sm.t[:, 0:1], S.t[:, 0:nk], axis=AX.X), reads=[S.r], writes=[sm.r])
                k.ts("dve", sm.t[:, 1:2], sm.t[:, 0:1], -SCALE, None, ALU.mult, reads=[sm.r], writes=[sm.r])
                P = P_p.next()
                k.act(P.t[:, 0:nk], S.t[:, 0:nk], AF.Exp, reads=[S.r, sm.r], writes=[P.r, sm.r], scale=SCALE, bias=sm.t[:, 1:2],
                      accum_out=sm.t[:, 2:3])
                PT = PT_p.next()
                for j0 in range(0, t + 1, 8):
                    j1 = min(t + 1, j0 + 8)
                    for j in range(j0, j1):
                        k.tr(bankb(6, 128, (j - j0) * 128, (j - j0 + 1) * 128), P.t[:, j * 128:(j + 1) * 128], identb_t[:],
                             reads=[P.r, rconst], writes=[rb[6]], accum=(j > j0))
                    k.cp("act" if (j0 // 8) % 2 == 0 else "dve", PT.t[:, j0:j1, :],
                         bankb(6, 128, 0, (j1 - j0) * 128).rearrange("p (j s) -> p j s", s=128), reads=[rb[6]], writes=[PT.r], accum=(j0 > 0))
                for j in range(t + 1):
                    k.mm(bank(7, 128, 0, 128), PT.t[:, j, :], v_hist.t[:, j, h * 128:(h + 1) * 128],
                         j == 0, j == t, reads=[PT.r, v_hist.r], writes=[rb[7]])
                k.op("dve", lambda e, sm=sm: e.reciprocal(sm.t[:, 3:4], sm.t[:, 2:3]), reads=[sm.r], writes=[sm.r])
                k.act(om.t[:, h * 128:(h + 1) * 128], bank(7, 128, 0, 128), AF.Identity, reads=[rb[7], sm.r], writes=[om.r], accum=(h > 0),
                      scale=sm.t[:, 3:4])
            for h in range(4):
                k.tr(bankb(6, 128, h * 128, (h + 1) * 128), om.t[:, h * 128:(h + 1) * 128], identb_t[:], reads=[om.r, rconst],
                     writes=[rb[6]], accum=(h > 0))
            k.cp("act", omT.t[:, :, t * 128:(t + 1) * 128], bankb(6, 128, 0, 512).rearrange("p (h s) -> p h s", s=128),
                 reads=[rb[6]], writes=[omT.r], accum=(t > 0))
        k.release()
        if stage >= 3 and not skip_ms:
            moba_sample(qTs_b, qTs_f, kTs_b, v_s, omT)
        k.release()

    def moba_sample(qTs_b, qTs_f, kTs_b, v_s, omT):
        kpg_p = k.pool(4, [128, 512], F32, "kpg")
        vpg_p = k.pool(4, [128, 512], F32, "vpg")
        kb_p = k.pool(2, [128, 512], BF16, "kb")
        kTq_p = k.pool(2, [128, 4, T], BF16, "kTq")
        Vq_p = k.pool(2, [128, 16, 4, 132], BF16, "Vq")
        E_p = k.pool(2, [128, 17, 4, SR], BF16, "Eb")
        for b_ in Vq_p.bufs:
            k.memset("pool", b_.t[:], 1.0, writes=[b_.r])
        for b_ in E_p.bufs:
            k.memset("pool", b_.t[:], 0.0, writes=[b_.r])
        kms_p = k.pool(2, [128, 4, 8], F32, "kms")
        ksum_p = k.pool(2, [128, 4], F32, "ksum2")
        prod_p = k.pool(1, [128, 4, 8, 4], F32, "prod")
        gs_p = k.pool(1, [128, 16, 8], F32, "gs")
        cmp_p = k.pool(1, [128, 16, 8, 8], F32, "cmp2")
        comb_p = k.pool(1, [128, 16, 8], F32, "comb")
        pm_p = k.pool(1, [128, 16], F32, "pm")
        m16_p = k.pool(1, [16, 20], F32, "m16")
        X_p = k.pool(1, [128, 16, 16], F32, "X")
        Xn_p = k.pool(1, [SR, 16], F32, "Xn")
        for b in range(NS):
            kTq = kTq_p.next(); Vq = Vq_p.next(); Eb = E_p.next(); kms = kms_p.next()
            for j in range(16):
                col = b * 16 + j
                kpg = kpg_p.next(); vpg = vpg_p.next()
                k.gather(kpg.t[:], ck, idx.t[:, col:col + 1], reads=[idx.r], writes=[kpg.r])
                k.gather(vpg.t[:], cv, idx.t[:, col:col + 1], reads=[idx.r], writes=[vpg.r])
                kb = kb_p.next()
                k.cp("dve", kb.t[:], kpg.t[:], reads=[kpg.r], writes=[kb.r])
                k.cp("pool", Vq.t[:, j, :, 0:128], vpg.t[:].rearrange("p (h d) -> p h d", d=128), reads=[vpg.r], writes=[Vq.r],
                     accum=(j > 0))
                pb = 1 + j % 2
                for h in range(4):
                    k.tr(bankb(pb, 128, h * 128, (h + 1) * 128), kb.t[:, h * 128:(h + 1) * 128], identb_t[:],
                         reads=[kb.r, rconst], writes=[rb[pb]], accum=(h > 0))
                kvw = bankb(pb, 128, 0, 512).rearrange("p (h s) -> p h s", s=128)
                k.cp("act", kTq.t[:, :, j * 128:(j + 1) * 128], kvw, reads=[rb[pb]], writes=[kTq.r], accum=(j > 0))
                ksum = ksum_p.next()
                k.op("dve", lambda e, ksum=ksum, kvw=kvw: e.tensor_reduce(ksum.t[:], kvw, axis=AX.X, op=ALU.add), reads=[rb[pb]], writes=[ksum.r])
                if j % 2 == 0:
                    k.cp("pool", kms.t[:, :, j // 2], ksum.t[:], reads=[ksum.r], writes=[kms.r], accum=(j > 0))
                else:
                    k.tt("pool", kms.t[:, :, j // 2], kms.t[:, :, j // 2], ksum.t[:], ALU.add, reads=[ksum.r, kms.r], writes=[kms.r])
            prod = prod_p.next()
            qf = qTs_f.t[:, :, 4 * b:4 * b + 4]
            k.tt("dve", prod.t[:], bc(kms.t[:], 3, 4), bc(qf, 2, 8), ALU.mult, reads=[kms.r, qTs_f.r], writes=[prod.r])
            k.mm(bank(3, 128, 0, 128), onesf[:], prod.t[:].rearrange("p h n q -> p (h n q)"), True, True,
                 reads=[rconst, prod.r], writes=[rb[3]])
            gs = gs_p.next(); cmpb = cmp_p.next(); comb = comb_p.next()
            k.cp("act", gs.t[:].rearrange("p (h q) n -> p h q n", q=4),
                 bank(3, 128, 0, 128).rearrange("p (h n q) -> p h q n", h=4, n=8), reads=[rb[3]], writes=[gs.r])
            k.tt("dve", cmpb.t[:], bc(gs.t[:], 2, 8), bc(gs.t[:], 3, 8), ALU.is_gt, reads=[gs.r], writes=[cmpb.r])
            k.op("dve", lambda e, gs=gs, cmpb=cmpb: e.tensor_reduce(gs.t[:], cmpb.t[:], axis=AX.X, op=ALU.add), reads=[cmpb.r], writes=[gs.r])
            k.ts("dve", comb.t[:], gs.t[:], 2.5, NEG, ALU.is_gt, ALU.mult, reads=[gs.r], writes=[comb.r])
            for j in range(16):
                for h in range(4):
                    c0 = j * 16 + h * 4
                    k.mm(bank(0, 128, c0, c0 + 4), kTq.t[:, h, j * 128:(j + 1) * 128], qTs_b.t[:, h, 4 * b:4 * b + 4], True, True,
                         reads=[kTq.r, qTs_b.r], writes=[rb[0]], inc=(j == 15 and h == 3))
            for h in range(4):
                k.mm(bank(0, SR, 256 + h * 4, 260 + h * 4), kTs_b.t[:, h, :], qTs_b.t[:, h, 4 * b:4 * b + 4], True, True,
                     reads=[kTs_b.r, qTs_b.r], writes=[rb[0]], inc=(h == 3))
            pm = pm_p.next(); m16 = m16_p.next()
            k.op("dve", lambda e, pm=pm: e.tensor_reduce(pm.t[:], bank(0, 128, 0, 256).rearrange("p (j c) -> p c j", c=16), axis=AX.X, op=ALU.max),
                 reads=[rb[0]], writes=[pm.r])
            k.tt("dve", pm.t[0:SR, :], pm.t[0:SR, :], bank(0, SR, 256, 272), ALU.max, reads=[pm.r, rb[0]], writes=[pm.r])
            k.tr(bank(3, 16, 128, 256), pm.t[:], ident[:], reads=[pm.r, rconst], writes=[rb[3]])
            k.op("dve", lambda e, m16=m16: e.reduce_max(m16.t[:, 16:17], bank(3, 16, 128, 256), axis=AX.X), reads=[rb[3]], writes=[m16.r])
            k.ts("dve", m16.t[:, 0:16], ident[0:16, 0:16], m16.t[:, 16:17], None, ALU.mult, reads=[m16.r, rconst], writes=[m16.r])
            k.mm(bank(3, 128, 256, 272), onesf[0:16, :], m16.t[:, 0:16], True, True, reads=[rconst, m16.r], writes=[rb[3]])
            mbc = bank(3, 128, 256, 272)
            k.tt("dve", comb.t[:], comb.t[:], bc(mbc, 2, 8), ALU.subtract, reads=[comb.r, rb[3]], writes=[comb.r])
            X = X_p.next(); Xn = Xn_p.next()
            k.tt("dve", X.t[:].rearrange("p (n two) c -> p n two c", two=2),
                 bank(0, 128, 0, 256).rearrange("p (n two c) -> p n two c", two=2, c=16),
                 bc(comb.t[:].rearrange("p c n -> p n c"), 2, 2), ALU.add, reads=[rb[0], comb.r], writes=[X.r])
            k.tt("dve", Xn.t[:], bank(0, SR, 256, 272), nmask[:, b, :], ALU.add, reads=[rb[0], rconst], writes=[Xn.r])
            k.tt("dve", Xn.t[:], Xn.t[:], bank(3, SR, 256, 272), ALU.subtract, reads=[Xn.r, rb[3]], writes=[Xn.r])
            k.act(Eb.t[:, 0:16, :, 4 * b:4 * b + 4], X.t[:].rearrange("p j (h q) -> p j h q", q=4), AF.Exp, reads=[X.r], writes=[Eb.r], scale=SCALE)
            k.act(Eb.t[0:SR, 16, :, 4 * b:4 * b + 4], Xn.t[:].rearrange("p (h q) -> p h q", q=4), AF.Exp, reads=[Xn.r], writes=[Eb.r], scale=SCALE)
            for h in range(4):
                ob = bank(4 + h, SR, 0, 132)
                for j in range(16):
                    k.mm(ob, Eb.t[:, j, h, :], Vq.t[:, j, h, :], (b == 0 and j == 0), False,
                         reads=[Eb.r, Vq.r], writes=[rb[4 + h]], inc=False)
                k.mm(ob, Eb.t[0:SR, 16, h, :], v_s.t[:, h, :], False, (b == NS - 1),
                     reads=[Eb.r, v_s.r], writes=[rb[4 + h]], inc=True)
            k.memset("pool", Eb.t[:, :, :, 4 * b:4 * b + 4], 0.0, writes=[Eb.r])
        rin = k.buf([SR, 4], F32, "rin"); oms = k.buf([SR, 512], BF16, "oms")
        for h in range(4):
            c0 = (4 + h) * 512
            k.op("dve", lambda e, h=h, c0=c0: e.reciprocal(rin.t[:, h:h + 1], ps[0:SR, c0 + 128:c0 + 129]),
                 reads=[rb[4 + h]], writes=[rin.r], accum=(h > 0))
        for h in range(4):
            c0 = (4 + h) * 512
            k.act(oms.t[:, h * 128:(h + 1) * 128], ps[0:SR, c0:c0 + 128], AF.Identity, reads=[rb[4 + h], rin.r], writes=[oms.r],
                  accum=(h > 0), scale=rin.t[:, h:h + 1])
        for h in range(4):
            k.tr(bankb(1, 128, h * SR, (h + 1) * SR), oms.t[:, h * 128:(h + 1) * 128], identb_t[0:SR, 0:SR], reads=[oms.r, rconst],
                 writes=[rb[1]], accum=(h > 0))
        k.cp("act", omT.t[:, :, T:T + SR], bankb(1, 128, 0, 4 * SR).rearrange("p (h s) -> p h s", s=SR), reads=[rb[1]], writes=[omT.r])

    def pass_ret(mods_mix, orT):
        k.mark()
        wr = k.buf([128, 8, 2048], BF16, "wr")
        k.mark()
        stg = k.pool(2, [128, 8 * 512], F32, "stg")
        for g in range(4):
            load_w_bf16(wr.t[:, :, g * 512:(g + 1) * 512], wr.r,
                        w_in[:, g * 512:(g + 1) * 512].rearrange("(kc p) n -> p kc n", p=128), stg.next(), first=(g == 0))
        k.release()
        Sst = k.buf([128, 4, 128], F32, "Sst"); Sbf = k.buf([128, 4, 128], BF16, "Sbf")
        k.memset("pool", Sst.t[:], 0.0, writes=[Sst.r]); k.memset("pool", Sbf.t[:], 0.0, writes=[Sbf.r])
        wk = {"hs": k.pool(1, [SR, D], F32, "hs")}
        xt_p = k.pool(2, [128, D], F32, "xt"); hT_p = k.pool(2, [128, 8, 128], BF16, "hT")
        rot_p = k.pool(2, [128, 4, 128], F32, "rot"); qk_p = k.pool(2, [128, 8, 128], F32, "qkrot")
        tmp_p = k.pool(1, [128, 8, 128], F32, "rtmp")
        vb_p = k.pool(2, [128, 512], BF16, "vb"); sg_p = k.pool(2, [128, 512], F32, "sg")
        kd_p = k.pool(2, [128, 4, 128], BF16, "kd")
        qTb_p = k.pool(2, [128, 4, 128], BF16, "qTb"); kTb_p = k.pool(2, [128, 4, 128], BF16, "kTb")
        qdT_p = k.pool(2, [128, 4, 128], BF16, "qdT"); att_p = k.pool(2, [128, 4, 128], BF16, "att")
        ss_p = k.pool(2, [128, 8], F32, "ss"); junk_p = k.pool(1, [128, 128], F32, "junk")
        or_p = k.pool(2, [128, 512], BF16, "or")
        for t in range(NT + 1):
            rows = 128 if t < NT else SR
            if t < NT:
                xt = xt_p.next()
                k.dma("sp", xt.t[:], xp[t * 128:(t + 1) * 128, :], writes=[xt.r])
                xin, xin_reg = xt.t[:], xt.r
            else:
                xin, xin_reg = x_s.t[:], x_s.r
            hT = hT_p.next()
            make_hT(xin, xin_reg, rows, hT.t[:, :, 0:rows], hT.r, 0, 0, mods_s=mods_mix.t, mods_reg=mods_mix.r, wk=wk, pbanks=(0, 1))
            rot = rot_p.next()
            k.dma("sp", rot.t[0:rows], rot_d[t, 0:rows], writes=[rot.r])
            for g in range(4):
                for kc in range(8):
                    k.mm(bank(4 + g, rows), hT.t[:, kc, 0:rows], wr.t[:, kc, g * 512:(g + 1) * 512], kc == 0, kc == 7,
                         reads=[hT.r, wr.r], writes=[rb[4 + g]])
            qk = qk_p.next(); tmp = tmp_p.next()
            zqk = ps[0:rows, 4 * 512:6 * 512].rearrange("p (g d) -> p g d", d=128)
            rotate(zqk, [rb[4], rb[5]], rows, 8, rot.t[0:rows, 0, :], rot.t[0:rows, 1, :], rot.r,
                   qk.t[0:rows], qk.r, tmp.t[0:rows], tmp.r, "pair")
            vb = vb_p.next(); sg = sg_p.next()
            k.cp("act", vb.t[0:rows], bank(6, rows), reads=[rb[6]], writes=[vb.r])
            k.act(sg.t[0:rows], bank(7, rows), AF.Silu, reads=[rb[7]], writes=[sg.r])
            kdc = rkd if t < NT else rkds
            kd = kd_p.next()
            k.tt("pool", kd.t[0:rows], qk.t[0:rows, 4:8, :], bc(kdc[0:rows, :], 2, 128), ALU.mult, reads=[qk.r, rconst], writes=[kd.r])
            for g in range(8):
                b = 2 + g // 4
                k.tr(bank(b, 128, (g % 4) * 128, (g % 4) * 128 + rows), qk.t[0:rows, g, :], ident[0:rows, 0:rows],
                     reads=[qk.r, rconst], writes=[rb[b]], accum=(g % 4 > 0))
            qv = bank(2).rearrange("p (h s) -> p h s", s=128)[:, :, 0:rows]
            kv = bank(3).rearrange("p (h s) -> p h s", s=128)[:, :, 0:rows]
            qTb = qTb_p.next(); kTb = kTb_p.next(); qdT = qdT_p.next()
            k.cp("act", qTb.t[:, :, 0:rows], qv, reads=[rb[2]], writes=[qTb.r])
            k.cp("act", kTb.t[:, :, 0:rows], kv, reads=[rb[3]], writes=[kTb.r])
            qdc = rqd if t < NT else rqds
            k.tt("dve", qdT.t[:, :, 0:rows], qv, qdc[:, :, 0:rows], ALU.mult, reads=[rb[2], rconst], writes=[qdT.r])
            for h in range(4):
                k.mm(bank(0, rows, h * 128, h * 128 + rows), kTb.t[:, h, 0:rows], qTb.t[:, h, 0:rows], True, True,
                     reads=[kTb.r, qTb.r], writes=[rb[0]])
            att = att_p.next()
            dmc = rdm if t < NT else rdms
            k.tt("dve", att.t[0:rows, :, 0:rows], bank(0, rows).rearrange("p (h s) -> p h s", s=128)[:, :, 0:rows], dmc[0:rows, :, 0:rows],
                 ALU.mult, reads=[rb[0], rconst], writes=[att.r])
            if t < NT:
                for h in range(4):
                    ob = bank(1, 128, h * 128, (h + 1) * 128)
                    k.mm(ob, att.t[:, h, :], vb.t[:, h * 128:(h + 1) * 128], True, False, reads=[att.r, vb.r], writes=[rb[1]])
                    k.mm(ob, qdT.t[:, h, :], Sbf.t[:, h, :], False, True, reads=[qdT.r, Sbf.r], writes=[rb[1]])
                for h in range(4):
                    k.mm(bank(2, 128, h * 128, (h + 1) * 128), kd.t[:, h, :], vb.t[:, h * 128:(h + 1) * 128], True, True,
                         reads=[kd.r, vb.r], writes=[rb[2]])
                for h in range(4):
                    k.stt(Sst.t[:, h, :], Sst.t[:, h, :], math.exp(128.0 * LOGG[h]), bank(2, 128, h * 128, (h + 1) * 128), ALU.mult, ALU.add,
                          reads=[Sst.r, rb[2]], writes=[Sst.r])
                k.cp("pool", Sbf.t[:], Sst.t[:], reads=[Sst.r], writes=[Sbf.r])
                if t == NT - 1:
                    k.dma("sp", rpo.rearrange("(h d) v -> d h v", d=128), Sst.t[:], reads=[Sst.r], is_output=True)
            else:
                qdm = k.buf([128, NS, 4, SR], BF16, "qdm")
                k.memset("pool", qdm.t[:], 0.0, writes=[qdm.r])
                base = qdm.t[:]
                pstr = base.ap[0][0]
                for h in range(4):
                    dst = bass.AP(qdm.t, base.offset + h * SR, [[pstr, 128], [4 * SR + 4, NS], [1, 4]])
                    k.cp("pool", dst, qdT.t[:, h, 0:SR].rearrange("p (s j) -> p s j", j=4), reads=[qdT.r, qdm.r], writes=[qdm.r])
                for h in range(4):
                    ob = bank(4 + h, SR, 0, 128)
                    k.mm(ob, att.t[0:SR, h, 0:SR], vb.t[0:SR, h * 128:(h + 1) * 128], True, False, reads=[att.r, vb.r], writes=[rb[4 + h]], inc=True)
                s0_p = k.pool(2, [128, 4, 128], F32, "s0"); s0b_p = k.pool(2, [128, 4, 128], BF16, "s0b")
                kdm_p = k.pool(2, [SR, 4, 128], BF16, "kdm"); sn_p = k.pool(2, [128, 4, 128], F32, "sn")
                for sq in range(NS):
                    s0 = s0_p.next(); s0b = s0b_p.next()
                    k.dma("sp", s0.t[:], sret[sq * 512:(sq + 1) * 512, :].rearrange("(h d) v -> d h v", d=128), writes=[s0.r])
                    k.cp("pool", s0b.t[:], s0.t[:], reads=[s0.r], writes=[s0b.r])
                    for h in range(4):
                        k.mm(bank(4 + h, SR, 0, 128), qdm.t[:, sq, h, :], s0b.t[:, h, :], False, (sq == NS - 1),
                             reads=[qdm.r, s0b.r], writes=[rb[4 + h]], inc=True)
                    kdm = kdm_p.next()
                    k.ts("dve", kdm.t[:], kd.t[0:SR], blk[:, sq:sq + 1], None, ALU.mult, reads=[kd.r, rconst], writes=[kdm.r])
                    ub = 2 + sq % 2
                    for h in range(4):
                        k.mm(bank(ub, 128, h * 128, (h + 1) * 128), kdm.t[:, h, :], vb.t[0:SR, h * 128:(h + 1) * 128], True, True,
                             reads=[kdm.r, vb.r], writes=[rb[ub]])
                    sn = sn_p.next()
                    for h in range(4):
                        k.stt(sn.t[:, h, :], s0.t[:, h, :], math.exp(4.0 * LOGG[h]), bank(ub, 128, h * 128, (h + 1) * 128), ALU.mult, ALU.add,
                              reads=[s0.r, rb[ub]], writes=[sn.r], accum=(h > 0))
                    k.dma("sp", rso[sq * 512:(sq + 1) * 512, :].rearrange("(h d) v -> d h v", d=128), sn.t[:], reads=[sn.r], is_output=True)
            ss = ss_p.next(); junk = junk_p.next()

            def obank(h):
                return (bank(1, rows, h * 128, (h + 1) * 128), rb[1]) if t < NT else (bank(4 + h, rows, 0, 128), rb[4 + h])

            for h in range(4):
                oap, oreg = obank(h)
                k.act(junk.t[0:rows], oap, AF.Square, reads=[oreg], writes=[junk.r, ss.r],
                      accum_out=ss.t[0:rows, h:h + 1])
            k.ts("dve", ss.t[0:rows, 4:8], ss.t[0:rows, 0:4], 1.0 / 128.0, GN_EPS, ALU.mult, ALU.add, reads=[ss.r], writes=[ss.r])
            k.act(ss.t[0:rows, 4:8], ss.t[0:rows, 4:8], AF.Sqrt, reads=[ss.r], writes=[ss.r])
            k.op("dve", lambda e, ss=ss, rows=rows: e.reciprocal(ss.t[0:rows, 4:8], ss.t[0:rows, 4:8]), reads=[ss.r], writes=[ss.r])
            orr = or_p.next()
            for h in range(4):
                oap, oreg = obank(h)
                k.stt(orr.t[0:rows, h * 128:(h + 1) * 128], oap, ss.t[0:rows, 4 + h:5 + h],
                      sg.t[0:rows, h * 128:(h + 1) * 128], ALU.mult, ALU.mult, reads=[oreg, ss.r, sg.r], writes=[orr.r], accum=(h > 0))
            for h in range(4):
                k.tr(bankb(3, 128, h * 128, h * 128 + rows), orr.t[0:rows, h * 128:(h + 1) * 128], identb_t[0:rows, 0:rows],
                     reads=[orr.r, rconst], writes=[rb[3]], accum=(h > 0))
            k.cp("act", orT.t[:, :, t * 128:t * 128 + rows], bankb(3, 128, 0, 512).rearrange("p (h s) -> p h s", s=128)[:, :, 0:rows],
                 reads=[rb[3]], writes=[orT.r], accum=(t > 0))
        k.release()

    def post_norm(y_ap, y_regs, xt_ap, xt_reg, rows, gate_ap, gate_reg, lng, lnb, gb_reg, wk, bias_ap=None, bias_reg=None):
        rr = wk["rr"].next()
        if bias_ap is not None:
            k.tt("dve", rr.t[0:rows], y_ap, bias_ap[0:rows], ALU.add, reads=list(y_regs) + [bias_reg], writes=[rr.r])
            k.tt("pool", rr.t[0:rows], rr.t[0:rows], gate_ap, ALU.mult, reads=[rr.r, gate_reg], writes=[rr.r])
        else:
            k.tt("dve", rr.t[0:rows], y_ap, gate_ap, ALU.mult, reads=list(y_regs) + [gate_reg], writes=[rr.r])
        k.stt(xt_ap, xt_ap, ALPHA, rr.t[0:rows], ALU.mult, ALU.add, reads=[xt_reg, rr.r], writes=[xt_reg])
        layer_norm(xt_ap, xt_reg, rows, lng, lnb, gb_reg, xt_ap, xt_reg, wk)

    def ln_work(n=2):
        rr = k.pool(n, [128, D], F32, "rr")
        return {"st": k.pool(2, [128, 2, 6], F32, "st"), "mv": k.pool(2, [128, 4], F32, "mv"),
                "xn": k.pool(n, [128, D], F32, "xn"), "rr": rr, "hs": rr}

    def load_ln(i):
        g = k.buf([128, D], F32, "lng"); b = k.buf([128, D], F32, "lnb")
        k.dma("sp", g.t[:], ln_g[i:i + 1, :].partition_broadcast(128), writes=[g.r])
        k.dma("sp", b.t[:], ln_b[i:i + 1, :].partition_broadcast(128), writes=[g.r], anchor=g.r, accum=True)
        return g, b

    def pass_out(mods_mix, g1p, orT, omT):
        k.mark()
        wo = k.buf([128, 8, D], BF16, "wo")
        k.mark()
        stg = k.pool(2, [128, 8 * 512], F32, "stg")
        for g in range(2):
            load_w_bf16(wo.t[:, :, g * 512:(g + 1) * 512], wo.r,
                        w_out[:, g * 512:(g + 1) * 512].rearrange("(kc p) n -> p kc n", p=128), stg.next(), first=(g == 0))
        k.release()
        lng, lnb = load_ln(0)
        wk = ln_work()
        for t in range(NT + 1):
            rows = 128 if t < NT else SR
            if t < NT:
                k.dma("sp", x_all[:, t, :], xp[t * 128:(t + 1) * 128, :], writes=[rx[t]])
                xt_ap, xt_reg = x_all[:, t, :], rx[t]
                gate_ap, gate_reg = g1p.t[:], g1p.r
            else:
                xt_ap, xt_reg = x_s.t[:], x_s.r
                gate_ap, gate_reg = mods_mix.t[:, 2 * D:3 * D], mods_mix.r
            bp = 4 * (t % 2)
            for half in range(2):
                for c in range(8):
                    src = orT if c < 4 else omT
                    k.mm(bank(bp + half, rows), src.t[:, c % 4, t * 128:t * 128 + rows], wo.t[:, c, half * 512:(half + 1) * 512],
                         c == 0, c == 7, reads=[src.r, wo.r], writes=[rb[bp + half]])
            post_norm(ps[0:rows, bp * 512:bp * 512 + D], [rb[bp], rb[bp + 1]], xt_ap, xt_reg, rows, gate_ap, gate_reg,
                      lng.t, lnb.t, lng.r, wk)
        k.release()

    def ffn(l, final):
        k.mark()
        g2s = k.buf([SR, D], F32, "g2s"); g2p = k.buf([128, D], F32, "g2p")
        k.dma("sp", g2s.t[:], sc_mods[l][:, 2 * D:3 * D], reads=[r_scm[l]], writes=[g2s.r])
        k.dma("sp", g2p.t[:], sc_g2p[l].partition_broadcast(128), reads=[r_scg[l]], writes=[g2p.r])
        hT = k.buf([128, 8, T + SR], BF16, "hT_all")
        ffp = k.buf([128, NFC, 4], F32, "ffp"); ust = k.buf([128, NFC, 2 * NS], F32, "ust")
        fo = k.buf([128, NFC, 2], F32, "fo"); fs = k.buf([128, NFC, NS, 2], F32, "fs")
        k.mark()
        mf = k.buf([SR, 2 * D], F32, "mf")
        k.dma("sp", mf.t[:], sc_mods[l][:, 0:2 * D], reads=[r_scm[l]], writes=[mf.r])
        p4 = k.buf([4, DFF], F32, "p4"); p32 = k.buf([2 * NS, DFF], F32, "p32")
        k.dma("sp", p4.t[:], ffp_d[l], writes=[p4.r])
        k.dma("sp", p32.t[:], sffn[l], writes=[p32.r])
        for c0 in range(0, NFC, 4):
            c1 = min(NFC, c0 + 4)
            for c in range(c0, c1):
                k.tr(bank(0, 128, (c - c0) * 4, (c - c0) * 4 + 4), p4.t[:, c * 128:(c + 1) * 128], ident[0:4, 0:4], reads=[p4.r, rconst],
                     writes=[rb[0]], accum=(c > c0))
                k.tr(bank(1, 128, (c - c0) * 32, (c - c0) * 32 + 32), p32.t[:, c * 128:(c + 1) * 128], ident[0:32, 0:32], reads=[p32.r, rconst],
                     writes=[rb[1]], accum=(c > c0))
            k.cp("act", ffp.t[:, c0:c1, :], bank(0, 128, 0, (c1 - c0) * 4).rearrange("p (c j) -> p c j", j=4), reads=[rb[0]], writes=[ffp.r], accum=(c0 > 0))
            k.cp("act", ust.t[:, c0:c1, :], bank(1, 128, 0, (c1 - c0) * 32).rearrange("p (c j) -> p c j", j=32), reads=[rb[1]], writes=[ust.r], accum=(c0 > 0))
        wkh = {"hs": k.pool(1, [SR, D], F32, "hs")}
        for t in range(NT):
            make_hT(x_all[:, t, :], rx[t], 128, hT.t[:, :, t * 128:(t + 1) * 128], hT.r, l, 3, pbanks=(2 + 2 * (t % 2), 3 + 2 * (t % 2)), first=(t == 0))
            k.ts("pool", x_all[:, t, :], x_all[:, t, :], ALPHA, None, ALU.mult, reads=[rx[t]], writes=[rx[t]])
        make_hT(x_s.t[:], x_s.r, SR, hT.t[:, :, T:T + SR], hT.r, l, 3, mods_s=mf.t, mods_reg=mf.r, wk=wkh, pbanks=(2, 3), first=False)
        k.ts("pool", x_s.t[:], x_s.t[:], ALPHA, None, ALU.mult, reads=[x_s.r], writes=[x_s.r])
        k.release()
        k.mark()
        G = 2
        stg = k.pool(2, [128, 2048], F32, "stg")
        wu_p = k.pool(2, [128, 8, G * 128], BF16, "wu"); wv_p = k.pool(2, [128, 8, G * 128], BF16, "wv")
        wds_p = k.pool(2, [128, G, D], BF16, "wds"); wdu_p = k.pool(1, [128, G, D], BF16, "wdu")
        UW = 2 + T + 6 * NS
        u_p = k.pool(2, [128, UW], F32, "u_sb"); a_p = k.pool(1, [128, UW], F32, "acc")
        gT_p = k.pool(1, [128, G, T + SR], BF16, "gT")
        nb = [0]

        for g in range(NFC // G):
            f0 = g * G * 128
            wu = wu_p.next(); wv = wv_p.next(); wds = wds_p.next(); wdu = wdu_p.next()
            load_w_bf16(wu.t[:], wu.r, ffn_up[l, :, f0:f0 + G * 128].rearrange("(kc p) n -> p kc n", p=128), stg.next())
            load_w_bf16(wv.t[:], wv.r, ffn_up[l, :, DFF + f0:DFF + f0 + G * 128].rearrange("(kc p) n -> p kc n", p=128), stg.next())
            st = stg.next()
            stv = st.t[:, 0:G * D].rearrange("p (a b) -> p a b", b=D)
            k.dma("sp", stv, ffn_down[l, f0:f0 + G * 128, :].rearrange("(c p) n -> p c n", p=128), writes=[st.r])
            k.cp("pool", wdu.t[:], stv, reads=[st.r], writes=[wdu.r])
            k.tt("pool", wds.t[:], stv, bc(g2p.t[:], 1, G), ALU.mult, reads=[st.r, g2p.r], writes=[wds.r])
            gT = gT_p.next()
            for c in range(G):
                fc = g * G + c
                u = u_p.next(); acc = a_p.next()
                w0, w1, w2, bb = (ffp.t[:, fc, j:j + 1] for j in range(4))
                k.memset("pool", u.t[:, 0:2], 0.0, writes=[u.r])
                usv = u.t[:, 2 + T:UW].rearrange("p (s j) -> p s j", j=6)
                asv = acc.t[:, 2 + T:UW].rearrange("p (s j) -> p s j", j=6)
                k.cp("pool", usv[:, :, 0:2], ust.t[:, fc, :].rearrange("p (s j) -> p s j", j=2), reads=[ust.r], writes=[u.r], accum=True)
                for n in range(5):
                    ncol = 512 if n < 4 else SR
                    t0 = n * 512
                    nb[0] += 1
                    bu = nb[0] % 2; bv = 2 + nb[0] % 2
                    for kc in range(8):
                        k.mm(bank(bu, 128, 0, ncol), wu.t[:, kc, c * 128:(c + 1) * 128], hT.t[:, kc, t0:t0 + ncol], kc == 0, kc == 7,
                             reads=[wu.r, hT.r], writes=[rb[bu]])
                    for kc in range(8):
                        k.mm(bank(bv, 128, 0, ncol), wv.t[:, kc, c * 128:(c + 1) * 128], hT.t[:, kc, t0:t0 + ncol], kc == 0, kc == 7,
                             reads=[wv.r, hT.r], writes=[rb[bv]])
                    if n < 4:
                        lo, hi = 2 + t0, 2 + t0 + 512
                        k.cp("act", u.t[:, lo:hi], bank(bu), reads=[rb[bu]], writes=[u.r], accum=True)
                    else:
                        lo, hi = 2 + T + 2, UW
                        k.cp("act", usv[:, :, 2:6], bank(bu, 128, 0, SR).rearrange("p (s j) -> p s j", j=4), reads=[rb[bu]], writes=[u.r], accum=True)
                    k.act(acc.t[:, lo:hi], u.t[:, lo:hi], AF.Identity, reads=[u.r, ffp.r], writes=[acc.r], accum=(n > 0), scale=w2, bias=bb)
                    k.stt(acc.t[:, lo:hi], u.t[:, lo - 1:hi - 1], w1, acc.t[:, lo:hi], ALU.mult, ALU.add, reads=[u.r, acc.r, ffp.r], writes=[acc.r])
                    k.stt(acc.t[:, lo:hi], u.t[:, lo - 2:hi - 2], w0, acc.t[:, lo:hi], ALU.mult, ALU.add, reads=[u.r, acc.r, ffp.r], writes=[acc.r])
                    k.act(acc.t[:, lo:hi], acc.t[:, lo:hi], AF.Gelu, reads=[acc.r], writes=[acc.r])
                    if n < 4:
                        k.tt("dve", gT.t[:, c, t0:t0 + 512], acc.t[:, lo:hi], bank(bv), ALU.mult, reads=[acc.r, rb[bv]], writes=[gT.r],
                             accum=not (c == 0 and n == 0))
                    else:
                        k.tt("dve", gT.t[:, c, T:T + SR].rearrange("p (s j) -> p s j", j=4), asv[:, :, 2:6],
                             bank(bv, 128, 0, SR).rearrange("p (s j) -> p s j", j=4), ALU.mult, reads=[acc.r, rb[bv]], writes=[gT.r], accum=True)
                k.cp("pool", fo.t[:, fc, :], u.t[:, T:T + 2], reads=[u.r], writes=[fo.r], accum=True)
                k.cp("pool", fs.t[:, fc, :, :], usv[:, :, 4:6], reads=[u.r], writes=[fs.r], accum=True)
            for t in range(NT + 1):
                rows = 128 if t < NT else SR
                bp = 4 + 2 * (t % 2)
                wd = wds if t < NT else wdu
                for half in range(2):
                    for c in range(G):
                        k.mm(bank(bp + half, rows), gT.t[:, c, t * 128:t * 128 + rows], wd.t[:, c, half * 512:(half + 1) * 512],
                             c == 0, c == G - 1, reads=[gT.r, wd.r], writes=[rb[bp + half]])
                yv = ps[0:rows, bp * 512:bp * 512 + D]
                if t < NT:
                    k.tt("dve", x_all[:, t, :], x_all[:, t, :], yv, ALU.add, reads=[rx[t], rb[bp], rb[bp + 1]], writes=[rx[t]])
                else:
                    rs_ = a_p.next()
                    k.tt("dve", rs_.t[0:SR, 0:D], yv, g2s.t[:], ALU.mult, reads=[rb[bp], rb[bp + 1], g2s.r], writes=[rs_.r])
                    k.tt("pool", x_s.t[:], x_s.t[:], rs_.t[0:SR, 0:D], ALU.add, reads=[x_s.r, rs_.r], writes=[x_s.r])
        k.release()
        k.mark()
        fo_tok = k.buf([2, DFF], F32, "fo_tok"); fs_tok = k.buf([2 * NS, DFF], F32, "fs_tok")
        for c0 in range(0, NFC, 4):
            c1 = min(NFC, c0 + 4)
            for c in range(c0, c1):
                k.tr(bank(0, 2, (c - c0) * 128, (c - c0 + 1) * 128), fo.t[:, c, :], ident[:], reads=[fo.r, rconst], writes=[rb[0]], accum=(c > c0))
                k.tr(bank(1, 2 * NS, (c - c0) * 128, (c - c0 + 1) * 128), fs.t[:, c, :, :].rearrange("p s j -> p (s j)"), ident[:],
                     reads=[fs.r, rconst], writes=[rb[1]], accum=(c > c0))
            k.cp("act", fo_tok.t[:, c0 * 128:c1 * 128], bank(0, 2, 0, (c1 - c0) * 128), reads=[rb[0]], writes=[fo_tok.r], accum=(c0 > 0))
            k.cp("act", fs_tok.t[:, c0 * 128:c1 * 128], bank(1, 2 * NS, 0, (c1 - c0) * 128), reads=[rb[1]], writes=[fs_tok.r], accum=(c0 > 0))
        k.dma("sp", fpo[l], fo_tok.t[:], reads=[fo_tok.r], is_output=True)
        k.dma("sp", fso[l], fs_tok.t[:], reads=[fs_tok.r], is_output=True)
        lng, lnb = load_ln(2 * l + 1)
        wk = ln_work()
        for t in range(NT + 1):
            rows = 128 if t < NT else SR
            xt_ap, xt_reg = (x_all[:, t, :], rx[t]) if t < NT else (x_s.t[:], x_s.r)
            layer_norm(xt_ap, xt_reg, rows, lng.t, lnb.t, lng.r, xt_ap, xt_reg, wk)
            if final:
                k.dma("sp", yp[t * 128:(t + 1) * 128, :] if t < NT else ys, xt_ap, reads=[xt_reg], is_output=True)
        k.release()
        k.release()

    def conformer(mods_mix, g1p):
        k.mark()
        w1 = k.buf([128, 8, 2048], BF16, "w1"); w2 = k.buf([128, 8, D], BF16, "w2")
        cf = k.buf([128, 8, 36], F32, "cf")
        k.mark()
        stg = k.pool(2, [128, 8 * 512], F32, "stg")
        for g in range(4):
            load_w_bf16(w1.t[:, :, g * 512:(g + 1) * 512], w1.r, pw1[:, g * 512:(g + 1) * 512].rearrange("(kc p) n -> p kc n", p=128),
                        stg.next(), first=(g == 0))
        for g in range(2):
            load_w_bf16(w2.t[:, :, g * 512:(g + 1) * 512], w2.r, pw2[:, g * 512:(g + 1) * 512].rearrange("(kc p) n -> p kc n", p=128),
                        stg.next(), first=(g == 0))
        p36 = k.buf([36, D], F32, "p36")
        k.dma("sp", p36.t[:], cfp_d, writes=[p36.r])
        for c in range(8):
            k.tr(bank(0, 128, c * 36, c * 36 + 36), p36.t[:, c * 128:(c + 1) * 128], ident[0:36, 0:36], reads=[p36.r, rconst],
                 writes=[rb[0]], accum=(c > 0))
        k.cp("act", cf.t[:], bank(0, 128, 0, 288).rearrange("p (c j) -> p c j", j=36), reads=[rb[0]], writes=[cf.r])
        k.release()
        b2 = k.buf([128, D], F32, "b2")
        k.dma("sp", b2.t[:], b_pw2.partition_broadcast(128), writes=[b2.r])
        lng, lnb = load_ln(2)
        wk = ln_work(1)
        BS = 256
        NB = T // BS
        TPB = BS // 128
        GW = 34 * NS
        sconv_p = k.pool(1, [120, 4, 128], F32, "sconv_sb")
        carry = k.buf([128, 8, 30], F32, "carry")
        k.memset("pool", carry.t[:], 0.0, writes=[carry.r])
        hTb = k.buf([128, 8, BS], BF16, "hTb")
        yc = k.buf([128, 8, BS], F32, "yc")
        sT = k.buf([128, 8, BS], BF16, "sT")
        glu_p = k.pool(2, [128, GW], F32, "glu"); sig_p = k.pool(2, [128, BS], F32, "sig")
        acc_p = k.pool(2, [128, GW], F32, "cacc"); tmpc_p = k.pool(1, [128, GW], F32, "ctmp")
        ycb_p = k.pool(2, [128, BS], BF16, "ycb"); ysq_p = k.pool(2, [128, BS], BF16, "ysq")
        mean = k.buf([128, BS], F32, "mean"); rstd = k.buf([128, BS], F32, "rstd"); xn_p = k.pool(2, [128, BS], F32, "cxn")
        gnew = k.buf([128, 8, SR], F32, "gnew")
        for B in range(NB + 1):
            prompt = B < NB
            ncol = BS if prompt else SR
            if prompt:
                for j in range(TPB):
                    t = TPB * B + j
                    make_hT(x_all[:, t, :], rx[t], 128, hTb.t[:, :, j * 128:(j + 1) * 128], hTb.r, 1, 0, pbanks=(2, 3), first=(j == 0))
            else:
                make_hT(x_s.t[:], x_s.r, SR, hTb.t[:, :, 0:SR], hTb.r, 1, 0, mods_s=mods_mix.t, mods_reg=mods_mix.r, wk=wk, pbanks=(2, 3))
            NO = BS if prompt else GW - 30
            s1b = 6 if prompt else 0
            for c in range(8):
                for kc in range(8):
                    k.mm(bank(4, 128, 0, ncol), w1.t[:, kc, c * 128:(c + 1) * 128], hTb.t[:, kc, 0:ncol], kc == 0, kc == 7,
                         reads=[w1.r, hTb.r], writes=[rb[4]])
                for kc in range(8):
                    k.mm(bank(5, 128, 0, ncol), w1.t[:, kc, D + c * 128:D + (c + 1) * 128], hTb.t[:, kc, 0:ncol], kc == 0, kc == 7,
                         reads=[w1.r, hTb.r], writes=[rb[5]])
                sig = sig_p.next(); glu = glu_p.next()
                k.act(sig.t[:, 0:ncol], bank(5, 128, 0, ncol), AF.Sigmoid, reads=[rb[5], cf.r], writes=[sig.r], bias=cf.t[:, c, 35:36])
                if prompt:
                    k.cp("pool", glu.t[:, 0:30], carry.t[:, c, :], reads=[carry.r], writes=[glu.r])
                    k.stt(glu.t[:, 30:30 + BS], bank(4, 128, 0, BS), cf.t[:, c, 34:35], sig.t[:, 0:BS], ALU.add, ALU.mult,
                          reads=[rb[4], cf.r, sig.r], writes=[glu.r], accum=True)
                    k.cp("pool", carry.t[:, c, :], glu.t[:, BS:BS + 30], reads=[glu.r], writes=[carry.r])
                else:
                    gv = glu.t[:, 0:GW].rearrange("p (s j) -> p s j", j=34)
                    scv = sconv_p.next()
                    for q in range(4):
                        k.dma("sp", scv.t[:, q, :], sconv[q * 120:(q + 1) * 120, c * 128:(c + 1) * 128], writes=[scv.r], accum=(q > 0))
                    for q in range(4):
                        k.tr(bank(6, 128, q * 120, (q + 1) * 120), scv.t[:, q, :], ident[0:120, 0:120],
                             reads=[scv.r, rconst], writes=[rb[6]], accum=(q > 0))
                    k.cp("act", gv[:, :, 0:30], bank(6, 128, 0, 480).rearrange("p (s j) -> p s j", j=30), reads=[rb[6]], writes=[glu.r])
                    k.stt(gv[:, :, 30:34], bank(4, 128, 0, SR).rearrange("p (s j) -> p s j", j=4), cf.t[:, c, 34:35],
                          sig.t[:, 0:SR].rearrange("p (s j) -> p s j", j=4), ALU.add, ALU.mult, reads=[rb[4], cf.r, sig.r], writes=[glu.r], accum=True)
                    k.cp("pool", gnew.t[:, c, :].rearrange("p (s j) -> p s j", j=4), gv[:, :, 30:34], reads=[glu.r], writes=[gnew.r], accum=(c > 0))
                acc = acc_p.next()
                k.act(acc.t[:, 0:NO], glu.t[:, 0:NO], AF.Identity, reads=[glu.r, cf.r], writes=[acc.r], scale=cf.t[:, c, 0:1], bias=cf.t[:, c, 31:32])
                on_pool = c in (2, 5)
                for j in range(1, 31):
                    if on_pool:
                        tmpc = tmpc_p.next()
                        k.ts("pool", tmpc.t[:, 0:NO], glu.t[:, j:j + NO], cf.t[:, c, j:j + 1], None, ALU.mult, reads=[glu.r, cf.r], writes=[tmpc.r])
                        k.tt("pool", acc.t[:, 0:NO], acc.t[:, 0:NO], tmpc.t[:, 0:NO], ALU.add, reads=[acc.r, tmpc.r], writes=[acc.r])
                    else:
                        k.stt(acc.t[:, 0:NO], glu.t[:, j:j + NO], cf.t[:, c, j:j + 1], acc.t[:, 0:NO], ALU.mult, ALU.add,
                              reads=[glu.r, cf.r, acc.r], writes=[acc.r])
                ycb = ycb_p.next(); ysq = ysq_p.next()
                if prompt:
                    ysrc = acc.t[:, 0:BS]
                    k.cp("pool", yc.t[:, c, :], ysrc, reads=[acc.r], writes=[yc.r], accum=(c > 0))
                    k.cp("act", ycb.t[:, 0:ncol], ysrc, reads=[acc.r], writes=[ycb.r])
                    k.act(ysq.t[:, 0:ncol], ysrc, AF.Square, reads=[acc.r], writes=[ysq.r])
                else:
                    ysrc = acc.t[:, 0:GW].rearrange("p (s j) -> p s j", j=34)[:, :, 0:4]
                    k.cp("pool", yc.t[:, c, 0:SR].rearrange("p (s j) -> p s j", j=4), ysrc, reads=[acc.r], writes=[yc.r], accum=(c > 0))
                    k.cp("act", ycb.t[:, 0:ncol].rearrange("p (s j) -> p s j", j=4), ysrc, reads=[acc.r], writes=[ycb.r])
                    k.act(ysq.t[:, 0:ncol].rearrange("p (s j) -> p s j", j=4), ysrc, AF.Square, reads=[acc.r], writes=[ysq.r])
                k.mm(bank(s1b, 128, 0, ncol), onesb[:], ycb.t[:, 0:ncol], c == 0, c == 7, reads=[rconst, ycb.r], writes=[rb[s1b]], inc=True)
                k.mm(bank(7, 128, 0, ncol), onesb[:], ysq.t[:, 0:ncol], c == 0, c == 7, reads=[rconst, ysq.r], writes=[rb[7]], inc=True)
            k.act(mean.t[:, 0:ncol], bank(s1b, 128, 0, ncol), AF.Identity, reads=[rb[s1b]], writes=[mean.r], scale=1.0 / D)
            k.tt("pool", rstd.t[:, 0:ncol], mean.t[:, 0:ncol], mean.t[:, 0:ncol], ALU.mult, reads=[mean.r], writes=[rstd.r])
            k.stt(rstd.t[:, 0:ncol], bank(7, 128, 0, ncol), 1.0 / D, rstd.t[:, 0:ncol], ALU.mult, ALU.subtract, reads=[rb[7], rstd.r], writes=[rstd.r])
            k.ts("dve", rstd.t[:, 0:ncol], rstd.t[:, 0:ncol], LN_EPS, None, ALU.add, reads=[rstd.r], writes=[rstd.r])
            k.act(rstd.t[:, 0:ncol], rstd.t[:, 0:ncol], AF.Sqrt, reads=[rstd.r], writes=[rstd.r])
            k.op("dve", lambda e, ncol=ncol: e.reciprocal(rstd.t[:, 0:ncol], rstd.t[:, 0:ncol]), reads=[rstd.r], writes=[rstd.r])
            for c in range(8):
                xn = xn_p.next()
                k.tt("pool", xn.t[:, 0:ncol], yc.t[:, c, 0:ncol], mean.t[:, 0:ncol], ALU.subtract, reads=[yc.r, mean.r], writes=[xn.r])
                k.tt("dve", xn.t[:, 0:ncol], xn.t[:, 0:ncol], rstd.t[:, 0:ncol], ALU.mult, reads=[xn.r, rstd.r], writes=[xn.r])
                k.act(sT.t[:, c, 0:ncol], xn.t[:, 0:ncol], AF.Silu, reads=[xn.r, cf.r], writes=[sT.r], accum=(c > 0),
                      scale=cf.t[:, c, 32:33], bias=cf.t[:, c, 33:34])
            for j in range(TPB if prompt else 1):
                rows = 128 if prompt else SR
                t = TPB * B + j
                bp = 4 * (j % 2) if prompt else 2
                for half in range(2):
                    for c in range(8):
                        k.mm(bank(bp + half, rows), sT.t[:, c, j * 128:j * 128 + rows], w2.t[:, c, half * 512:(half + 1) * 512], c == 0, c == 7,
                             reads=[sT.r, w2.r], writes=[rb[bp + half]])
                if prompt:
                    xt_ap, xt_reg, gate_ap, gate_reg = x_all[:, t, :], rx[t], g1p.t[:], g1p.r
                else:
                    xt_ap, xt_reg, gate_ap, gate_reg = x_s.t[:], x_s.r, mods_mix.t[:, 2 * D:3 * D], mods_mix.r
                post_norm(ps[0:rows, bp * 512:bp * 512 + D], [rb[bp], rb[bp + 1]], xt_ap, xt_reg, rows, gate_ap, gate_reg,
                          lng.t, lnb.t, lng.r, wk, bias_ap=b2.t, bias_reg=b2.r)
            if B == NB - 1:
                cp_tok = wk["xn"].next()
                for c in range(8):
                    k.tr(bank(2 + c // 4, 30, (c % 4) * 128, (c % 4 + 1) * 128), carry.t[:, c, :], ident[:], reads=[carry.r, rconst],
                         writes=[rb[2 + c // 4]], accum=(c % 4 > 0))
                k.cp("act", cp_tok.t[0:30, :], ps[0:30, 2 * 512:2 * 512 + D], reads=[rb[2], rb[3]], writes=[cp_tok.r])
                k.dma("sp", cpo, cp_tok.t[0:30, :], reads=[cp_tok.r], is_output=True)
        r_cso = Reg()
        k.dma("sp", cso.rearrange("(s j) f -> s j f", j=30)[:, 0:26, :], sconv.rearrange("(s j) f -> s j f", j=30)[:, 4:30, :],
              reads=[], writes=[r_cso], is_output=True)
        cs_tok = wk["xn"].next()
        for c in range(8):
            k.tr(bank(2 + c // 4, SR, (c % 4) * 128, (c % 4 + 1) * 128), gnew.t[:, c, :], ident[:], reads=[gnew.r, rconst],
                 writes=[rb[2 + c // 4]], accum=(c % 4 > 0))
        k.cp("act", cs_tok.t[0:SR, :], ps[0:SR, 2 * 512:2 * 512 + D], reads=[rb[2], rb[3]], writes=[cs_tok.r])
        for sq in range(NS):
            k.dma("sp", cso[sq * 30 + 26:sq * 30 + 30, :], cs_tok.t[sq * 4:sq * 4 + 4, :], reads=[cs_tok.r], is_output=True)
        k.release()

    k.limit = k.sb_top
    k.mark()
    rdm = cload([128, 4, 128], rdm_d); rqd = cload([128, 4, 128], rqd_d); rkd = cload([128, 4], rkd_d)
    rdms = cload([64, 4, 64], rdms_d); rqds = cload([128, 4, 64], rqds_d); rkds = cload([64, 4], rkds_d)
    blk = cload([64, 16], blk_d); tri = cload([128, 128], tri_d); nmask = cload([64, 16, 16], nmask_d)
    idx = k.buf([128, NS * 16], I32)
    pti = cload([128, NS * 16], ptd.partition_broadcast(128), dt=I32)
    idxf = k.sb([128, NS * 16], F32)
    k.cp("dve", idxf[:], pti[:], reads=[rconst], writes=[idx.r])
    k.stt(idxf[:], idxf[:], 128.0, iota[:].broadcast_to([128, NS * 16]), ALU.mult, ALU.add, reads=[rconst, idx.r], writes=[idx.r])
    k.cp("dve", idx.t[:], idxf[:], reads=[idx.r], writes=[idx.r])
    mods_mix0 = k.buf([SR, 3 * D], F32, "mods_mix")
    g1p0 = k.buf([128, D], F32, "g1p")
    adaln(0, mods_mix0, g1p0)
    omT = k.buf([128, 4, T + SR], BF16, "omT")
    orT = k.buf([128, 4, T + SR], BF16, "orT")
    pass_moba(mods_mix0, omT)
    if stage >= 4:
        pass_ret(mods_mix0, orT)
    k.limit = XOFF
    if stage >= 5:
        pass_out(mods_mix0, g1p0, orT, omT)
    k.release()
    if stage >= 6:
        ffn(0, final=False)
    if stage >= 7:
        k.mark()
        mods_mix1 = k.buf([SR, 3 * D], F32, "mods_mix")
        g1p1 = k.buf([128, D], F32, "g1p")
        adaln(1, mods_mix1, g1p1)
        conformer(mods_mix1, g1p1)
        k.release()
    if stage >= 8:
        ffn(1, final=True)
    k.finish()
    print("instructions:", k.ninst, "sems:", k.nsem, "sbuf_off:", k.sb_off)
    return nc


def _consts():
    f32 = np.float32
    c = {}
    c["ident"] = np.eye(128, dtype=f32)
    c["iota"] = np.arange(128, dtype=f32).reshape(128, 1)
    theta = f32(10000.0)
    inv_m = (theta ** (-np.arange(0, 128, 2, dtype=f32) / f32(128))).astype(f32)
    inv_r = (f32(1.0) / (theta ** np.linspace(0.0, 1.0, 64, dtype=f32))).astype(f32)
    rot = np.zeros((17, 128, 4, 128), f32)
    for t in range(17):
        if t < 16:
            pos = (t * 128 + np.arange(128)).astype(f32)
        else:
            pos = (2048 + (np.arange(128) % 4)).astype(f32)
        am = (pos[:, None] * inv_m[None, :]).astype(f32)
        ar = (pos[:, None] * inv_r[None, :]).astype(f32)
        cm, sm = np.cos(am).astype(f32), np.sin(am).astype(f32)
        cr, sr = np.cos(ar).astype(f32), np.sin(ar).astype(f32)
        rot[t, :, 0, 0::2] = cr; rot[t, :, 0, 1::2] = cr
        rot[t, :, 1, 0::2] = -sr; rot[t, :, 1, 1::2] = sr
        rot[t, :, 2, 0:64] = cm; rot[t, :, 2, 64:128] = cm
        rot[t, :, 3, 0:64] = -sm; rot[t, :, 3, 64:128] = sm
    c["rot"] = rot
    lg = np.array(LOGG, dtype=np.float64)
    i = np.arange(128, dtype=np.float64)
    rdm = np.zeros((128, 4, 128), np.float64)
    for h in range(4):
        diff = i[None, :] - i[:, None]
        rdm[:, h, :] = np.where(diff >= 0, np.exp(np.maximum(diff, 0) * lg[h]), 0.0) * SCALE
    c["rdm"] = rdm.astype(f32)
    c["rqd"] = np.broadcast_to(np.exp((i[None, None, :] + 1.0) * lg[None, :, None]), (128, 4, 128)).astype(f32).copy()
    c["rkd"] = (np.exp((127.0 - i)[:, None] * lg[None, :]) * SCALE).astype(f32)
    r = np.arange(64)
    seq, ii = r // 4, (r % 4).astype(np.float64)
    rdms = np.zeros((64, 4, 64), np.float64)
    for h in range(4):
        diff = ii[None, :] - ii[:, None]
        same = seq[None, :] == seq[:, None]
        rdms[:, h, :] = np.where(same & (diff >= 0), np.exp(np.maximum(diff, 0) * lg[h]), 0.0) * SCALE
    c["rdms"] = rdms.astype(f32)
    c["rqds"] = np.broadcast_to(np.exp((ii[None, None, :] + 1.0) * lg[None, :, None]), (128, 4, 64)).astype(f32).copy()
    c["rkds"] = (np.exp((3.0 - ii)[:, None] * lg[None, :]) * SCALE).astype(f32)
    blk = np.zeros((64, 16), f32)
    blk[r, seq] = 1.0
    c["blk"] = blk
    tri = np.where(np.arange(128)[None, :] <= np.arange(128)[:, None], 0.0, NEG).astype(f32)
    c["tri"] = tri
    nm = np.full((64, 16, 16), NEG, f32)
    for b in range(16):
        for tq in range(4):
            for kk in range(tq + 1):
                nm[b * 4 + kk, b, tq::4] = 0.0
    c["nmask"] = nm
    Ep = np.zeros((18, 128), f32); Ep[0, :] = 1.0; Ep[17, :] = 1.0
    Es = np.zeros((18, 64), f32); Es[17, :] = 1.0
    for s in range(16):
        Es[1 + s, 4 * s:4 * s + 4] = 1.0
    ep = np.zeros((18, 1), f32); ep[0, 0] = 1.0; ep[17, 0] = 1.0
    c["Ep"], c["Es"], c["ep"] = Ep, Es, ep
    return c


def make_in_maps(x_prompt, x_sample, cache_k, cache_v, state_ret, state_conv, state_ffn, page_table, c_prompt, c_sample,
                 ab_w_in, ab_w_out, cf_w_pw1, cf_b_pw1, cf_w_dw, cf_b_dw, cf_ln_g, cf_ln_b, cf_w_pw2, cf_b_pw2,
                 ffn_w_up, ffn_w_dw, ffn_b_dw, ffn_w_down, ada_w, ada_b, ln_g, ln_b):
    A = lambda a: np.ascontiguousarray(np.asarray(a))
    consts = _consts()
    ck = A(cache_k).reshape(-1, 512)
    cv = A(cache_v).reshape(-1, 512)
    cfp = A(np.concatenate([np.asarray(cf_w_dw)[0], np.asarray(cf_b_dw)[0][None], np.asarray(cf_ln_g)[0][None],
                            np.asarray(cf_ln_b)[0][None], np.asarray(cf_b_pw1)[0].reshape(2, D)], axis=0))
    ffp = A(np.concatenate([np.asarray(ffn_w_dw), np.asarray(ffn_b_dw)[:, None, :]], axis=1))
    shared = {
        "ck": ck, "cv": cv, "w_in": A(ab_w_in)[0], "w_out": A(ab_w_out)[0], "pw1": A(cf_w_pw1)[0], "cfp": cfp,
        "pw2": A(cf_w_pw2)[0], "b_pw2": A(cf_b_pw2).reshape(1, D), "ffn_up": A(ffn_w_up), "ffp": ffp,
        "ffn_down": A(ffn_w_down), "ada_w": A(ada_w), "ada_b": A(ada_b), "ln_g": A(ln_g).reshape(4, D),
        "ln_b": A(ln_b).reshape(4, D),
    }
    shared.update(consts)
    maps = []
    for c in range(NCORES):
        s0, s1 = c * NS, (c + 1) * NS
        m = dict(shared)
        m["xp"] = A(x_prompt[c])
        m["xs"] = A(np.asarray(x_sample)[s0:s1].reshape(SR, D))
        m["call"] = A(np.concatenate([np.asarray(c_prompt)[c:c + 1], np.asarray(c_sample)[s0:s1]], axis=0))
        m["pt"] = A(np.asarray(page_table)[s0:s1].reshape(1, NS * 16).astype(np.int32))
        m["sret"] = A(np.asarray(state_ret)[0, s0:s1].reshape(NS * 512, 128))
        m["sconv"] = A(np.asarray(state_conv)[0, s0:s1].reshape(NS * 30, D))
        m["sffn"] = A(np.asarray(state_ffn)[:, s0:s1].reshape(2, NS * 2, DFF))
        maps.append(m)
    return maps


def assemble(results):
    R = results
    cat = lambda name: [r[name] for r in R]
    y_prompt = np.stack(cat("yp"), 0)
    y_sample = np.concatenate(cat("ys"), 0).reshape(128, 4, D)
    k_prompt = np.stack(cat("kp"), 0).reshape(1, 8, T, 4, 128)
    v_prompt = np.stack(cat("vp"), 0).reshape(1, 8, T, 4, 128)
    k_sample = np.concatenate(cat("ks"), 0).reshape(1, 128, 4, 4, 128)
    v_sample = np.concatenate(cat("vs"), 0).reshape(1, 128, 4, 4, 128)
    ret_prompt = np.stack(cat("rpo"), 0).reshape(1, 8, 4, 128, 128)
    ret_sample = np.concatenate(cat("rso"), 0).reshape(1, 128, 4, 128, 128)
    conv_prompt = np.stack(cat("cpo"), 0).reshape(1, 8, 30, D)
    conv_sample = np.concatenate(cat("cso"), 0).reshape(1, 128, 30, D)
    ffn_prompt = np.stack(cat("fpo"), 1).reshape(2, 8, 2, DFF)
    ffn_sample = np.concatenate([r["fso"].reshape(2, NS, 2, DFF) for r in R], 1)
    outs = (y_prompt, y_sample, k_prompt, v_prompt, k_sample, v_sample, ret_prompt, ret_sample,
            conv_prompt, conv_sample, ffn_prompt, ffn_sample)
    return tuple(np.ascontiguousarray(o, dtype=np.float32) for o in outs)


def kernel(**inputs):
    nc = build()
    maps = make_in_maps(**inputs)
    res = run_bass_kernel_spmd(nc, maps, core_ids=list(range(NCORES)))
    return assemble(res.results)
```

```python
import math
import numpy as np
import ml_dtypes
import concourse.bass as bass
import concourse.mybir as mybir
from concourse.bass_utils import run_bass_kernel_spmd

F32 = mybir.dt.float32
BF16 = mybir.dt.bfloat16
I32 = mybir.dt.int32
ALU = mybir.AluOpType
AF = mybir.ActivationFunctionType
AX = mybir.AxisListType

NCORES = 8
D = 1024
T = 2048
NT = 16
NS = 16
SR = 64
DFF = 2816
NFC = 22
ALPHA = 4.0 ** 0.25
LN_EPS = 1e-5
GN_EPS = 1e-6
SCALE = 128.0 ** -0.5
NEG = -1.0e30
LOGG = [math.log1p(-2.0 ** (-5.0 - h)) for h in range(4)]


class Reg:
    __slots__ = ("w", "r", "p", "dsem", "dcnt", "psum")

    def __init__(self, psum=False):
        self.psum = psum
        self.w = {}
        self.r = {}
        self.p = {}
        self.dsem = None
        self.dcnt = 0


class Buf:
    __slots__ = ("t", "r")

    def __init__(self, t):
        self.t = t
        self.r = Reg()


class Pool:
    def __init__(self, bufs):
        self.bufs = bufs
        self.i = 0

    def next(self):
        b = self.bufs[self.i % len(self.bufs)]
        self.i += 1
        return b


def _merge(dst, src):
    for s, v in src.items():
        if dst.get(s, 0) < v:
            dst[s] = v


class KB:
    def __init__(self, nc):
        self.nc = nc
        self.eng = {"pe": nc.tensor, "act": nc.scalar, "dve": nc.vector, "pool": nc.gpsimd, "sp": nc.sync}
        self.esem = {e: nc.alloc_semaphore("es_" + e) for e in ("pe", "act", "dve", "pool")}
        self.ecnt = {e: 0 for e in self.esem}
        self.seen = {e: {} for e in self.eng}
        self.nsem = 4
        self.out_toks = {}
        self.sb_off = (nc.sbuf_base + 63) // 64 * 64
        self.sb_top = nc.sbuf_top
        self.sb_marks = []
        self.uid = 0
        self.ninst = 0
        self.anchors = []
        self.limit = self.sb_top

    def barrier(self):
        for e, E in self.eng.items():
            seen = self.seen[e]
            for e2, s in self.esem.items():
                v = self.ecnt[e2]
                if v > seen.get(s, 0) and not (e == "pe" and e2 == "pe"):
                    E.wait_ge(s, v)
                    seen[s] = v
                    self.ninst += 1
            for a in self.anchors:
                if a.dcnt > seen.get(a.dsem, 0):
                    E.wait_ge(a.dsem, a.dcnt)
                    seen[a.dsem] = a.dcnt
                    self.ninst += 1

    def sb(self, shape, dtype, name=None):
        self.uid += 1
        nm = (name or "t") + "_%d" % self.uid
        esz = 2 if dtype == BF16 else 4
        n = 1
        for s in shape[1:]:
            n *= s
        nbytes = (n * esz + 63) // 64 * 64
        off = self.sb_off
        self.sb_off += nbytes
        assert self.sb_off <= self.limit, "SBUF overflow %d > %d (%s)" % (self.sb_off, self.limit, nm)
        return self.nc.alloc_sbuf_tensor_at(nm, list(shape), dtype, offset=off)

    def buf(self, shape, dtype, name=None):
        return Buf(self.sb(shape, dtype, name))

    def pool(self, n, shape, dtype, name=None):
        return Pool([self.buf(shape, dtype, name) for _ in range(n)])

    def mark(self):
        self.sb_marks.append(self.sb_off)

    def release(self):
        self.sb_off = self.sb_marks.pop()
        self.barrier()

    def _waits(self, eng, reads, writes, accum):
        need = {}
        mysem0 = self.esem.get(eng)
        for r in reads:
            _merge(need, r.w)
            if r.psum:
                for s_, v_ in r.r.items():
                    if s_ is not mysem0 and need.get(s_, 0) < v_:
                        need[s_] = v_
        for w in writes:
            if accum and not w.r:
                _merge(need, w.p)
            else:
                _merge(need, w.r)
                _merge(need, w.w)
        E = self.eng[eng]
        mysem = self.esem.get(eng)
        seen = self.seen[eng]
        for s, v in need.items():
            if eng == "pe" and s is mysem:
                continue
            if seen.get(s, 0) >= v:
                continue
            E.wait_ge(s, v)
            self.ninst += 1
            seen[s] = v

    def _update(self, tok, reads, writes, accum):
        s, v = tok
        for r in reads:
            if r.r.get(s, 0) < v:
                r.r[s] = v
        for w in writes:
            if accum and not w.r:
                if w.w.get(s, 0) < v:
                    w.w[s] = v
            else:
                p = dict(w.w)
                _merge(p, w.r)
                w.p = p
                w.w = {s: v}
                w.r = {}

    def op(self, eng, fn, reads=(), writes=(), accum=False, inc=True):
        self._waits(eng, reads, writes, accum)
        ins = fn(self.eng[eng])
        self.ninst += 1
        if inc:
            self.ecnt[eng] += 1
            ins.then_inc(self.esem[eng], 1)
        tok = (self.esem[eng], self.ecnt[eng] + (0 if inc else 1))
        self._update(tok, reads, writes, accum)
        return ins

    def _dma_any(self, q, mk, reads, writes, anchor, accum, is_output):
        if anchor is None:
            anchor = writes[0] if writes else reads[0]
        if anchor.dsem is None:
            anchor.dsem = self.nc.alloc_semaphore("ds_%d" % self.nsem)
            self.nsem += 1
            self.anchors.append(anchor)
        self._waits(q, reads, writes, accum)
        ins = mk(self.eng[q])
        self.ninst += 1
        anchor.dcnt += 16
        ins.then_inc(anchor.dsem, 16)
        tok = (anchor.dsem, anchor.dcnt)
        self._update(tok, reads, writes, accum)
        if is_output:
            self.out_toks[anchor.dsem] = anchor.dcnt
        return ins

    def dma(self, q, out, in_, reads=(), writes=(), anchor=None, accum=False, is_output=False, **kw):
        return self._dma_any(q, lambda e: e.dma_start(out=out, in_=in_, **kw), reads, writes, anchor, accum, is_output)

    def gather(self, out, table, idx_ap, reads=(), writes=(), accum=False):
        return self._dma_any(
            "pool",
            lambda e: e.indirect_dma_start(out=out, out_offset=None, in_=table,
                                           in_offset=bass.IndirectOffsetOnAxis(ap=idx_ap, axis=0)),
            reads, writes, None, accum, False)

    def finish(self):
        sp = self.eng["sp"]
        for s, v in self.out_toks.items():
            sp.wait_ge(s, v)
        for e, s in self.esem.items():
            if self.ecnt[e] > 0:
                sp.wait_ge(s, self.ecnt[e])

    def mm(self, out, lhsT, rhs, start, stop, reads=(), writes=(), inc=None):
        return self.op("pe", lambda e: e.matmul(out, lhsT, rhs, start=start, stop=stop),
                       reads, writes, accum=not start, inc=(stop if inc is None else inc))

    def tr(self, out, in_, ident, reads=(), writes=(), accum=False):
        return self.op("pe", lambda e: e.transpose(out, in_, ident), reads, writes, accum=accum)

    def act(self, out, in_, func, reads=(), writes=(), accum=False, **kw):
        return self.op("act", lambda e: e.activation(out, in_, func, **kw), reads, writes, accum=accum)

    def tt(self, eng, out, in0, in1, op, reads=(), writes=(), accum=False):
        return self.op(eng, lambda e: e.tensor_tensor(out, in0, in1, op), reads, writes, accum=accum)

    def ts(self, eng, out, in0, s1, s2, op0, op1=None, reads=(), writes=(), accum=False):
        if op1 is None:
            return self.op(eng, lambda e: e.tensor_scalar(out, in0, s1, None, op0), reads, writes, accum=accum)
        return self.op(eng, lambda e: e.tensor_scalar(out, in0, s1, s2, op0, op1), reads, writes, accum=accum)

    def stt(self, out, in0, scalar, in1, op0, op1, reads=(), writes=(), accum=False):
        return self.op("dve", lambda e: e.scalar_tensor_tensor(out, in0, scalar, in1, op0, op1), reads, writes, accum=accum)

    def cp(self, eng, out, in_, reads=(), writes=(), accum=False):
        if eng == "act":
            return self.op("act", lambda e: e.copy(out, in_), reads, writes, accum=accum)
        return self.op(eng, lambda e: e.tensor_copy(out, in_), reads, writes, accum=accum)

    def memset(self, eng, ap, val, writes=(), accum=False):
        return self.op(eng, lambda e: e.memset(ap, val), (), writes, accum=accum)


def bc(ap, axis, n):
    a = ap.unsqueeze(axis)
    shp = list(a.shape)
    shp[axis] = n
    return a.broadcast_to(shp)


def build(stage=99, nphys=2560, skip_ms=False):
    nc = bass.Bass("TRN2", target_bir_lowering=False)
    k = KB(nc)

    def din(name, shape, dt=F32):
        return nc.dram_tensor(name, list(shape), dt, kind="ExternalInput").ap()

    def dout(name, shape):
        return nc.dram_tensor(name, list(shape), F32, kind="ExternalOutput").ap()

    xp = din("xp", [T, D]); xs_d = din("xs", [SR, D]); call = din("call", [17, D])
    ck = din("ck", [nphys * 128, 512]); cv = din("cv", [nphys * 128, 512])
    ptd = din("pt", [1, NS * 16], I32)
    sret = din("sret", [NS * 4 * 128, 128]); sconv = din("sconv", [NS * 30, D]); sffn = din("sffn", [2, NS * 2, DFF])
    w_in = din("w_in", [D, 3584]); w_out = din("w_out", [D, D]); pw1 = din("pw1", [D, 2048])
    cfp_d = din("cfp", [36, D]); pw2 = din("pw2", [D, D]); b_pw2 = din("b_pw2", [1, D])
    ffn_up = din("ffn_up", [2, D, 2 * DFF]); ffp_d = din("ffp", [2, 4, DFF]); ffn_down = din("ffn_down", [2, DFF, D])
    ada_w = din("ada_w", [2, D, 6 * D]); ada_b = din("ada_b", [2, 6 * D])
    ln_g = din("ln_g", [4, D]); ln_b = din("ln_b", [4, D])
    ident_d = din("ident", [128, 128]); iota_d = din("iota", [128, 1])
    rot_d = din("rot", [17, 128, 4, 128])
    rdm_d = din("rdm", [128, 4, 128]); rqd_d = din("rqd", [128, 4, 128]); rkd_d = din("rkd", [128, 4])
    rdms_d = din("rdms", [64, 4, 64]); rqds_d = din("rqds", [128, 4, 64]); rkds_d = din("rkds", [64, 4])
    blk_d = din("blk", [64, 16]); tri_d = din("tri", [128, 128]); nmask_d = din("nmask", [64, 16, 16])
    Ep_d = din("Ep", [18, 128]); Es_d = din("Es", [18, 64]); ep_d = din("ep", [18, 1])

    yp = dout("yp", [T, D]); ys = dout("ys", [SR, D])
    kp = dout("kp", [T, 512]); vp = dout("vp", [T, 512]); ks = dout("ks", [SR, 512]); vs = dout("vs", [SR, 512])
    rpo = dout("rpo", [512, 128]); rso = dout("rso", [NS * 512, 128])
    cpo = dout("cpo", [30, D]); cso = dout("cso", [NS * 30, D])
    fpo = dout("fpo", [2, 2, DFF]); fso = dout("fso", [2, NS * 2, DFF])

    sc_mods = [nc.dram_tensor("sc_mods%d" % l, [SR, 3 * D], F32).ap() for l in range(2)]
    sc_g2p = [nc.dram_tensor("sc_g2p%d" % l, [1, D], F32).ap() for l in range(2)]
    r_scm = [Reg(), Reg()]
    r_scg = [Reg(), Reg()]

    ps = nc.alloc_psum_tensor("ps", [128, 4096], F32)
    psb = ps[:].bitcast(BF16)
    rb = [Reg(psum=True) for _ in range(8)]

    def bank(i, rows=128, c0=0, c1=512):
        return ps[0:rows, i * 512 + c0:i * 512 + c1]

    def bankb(i, rows=128, c0=0, c1=1024):
        return psb[0:rows, i * 1024 + c0:i * 1024 + c1]

    rconst = Reg()

    def cload(shape, src, dt=F32, q="sp"):
        t = k.sb(shape, dt)
        k.dma(q, t[:], src, writes=[rconst], anchor=rconst, accum=True)
        return t

    ident = cload([128, 128], ident_d)
    iota = cload([128, 1], iota_d)
    Ep = cload([18, 128], Ep_d); Es = cload([18, 64], Es_d); ep = cload([18, 1], ep_d)
    identb_t = k.sb([128, 128], BF16)
    onesb = k.sb([128, 128], BF16)
    onesf = k.sb([128, 128], F32)
    k.memset("pool", onesf[:], 1.0, writes=[rconst], accum=True)
    k.cp("dve", identb_t[:], ident[:], reads=[rconst], writes=[rconst], accum=True)
    k.memset("pool", onesb[:], 1.0, writes=[rconst], accum=True)
    XOFF = (k.sb_top - NT * D * 4) // 64 * 64
    x_all = nc.alloc_sbuf_tensor_at("x_all", [128, NT, D], F32, offset=XOFF)
    rx = [Reg() for _ in range(NT)]
    x_s = k.buf([SR, D], F32)
    k.dma("sp", x_s.t[:], xs_d, writes=[x_s.r])
    modT = k.buf([128, 2, 6, 8], F32)
    scT = k.buf([128, 8, 32], BF16)
    k.mark()
    callsb = cload([17, D], call)
    scs = k.buf([17, D], F32)
    k.act(scs.t[:], callsb[:], AF.Silu, reads=[rconst], writes=[scs.r])
    for c in range(8):
        k.tr(bank(0, 128, c * 32, c * 32 + 17), scs.t[0:17, c * 128:(c + 1) * 128], ident[0:17, 0:17],
             reads=[scs.r, rconst], writes=[rb[0]], accum=(c > 0))
    k.cp("dve", scT.t[:, :, 0:17], bank(0, 128, 0, 256).rearrange("p (c j) -> p c j", j=32)[:, :, 0:17],
         reads=[rb[0]], writes=[scT.r])
    k.release()

    def load_w_bf16(dst_ap, dst_reg, src_ap, stg, eng="pool", first=True, mul=None, mul_reg=None):
        shp = list(src_ap.shape)
        if len(shp) == 3:
            st = stg.t[:, 0:shp[1] * shp[2]].rearrange("p (a b) -> p a b", b=shp[2])
        else:
            st = stg.t[:, 0:shp[1]]
        k.dma("sp", st, src_ap, writes=[stg.r])
        if mul is None:
            k.cp(eng, dst_ap, st, reads=[stg.r], writes=[dst_reg], accum=not first)
        else:
            k.tt(eng, dst_ap, st, mul, ALU.mult, reads=[stg.r, mul_reg], writes=[dst_reg], accum=not first)

    def layer_norm(xin, xin_reg, rows, gam, bet, gb_reg, out_ap, out_reg, wk):
        st = wk["st"].next(); mv = wk["mv"].next()
        xv = xin.rearrange("p (c f) -> p c f", f=512)
        for c in range(2):
            k.op("dve", lambda e, c=c: e.bn_stats(st.t[0:rows, c, :], xv[:, c, :]), reads=[xin_reg], writes=[st.r], accum=(c > 0))
        k.op("dve", lambda e: e.bn_aggr(mv.t[0:rows, 0:2], st.t[0:rows, :, :]), reads=[st.r], writes=[mv.r])
        k.ts("dve", mv.t[0:rows, 2:3], mv.t[0:rows, 1:2], LN_EPS, None, ALU.add, reads=[mv.r], writes=[mv.r])
        k.act(mv.t[0:rows, 2:3], mv.t[0:rows, 2:3], AF.Sqrt, reads=[mv.r], writes=[mv.r])
        k.op("dve", lambda e: e.reciprocal(mv.t[0:rows, 2:3], mv.t[0:rows, 2:3]), reads=[mv.r], writes=[mv.r])
        k.stt(mv.t[0:rows, 3:4], mv.t[0:rows, 0:1], -1.0, mv.t[0:rows, 2:3], ALU.mult, ALU.mult, reads=[mv.r], writes=[mv.r])
        xn = wk["xn"].next()
        k.act(xn.t[0:rows, :], xin, AF.Identity, reads=[xin_reg, mv.r], writes=[xn.r], scale=mv.t[0:rows, 2:3], bias=mv.t[0:rows, 3:4])
        k.tt("pool", xn.t[0:rows, :], xn.t[0:rows, :], gam[0:rows, :], ALU.mult, reads=[xn.r, gb_reg], writes=[xn.r])
        k.tt("pool", out_ap, xn.t[0:rows, :], bet[0:rows, :], ALU.add, reads=[xn.r, gb_reg], writes=[out_reg])

    def make_hT(xin, xin_reg, rows, hT_ap, hT_reg, layer, which, mods_s=None, mods_reg=None, wk=None, pbanks=(0, 1), first=True):
        if rows == 128:
            src, src_reg = xin, xin_reg
        else:
            hs = wk["hs"].next()
            k.tt("dve", hs.t[0:rows], xin, mods_s[:, D:2 * D], ALU.mult, reads=[xin_reg, mods_reg], writes=[hs.r])
            k.tt("dve", hs.t[0:rows], hs.t[0:rows], mods_s[:, 0:D], ALU.add, reads=[hs.r, mods_reg], writes=[hs.r])
            src, src_reg = hs.t[0:rows], hs.r
        for c in range(8):
            b = pbanks[c // 4]
            k.tr(bank(b, 128, (c % 4) * 128, (c % 4) * 128 + rows), src[:, c * 128:(c + 1) * 128], ident[0:rows, 0:rows],
                 reads=[src_reg, rconst], writes=[rb[b]], accum=(c % 4 > 0))
        for c in range(8):
            b = pbanks[c // 4]
            pin = bank(b, 128, (c % 4) * 128, (c % 4) * 128 + rows)
            if rows == 128:
                k.act(hT_ap[:, c, :], pin, AF.Identity, reads=[rb[b], modT.r], writes=[hT_reg], accum=not (first and c == 0),
                      scale=modT.t[:, layer, which + 1, c:c + 1], bias=modT.t[:, layer, which, c:c + 1])
            else:
                k.cp("act", hT_ap[:, c, :], pin, reads=[rb[b]], writes=[hT_reg], accum=not (first and c == 0))

    def rotate(xv, xregs, rows, G, Ct, St, tab_reg, out3, out_reg, tmp3, tmp_reg, mode):
        if mode == "half":
            x4 = xv.rearrange("p g (two j) -> p g two j", two=2)
            t4 = tmp3.rearrange("p g (two j) -> p g two j", two=2)
            a0, a1 = x4[:, :, 1, :], x4[:, :, 0, :]
            d0, d1 = t4[:, :, 0, :], t4[:, :, 1, :]
            s0, s1 = St[:, 0:64], St[:, 64:128]
        else:
            x4 = xv.rearrange("p g (j two) -> p g j two", two=2)
            t4 = tmp3.rearrange("p g (j two) -> p g j two", two=2)
            a0, a1 = x4[:, :, :, 1], x4[:, :, :, 0]
            d0, d1 = t4[:, :, :, 0], t4[:, :, :, 1]
            Sv = St.rearrange("p (j two) -> p j two", two=2)
            s0, s1 = Sv[:, :, 0], Sv[:, :, 1]
        k.tt("dve", d0, a0, bc(s0, 1, G), ALU.mult, reads=list(xregs) + [tab_reg], writes=[tmp_reg])
        k.tt("dve", d1, a1, bc(s1, 1, G), ALU.mult, reads=list(xregs) + [tab_reg], writes=[tmp_reg], accum=True)
        k.tt("dve", out3, xv, bc(Ct, 1, G), ALU.mult, reads=list(xregs) + [tab_reg], writes=[out_reg])
        k.tt("pool", out3, out3, tmp3, ALU.add, reads=[out_reg, tmp_reg], writes=[out_reg])

    def adaln(l, mods_mix, g1p):
        k.mark()
        stg = k.pool(2, [128, 8 * 512], F32, "ada_stg")
        wbp = k.pool(2, [128, 8, 512], BF16, "ada_wb")
        m17p = k.pool(2, [18, 512], F32, "m17")
        outp = k.pool(2, [128, 512], F32, "ada_out")
        for j in range(12):
            which, half = divmod(j, 2)
            wb = wbp.next()
            load_w_bf16(wb.t[:], wb.r, ada_w[l, :, j * 512:(j + 1) * 512].rearrange("(kc p) n -> p kc n", p=128), stg.next())
            m17 = m17p.next()
            k.dma("sp", m17.t[17:18, :], ada_b[l:l + 1, j * 512:(j + 1) * 512], writes=[m17.r])
            for kc in range(8):
                k.mm(bank(0, 17), scT.t[:, kc, 0:17], wb.t[:, kc, :], kc == 0, kc == 7, reads=[scT.r, wb.r], writes=[rb[0]])
            k.cp("act", m17.t[0:17, :], bank(0, 17), reads=[rb[0]], writes=[m17.r], accum=True)
            plus1 = 1.0 if which in (1, 2, 4, 5) else 0.0
            k.mm(bank(1, 64), Es[:], m17.t[:], True, True, reads=[rconst, m17.r], writes=[rb[1]])
            if which < 3:
                k.ts("dve", mods_mix.t[:, j * 512:(j + 1) * 512], bank(1, 64), plus1, None, ALU.add,
                     reads=[rb[1]], writes=[mods_mix.r], accum=(j > 0))
            else:
                o = outp.next()
                k.ts("dve", o.t[0:64, :], bank(1, 64), plus1, None, ALU.add, reads=[rb[1]], writes=[o.r])
                k.dma("sp", sc_mods[l][:, (j - 6) * 512:(j - 5) * 512], o.t[0:64, :], reads=[o.r], writes=[r_scm[l]], accum=(j > 6))
            if which in (2, 5):
                k.mm(bank(2), Ep[:], m17.t[:], True, True, reads=[rconst, m17.r], writes=[rb[2]])
                if which == 2:
                    k.ts("dve", g1p.t[:, half * 512:(half + 1) * 512], bank(2), 1.0, None, ALU.add,
                         reads=[rb[2]], writes=[g1p.r], accum=(half > 0))
                else:
                    o = outp.next()
                    k.ts("dve", o.t[:, :], bank(2), 1.0, None, ALU.add, reads=[rb[2]], writes=[o.r])
                    k.dma("sp", sc_g2p[l][:, half * 512:(half + 1) * 512], o.t[0:1, :], reads=[o.r], writes=[r_scg[l]], accum=(half > 0))
            for cc in range(4):
                k.mm(bank(3, 128, cc, cc + 1), m17.t[:, cc * 128:(cc + 1) * 128], ep[:], True, True,
                     reads=[m17.r, rconst], writes=[rb[3]])
            k.ts("dve", modT.t[:, l, which, half * 4:(half + 1) * 4], bank(3, 128, 0, 4), plus1, None, ALU.add,
                 reads=[rb[3]], writes=[modT.r], accum=True)
        k.release()

    def pass_moba(mods_mix, omT):
        k.mark()
        wm = k.buf([128, 8, 1536], BF16, "wm")
        qTs_b = k.buf([128, 4, SR], BF16, "qTs_b"); qTs_f = k.buf([128, 4, SR], F32, "qTs_f")
        kTs_b = k.buf([128, 4, SR], BF16, "kTs_b"); v_s = k.buf([SR, 4, 132], BF16, "v_s")
        k.mark()
        kT_hist = k.buf([128, 4, T], BF16, "kT_hist")
        v_hist = k.buf([128, NT, 512], BF16, "v_hist")
        kmT = k.buf([128, 4, 8], F32, "kmT")
        k.mark()
        stg = k.pool(2, [128, 8 * 512], F32, "stg")
        for g in range(3):
            load_w_bf16(wm.t[:, :, g * 512:(g + 1) * 512], wm.r,
                        w_in[:, 2048 + g * 512:2048 + (g + 1) * 512].rearrange("(kc p) n -> p kc n", p=128),
                        stg.next(), first=(g == 0))
        k.release()
        wk = {"hs": k.pool(1, [SR, D], F32, "hs")}
        xt_p = k.pool(2, [128, D], F32, "xt")
        hT_p = k.pool(2, [128, 8, 128], BF16, "hT")
        rot_p = k.pool(2, [128, 4, 128], F32, "rot")
        qk_p = k.pool(2, [128, 8, 128], F32, "qkrot")
        tmp_p = k.pool(1, [128, 8, 128], F32, "rtmp")
        vf_p = k.pool(2, [128, 512], F32, "vf")
        qTb_p = k.pool(2, [128, 4, 128], BF16, "qTb")
        qTf_p = k.pool(2, [128, 4, 128], F32, "qTf")
        ksum_p = k.pool(2, [128, 4], F32, "ksum")
        gate_p = k.pool(1, [128, 4, 8], F32, "gate")
        cmp_p = k.pool(1, [128, 4, 8, 8], F32, "cmp")
        bias_p = k.pool(2, [128, 4, 8], F32, "bias")
        S_p = k.pool(1, [128, T], F32, "S")
        P_p = k.pool(2, [128, T], BF16, "P")
        PT_p = k.pool(2, [128, NT, 128], BF16, "PT")
        sm_p = k.pool(4, [128, 4], F32, "sm")
        om_p = k.pool(2, [128, 512], BF16, "om")

        for t in range(NT + 1):
            rows = 128 if t < NT else SR
            if t < NT:
                xt = xt_p.next()
                k.dma("sp", xt.t[:], xp[t * 128:(t + 1) * 128, :], writes=[xt.r])
                xin, xin_reg = xt.t[:], xt.r
            else:
                xin, xin_reg = x_s.t[:], x_s.r
            hT = hT_p.next()
            make_hT(xin, xin_reg, rows, hT.t[:, :, 0:rows], hT.r, 0, 0, mods_s=mods_mix.t, mods_reg=mods_mix.r, wk=wk, pbanks=(0, 1))
            rot = rot_p.next()
            k.dma("sp", rot.t[0:rows], rot_d[t, 0:rows], writes=[rot.r])
            for g in range(3):
                for kc in range(8):
                    k.mm(bank(4 + g, rows), hT.t[:, kc, 0:rows], wm.t[:, kc, g * 512:(g + 1) * 512], kc == 0, kc == 7,
                         reads=[hT.r, wm.r], writes=[rb[4 + g]])
            qk = qk_p.next(); tmp = tmp_p.next()
            zqk = ps[0:rows, 4 * 512:6 * 512].rearrange("p (g d) -> p g d", d=128)
            rotate(zqk, [rb[4], rb[5]], rows, 8, rot.t[0:rows, 2, :], rot.t[0:rows, 3, :], rot.r,
                   qk.t[0:rows], qk.r, tmp.t[0:rows], tmp.r, "half")
            vf = vf_p.next()
            k.cp("act", vf.t[0:rows], bank(6, rows), reads=[rb[6]], writes=[vf.r])
            kdst = kp[t * 128:(t + 1) * 128, :] if t < NT else ks
            vdst = vp[t * 128:(t + 1) * 128, :] if t < NT else vs
            k.dma("sp", kdst, qk.t[0:rows, 4:8, :].rearrange("p g d -> p (g d)"), reads=[qk.r], is_output=True)
            k.dma("sp", vdst, vf.t[0:rows], reads=[vf.r], is_output=True)
            if stage < 2:
                continue
            if t < NT:
                k.cp("pool", v_hist.t[:, t, :], vf.t[:], reads=[vf.r], writes=[v_hist.r], accum=(t > 0))
            else:
                k.memset("pool", v_s.t[:], 1.0, writes=[v_s.r])
                k.cp("pool", v_s.t[:, :, 0:128], vf.t[0:SR].rearrange("p (h d) -> p h d", d=128), reads=[vf.r], writes=[v_s.r])
            for g in range(8):
                b = 2 + g // 4
                k.tr(bank(b, 128, (g % 4) * 128, (g % 4) * 128 + rows), qk.t[0:rows, g, :], ident[0:rows, 0:rows],
                     reads=[qk.r, rconst], writes=[rb[b]], accum=(g % 4 > 0))
            qv = bank(2).rearrange("p (h s) -> p h s", s=128)[:, :, 0:rows]
            kv = bank(3).rearrange("p (h s) -> p h s", s=128)[:, :, 0:rows]
            if t < NT:
                qTb = qTb_p.next(); qTf = qTf_p.next()
                k.cp("act", qTb.t[:], qv, reads=[rb[2]], writes=[qTb.r])
                k.cp("dve", qTf.t[:], qv, reads=[rb[2]], writes=[qTf.r])
                k.cp("act", kT_hist.t[:, :, t * 128:(t + 1) * 128], kv, reads=[rb[3]], writes=[kT_hist.r], accum=(t > 0))
                ksum = ksum_p.next()
                k.op("dve", lambda e, ksum=ksum, kv=kv: e.tensor_reduce(ksum.t[:], kv, axis=AX.X, op=ALU.add), reads=[rb[3]], writes=[ksum.r])
                if t % 2 == 0:
                    k.cp("pool", kmT.t[:, :, t // 2], ksum.t[:], reads=[ksum.r], writes=[kmT.r], accum=True)
                else:
                    k.tt("pool", kmT.t[:, :, t // 2], kmT.t[:, :, t // 2], ksum.t[:], ALU.add, reads=[ksum.r, kmT.r], writes=[kmT.r])
            else:
                k.cp("act", qTs_b.t[:], qv, reads=[rb[2]], writes=[qTs_b.r])
                k.cp("dve", qTs_f.t[:], qv, reads=[rb[2]], writes=[qTs_f.r])
                k.cp("act", kTs_b.t[:], kv, reads=[rb[3]], writes=[kTs_b.r])
                continue
            own = t // 2
            bias = None
            if own >= 4:
                for h in range(4):
                    k.mm(bank(7, 128, 448 + h * 8, 448 + h * 8 + own), qTf.t[:, h, :], kmT.t[:, h, 0:own], True, True,
                         reads=[qTf.r, kmT.r], writes=[rb[7]])
                gate = gate_p.next(); cmpb = cmp_p.next(); bias = bias_p.next()
                gpv = bank(7, 128, 448, 480).rearrange("p (h n) -> p h n", n=8)[:, :, 0:own]
                k.cp("act", gate.t[:, :, 0:own], gpv, reads=[rb[7]], writes=[gate.r])
                gv = gate.t[:, :, 0:own]
                k.tt("dve", cmpb.t[:, :, 0:own, 0:own], bc(gv, 2, own), bc(gv, 3, own), ALU.is_gt, reads=[gate.r], writes=[cmpb.r])
                k.op("dve", lambda e, gate=gate, cmpb=cmpb, own=own: e.tensor_reduce(gate.t[:, :, 0:own], cmpb.t[:, :, 0:own, 0:own], axis=AX.X, op=ALU.add),
                     reads=[cmpb.r], writes=[gate.r])
                k.ts("dve", bias.t[:, :, 0:own], gate.t[:, :, 0:own], 2.5, NEG, ALU.is_gt, ALU.mult, reads=[gate.r], writes=[bias.r])
            nk = (t + 1) * 128
            om = om_p.next()
            for h in range(4):
                for c0 in range(0, nk, 512):
                    c1 = min(nk, c0 + 512)
                    b = c0 // 512
                    k.mm(bank(b, 128, 0, c1 - c0), qTb.t[:, h, :], kT_hist.t[:, h, c0:c1], True, True,
                         reads=[qTb.r, kT_hist.r], writes=[rb[b]])
                nb_used = (nk + 511) // 512
                sregs = [rb[i] for i in range(nb_used)]
                S = S_p.next()
                npast = own * 256
                first = True
                if npast > 0:
                    if bias is not None:
                        k.tt("dve", S.t[:, 0:npast].rearrange("p (n s) -> p n s", s=256),
                             ps[:, 0:npast].rearrange("p (n s) -> p n s", s=256), bc(bias.t[:, h, 0:own], 2, 256), ALU.add,
                             reads=sregs + [bias.r], writes=[S.r])
                    else:
                        k.cp("act", S.t[:, 0:npast], ps[:, 0:npast], reads=sregs, writes=[S.r])
                    first = False
                if t % 2 == 1:
                    k.cp("act", S.t[:, npast:npast + 128], ps[:, npast:npast + 128], reads=sregs, writes=[S.r], accum=not first)
                    first = False
                k.tt("dve", S.t[:, nk - 128:nk], ps[:, nk - 128:nk], tri[:], ALU.add, reads=sregs + [rconst], writes=[S.r], accum=not first)
                sm = sm_p.next()
                k.op("dve", lambda e, sm=sm, S=S, nk=nk: e.reduce_max(sm.t[:, 0:1], S.t[:, 0:nk], axis=AX.X), reads=[S.r], writes=[sm.r])
                k.ts("dve", sm.t[:, 1:2], sm.t[:, 0:1], -SCALE, None, ALU.mult, reads=[sm.r], writes=[sm.r])
                P = P_p.next()
                k.act(P.t[:, 0:nk], S.t[:, 0:nk], AF.Exp, reads=[S.r, sm.r], writes=[P.r, sm.r], scale=SCALE, bias=sm.t[:, 1:2],
                      accum_out=sm.t[:, 2:3])
                PT = PT_p.next()
                for j0 in range(0, t + 1, 8):
                    j1 = min(t + 1, j0 + 8)
                    for j in range(j0, j1):
                        k.tr(bankb(6, 128, (j - j0) * 128, (j - j0 + 1) * 128), P.t[:, j * 128:(j + 1) * 128], identb_t[:],
                             reads=[P.r, rconst], writes=[rb[6]], accum=(j > j0))
                    k.cp("act" if (j0 // 8) % 2 == 0 else "dve", PT.t[:, j0:j1, :],
                         bankb(6, 128, 0, (j1 - j0) * 128).rearrange("p (j s) -> p j s", s=128), reads=[rb[6]], writes=[PT.r], accum=(j0 > 0))
                for j in range(t + 1):
                    k.mm(bank(7, 128, 0, 128), PT.t[:, j, :], v_hist.t[:, j, h * 128:(h + 1) * 128],
                         j == 0, j == t, reads=[PT.r, v_hist.r], writes=[rb[7]])
                k.op("dve", lambda e, sm=sm: e.reciprocal(sm.t[:, 3:4], sm.t[:, 2:3]), reads=[sm.r], writes=[sm.r])
                k.act(om.t[:, h * 128:(h + 1) * 128], bank(7, 128, 0, 128), AF.Identity, reads=[rb[7], sm.r], writes=[om.r], accum=(h > 0),
                      scale=sm.t[:, 3:4])
            for h in range(4):
                k.tr(bankb(6, 128, h * 128, (h + 1) * 128), om.t[:, h * 128:(h + 1) * 128], identb_t[:], reads=[om.r, rconst],
                     writes=[rb[6]], accum=(h > 0))
            k.cp("act", omT.t[:, :, t * 128:(t + 1) * 128], bankb(6, 128, 0, 512).rearrange("p (h s) -> p h s", s=128),
                 reads=[rb[6]], writes=[omT.r], accum=(t > 0))
        k.release()
        if stage >= 3 and not skip_ms:
            moba_sample(qTs_b, qTs_f, kTs_b, v_s, omT)
        k.release()

    def moba_sample(qTs_b, qTs_f, kTs_b, v_s, omT):
        kpg_p = k.pool(4, [128, 512], F32, "kpg")
        vpg_p = k.pool(4, [128, 512], F32, "vpg")
        kb_p = k.pool(2, [128, 512], BF16, "kb")
        kTq_p = k.pool(2, [128, 4, T], BF16, "kTq")
        Vq_p = k.pool(2, [128, 16, 4, 132], BF16, "Vq")
        E_p = k.pool(2, [128, 17, 4, SR], BF16, "Eb")
        for b_ in Vq_p.bufs:
            k.memset("pool", b_.t[:], 1.0, writes=[b_.r])
        for b_ in E_p.bufs:
            k.memset("pool", b_.t[:], 0.0, writes=[b_.r])
        kms_p = k.pool(2, [128, 4, 8], F32, "kms")
        ksum_p = k.pool(2, [128, 4], F32, "ksum2")
        prod_p = k.pool(1, [128, 4, 8, 4], F32, "prod")
        gs_p = k.pool(1, [128, 16, 8], F32, "gs")
        cmp_p = k.pool(1, [128, 16, 8, 8], F32, "cmp2")
        comb_p = k.pool(1, [128, 16, 8], F32, "comb")
        pm_p = k.pool(1, [128, 16], F32, "pm")
        m16_p = k.pool(1, [16, 20], F32, "m16")
        X_p = k.pool(1, [128, 16, 16], F32, "X")
        Xn_p = k.pool(1, [SR, 16], F32, "Xn")
        for b in range(NS):
            kTq = kTq_p.next(); Vq = Vq_p.next(); Eb = E_p.next(); kms = kms_p.next()
            for j in range(16):
                col = b * 16 + j
                kpg = kpg_p.next(); vpg = vpg_p.next()
                k.gather(kpg.t[:], ck, idx.t[:, col:col + 1], reads=[idx.r], writes=[kpg.r])
                k.gather(vpg.t[:], cv, idx.t[:, col:col + 1], reads=[idx.r], writes=[vpg.r])
                kb = kb_p.next()
                k.cp("dve", kb.t[:], kpg.t[:], reads=[kpg.r], writes=[kb.r])
                k.cp("pool", Vq.t[:, j, :, 0:128], vpg.t[:].rearrange("p (h d) -> p h d", d=128), reads=[vpg.r], writes=[Vq.r],
                     accum=(j > 0))
                pb = 1 + j % 2
                for h in range(4):
                    k.tr(bankb(pb, 128, h * 128, (h + 1) * 128), kb.t[:, h * 128:(h + 1) * 128], identb_t[:],
                         reads=[kb.r, rconst], writes=[rb[pb]], accum=(h > 0))
                kvw = bankb(pb, 128, 0, 512).rearrange("p (h s) -> p h s", s=128)
                k.cp("act", kTq.t[:, :, j * 128:(j + 1) * 128], kvw, reads=[rb[pb]], writes=[kTq.r], accum=(j > 0))
                ksum = ksum_p.next()
                k.op("dve", lambda e, ksum=ksum, kvw=kvw: e.tensor_reduce(ksum.t[:], kvw, axis=AX.X, op=ALU.add), reads=[rb[pb]], writes=[ksum.r])
                if j % 2 == 0:
                    k.cp("pool", kms.t[:, :, j // 2], ksum.t[:], reads=[ksum.r], writes=[kms.r], accum=(j > 0))
                else:
                    k.tt("pool", kms.t[:, :, j // 2], kms.t[:, :, j // 2], ksum.t[:], ALU.add, reads=[ksum.r, kms.r], writes=[kms.r])
            prod = prod_p.next()
            qf = qTs_f.t[:, :, 4 * b:4 * b + 4]
            k.tt("dve", prod.t[:], bc(kms.t[:], 3, 4), bc(qf, 2, 8), ALU.mult, reads=[kms.r, qTs_f.r], writes=[prod.r])
            k.mm(bank(3, 128, 0, 128), onesf[:], prod.t[:].rearrange("p h n q -> p (h n q)"), True, True,
                 reads=[rconst, prod.r], writes=[rb[3]])
            gs = gs_p.next(); cmpb = cmp_p.next(); comb = comb_p.next()
            k.cp("act", gs.t[:].rearrange("p (h q) n -> p h q n", q=4),
                 bank(3, 128, 0, 128).rearrange("p (h n q) -> p h q n", h=4, n=8), reads=[rb[3]], writes=[gs.r])
            k.tt("dve", cmpb.t[:], bc(gs.t[:], 2, 8), bc(gs.t[:], 3, 8), ALU.is_gt, reads=[gs.r], writes=[cmpb.r])
            k.op("dve", lambda e, gs=gs, cmpb=cmpb: e.tensor_reduce(gs.t[:], cmpb.t[:], axis=AX.X, op=ALU.add), reads=[cmpb.r], writes=[gs.r])
            k.ts("dve", comb.t[:], gs.t[:], 2.5, NEG, ALU.is_gt, ALU.mult, reads=[gs.r], writes=[comb.r])
            for j in range(16):
                for h in range(4):
                    c0 = j * 16 + h * 4
                    k.mm(bank(0, 128, c0, c0 + 4), kTq.t[:, h, j * 128:(j + 1) * 128], qTs_b.t[:, h, 4 * b:4 * b + 4], True, True,
                         reads=[kTq.r, qTs_b.r], writes=[rb[0]], inc=(j == 15 and h == 3))
            for h in range(4):
                k.mm(bank(0, SR, 256 + h * 4, 260 + h * 4), kTs_b.t[:, h, :], qTs_b.t[:, h, 4 * b:4 * b + 4], True, True,
                     reads=[kTs_b.r, qTs_b.r], writes=[rb[0]], inc=(h == 3))
            pm = pm_p.next(); m16 = m16_p.next()
            k.op("dve", lambda e, pm=pm: e.tensor_reduce(pm.t[:], bank(0, 128, 0, 256).rearrange("p (j c) -> p c j", c=16), axis=AX.X, op=ALU.max),
                 reads=[rb[0]], writes=[pm.r])
            k.tt("dve", pm.t[0:SR, :], pm.t[0:SR, :], bank(0, SR, 256, 272), ALU.max, reads=[pm.r, rb[0]], writes=[pm.r])
            k.tr(bank(3, 16, 128, 256), pm.t[:], ident[:], reads=[pm.r, rconst], writes=[rb[3]])
            k.op("dve", lambda e, m16=m16: e.reduce_max(m16.t[:, 16:17], bank(3, 16, 128, 256), axis=AX.X), reads=[rb[3]], writes=[m16.r])
            k.ts("dve", m16.t[:, 0:16], ident[0:16, 0:16], m16.t[:, 16:17], None, ALU.mult, reads=[m16.r, rconst], writes=[m16.r])
            k.mm(bank(3, 128, 256, 272), onesf[0:16, :], m16.t[:, 0:16], True, True, reads=[rconst, m16.r], writes=[rb[3]])
            mbc = bank(3, 128, 256, 272)
            k.tt("dve", comb.t[:], comb.t[:], bc(mbc, 2, 8), ALU.subtract, reads=[comb.r, rb[3]], writes=[comb.r])
            X = X_p.next(); Xn = Xn_p.next()
            k.tt("dve", X.t[:].rearrange("p (n two) c -> p n two c", two=2),
                 bank(0, 128, 0, 256).rearrange("p (n two c) -> p n two c", two=2, c=16),
                 bc(comb.t[:].rearrange("p c n -> p n c"), 2, 2), ALU.add, reads=[rb[0], comb.r], writes=[X.r])
            k.tt("dve", Xn.t[:], bank(0, SR, 256, 272), nmask[:, b, :], ALU.add, reads=[rb[0], rconst], writes=[Xn.r])
            k.tt("dve", Xn.t[:], Xn.t[:], bank(3, SR, 256, 272), ALU.subtract, reads=[Xn.r, rb[3]], writes=[Xn.r])
            k.act(Eb.t[:, 0:16, :, 4 * b:4 * b + 4], X.t[:].rearrange("p j (h q) -> p j h q", q=4), AF.Exp, reads=[X.r], writes=[Eb.r], scale=SCALE)
            k.act(Eb.t[0:SR, 16, :, 4 * b:4 * b + 4], Xn.t[:].rearrange("p (h q) -> p h q", q=4), AF.Exp, reads=[Xn.r], writes=[Eb.r], scale=SCALE)
            for h in range(4):
                ob = bank(4 + h, SR, 0, 132)
                for j in range(16):
                    k.mm(ob, Eb.t[:, j, h, :], Vq.t[:, j, h, :], (b == 0 and j == 0), False,
                         reads=[Eb.r, Vq.r], writes=[rb[4 + h]], inc=False)
                k.mm(ob, Eb.t[0:SR, 16, h, :], v_s.t[:, h, :], False, (b == NS - 1),
                     reads=[Eb.r, v_s.r], writes=[rb[4 + h]], inc=True)
            k.memset("pool", Eb.t[:, :, :, 4 * b:4 * b + 4], 0.0, writes=[Eb.r])
        rin = k.buf([SR, 4], F32, "rin"); oms = k.buf([SR, 512], BF16, "oms")
        for h in range(4):
            c0 = (4 + h) * 512
            k.op("dve", lambda e, h=h, c0=c0: e.reciprocal(rin.t[:, h:h + 1], ps[0:SR, c0 + 128:c0 + 129]),
                 reads=[rb[4 + h]], writes=[rin.r], accum=(h > 0))
        for h in range(4):
            c0 = (4 + h) * 512
            k.act(oms.t[:, h * 128:(h + 1) * 128], ps[0:SR, c0:c0 + 128], AF.Identity, reads=[rb[4 + h], rin.r], writes=[oms.r],
                  accum=(h > 0), scale=rin.t[:, h:h + 1])
        for h in range(4):
            k.tr(bankb(1, 128, h * SR, (h + 1) * SR), oms.t[:, h * 128:(h + 1) * 128], identb_t[0:SR, 0:SR], reads=[oms.r, rconst],
                 writes=[rb[1]], accum=(h > 0))
        k.cp("act", omT.t[:, :, T:T + SR], bankb(1, 128, 0, 4 * SR).rearrange("p (h s) -> p h s", s=SR), reads=[rb[1]], writes=[omT.r])

    def pass_ret(mods_mix, orT):
        k.mark()
        wr = k.buf([128, 8, 2048], BF16, "wr")
        k.mark()
        stg = k.pool(2, [128, 8 * 512], F32, "stg")
        for g in range(4):
            load_w_bf16(wr.t[:, :, g * 512:(g + 1) * 512], wr.r,
                        w_in[:, g * 512:(g + 1) * 512].rearrange("(kc p) n -> p kc n", p=128), stg.next(), first=(g == 0))
        k.release()
        Sst = k.buf([128, 4, 128], F32, "Sst"); Sbf = k.buf([128, 4, 128], BF16, "Sbf")
        k.memset("pool", Sst.t[:], 0.0, writes=[Sst.r]); k.memset("pool", Sbf.t[:], 0.0, writes=[Sbf.r])
        wk = {"hs": k.pool(1, [SR, D], F32, "hs")}
        xt_p = k.pool(2, [128, D], F32, "xt"); hT_p = k.pool(2, [128, 8, 128], BF16, "hT")
        rot_p = k.pool(2, [128, 4, 128], F32, "rot"); qk_p = k.pool(2, [128, 8, 128], F32, "qkrot")
        tmp_p = k.pool(1, [128, 8, 128], F32, "rtmp")
        vb_p = k.pool(2, [128, 512], BF16, "vb"); sg_p = k.pool(2, [128, 512], F32, "sg")
        kd_p = k.pool(2, [128, 4, 128], BF16, "kd")
        qTb_p = k.pool(2, [128, 4, 128], BF16, "qTb"); kTb_p = k.pool(2, [128, 4, 128], BF16, "kTb")
        qdT_p = k.pool(2, [128, 4, 128], BF16, "qdT"); att_p = k.pool(2, [128, 4, 128], BF16, "att")
        ss_p = k.pool(2, [128, 8], F32, "ss"); junk_p = k.pool(1, [128, 128], F32, "junk")
        or_p = k.pool(2, [128, 512], BF16, "or")
        for t in range(NT + 1):
            rows = 128 if t < NT else SR
            if t < NT:
                xt = xt_p.next()
                k.dma("sp", xt.t[:], xp[t * 128:(t + 1) * 128, :], writes=[xt.r])
                xin, xin_reg = xt.t[:], xt.r
            else:
                xin, xin_reg = x_s.t[:], x_s.r
            hT = hT_p.next()
            make_hT(xin, xin_reg, rows, hT.t[:, :, 0:rows], hT.r, 0, 0, mods_s=mods_mix.t, mods_reg=mods_mix.r, wk=wk, pbanks=(0, 1))
            rot = rot_p.next()
            k.dma("sp", rot.t[0:rows], rot_d[t, 0:rows], writes=[rot.r])
            for g in range(4):
                for kc in range(8):
                    k.mm(bank(4 + g, rows), hT.t[:, kc, 0:rows], wr.t[:, kc, g * 512:(g + 1) * 512], kc == 0, kc == 7,
                         reads=[hT.r, wr.r], writes=[rb[4 + g]])
            qk = qk_p.next(); tmp = tmp_p.next()
            zqk = ps[0:rows, 4 * 512:6 * 512].rearrange("p (g d) -> p g d", d=128)
            rotate(zqk, [rb[4], rb[5]], rows, 8, rot.t[0:rows, 0, :], rot.t[0:rows, 1, :], rot.r,
                   qk.t[0:rows], qk.r, tmp.t[0:rows], tmp.r, "pair")
            vb = vb_p.next(); sg = sg_p.next()
            k.cp("act", vb.t[0:rows], bank(6, rows), reads=[rb[6]], writes=[vb.r])
            k.act(sg.t[0:rows], bank(7, rows), AF.Silu, reads=[rb[7]], writes=[sg.r])
            kdc = rkd if t < NT else rkds
            kd = kd_p.next()
            k.tt("pool", kd.t[0:rows], qk.t[0:rows, 4:8, :], bc(kdc[0:rows, :], 2, 128), ALU.mult, reads=[qk.r, rconst], writes=[kd.r])
            for g in range(8):
                b = 2 + g // 4
                k.tr(bank(b, 128, (g % 4) * 128, (g % 4) * 128 + rows), qk.t[0:rows, g, :], ident[0:rows, 0:rows],
                     reads=[qk.r, rconst], writes=[rb[b]], accum=(g % 4 > 0))
            qv = bank(2).rearrange("p (h s) -> p h s", s=128)[:, :, 0:rows]
            kv = bank(3).rearrange("p (h s) -> p h s", s=128)[:, :, 0:rows]
            qTb = qTb_p.next(); kTb = kTb_p.next(); qdT = qdT_p.next()
            k.cp("act", qTb.t[:, :, 0:rows], qv, reads=[rb[2]], writes=[qTb.r])
            k.cp("act", kTb.t[:, :, 0:rows], kv, reads=[rb[3]], writes=[kTb.r])
            qdc = rqd if t < NT else rqds
            k.tt("dve", qdT.t[:, :, 0:rows], qv, qdc[:, :, 0:rows], ALU.mult, reads=[rb[2], rconst], writes=[qdT.r])
            for h in range(4):
                k.mm(bank(0, rows, h * 128, h * 128 + rows), kTb.t[:, h, 0:rows], qTb.t[:, h, 0:rows], True, True,
                     reads=[kTb.r, qTb.r], writes=[rb[0]])
            att = att_p.next()
            dmc = rdm if t < NT else rdms
            k.tt("dve", att.t[0:rows, :, 0:rows], bank(0, rows).rearrange("p (h s) -> p h s", s=128)[:, :, 0:rows], dmc[0:rows, :, 0:rows],
                 ALU.mult, reads=[rb[0], rconst], writes=[att.r])
            if t < NT:
                for h in range(4):
                    ob = bank(1, 128, h * 128, (h + 1) * 128)
                    k.mm(ob, att.t[:, h, :], vb.t[:, h * 128:(h + 1) * 128], True, False, reads=[att.r, vb.r], writes=[rb[1]])
                    k.mm(ob, qdT.t[:, h, :], Sbf.t[:, h, :], False, True, reads=[qdT.r, Sbf.r], writes=[rb[1]])
                for h in range(4):
                    k.mm(bank(2, 128, h * 128, (h + 1) * 128), kd.t[:, h, :], vb.t[:, h * 128:(h + 1) * 128], True, True,
                         reads=[kd.r, vb.r], writes=[rb[2]])
                for h in range(4):
                    k.stt(Sst.t[:, h, :], Sst.t[:, h, :], math.exp(128.0 * LOGG[h]), bank(2, 128, h * 128, (h + 1) * 128), ALU.mult, ALU.add,
                          reads=[Sst.r, rb[2]], writes=[Sst.r])
                k.cp("pool", Sbf.t[:], Sst.t[:], reads=[Sst.r], writes=[Sbf.r])
                if t == NT - 1:
                    k.dma("sp", rpo.rearrange("(h d) v -> d h v", d=128), Sst.t[:], reads=[Sst.r], is_output=True)
            else:
                qdm = k.buf([128, NS, 4, SR], BF16, "qdm")
                k.memset("pool", qdm.t[:], 0.0, writes=[qdm.r])
                base = qdm.t[:]
                pstr = base.ap[0][0]
                for h in range(4):
                    dst = bass.AP(qdm.t, base.offset + h * SR, [[pstr, 128], [4 * SR + 4, NS], [1, 4]])
                    k.cp("pool", dst, qdT.t[:, h, 0:SR].rearrange("p (s j) -> p s j", j=4), reads=[qdT.r, qdm.r], writes=[qdm.r])
                for h in range(4):
                    ob = bank(4 + h, SR, 0, 128)
                    k.mm(ob, att.t[0:SR, h, 0:SR], vb.t[0:SR, h * 128:(h + 1) * 128], True, False, reads=[att.r, vb.r], writes=[rb[4 + h]], inc=True)
                s0_p = k.pool(2, [128, 4, 128], F32, "s0"); s0b_p = k.pool(2, [128, 4, 128], BF16, "s0b")
                kdm_p = k.pool(2, [SR, 4, 128], BF16, "kdm"); sn_p = k.pool(2, [128, 4, 128], F32, "sn")
                for sq in range(NS):
                    s0 = s0_p.next(); s0b = s0b_p.next()
                    k.dma("sp", s0.t[:], sret[sq * 512:(sq + 1) * 512, :].rearrange("(h d) v -> d h v", d=128), writes=[s0.r])
                    k.cp("pool", s0b.t[:], s0.t[:], reads=[s0.r], writes=[s0b.r])
                    for h in range(4):
                        k.mm(bank(4 + h, SR, 0, 128), qdm.t[:, sq, h, :], s0b.t[:, h, :], False, (sq == NS - 1),
                             reads=[qdm.r, s0b.r], writes=[rb[4 + h]], inc=True)
                    kdm = kdm_p.next()
                    k.ts("dve", kdm.t[:], kd.t[0:SR], blk[:, sq:sq + 1], None, ALU.mult, reads=[kd.r, rconst], writes=[kdm.r])
                    ub = 2 + sq % 2
                    for h in range(4):
                        k.mm(bank(ub, 128, h * 128, (h + 1) * 128), kdm.t[:, h, :], vb.t[0:SR, h * 128:(h + 1) * 128], True, True,
                             reads=[kdm.r, vb.r], writes=[rb[ub]])
                    sn = sn_p.next()
                    for h in range(4):
                        k.stt(sn.t[:, h, :], s0.t[:, h, :], math.exp(4.0 * LOGG[h]), bank(ub, 128, h * 128, (h + 1) * 128), ALU.mult, ALU.add,
                              reads=[s0.r, rb[ub]], writes=[sn.r], accum=(h > 0))
                    k.dma("sp", rso[sq * 512:(sq + 1) * 512, :].rearrange("(h d) v -> d h v", d=128), sn.t[:], reads=[sn.r], is_output=True)
            ss = ss_p.next(); junk = junk_p.next()

            def obank(h):
                return (bank(1, rows, h * 128, (h + 1) * 128), rb[1]) if t < NT else (bank(4 + h, rows, 0, 128), rb[4 + h])

            for h in range(4):
                oap, oreg = obank(h)
                k.act(junk.t[0:rows], oap, AF.Square, reads=[oreg], writes=[junk.r, ss.r],
                      accum_out=ss.t[0:rows, h:h + 1])
            k.ts("dve", ss.t[0:rows, 4:8], ss.t[0:rows, 0:4], 1.0 / 128.0, GN_EPS, ALU.mult, ALU.add, reads=[ss.r], writes=[ss.r])
            k.act(ss.t[0:rows, 4:8], ss.t[0:rows, 4:8], AF.Sqrt, reads=[ss.r], writes=[ss.r])
            k.op("dve", lambda e, ss=ss, rows=rows: e.reciprocal(ss.t[0:rows, 4:8], ss.t[0:rows, 4:8]), reads=[ss.r], writes=[ss.r])
            orr = or_p.next()
            for h in range(4):
                oap, oreg = obank(h)
                k.stt(orr.t[0:rows, h * 128:(h + 1) * 128], oap, ss.t[0:rows, 4 + h:5 + h],
                      sg.t[0:rows, h * 128:(h + 1) * 128], ALU.mult, ALU.mult, reads=[oreg, ss.r, sg.r], writes=[orr.r], accum=(h > 0))
            for h in range(4):
                k.tr(bankb(3, 128, h * 128, h * 128 + rows), orr.t[0:rows, h * 128:(h + 1) * 128], identb_t[0:rows, 0:rows],
                     reads=[orr.r, rconst], writes=[rb[3]], accum=(h > 0))
            k.cp("act", orT.t[:, :, t * 128:t * 128 + rows], bankb(3, 128, 0, 512).rearrange("p (h s) -> p h s", s=128)[:, :, 0:rows],
                 reads=[rb[3]], writes=[orT.r], accum=(t > 0))
        k.release()

    def post_norm(y_ap, y_regs, xt_ap, xt_reg, rows, gate_ap, gate_reg, lng, lnb, gb_reg, wk, bias_ap=None, bias_reg=None):
        rr = wk["rr"].next()
        if bias_ap is not None:
            k.tt("dve", rr.t[0:rows], y_ap, bias_ap[0:rows], ALU.add, reads=list(y_regs) + [bias_reg], writes=[rr.r])
            k.tt("pool", rr.t[0:rows], rr.t[0:rows], gate_ap, ALU.mult, reads=[rr.r, gate_reg], writes=[rr.r])
        else:
            k.tt("dve", rr.t[0:rows], y_ap, gate_ap, ALU.mult, reads=list(y_regs) + [gate_reg], writes=[rr.r])
        k.stt(xt_ap, xt_ap, ALPHA, rr.t[0:rows], ALU.mult, ALU.add, reads=[xt_reg, rr.r], writes=[xt_reg])
        layer_norm(xt_ap, xt_reg, rows, lng, lnb, gb_reg, xt_ap, xt_reg, wk)

    def ln_work(n=2):
        rr = k.pool(n, [128, D], F32, "rr")
        return {"st": k.pool(2, [128, 2, 6], F32, "st"), "mv": k.pool(2, [128, 4], F32, "mv"),
                "xn": k.pool(n, [128, D], F32, "xn"), "rr": rr, "hs": rr}

    def load_ln(i):
        g = k.buf([128, D], F32, "lng"); b = k.buf([128, D], F32, "lnb")
        k.dma("sp", g.t[:], ln_g[i:i + 1, :].partition_broadcast(128), writes=[g.r])
        k.dma("sp", b.t[:], ln_b[i:i + 1, :].partition_broadcast(128), writes=[g.r], anchor=g.r, accum=True)
        return g, b

    def pass_out(mods_mix, g1p, orT, omT):
        k.mark()
        wo = k.buf([128, 8, D], BF16, "wo")
        k.mark()
        stg = k.pool(2, [128, 8 * 512], F32, "stg")
        for g in range(2):
            load_w_bf16(wo.t[:, :, g * 512:(g + 1) * 512], wo.r,
                        w_out[:, g * 512:(g + 1) * 512].rearrange("(kc p) n -> p kc n", p=128), stg.next(), first=(g == 0))
        k.release()
        lng, lnb = load_ln(0)
        wk = ln_work()
        for t in range(NT + 1):
            rows = 128 if t < NT else SR
            if t < NT:
                k.dma("sp", x_all[:, t, :], xp[t * 128:(t + 1) * 128, :], writes=[rx[t]])
                xt_ap, xt_reg = x_all[:, t, :], rx[t]
                gate_ap, gate_reg = g1p.t[:], g1p.r
            else:
                xt_ap, xt_reg = x_s.t[:], x_s.r
                gate_ap, gate_reg = mods_mix.t[:, 2 * D:3 * D], mods_mix.r
            bp = 4 * (t % 2)
            for half in range(2):
                for c in range(8):
                    src = orT if c < 4 else omT
                    k.mm(bank(bp + half, rows), src.t[:, c % 4, t * 128:t * 128 + rows], wo.t[:, c, half * 512:(half + 1) * 512],
                         c == 0, c == 7, reads=[src.r, wo.r], writes=[rb[bp + half]])
            post_norm(ps[0:rows, bp * 512:bp * 512 + D], [rb[bp], rb[bp + 1]], xt_ap, xt_reg, rows, gate_ap, gate_reg,
                      lng.t, lnb.t, lng.r, wk)
        k.release()

    def ffn(l, final):
        k.mark()
        g2s = k.buf([SR, D], F32, "g2s"); g2p = k.buf([128, D], F32, "g2p")
        k.dma("sp", g2s.t[:], sc_mods[l][:, 2 * D:3 * D], reads=[r_scm[l]], writes=[g2s.r])
        k.dma("sp", g2p.t[:], sc_g2p[l].partition_broadcast(128), reads=[r_scg[l]], writes=[g2p.r])
        hT = k.buf([128, 8, T + SR], BF16, "hT_all")
        ffp = k.buf([128, NFC, 4], F32, "ffp"); ust = k.buf([128, NFC, 2 * NS], F32, "ust")
        fo = k.buf([128, NFC, 2], F32, "fo"); fs = k.buf([128, NFC, NS, 2], F32, "fs")
        k.mark()
        mf = k.buf([SR, 2 * D], F32, "mf")
        k.dma("sp", mf.t[:], sc_mods[l][:, 0:2 * D], reads=[r_scm[l]], writes=[mf.r])
        p4 = k.buf([4, DFF], F32, "p4"); p32 = k.buf([2 * NS, DFF], F32, "p32")
        k.dma("sp", p4.t[:], ffp_d[l], writes=[p4.r])
        k.dma("sp", p32.t[:], sffn[l], writes=[p32.r])
        for c0 in range(0, NFC, 4):
            c1 = min(NFC, c0 + 4)
            for c in range(c0, c1):
                k.tr(bank(0, 128, (c - c0) * 4, (c - c0) * 4 + 4), p4.t[:, c * 128:(c + 1) * 128], ident[0:4, 0:4], reads=[p4.r, rconst],
                     writes=[rb[0]], accum=(c > c0))
                k.tr(bank(1, 128, (c - c0) * 32, (c - c0) * 32 + 32), p32.t[:, c * 128:(c + 1) * 128], ident[0:32, 0:32], reads=[p32.r, rconst],
                     writes=[rb[1]], accum=(c > c0))
            k.cp("act", ffp.t[:, c0:c1, :], bank(0, 128, 0, (c1 - c0) * 4).rearrange("p (c j) -> p c j", j=4), reads=[rb[0]], writes=[ffp.r], accum=(c0 > 0))
            k.cp("act", ust.t[:, c0:c1, :], bank(1, 128, 0, (c1 - c0) * 32).rearrange("p (c j) -> p c j", j=32), reads=[rb[1]], writes=[ust.r], accum=(c0 > 0))
        wkh = {"hs": k.pool(1, [SR, D], F32, "hs")}
        for t in range(NT):
            make_hT(x_all[:, t, :], rx[t], 128, hT.t[:, :, t * 128:(t + 1) * 128], hT.r, l, 3, pbanks=(2 + 2 * (t % 2), 3 + 2 * (t % 2)), first=(t == 0))
            k.ts("pool", x_all[:, t, :], x_all[:, t, :], ALPHA, None, ALU.mult, reads=[rx[t]], writes=[rx[t]])
        make_hT(x_s.t[:], x_s.r, SR, hT.t[:, :, T:T + SR], hT.r, l, 3, mods_s=mf.t, mods_reg=mf.r, wk=wkh, pbanks=(2, 3), first=False)
        k.ts("pool", x_s.t[:], x_s.t[:], ALPHA, None, ALU.mult, reads=[x_s.r], writes=[x_s.r])
        k.release()
        k.mark()
        G = 2
        stg = k.pool(2, [128, 2048], F32, "stg")
        wu_p = k.pool(2, [128, 8, G * 128], BF16, "wu"); wv_p = k.pool(2, [128, 8, G * 128], BF16, "wv")
        wds_p = k.pool(2, [128, G, D], BF16, "wds"); wdu_p = k.pool(1, [128, G, D], BF16, "wdu")
        UW = 2 + T + 6 * NS
        u_p = k.pool(2, [128, UW], F32, "u_sb"); a_p = k.pool(1, [128, UW], F32, "acc")
        gT_p = k.pool(1, [128, G, T + SR], BF16, "gT")
        vsb_p = k.pool(1, [128, T + SR], BF16, "vsb")
        nb = [0]

        for g in range(NFC // G):
            f0 = g * G * 128
            wu = wu_p.next(); wv = wv_p.next(); wds = wds_p.next(); wdu = wdu_p.next()
            load_w_bf16(wu.t[:], wu.r, ffn_up[l, :, f0:f0 + G * 128].rearrange("(kc p) n -> p kc n", p=128), stg.next())
            load_w_bf16(wv.t[:], wv.r, ffn_up[l, :, DFF + f0:DFF + f0 + G * 128].rearrange("(kc p) n -> p kc n", p=128), stg.next())
            st = stg.next()
            stv = st.t[:, 0:G * D].rearrange("p (a b) -> p a b", b=D)
            k.dma("sp", stv, ffn_down[l, f0:f0 + G * 128, :].rearrange("(c p) n -> p c n", p=128), writes=[st.r])
            k.cp("pool", wdu.t[:], stv, reads=[st.r], writes=[wdu.r])
            k.tt("pool", wds.t[:], stv, bc(g2p.t[:], 1, G), ALU.mult, reads=[st.r, g2p.r], writes=[wds.r])
            gT = gT_p.next()
            for c in range(G):
                fc = g * G + c
                u = u_p.next(); acc = a_p.next()
                w0, w1, w2, bb = (ffp.t[:, fc, j:j + 1] for j in range(4))
                k.memset("pool", u.t[:, 0:2], 0.0, writes=[u.r])
                usv = u.t[:, 2 + T:UW].rearrange("p (s j) -> p s j", j=6)
                asv = acc.t[:, 2 + T:UW].rearrange("p (s j) -> p s j", j=6)
                k.cp("pool", usv[:, :, 0:2], ust.t[:, fc, :].rearrange("p (s j) -> p s j", j=2), reads=[ust.r], writes=[u.r], accum=True)
                vsb = vsb_p.next()
                for n in range(5):
                    ncol = 512 if n < 4 else SR
                    t0 = n * 512
                    nb[0] += 1
                    bu = nb[0] % 2; bv = 2 + nb[0] % 2
                    for kc in range(8):
                        k.mm(bank(bu, 128, 0, ncol), wu.t[:, kc, c * 128:(c + 1) * 128], hT.t[:, kc, t0:t0 + ncol], kc == 0, kc == 7,
                             reads=[wu.r, hT.r], writes=[rb[bu]])
                    for kc in range(8):
                        k.mm(bank(bv, 128, 0, ncol), wv.t[:, kc, c * 128:(c + 1) * 128], hT.t[:, kc, t0:t0 + ncol], kc == 0, kc == 7,
                             reads=[wv.r, hT.r], writes=[rb[bv]])
                    if n < 4:
                        k.cp("act", u.t[:, 2 + t0:2 + t0 + 512], bank(bu), reads=[rb[bu]], writes=[u.r], accum=True)
                    else:
                        k.cp("act", usv[:, :, 2:6], bank(bu, 128, 0, SR).rearrange("p (s j) -> p s j", j=4), reads=[rb[bu]], writes=[u.r], accum=True)
                    k.cp("act", vsb.t[:, t0:t0 + ncol], bank(bv, 128, 0, ncol), reads=[rb[bv]], writes=[vsb.r], accum=(n > 0))
                lo, hi = 2, UW
                k.act(acc.t[:, lo:hi], u.t[:, lo:hi], AF.Identity, reads=[u.r, ffp.r], writes=[acc.r], scale=w2, bias=bb)
                k.stt(acc.t[:, lo:hi], u.t[:, lo - 1:hi - 1], w1, acc.t[:, lo:hi], ALU.mult, ALU.add, reads=[u.r, acc.r, ffp.r], writes=[acc.r])
                k.stt(acc.t[:, lo:hi], u.t[:, lo - 2:hi - 2], w0, acc.t[:, lo:hi], ALU.mult, ALU.add, reads=[u.r, acc.r, ffp.r], writes=[acc.r])
                k.act(acc.t[:, lo:hi], acc.t[:, lo:hi], AF.Gelu, reads=[acc.r], writes=[acc.r])
                k.tt("pool", gT.t[:, c, 0:T], acc.t[:, 2:2 + T], vsb.t[:, 0:T], ALU.mult, reads=[acc.r, vsb.r], writes=[gT.r], accum=(c > 0))
                k.tt("pool", gT.t[:, c, T:T + SR].rearrange("p (s j) -> p s j", j=4), asv[:, :, 2:6],
                     vsb.t[:, T:T + SR].rearrange("p (s j) -> p s j", j=4), ALU.mult, reads=[acc.r, vsb.r], writes=[gT.r], accum=True)
                k.cp("pool", fo.t[:, fc, :], u.t[:, T:T + 2], reads=[u.r], writes=[fo.r], accum=True)
                k.cp("pool", fs.t[:, fc, :, :], usv[:, :, 4:6], reads=[u.r], writes=[fs.r], accum=True)
            for t in range(NT + 1):
                rows = 128 if t < NT else SR
                bp = 4 + 2 * (t % 2)
                wd = wds if t < NT else wdu
                for half in range(2):
                    for c in range(G):
                        k.mm(bank(bp + half, rows), gT.t[:, c, t * 128:t * 128 + rows], wd.t[:, c, half * 512:(half + 1) * 512],
                             c == 0, c == G - 1, reads=[gT.r, wd.r], writes=[rb[bp + half]])
                yv = ps[0:rows, bp * 512:bp * 512 + D]
                if t < NT:
                    k.tt("dve", x_all[:, t, :], x_all[:, t, :], yv, ALU.add, reads=[rx[t], rb[bp], rb[bp + 1]], writes=[rx[t]])
                else:
                    rs_ = a_p.next()
                    k.tt("dve", rs_.t[0:SR, 0:D], yv, g2s.t[:], ALU.mult, reads=[rb[bp], rb[bp + 1], g2s.r], writes=[rs_.r])
                    k.tt("pool", x_s.t[:], x_s.t[:], rs_.t[0:SR, 0:D], ALU.add, reads=[x_s.r, rs_.r], writes=[x_s.r])
        k.release()
        k.mark()
        fo_tok = k.buf([2, DFF], F32, "fo_tok"); fs_tok = k.buf([2 * NS, DFF], F32, "fs_tok")
        for c0 in range(0, NFC, 4):
            c1 = min(NFC, c0 + 4)
            for c in range(c0, c1):
                k.tr(bank(0, 2, (c - c0) * 128, (c - c0 + 1) * 128), fo.t[:, c, :], ident[:], reads=[fo.r, rconst], writes=[rb[0]], accum=(c > c0))
                k.tr(bank(1, 2 * NS, (c - c0) * 128, (c - c0 + 1) * 128), fs.t[:, c, :, :].rearrange("p s j -> p (s j)"), ident[:],
                     reads=[fs.r, rconst], writes=[rb[1]], accum=(c > c0))
            k.cp("act", fo_tok.t[:, c0 * 128:c1 * 128], bank(0, 2, 0, (c1 - c0) * 128), reads=[rb[0]], writes=[fo_tok.r], accum=(c0 > 0))
            k.cp("act", fs_tok.t[:, c0 * 128:c1 * 128], bank(1, 2 * NS, 0, (c1 - c0) * 128), reads=[rb[1]], writes=[fs_tok.r], accum=(c0 > 0))
        k.dma("sp", fpo[l], fo_tok.t[:], reads=[fo_tok.r], is_output=True)
        k.dma("sp", fso[l], fs_tok.t[:], reads=[fs_tok.r], is_output=True)
        lng, lnb = load_ln(2 * l + 1)
        wk = ln_work()
        for t in range(NT + 1):
            rows = 128 if t < NT else SR
            xt_ap, xt_reg = (x_all[:, t, :], rx[t]) if t < NT else (x_s.t[:], x_s.r)
            layer_norm(xt_ap, xt_reg, rows, lng.t, lnb.t, lng.r, xt_ap, xt_reg, wk)
            if final:
                k.dma("sp", yp[t * 128:(t + 1) * 128, :] if t < NT else ys, xt_ap, reads=[xt_reg], is_output=True)
        k.release()
        k.release()

    def conformer(mods_mix, g1p):
        k.mark()
        w1 = k.buf([128, 8, 2048], BF16, "w1"); w2 = k.buf([128, 8, D], BF16, "w2")
        cf = k.buf([128, 8, 36], F32, "cf")
        k.mark()
        stg = k.pool(2, [128, 8 * 512], F32, "stg")
        for g in range(4):
            load_w_bf16(w1.t[:, :, g * 512:(g + 1) * 512], w1.r, pw1[:, g * 512:(g + 1) * 512].rearrange("(kc p) n -> p kc n", p=128),
                        stg.next(), first=(g == 0))
        for g in range(2):
            load_w_bf16(w2.t[:, :, g * 512:(g + 1) * 512], w2.r, pw2[:, g * 512:(g + 1) * 512].rearrange("(kc p) n -> p kc n", p=128),
                        stg.next(), first=(g == 0))
        p36 = k.buf([36, D], F32, "p36")
        k.dma("sp", p36.t[:], cfp_d, writes=[p36.r])
        for c in range(8):
            k.tr(bank(0, 128, c * 36, c * 36 + 36), p36.t[:, c * 128:(c + 1) * 128], ident[0:36, 0:36], reads=[p36.r, rconst],
                 writes=[rb[0]], accum=(c > 0))
        k.cp("act", cf.t[:], bank(0, 128, 0, 288).rearrange("p (c j) -> p c j", j=36), reads=[rb[0]], writes=[cf.r])
        k.release()
        b2 = k.buf([128, D], F32, "b2")
        k.dma("sp", b2.t[:], b_pw2.partition_broadcast(128), writes=[b2.r])
        lng, lnb = load_ln(2)
        wk = ln_work(1)
        BS = 256
        NB = T // BS
        TPB = BS // 128
        GW = 34 * NS
        sconv_p = k.pool(1, [120, 4, 128], F32, "sconv_sb")
        carry = k.buf([128, 8, 30], F32, "carry")
        k.memset("pool", carry.t[:], 0.0, writes=[carry.r])
        hTb = k.buf([128, 8, BS], BF16, "hTb")
        yc = k.buf([128, 8, BS], F32, "yc")
        sT = k.buf([128, 8, BS], BF16, "sT")
        glu_p = k.pool(2, [128, GW], F32, "glu"); sig_p = k.pool(2, [128, BS], F32, "sig")
        glub_p = k.pool(2, [128, GW], BF16, "glub")
        dg_p = k.pool(2, [128, 31, 128], BF16, "dg")
        ycb_p = k.pool(2, [128, BS], BF16, "ycb"); ysq_p = k.pool(2, [128, BS], BF16, "ysq")
        mean = k.buf([128, BS], F32, "mean"); rstd = k.buf([128, BS], F32, "rstd"); xn_p = k.pool(2, [128, BS], F32, "cxn")
        gnew = k.buf([128, 8, SR], F32, "gnew")
        for B in range(NB + 1):
            prompt = B < NB
            ncol = BS if prompt else SR
            if prompt:
                for j in range(TPB):
                    t = TPB * B + j
                    make_hT(x_all[:, t, :], rx[t], 128, hTb.t[:, :, j * 128:(j + 1) * 128], hTb.r, 1, 0, pbanks=(2, 3), first=(j == 0))
            else:
                make_hT(x_s.t[:], x_s.r, SR, hTb.t[:, :, 0:SR], hTb.r, 1, 0, mods_s=mods_mix.t, mods_reg=mods_mix.r, wk=wk, pbanks=(2, 3))
            NO = BS if prompt else GW - 30
            s1b = 6 if prompt else 0
            for c in range(8):
                for kc in range(8):
                    k.mm(bank(4, 128, 0, ncol), w1.t[:, kc, c * 128:(c + 1) * 128], hTb.t[:, kc, 0:ncol], kc == 0, kc == 7,
                         reads=[w1.r, hTb.r], writes=[rb[4]])
                for kc in range(8):
                    k.mm(bank(5, 128, 0, ncol), w1.t[:, kc, D + c * 128:D + (c + 1) * 128], hTb.t[:, kc, 0:ncol], kc == 0, kc == 7,
                         reads=[w1.r, hTb.r], writes=[rb[5]])
                sig = sig_p.next(); glu = glu_p.next()
                k.act(sig.t[:, 0:ncol], bank(5, 128, 0, ncol), AF.Sigmoid, reads=[rb[5], cf.r], writes=[sig.r], bias=cf.t[:, c, 35:36])
                if prompt:
                    k.cp("pool", glu.t[:, 0:30], carry.t[:, c, :], reads=[carry.r], writes=[glu.r])
                    k.stt(glu.t[:, 30:30 + BS], bank(4, 128, 0, BS), cf.t[:, c, 34:35], sig.t[:, 0:BS], ALU.add, ALU.mult,
                          reads=[rb[4], cf.r, sig.r], writes=[glu.r], accum=True)
                    k.cp("pool", carry.t[:, c, :], glu.t[:, BS:BS + 30], reads=[glu.r], writes=[carry.r])
                else:
                    gv = glu.t[:, 0:GW].rearrange("p (s j) -> p s j", j=34)
                    scv = sconv_p.next()
                    for q in range(4):
                        k.dma("sp", scv.t[:, q, :], sconv[q * 120:(q + 1) * 120, c * 128:(c + 1) * 128], writes=[scv.r], accum=(q > 0))
                    for q in range(4):
                        k.tr(bank(6, 128, q * 120, (q + 1) * 120), scv.t[:, q, :], ident[0:120, 0:120],
                             reads=[scv.r, rconst], writes=[rb[6]], accum=(q > 0))
                    k.cp("act", gv[:, :, 0:30], bank(6, 128, 0, 480).rearrange("p (s j) -> p s j", j=30), reads=[rb[6]], writes=[glu.r])
                    k.stt(gv[:, :, 30:34], bank(4, 128, 0, SR).rearrange("p (s j) -> p s j", j=4), cf.t[:, c, 34:35],
                          sig.t[:, 0:SR].rearrange("p (s j) -> p s j", j=4), ALU.add, ALU.mult, reads=[rb[4], cf.r, sig.r], writes=[glu.r], accum=True)
                    k.cp("pool", gnew.t[:, c, :].rearrange("p (s j) -> p s j", j=4), gv[:, :, 30:34], reads=[glu.r], writes=[gnew.r], accum=(c > 0))
                dg = dg_p.next()
                k.tt("pool", dg.t[:], bc(identb_t[:], 1, 31), bc(cf.t[:, c, 0:31], 2, 128), ALU.mult, reads=[rconst, cf.r], writes=[dg.r])
                glub = glub_p.next()
                W = 30 + BS if prompt else GW
                k.cp("act", glub.t[:, 0:W], glu.t[:, 0:W], reads=[glu.r], writes=[glub.r])
                cb = (c % 2) if prompt else 1
                for j in range(31):
                    if prompt:
                        rhs = glub.t[:, j:j + BS]
                    else:
                        rhs = glub.t[:, 0:GW].rearrange("p (s j) -> p s j", j=34)[:, :, j:j + 4]
                    k.mm(bank(cb, 128, 0, ncol), dg.t[:, j, :], rhs, j == 0, j == 30, reads=[dg.r, glub.r], writes=[rb[cb]])
                k.act(yc.t[:, c, 0:ncol], bank(cb, 128, 0, ncol), AF.Identity, reads=[rb[cb], cf.r], writes=[yc.r], accum=(c > 0),
                      bias=cf.t[:, c, 31:32])
                ycb = ycb_p.next(); ysq = ysq_p.next()
                k.cp("pool", ycb.t[:, 0:ncol], yc.t[:, c, 0:ncol], reads=[yc.r], writes=[ycb.r])
                k.act(ysq.t[:, 0:ncol], yc.t[:, c, 0:ncol], AF.Square, reads=[yc.r], writes=[ysq.r])
                k.mm(bank(s1b, 128, 0, ncol), onesb[:], ycb.t[:, 0:ncol], c == 0, c == 7, reads=[rconst, ycb.r], writes=[rb[s1b]], inc=True)
                k.mm(bank(7, 128, 0, ncol), onesb[:], ysq.t[:, 0:ncol], c == 0, c == 7, reads=[rconst, ysq.r], writes=[rb[7]], inc=True)
            k.act(mean.t[:, 0:ncol], bank(s1b, 128, 0, ncol), AF.Identity, reads=[rb[s1b]], writes=[mean.r], scale=1.0 / D)
            k.tt("pool", rstd.t[:, 0:ncol], mean.t[:, 0:ncol], mean.t[:, 0:ncol], ALU.mult, reads=[mean.r], writes=[rstd.r])
            k.stt(rstd.t[:, 0:ncol], bank(7, 128, 0, ncol), 1.0 / D, rstd.t[:, 0:ncol], ALU.mult, ALU.subtract, reads=[rb[7], rstd.r], writes=[rstd.r])
            k.ts("dve", rstd.t[:, 0:ncol], rstd.t[:, 0:ncol], LN_EPS, None, ALU.add, reads=[rstd.r], writes=[rstd.r])
            k.act(rstd.t[:, 0:ncol], rstd.t[:, 0:ncol], AF.Sqrt, reads=[rstd.r], writes=[rstd.r])
            k.op("dve", lambda e, ncol=ncol: e.reciprocal(rstd.t[:, 0:ncol], rstd.t[:, 0:ncol]), reads=[rstd.r], writes=[rstd.r])
            for c in range(8):
                xn = xn_p.next()
                k.tt("pool", xn.t[:, 0:ncol], yc.t[:, c, 0:ncol], mean.t[:, 0:ncol], ALU.subtract, reads=[yc.r, mean.r], writes=[xn.r])
                k.tt("dve", xn.t[:, 0:ncol], xn.t[:, 0:ncol], rstd.t[:, 0:ncol], ALU.mult, reads=[xn.r, rstd.r], writes=[xn.r])
                k.act(sT.t[:, c, 0:ncol], xn.t[:, 0:ncol], AF.Silu, reads=[xn.r, cf.r], writes=[sT.r], accum=(c > 0),
                      scale=cf.t[:, c, 32:33], bias=cf.t[:, c, 33:34])
            for j in range(TPB if prompt else 1):
                rows = 128 if prompt else SR
                t = TPB * B + j
                bp = 4 * (j % 2) if prompt else 2
                for half in range(2):
                    for c in range(8):
                        k.mm(bank(bp + half, rows), sT.t[:, c, j * 128:j * 128 + rows], w2.t[:, c, half * 512:(half + 1) * 512], c == 0, c == 7,
                             reads=[sT.r, w2.r], writes=[rb[bp + half]])
                if prompt:
                    xt_ap, xt_reg, gate_ap, gate_reg = x_all[:, t, :], rx[t], g1p.t[:], g1p.r
                else:
                    xt_ap, xt_reg, gate_ap, gate_reg = x_s.t[:], x_s.r, mods_mix.t[:, 2 * D:3 * D], mods_mix.r
                post_norm(ps[0:rows, bp * 512:bp * 512 + D], [rb[bp], rb[bp + 1]], xt_ap, xt_reg, rows, gate_ap, gate_reg,
                          lng.t, lnb.t, lng.r, wk, bias_ap=b2.t, bias_reg=b2.r)
            if B == NB - 1:
                cp_tok = wk["xn"].next()
                for c in range(8):
                    k.tr(bank(2 + c // 4, 30, (c % 4) * 128, (c % 4 + 1) * 128), carry.t[:, c, :], ident[:], reads=[carry.r, rconst],
                         writes=[rb[2 + c // 4]], accum=(c % 4 > 0))
                k.cp("act", cp_tok.t[0:30, :], ps[0:30, 2 * 512:2 * 512 + D], reads=[rb[2], rb[3]], writes=[cp_tok.r])
                k.dma("sp", cpo, cp_tok.t[0:30, :], reads=[cp_tok.r], is_output=True)
        r_cso = Reg()
        k.dma("sp", cso.rearrange("(s j) f -> s j f", j=30)[:, 0:26, :], sconv.rearrange("(s j) f -> s j f", j=30)[:, 4:30, :],
              reads=[], writes=[r_cso], is_output=True)
        cs_tok = wk["xn"].next()
        for c in range(8):
            k.tr(bank(2 + c // 4, SR, (c % 4) * 128, (c % 4 + 1) * 128), gnew.t[:, c, :], ident[:], reads=[gnew.r, rconst],
                 writes=[rb[2 + c // 4]], accum=(c % 4 > 0))
        k.cp("act", cs_tok.t[0:SR, :], ps[0:SR, 2 * 512:2 * 512 + D], reads=[rb[2], rb[3]], writes=[cs_tok.r])
        for sq in range(NS):
            k.dma("sp", cso[sq * 30 + 26:sq * 30 + 30, :], cs_tok.t[sq * 4:sq * 4 + 4, :], reads=[cs_tok.r], is_output=True)
        k.release()

    k.limit = k.sb_top
    k.mark()
    rdm = cload([128, 4, 128], rdm_d); rqd = cload([128, 4, 128], rqd_d); rkd = cload([128, 4], rkd_d)
    rdms = cload([64, 4, 64], rdms_d); rqds = cload([128, 4, 64], rqds_d); rkds = cload([64, 4], rkds_d)
    blk = cload([64, 16], blk_d); tri = cload([128, 128], tri_d); nmask = cload([64, 16, 16], nmask_d)
    idx = k.buf([128, NS * 16], I32)
    pti = cload([128, NS * 16], ptd.partition_broadcast(128), dt=I32)
    idxf = k.sb([128, NS * 16], F32)
    k.cp("dve", idxf[:], pti[:], reads=[rconst], writes=[idx.r])
    k.stt(idxf[:], idxf[:], 128.0, iota[:].broadcast_to([128, NS * 16]), ALU.mult, ALU.add, reads=[rconst, idx.r], writes=[idx.r])
    k.cp("dve", idx.t[:], idxf[:], reads=[idx.r], writes=[idx.r])
    mods_mix0 = k.buf([SR, 3 * D], F32, "mods_mix")
    g1p0 = k.buf([128, D], F32, "g1p")
    with nc.named_scope("adaln0"):
        adaln(0, mods_mix0, g1p0)
    omT = k.buf([128, 4, T + SR], BF16, "omT")
    orT = k.buf([128, 4, T + SR], BF16, "orT")
    with nc.named_scope("passM"):
        pass_moba(mods_mix0, omT)
    if stage >= 4:
        with nc.named_scope("passR"):
            pass_ret(mods_mix0, orT)
    k.limit = XOFF
    if stage >= 5:
        with nc.named_scope("passO"):
            pass_out(mods_mix0, g1p0, orT, omT)
    k.release()
    if stage >= 6:
        with nc.named_scope("ffn0"):
            ffn(0, final=False)
    if stage >= 7:
        k.mark()
        mods_mix1 = k.buf([SR, 3 * D], F32, "mods_mix")
        g1p1 = k.buf([128, D], F32, "g1p")
        with nc.named_scope("adaln1"):
            adaln(1, mods_mix1, g1p1)
        with nc.named_scope("conformer"):
            conformer(mods_mix1, g1p1)
        k.release()
    if stage >= 8:
        with nc.named_scope("ffn1"):
            ffn(1, final=True)
    k.finish()
    print("instructions:", k.ninst, "sems:", k.nsem, "sbuf_off:", k.sb_off)
    return nc


def _consts():
    f32 = np.float32
    c = {}
    c["ident"] = np.eye(128, dtype=f32)
    c["iota"] = np.arange(128, dtype=f32).reshape(128, 1)
    theta = f32(10000.0)
    inv_m = (theta ** (-np.arange(0, 128, 2, dtype=f32) / f32(128))).astype(f32)
    inv_r = (f32(1.0) / (theta ** np.linspace(0.0, 1.0, 64, dtype=f32))).astype(f32)
    rot = np.zeros((17, 128, 4, 128), f32)
    for t in range(17):
        if t < 16:
            pos = (t * 128 + np.arange(128)).astype(f32)
        else:
            pos = (2048 + (np.arange(128) % 4)).astype(f32)
        am = (pos[:, None] * inv_m[None, :]).astype(f32)
        ar = (pos[:, None] * inv_r[None, :]).astype(f32)
        cm, sm = np.cos(am).astype(f32), np.sin(am).astype(f32)
        cr, sr = np.cos(ar).astype(f32), np.sin(ar).astype(f32)
        rot[t, :, 0, 0::2] = cr; rot[t, :, 0, 1::2] = cr
        rot[t, :, 1, 0::2] = -sr; rot[t, :, 1, 1::2] = sr
        rot[t, :, 2, 0:64] = cm; rot[t, :, 2, 64:128] = cm
        rot[t, :, 3, 0:64] = -sm; rot[t, :, 3, 64:128] = sm
    c["rot"] = rot
    lg = np.array(LOGG, dtype=np.float64)
    i = np.arange(128, dtype=np.float64)
    rdm = np.zeros((128, 4, 128), np.float64)
    for h in range(4):
        diff = i[None, :] - i[:, None]
        rdm[:, h, :] = np.where(diff >= 0, np.exp(np.maximum(diff, 0) * lg[h]), 0.0) * SCALE
    c["rdm"] = rdm.astype(f32)
    c["rqd"] = np.broadcast_to(np.exp((i[None, None, :] + 1.0) * lg[None, :, None]), (128, 4, 128)).astype(f32).copy()
    c["rkd"] = (np.exp((127.0 - i)[:, None] * lg[None, :]) * SCALE).astype(f32)
    r = np.arange(64)
    seq, ii = r // 4, (r % 4).astype(np.float64)
    rdms = np.zeros((64, 4, 64), np.float64)
    for h in range(4):
        diff = ii[None, :] - ii[:, None]
        same = seq[None, :] == seq[:, None]
        rdms[:, h, :] = np.where(same & (diff >= 0), np.exp(np.maximum(diff, 0) * lg[h]), 0.0) * SCALE
    c["rdms"] = rdms.astype(f32)
    c["rqds"] = np.broadcast_to(np.exp((ii[None, None, :] + 1.0) * lg[None, :, None]), (128, 4, 64)).astype(f32).copy()
    c["rkds"] = (np.exp((3.0 - ii)[:, None] * lg[None, :]) * SCALE).astype(f32)
    blk = np.zeros((64, 16), f32)
    blk[r, seq] = 1.0
    c["blk"] = blk
    tri = np.where(np.arange(128)[None, :] <= np.arange(128)[:, None], 0.0, NEG).astype(f32)
    c["tri"] = tri
    nm = np.full((64, 16, 16), NEG, f32)
    for b in range(16):
        for tq in range(4):
            for kk in range(tq + 1):
                nm[b * 4 + kk, b, tq::4] = 0.0
    c["nmask"] = nm
    Ep = np.zeros((18, 128), f32); Ep[0, :] = 1.0; Ep[17, :] = 1.0
    Es = np.zeros((18, 64), f32); Es[17, :] = 1.0
    for s in range(16):
        Es[1 + s, 4 * s:4 * s + 4] = 1.0
    ep = np.zeros((18, 1), f32); ep[0, 0] = 1.0; ep[17, 0] = 1.0
    c["Ep"], c["Es"], c["ep"] = Ep, Es, ep
    return c


def make_in_maps(x_prompt, x_sample, cache_k, cache_v, state_ret, state_conv, state_ffn, page_table, c_prompt, c_sample,
                 ab_w_in, ab_w_out, cf_w_pw1, cf_b_pw1, cf_w_dw, cf_b_dw, cf_ln_g, cf_ln_b, cf_w_pw2, cf_b_pw2,
                 ffn_w_up, ffn_w_dw, ffn_b_dw, ffn_w_down, ada_w, ada_b, ln_g, ln_b):
    A = lambda a: np.ascontiguousarray(np.asarray(a))
    consts = _consts()
    ck = A(cache_k).reshape(-1, 512)
    cv = A(cache_v).reshape(-1, 512)
    cfp = A(np.concatenate([np.asarray(cf_w_dw)[0], np.asarray(cf_b_dw)[0][None], np.asarray(cf_ln_g)[0][None],
                            np.asarray(cf_ln_b)[0][None], np.asarray(cf_b_pw1)[0].reshape(2, D)], axis=0))
    ffp = A(np.concatenate([np.asarray(ffn_w_dw), np.asarray(ffn_b_dw)[:, None, :]], axis=1))
    shared = {
        "ck": ck, "cv": cv, "w_in": A(ab_w_in)[0], "w_out": A(ab_w_out)[0], "pw1": A(cf_w_pw1)[0], "cfp": cfp,
        "pw2": A(cf_w_pw2)[0], "b_pw2": A(cf_b_pw2).reshape(1, D), "ffn_up": A(ffn_w_up), "ffp": ffp,
        "ffn_down": A(ffn_w_down), "ada_w": A(ada_w), "ada_b": A(ada_b), "ln_g": A(ln_g).reshape(4, D),
        "ln_b": A(ln_b).reshape(4, D),
    }
    shared.update(consts)
    maps = []
    for c in range(NCORES):
        s0, s1 = c * NS, (c + 1) * NS
        m = dict(shared)
        m["xp"] = A(x_prompt[c])
        m["xs"] = A(np.asarray(x_sample)[s0:s1].reshape(SR, D))
        m["call"] = A(np.concatenate([np.asarray(c_prompt)[c:c + 1], np.asarray(c_sample)[s0:s1]], axis=0))
        m["pt"] = A(np.asarray(page_table)[s0:s1].reshape(1, NS * 16).astype(np.int32))
        m["sret"] = A(np.asarray(state_ret)[0, s0:s1].reshape(NS * 512, 128))
        m["sconv"] = A(np.asarray(state_conv)[0, s0:s1].reshape(NS * 30, D))
        m["sffn"] = A(np.asarray(state_ffn)[:, s0:s1].reshape(2, NS * 2, DFF))
        maps.append(m)
    return maps


def assemble(results):
    R = results
    cat = lambda name: [r[name] for r in R]
    y_prompt = np.stack(cat("yp"), 0)
    y_sample = np.concatenate(cat("ys"), 0).reshape(128, 4, D)
    k_prompt = np.stack(cat("kp"), 0).reshape(1, 8, T, 4, 128)
    v_prompt = np.stack(cat("vp"), 0).reshape(1, 8, T, 4, 128)
    k_sample = np.concatenate(cat("ks"), 0).reshape(1, 128, 4, 4, 128)
    v_sample = np.concatenate(cat("vs"), 0).reshape(1, 128, 4, 4, 128)
    ret_prompt = np.stack(cat("rpo"), 0).reshape(1, 8, 4, 128, 128)
    ret_sample = np.concatenate(cat("rso"), 0).reshape(1, 128, 4, 128, 128)
    conv_prompt = np.stack(cat("cpo"), 0).reshape(1, 8, 30, D)
    conv_sample = np.concatenate(cat("cso"), 0).reshape(1, 128, 30, D)
    ffn_prompt = np.stack(cat("fpo"), 1).reshape(2, 8, 2, DFF)
    ffn_sample = np.concatenate([r["fso"].reshape(2, NS, 2, DFF) for r in R], 1)
    outs = (y_prompt, y_sample, k_prompt, v_prompt, k_sample, v_sample, ret_prompt, ret_sample,
            conv_prompt, conv_sample, ffn_prompt, ffn_sample)
    return tuple(np.ascontiguousarray(o, dtype=np.float32) for o in outs)


def kernel(**inputs):
    nc = build()
    maps = make_in_maps(**inputs)
    res = run_bass_kernel_spmd(nc, maps, core_ids=list(range(NCORES)))
    return assemble(res.results)
```

```python
import math
import numpy as np
import ml_dtypes
import concourse.bass as bass
import concourse.mybir as mybir
from concourse.bass_utils import run_bass_kernel_spmd

F32 = mybir.dt.float32
BF16 = mybir.dt.bfloat16
I32 = mybir.dt.int32
ALU = mybir.AluOpType
AF = mybir.ActivationFunctionType
AX = mybir.AxisListType

NCORES = 8
D = 1024
T = 2048
NT = 16
NS = 16
SR = 64
DFF = 2816
NFC = 22
ALPHA = 4.0 ** 0.25
LN_EPS = 1e-5
GN_EPS = 1e-6
SCALE = 128.0 ** -0.5
NEG = -1.0e30
LOGG = [math.log1p(-2.0 ** (-5.0 - h)) for h in range(4)]


class Reg:
    __slots__ = ("w", "r", "p", "dsem", "dcnt", "psum")

    def __init__(self, psum=False):
        self.psum = psum
        self.w = {}
        self.r = {}
        self.p = {}
        self.dsem = None
        self.dcnt = 0


class Buf:
    __slots__ = ("t", "r")

    def __init__(self, t):
        self.t = t
        self.r = Reg()


class Pool:
    def __init__(self, bufs):
        self.bufs = bufs
        self.i = 0

    def next(self):
        b = self.bufs[self.i % len(self.bufs)]
        self.i += 1
        return b


def _merge(dst, src):
    for s, v in src.items():
        if dst.get(s, 0) < v:
            dst[s] = v


class KB:
    def __init__(self, nc):
        self.nc = nc
        self.eng = {"pe": nc.tensor, "act": nc.scalar, "dve": nc.vector, "pool": nc.gpsimd, "sp": nc.sync}
        self.esem = {e: nc.alloc_semaphore("es_" + e) for e in ("pe", "act", "dve", "pool")}
        self.ecnt = {e: 0 for e in self.esem}
        self.seen = {e: {} for e in self.eng}
        self.nsem = 4
        self.out_toks = {}
        self.sb_off = (nc.sbuf_base + 63) // 64 * 64
        self.sb_top = nc.sbuf_top
        self.sb_marks = []
        self.uid = 0
        self.ninst = 0
        self.anchors = []
        self.limit = self.sb_top

    def barrier(self):
        for e, E in self.eng.items():
            seen = self.seen[e]
            for e2, s in self.esem.items():
                v = self.ecnt[e2]
                if v > seen.get(s, 0) and not (e == "pe" and e2 == "pe"):
                    E.wait_ge(s, v)
                    seen[s] = v
                    self.ninst += 1
            for a in self.anchors:
                if a.dcnt > seen.get(a.dsem, 0):
                    E.wait_ge(a.dsem, a.dcnt)
                    seen[a.dsem] = a.dcnt
                    self.ninst += 1

    def sb(self, shape, dtype, name=None):
        self.uid += 1
        nm = (name or "t") + "_%d" % self.uid
        esz = 2 if dtype == BF16 else 4
        n = 1
        for s in shape[1:]:
            n *= s
        nbytes = (n * esz + 63) // 64 * 64
        off = self.sb_off
        self.sb_off += nbytes
        assert self.sb_off <= self.limit, "SBUF overflow %d > %d (%s)" % (self.sb_off, self.limit, nm)
        return self.nc.alloc_sbuf_tensor_at(nm, list(shape), dtype, offset=off)

    def buf(self, shape, dtype, name=None):
        return Buf(self.sb(shape, dtype, name))

    def pool(self, n, shape, dtype, name=None):
        return Pool([self.buf(shape, dtype, name) for _ in range(n)])

    def mark(self):
        self.sb_marks.append(self.sb_off)

    def release(self):
        self.sb_off = self.sb_marks.pop()
        self.barrier()

    def _waits(self, eng, reads, writes, accum):
        need = {}
        mysem0 = self.esem.get(eng)
        for r in reads:
            _merge(need, r.w)
            if r.psum:
                for s_, v_ in r.r.items():
                    if s_ is not mysem0 and need.get(s_, 0) < v_:
                        need[s_] = v_
        for w in writes:
            if accum and not w.r:
                _merge(need, w.p)
            else:
                _merge(need, w.r)
                _merge(need, w.w)
        E = self.eng[eng]
        mysem = self.esem.get(eng)
        seen = self.seen[eng]
        for s, v in need.items():
            if eng == "pe" and s is mysem:
                continue
            if seen.get(s, 0) >= v:
                continue
            E.wait_ge(s, v)
            self.ninst += 1
            seen[s] = v

    def _update(self, tok, reads, writes, accum):
        s, v = tok
        for r in reads:
            if r.r.get(s, 0) < v:
                r.r[s] = v
        for w in writes:
            if accum and not w.r:
                if w.w.get(s, 0) < v:
                    w.w[s] = v
            else:
                p = dict(w.w)
                _merge(p, w.r)
                w.p = p
                w.w = {s: v}
                w.r = {}

    def op(self, eng, fn, reads=(), writes=(), accum=False, inc=True):
        self._waits(eng, reads, writes, accum)
        ins = fn(self.eng[eng])
        self.ninst += 1
        if inc:
            self.ecnt[eng] += 1
            ins.then_inc(self.esem[eng], 1)
        tok = (self.esem[eng], self.ecnt[eng] + (0 if inc else 1))
        self._update(tok, reads, writes, accum)
        return ins

    def _dma_any(self, q, mk, reads, writes, anchor, accum, is_output):
        if anchor is None:
            anchor = writes[0] if writes else reads[0]
        if anchor.dsem is None:
            anchor.dsem = self.nc.alloc_semaphore("ds_%d" % self.nsem)
            self.nsem += 1
            self.anchors.append(anchor)
        self._waits(q, reads, writes, accum)
        ins = mk(self.eng[q])
        self.ninst += 1
        anchor.dcnt += 16
        ins.then_inc(anchor.dsem, 16)
        tok = (anchor.dsem, anchor.dcnt)
        self._update(tok, reads, writes, accum)
        if is_output:
            self.out_toks[anchor.dsem] = anchor.dcnt
        return ins

    def dma(self, q, out, in_, reads=(), writes=(), anchor=None, accum=False, is_output=False, **kw):
        return self._dma_any(q, lambda e: e.dma_start(out=out, in_=in_, **kw), reads, writes, anchor, accum, is_output)

    def gather(self, out, table, idx_ap, reads=(), writes=(), accum=False):
        return self._dma_any(
            "pool",
            lambda e: e.indirect_dma_start(out=out, out_offset=None, in_=table,
                                           in_offset=bass.IndirectOffsetOnAxis(ap=idx_ap, axis=0)),
            reads, writes, None, accum, False)

    def finish(self):
        sp = self.eng["sp"]
        for s, v in self.out_toks.items():
            sp.wait_ge(s, v)
        for e, s in self.esem.items():
            if self.ecnt[e] > 0:
                sp.wait_ge(s, self.ecnt[e])

    def mm(self, out, lhsT, rhs, start, stop, reads=(), writes=(), inc=None):
        return self.op("pe", lambda e: e.matmul(out, lhsT, rhs, start=start, stop=stop),
                       reads, writes, accum=not start, inc=(stop if inc is None else inc))

    def tr(self, out, in_, ident, reads=(), writes=(), accum=False):
        return self.op("pe", lambda e: e.transpose(out, in_, ident), reads, writes, accum=accum)

    def act(self, out, in_, func, reads=(), writes=(), accum=False, **kw):
        return self.op("act", lambda e: e.activation(out, in_, func, **kw), reads, writes, accum=accum)

    def tt(self, eng, out, in0, in1, op, reads=(), writes=(), accum=False):
        return self.op(eng, lambda e: e.tensor_tensor(out, in0, in1, op), reads, writes, accum=accum)

    def ts(self, eng, out, in0, s1, s2, op0, op1=None, reads=(), writes=(), accum=False):
        if op1 is None:
            return self.op(eng, lambda e: e.tensor_scalar(out, in0, s1, None, op0), reads, writes, accum=accum)
        return self.op(eng, lambda e: e.tensor_scalar(out, in0, s1, s2, op0, op1), reads, writes, accum=accum)

    def stt(self, out, in0, scalar, in1, op0, op1, reads=(), writes=(), accum=False):
        return self.op("dve", lambda e: e.scalar_tensor_tensor(out, in0, scalar, in1, op0, op1), reads, writes, accum=accum)

    def cp(self, eng, out, in_, reads=(), writes=(), accum=False):
        if eng == "act":
            return self.op("act", lambda e: e.copy(out, in_), reads, writes, accum=accum)
        return self.op(eng, lambda e: e.tensor_copy(out, in_), reads, writes, accum=accum)

    def memset(self, eng, ap, val, writes=(), accum=False):
        return self.op(eng, lambda e: e.memset(ap, val), (), writes, accum=accum)


def bc(ap, axis, n):
    a = ap.unsqueeze(axis)
    shp = list(a.shape)
    shp[axis] = n
    return a.broadcast_to(shp)


def build(stage=99, nphys=2560, skip_ms=False):
    nc = bass.Bass("TRN2", target_bir_lowering=False)
    k = KB(nc)

    def din(name, shape, dt=F32):
        return nc.dram_tensor(name, list(shape), dt, kind="ExternalInput").ap()

    def dout(name, shape):
        return nc.dram_tensor(name, list(shape), F32, kind="ExternalOutput").ap()

    xp = din("xp", [T, D]); xs_d = din("xs", [SR, D]); call = din("call", [17, D])
    ck = din("ck", [nphys * 128, 512]); cv = din("cv", [nphys * 128, 512])
    ptd = din("pt", [1, NS * 16], I32)
    sret = din("sret", [NS * 4 * 128, 128]); sconv = din("sconv", [NS * 30, D]); sffn = din("sffn", [2, NS * 2, DFF])
    w_in = din("w_in", [D, 3584]); w_out = din("w_out", [D, D]); pw1 = din("pw1", [D, 2048])
    cfp_d = din("cfp", [36, D]); pw2 = din("pw2", [D, D]); b_pw2 = din("b_pw2", [1, D])
    ffn_up = din("ffn_up", [2, D, 2 * DFF]); ffp_d = din("ffp", [2, 4, DFF]); ffn_down = din("ffn_down", [2, DFF, D])
    ada_w = din("ada_w", [2, D, 6 * D]); ada_b = din("ada_b", [2, 6 * D])
    ln_g = din("ln_g", [4, D]); ln_b = din("ln_b", [4, D])
    ident_d = din("ident", [128, 128]); iota_d = din("iota", [128, 1])
    rot_d = din("rot", [17, 128, 4, 128])
    rdm_d = din("rdm", [128, 4, 128]); rqd_d = din("rqd", [128, 4, 128]); rkd_d = din("rkd", [128, 4])
    rdms_d = din("rdms", [64, 4, 64]); rqds_d = din("rqds", [128, 4, 64]); rkds_d = din("rkds", [64, 4])
    blk_d = din("blk", [64, 16]); tri_d = din("tri", [128, 128]); nmask_d = din("nmask", [64, 16, 16])
    Ep_d = din("Ep", [18, 128]); Es_d = din("Es", [18, 64]); ep_d = din("ep", [18, 1])

    yp = dout("yp", [T, D]); ys = dout("ys", [SR, D])
    kp = dout("kp", [T, 512]); vp = dout("vp", [T, 512]); ks = dout("ks", [SR, 512]); vs = dout("vs", [SR, 512])
    rpo = dout("rpo", [512, 128]); rso = dout("rso", [NS * 512, 128])
    cpo = dout("cpo", [30, D]); cso = dout("cso", [NS * 30, D])
    fpo = dout("fpo", [2, 2, DFF]); fso = dout("fso", [2, NS * 2, DFF])

    sc_mods = [nc.dram_tensor("sc_mods%d" % l, [SR, 3 * D], F32).ap() for l in range(2)]
    sc_g2p = [nc.dram_tensor("sc_g2p%d" % l, [1, D], F32).ap() for l in range(2)]
    r_scm = [Reg(), Reg()]
    r_scg = [Reg(), Reg()]

    ps = nc.alloc_psum_tensor("ps", [128, 4096], F32)
    psb = ps[:].bitcast(BF16)
    rb = [Reg(psum=True) for _ in range(8)]

    def bank(i, rows=128, c0=0, c1=512):
        return ps[0:rows, i * 512 + c0:i * 512 + c1]

    def bankb(i, rows=128, c0=0, c1=1024):
        return psb[0:rows, i * 1024 + c0:i * 1024 + c1]

    rconst = Reg()

    def cload(shape, src, dt=F32, q="sp"):
        t = k.sb(shape, dt)
        k.dma(q, t[:], src, writes=[rconst], anchor=rconst, accum=True)
        return t

    ident = cload([128, 128], ident_d)
    iota = cload([128, 1], iota_d)
    Ep = cload([18, 128], Ep_d); Es = cload([18, 64], Es_d); ep = cload([18, 1], ep_d)
    identb_t = k.sb([128, 128], BF16)
    onesb = k.sb([128, 128], BF16)
    onesf = k.sb([128, 128], F32)
    k.memset("pool", onesf[:], 1.0, writes=[rconst], accum=True)
    k.cp("dve", identb_t[:], ident[:], reads=[rconst], writes=[rconst], accum=True)
    k.memset("pool", onesb[:], 1.0, writes=[rconst], accum=True)
    XOFF = (k.sb_top - NT * D * 4) // 64 * 64
    x_all = nc.alloc_sbuf_tensor_at("x_all", [128, NT, D], F32, offset=XOFF)
    rx = [Reg() for _ in range(NT)]
    x_s = k.buf([SR, D], F32)
    k.dma("sp", x_s.t[:], xs_d, writes=[x_s.r])
    modT = k.buf([128, 2, 6, 8], F32)
    scT = k.buf([128, 8, 32], BF16)
    k.mark()
    callsb = cload([17, D], call)
    scs = k.buf([17, D], F32)
    k.act(scs.t[:], callsb[:], AF.Silu, reads=[rconst], writes=[scs.r])
    for c in range(8):
        k.tr(bank(0, 128, c * 32, c * 32 + 17), scs.t[0:17, c * 128:(c + 1) * 128], ident[0:17, 0:17],
             reads=[scs.r, rconst], writes=[rb[0]], accum=(c > 0))
    k.cp("dve", scT.t[:, :, 0:17], bank(0, 128, 0, 256).rearrange("p (c j) -> p c j", j=32)[:, :, 0:17],
         reads=[rb[0]], writes=[scT.r])
    k.release()

    cvt_ctr = [0]

    def load_w_bf16(dst_ap, dst_reg, src_ap, stg, eng=None, first=True, mul=None, mul_reg=None):
        shp = list(src_ap.shape)
        if len(shp) == 3:
            st = stg.t[:, 0:shp[1] * shp[2]].rearrange("p (a b) -> p a b", b=shp[2])
        else:
            st = stg.t[:, 0:shp[1]]
        k.dma("sp", st, src_ap, writes=[stg.r])
        if eng is None:
            cvt_ctr[0] += 1
            eng = "act" if cvt_ctr[0] % 2 else "dve"
        if mul is None:
            k.cp(eng, dst_ap, st, reads=[stg.r], writes=[dst_reg], accum=not first)
        else:
            k.tt("dve", dst_ap, st, mul, ALU.mult, reads=[stg.r, mul_reg], writes=[dst_reg], accum=not first)

    def layer_norm(xin, xin_reg, rows, gam, bet, gb_reg, out_ap, out_reg, wk):
        st = wk["st"].next(); mv = wk["mv"].next()
        xv = xin.rearrange("p (c f) -> p c f", f=512)
        for c in range(2):
            k.op("dve", lambda e, c=c: e.bn_stats(st.t[0:rows, c, :], xv[:, c, :]), reads=[xin_reg], writes=[st.r], accum=(c > 0))
        k.op("dve", lambda e: e.bn_aggr(mv.t[0:rows, 0:2], st.t[0:rows, :, :]), reads=[st.r], writes=[mv.r])
        k.ts("dve", mv.t[0:rows, 2:3], mv.t[0:rows, 1:2], LN_EPS, None, ALU.add, reads=[mv.r], writes=[mv.r])
        k.act(mv.t[0:rows, 2:3], mv.t[0:rows, 2:3], AF.Sqrt, reads=[mv.r], writes=[mv.r])
        k.op("dve", lambda e: e.reciprocal(mv.t[0:rows, 2:3], mv.t[0:rows, 2:3]), reads=[mv.r], writes=[mv.r])
        k.stt(mv.t[0:rows, 3:4], mv.t[0:rows, 0:1], -1.0, mv.t[0:rows, 2:3], ALU.mult, ALU.mult, reads=[mv.r], writes=[mv.r])
        xn = wk["xn"].next()
        k.act(xn.t[0:rows, :], xin, AF.Identity, reads=[xin_reg, mv.r], writes=[xn.r], scale=mv.t[0:rows, 2:3], bias=mv.t[0:rows, 3:4])
        k.tt("dve", xn.t[0:rows, :], xn.t[0:rows, :], gam[0:rows, :], ALU.mult, reads=[xn.r, gb_reg], writes=[xn.r])
        k.tt("dve", out_ap, xn.t[0:rows, :], bet[0:rows, :], ALU.add, reads=[xn.r, gb_reg], writes=[out_reg])

    def make_hT(xin, xin_reg, rows, hT_ap, hT_reg, layer, which, mods_s=None, mods_reg=None, wk=None, pbanks=(0, 1), first=True):
        if rows == 128:
            src, src_reg = xin, xin_reg
        else:
            hs = wk["hs"].next()
            k.tt("dve", hs.t[0:rows], xin, mods_s[:, D:2 * D], ALU.mult, reads=[xin_reg, mods_reg], writes=[hs.r])
            k.tt("dve", hs.t[0:rows], hs.t[0:rows], mods_s[:, 0:D], ALU.add, reads=[hs.r, mods_reg], writes=[hs.r])
            src, src_reg = hs.t[0:rows], hs.r
        for c in range(8):
            b = pbanks[c // 4]
            k.tr(bank(b, 128, (c % 4) * 128, (c % 4) * 128 + rows), src[:, c * 128:(c + 1) * 128], ident[0:rows, 0:rows],
                 reads=[src_reg, rconst], writes=[rb[b]], accum=(c % 4 > 0))
        for c in range(8):
            b = pbanks[c // 4]
            pin = bank(b, 128, (c % 4) * 128, (c % 4) * 128 + rows)
            if rows == 128:
                k.act(hT_ap[:, c, :], pin, AF.Identity, reads=[rb[b], modT.r], writes=[hT_reg], accum=not (first and c == 0),
                      scale=modT.t[:, layer, which + 1, c:c + 1], bias=modT.t[:, layer, which, c:c + 1])
            else:
                k.cp("act", hT_ap[:, c, :], pin, reads=[rb[b]], writes=[hT_reg], accum=not (first and c == 0))

    def rotate(xv, xregs, rows, G, Ct, St, tab_reg, out3, out_reg, tmp3, tmp_reg, mode):
        if mode == "half":
            x4 = xv.rearrange("p g (two j) -> p g two j", two=2)
            t4 = tmp3.rearrange("p g (two j) -> p g two j", two=2)
            a0, a1 = x4[:, :, 1, :], x4[:, :, 0, :]
            d0, d1 = t4[:, :, 0, :], t4[:, :, 1, :]
            s0, s1 = St[:, 0:64], St[:, 64:128]
        else:
            x4 = xv.rearrange("p g (j two) -> p g j two", two=2)
            t4 = tmp3.rearrange("p g (j two) -> p g j two", two=2)
            a0, a1 = x4[:, :, :, 1], x4[:, :, :, 0]
            d0, d1 = t4[:, :, :, 0], t4[:, :, :, 1]
            Sv = St.rearrange("p (j two) -> p j two", two=2)
            s0, s1 = Sv[:, :, 0], Sv[:, :, 1]
        k.tt("dve", d0, a0, bc(s0, 1, G), ALU.mult, reads=list(xregs) + [tab_reg], writes=[tmp_reg])
        k.tt("dve", d1, a1, bc(s1, 1, G), ALU.mult, reads=list(xregs) + [tab_reg], writes=[tmp_reg], accum=True)
        k.tt("dve", out3, xv, bc(Ct, 1, G), ALU.mult, reads=list(xregs) + [tab_reg], writes=[out_reg])
        k.tt("dve", out3, out3, tmp3, ALU.add, reads=[out_reg, tmp_reg], writes=[out_reg])

    def adaln(l, mods_mix, g1p):
        k.mark()
        stg = k.pool(2, [128, 8 * 512], F32, "ada_stg")
        wbp = k.pool(2, [128, 8, 512], BF16, "ada_wb")
        m17p = k.pool(2, [18, 512], F32, "m17")
        outp = k.pool(2, [128, 512], F32, "ada_out")
        for j in range(12):
            which, half = divmod(j, 2)
            wb = wbp.next()
            load_w_bf16(wb.t[:], wb.r, ada_w[l, :, j * 512:(j + 1) * 512].rearrange("(kc p) n -> p kc n", p=128), stg.next())
            m17 = m17p.next()
            k.dma("sp", m17.t[17:18, :], ada_b[l:l + 1, j * 512:(j + 1) * 512], writes=[m17.r])
            for kc in range(8):
                k.mm(bank(0, 17), scT.t[:, kc, 0:17], wb.t[:, kc, :], kc == 0, kc == 7, reads=[scT.r, wb.r], writes=[rb[0]])
            k.cp("act", m17.t[0:17, :], bank(0, 17), reads=[rb[0]], writes=[m17.r], accum=True)
            plus1 = 1.0 if which in (1, 2, 4, 5) else 0.0
            k.mm(bank(1, 64), Es[:], m17.t[:], True, True, reads=[rconst, m17.r], writes=[rb[1]])
            if which < 3:
                k.ts("dve", mods_mix.t[:, j * 512:(j + 1) * 512], bank(1, 64), plus1, None, ALU.add,
                     reads=[rb[1]], writes=[mods_mix.r], accum=(j > 0))
            else:
                o = outp.next()
                k.ts("dve", o.t[0:64, :], bank(1, 64), plus1, None, ALU.add, reads=[rb[1]], writes=[o.r])
                k.dma("sp", sc_mods[l][:, (j - 6) * 512:(j - 5) * 512], o.t[0:64, :], reads=[o.r], writes=[r_scm[l]], accum=(j > 6))
            if which in (2, 5):
                k.mm(bank(2), Ep[:], m17.t[:], True, True, reads=[rconst, m17.r], writes=[rb[2]])
                if which == 2:
                    k.ts("dve", g1p.t[:, half * 512:(half + 1) * 512], bank(2), 1.0, None, ALU.add,
                         reads=[rb[2]], writes=[g1p.r], accum=(half > 0))
                else:
                    o = outp.next()
                    k.ts("dve", o.t[:, :], bank(2), 1.0, None, ALU.add, reads=[rb[2]], writes=[o.r])
                    k.dma("sp", sc_g2p[l][:, half * 512:(half + 1) * 512], o.t[0:1, :], reads=[o.r], writes=[r_scg[l]], accum=(half > 0))
            for cc in range(4):
                k.mm(bank(3, 128, cc, cc + 1), m17.t[:, cc * 128:(cc + 1) * 128], ep[:], True, True,
                     reads=[m17.r, rconst], writes=[rb[3]])
            k.ts("dve", modT.t[:, l, which, half * 4:(half + 1) * 4], bank(3, 128, 0, 4), plus1, None, ALU.add,
                 reads=[rb[3]], writes=[modT.r], accum=True)
        k.release()

    def pass_moba(mods_mix, omT):
        k.mark()
        wm = k.buf([128, 8, 1536], BF16, "wm")
        qTs_b = k.buf([128, 4, SR], BF16, "qTs_b"); qTs_f = k.buf([128, 4, SR], F32, "qTs_f")
        kTs_b = k.buf([128, 4, SR], BF16, "kTs_b"); v_s = k.buf([SR, 4, 132], BF16, "v_s")
        k.mark()
        kT_hist = k.buf([128, 4, T], BF16, "kT_hist")
        v_hist = k.buf([128, NT, 512], BF16, "v_hist")
        kmT = k.buf([128, 4, 8], F32, "kmT")
        k.mark()
        stg = k.pool(2, [128, 8 * 512], F32, "stg")
        for g in range(3):
            load_w_bf16(wm.t[:, :, g * 512:(g + 1) * 512], wm.r,
                        w_in[:, 2048 + g * 512:2048 + (g + 1) * 512].rearrange("(kc p) n -> p kc n", p=128),
                        stg.next(), first=(g == 0))
        k.release()
        wk = {"hs": k.pool(1, [SR, D], F32, "hs")}
        xt_p = k.pool(2, [128, D], F32, "xt")
        hT_p = k.pool(2, [128, 8, 128], BF16, "hT")
        rot_p = k.pool(2, [128, 4, 128], F32, "rot")
        qk_p = k.pool(2, [128, 8, 128], F32, "qkrot")
        tmp_p = k.pool(1, [128, 8, 128], F32, "rtmp")
        vf_p = k.pool(2, [128, 512], F32, "vf")
        qTb_p = k.pool(2, [128, 4, 128], BF16, "qTb")
        qTf_p = k.pool(2, [128, 4, 128], F32, "qTf")
        ksum_p = k.pool(2, [128, 4], F32, "ksum")
        gate_p = k.pool(1, [128, 4, 8], F32, "gate")
        cmp_p = k.pool(1, [128, 4, 8, 8], F32, "cmp")
        bias_p = k.pool(2, [128, 4, 8], F32, "bias")
        S_p = k.pool(1, [128, T], F32, "S")
        P_p = k.pool(2, [128, T], BF16, "P")
        PT_p = k.pool(2, [128, NT, 128], BF16, "PT")
        sm_p = k.pool(4, [128, 4], F32, "sm")
        om_p = k.pool(2, [128, 512], BF16, "om")

        for t in range(NT + 1):
            rows = 128 if t < NT else SR
            if t < NT:
                xt = xt_p.next()
                k.dma("sp", xt.t[:], xp[t * 128:(t + 1) * 128, :], writes=[xt.r])
                xin, xin_reg = xt.t[:], xt.r
            else:
                xin, xin_reg = x_s.t[:], x_s.r
            hT = hT_p.next()
            make_hT(xin, xin_reg, rows, hT.t[:, :, 0:rows], hT.r, 0, 0, mods_s=mods_mix.t, mods_reg=mods_mix.r, wk=wk, pbanks=(0, 1))
            rot = rot_p.next()
            k.dma("sp", rot.t[0:rows], rot_d[t, 0:rows], writes=[rot.r])
            for g in range(3):
                for kc in range(8):
                    k.mm(bank(4 + g, rows), hT.t[:, kc, 0:rows], wm.t[:, kc, g * 512:(g + 1) * 512], kc == 0, kc == 7,
                         reads=[hT.r, wm.r], writes=[rb[4 + g]])
            qk = qk_p.next(); tmp = tmp_p.next()
            zqk = ps[0:rows, 4 * 512:6 * 512].rearrange("p (g d) -> p g d", d=128)
            rotate(zqk, [rb[4], rb[5]], rows, 8, rot.t[0:rows, 2, :], rot.t[0:rows, 3, :], rot.r,
                   qk.t[0:rows], qk.r, tmp.t[0:rows], tmp.r, "half")
            vf = vf_p.next()
            k.cp("act", vf.t[0:rows], bank(6, rows), reads=[rb[6]], writes=[vf.r])
            kdst = kp[t * 128:(t + 1) * 128, :] if t < NT else ks
            vdst = vp[t * 128:(t + 1) * 128, :] if t < NT else vs
            k.dma("sp", kdst, qk.t[0:rows, 4:8, :].rearrange("p g d -> p (g d)"), reads=[qk.r], is_output=True)
            k.dma("sp", vdst, vf.t[0:rows], reads=[vf.r], is_output=True)
            if stage < 2:
                continue
            if t < NT:
                k.cp("act", v_hist.t[:, t, :], bank(6), reads=[rb[6]], writes=[v_hist.r], accum=(t > 0))
            else:
                k.memset("pool", v_s.t[:], 1.0, writes=[v_s.r])
                k.cp("pool", v_s.t[:, :, 0:128], vf.t[0:SR].rearrange("p (h d) -> p h d", d=128), reads=[vf.r], writes=[v_s.r])
            for g in range(8):
                b = 2 + g // 4
                k.tr(bank(b, 128, (g % 4) * 128, (g % 4) * 128 + rows), qk.t[0:rows, g, :], ident[0:rows, 0:rows],
                     reads=[qk.r, rconst], writes=[rb[b]], accum=(g % 4 > 0))
            qv = bank(2).rearrange("p (h s) -> p h s", s=128)[:, :, 0:rows]
            kv = bank(3).rearrange("p (h s) -> p h s", s=128)[:, :, 0:rows]
            if t < NT:
                qTb = qTb_p.next(); qTf = qTf_p.next()
                k.cp("act", qTb.t[:], qv, reads=[rb[2]], writes=[qTb.r])
                k.cp("dve", qTf.t[:], qv, reads=[rb[2]], writes=[qTf.r])
                k.cp("act", kT_hist.t[:, :, t * 128:(t + 1) * 128], kv, reads=[rb[3]], writes=[kT_hist.r], accum=(t > 0))
                ksum = ksum_p.next()
                k.op("dve", lambda e, ksum=ksum, kv=kv: e.tensor_reduce(ksum.t[:], kv, axis=AX.X, op=ALU.add), reads=[rb[3]], writes=[ksum.r])
                if t % 2 == 0:
                    k.cp("pool", kmT.t[:, :, t // 2], ksum.t[:], reads=[ksum.r], writes=[kmT.r], accum=True)
                else:
                    k.tt("pool", kmT.t[:, :, t // 2], kmT.t[:, :, t // 2], ksum.t[:], ALU.add, reads=[ksum.r, kmT.r], writes=[kmT.r])
            else:
                k.cp("act", qTs_b.t[:], qv, reads=[rb[2]], writes=[qTs_b.r])
                k.cp("dve", qTs_f.t[:], qv, reads=[rb[2]], writes=[qTs_f.r])
                k.cp("act", kTs_b.t[:], kv, reads=[rb[3]], writes=[kTs_b.r])
                continue
            own = t // 2
            bias = None
            if own >= 4:
                for h in range(4):
                    k.mm(bank(7, 128, 448 + h * 8, 448 + h * 8 + own), qTf.t[:, h, :], kmT.t[:, h, 0:own], True, True,
                         reads=[qTf.r, kmT.r], writes=[rb[7]])
                gate = gate_p.next(); cmpb = cmp_p.next(); bias = bias_p.next()
                gpv = bank(7, 128, 448, 480).rearrange("p (h n) -> p h n", n=8)[:, :, 0:own]
                k.cp("act", gate.t[:, :, 0:own], gpv, reads=[rb[7]], writes=[gate.r])
                gv = gate.t[:, :, 0:own]
                k.tt("dve", cmpb.t[:, :, 0:own, 0:own], bc(gv, 2, own), bc(gv, 3, own), ALU.is_gt, reads=[gate.r], writes=[cmpb.r])
                k.op("dve", lambda e, gate=gate, cmpb=cmpb, own=own: e.tensor_reduce(gate.t[:, :, 0:own], cmpb.t[:, :, 0:own, 0:own], axis=AX.X, op=ALU.add),
                     reads=[cmpb.r], writes=[gate.r])
                k.ts("dve", bias.t[:, :, 0:own], gate.t[:, :, 0:own], 2.5, NEG, ALU.is_gt, ALU.mult, reads=[gate.r], writes=[bias.r])
            nk = (t + 1) * 128
            om = om_p.next()
            for h in range(4):
                for c0 in range(0, nk, 512):
                    c1 = min(nk, c0 + 512)
                    b = c0 // 512
                    k.mm(bank(b, 128, 0, c1 - c0), qTb.t[:, h, :], kT_hist.t[:, h, c0:c1], True, True,
                         reads=[qTb.r, kT_hist.r], writes=[rb[b]])
                nb_used = (nk + 511) // 512
                sregs = [rb[i] for i in range(nb_used)]
                S = S_p.next()
                npast = own * 256
                first = True
                if npast > 0:
                    if bias is not None:
                        k.tt("dve", S.t[:, 0:npast].rearrange("p (n s) -> p n s", s=256),
                             ps[:, 0:npast].rearrange("p (n s) -> p n s", s=256), bc(bias.t[:, h, 0:own], 2, 256), ALU.add,
                             reads=sregs + [bias.r], writes=[S.r])
                    else:
                        k.cp("act", S.t[:, 0:npast], ps[:, 0:npast], reads=sregs, writes=[S.r])
                    first = False
                if t % 2 == 1:
                    k.cp("act", S.t[:, npast:npast + 128], ps[:, npast:npast + 128], reads=sregs, writes=[S.r], accum=not first)
                    first = False
                k.tt("dve", S.t[:, nk - 128:nk], ps[:, nk - 128:nk], tri[:], ALU.add, reads=sregs + [rconst], writes=[S.r], accum=not first)
                sm = sm_p.next()
                k.op("dve", lambda e, sm=sm, S=S, nk=nk: e.reduce_max(sm.t[:, 0:1], S.t[:, 0:nk], axis=AX.X), reads=[S.r], writes=[sm.r])
                k.ts("dve", sm.t[:, 1:2], sm.t[:, 0:1], -SCALE, None, ALU.mult, reads=[sm.r], writes=[sm.r])
                P = P_p.next()
                k.act(P.t[:, 0:nk], S.t[:, 0:nk], AF.Exp, reads=[S.r, sm.r], writes=[P.r, sm.r], scale=SCALE, bias=sm.t[:, 1:2],
                      accum_out=sm.t[:, 2:3])
                PT = PT_p.next()
                for j0 in range(0, t + 1, 8):
                    j1 = min(t + 1, j0 + 8)
                    for j in range(j0, j1):
                        k.tr(bankb(6, 128, (j - j0) * 128, (j - j0 + 1) * 128), P.t[:, j * 128:(j + 1) * 128], identb_t[:],
                             reads=[P.r, rconst], writes=[rb[6]], accum=(j > j0))
                    k.cp("act" if (j0 // 8) % 2 == 0 else "dve", PT.t[:, j0:j1, :],
                         bankb(6, 128, 0, (j1 - j0) * 128).rearrange("p (j s) -> p j s", s=128), reads=[rb[6]], writes=[PT.r], accum=(j0 > 0))
                for j in range(t + 1):
                    k.mm(bank(7, 128, 0, 128), PT.t[:, j, :], v_hist.t[:, j, h * 128:(h + 1) * 128],
                         j == 0, j == t, reads=[PT.r, v_hist.r], writes=[rb[7]])
                k.op("dve", lambda e, sm=sm: e.reciprocal(sm.t[:, 3:4], sm.t[:, 2:3]), reads=[sm.r], writes=[sm.r])
                k.act(om.t[:, h * 128:(h + 1) * 128], bank(7, 128, 0, 128), AF.Identity, reads=[rb[7], sm.r], writes=[om.r], accum=(h > 0),
                      scale=sm.t[:, 3:4])
            for h in range(4):
                k.tr(bankb(6, 128, h * 128, (h + 1) * 128), om.t[:, h * 128:(h + 1) * 128], identb_t[:], reads=[om.r, rconst],
                     writes=[rb[6]], accum=(h > 0))
            k.cp("act", omT.t[:, :, t * 128:(t + 1) * 128], bankb(6, 128, 0, 512).rearrange("p (h s) -> p h s", s=128),
                 reads=[rb[6]], writes=[omT.r], accum=(t > 0))
        k.release()
        if stage >= 3 and not skip_ms:
            moba_sample(qTs_b, qTs_f, kTs_b, v_s, omT)
        k.release()

    def moba_sample(qTs_b, qTs_f, kTs_b, v_s, omT):
        kpg_p = k.pool(5, [128, 512], F32, "kpg")
        vpg_p = k.pool(5, [128, 512], F32, "vpg")
        kb_p = k.pool(2, [128, 512], BF16, "kb")
        kTq_p = k.pool(2, [128, 4, T], BF16, "kTq")
        Vq_p = k.pool(2, [128, 16, 4, 132], BF16, "Vq")
        E_p = k.pool(2, [128, 17, 4, SR], BF16, "Eb")
        for b_ in Vq_p.bufs:
            k.memset("pool", b_.t[:], 1.0, writes=[b_.r])
        for b_ in E_p.bufs:
            k.memset("pool", b_.t[:], 0.0, writes=[b_.r])
        kms_p = k.pool(2, [128, 4, 8], F32, "kms")
        ksum_p = k.pool(2, [128, 4], F32, "ksum2")
        prod_p = k.pool(1, [128, 4, 8, 4], F32, "prod")
        gs_p = k.pool(1, [128, 16, 8], F32, "gs")
        cmp_p = k.pool(1, [128, 16, 8, 8], F32, "cmp2")
        comb_p = k.pool(1, [128, 16, 8], F32, "comb")
        pm_p = k.pool(1, [128, 16], F32, "pm")
        m16_p = k.pool(1, [16, 20], F32, "m16")
        X_p = k.pool(1, [128, 16, 16], F32, "X")
        Xn_p = k.pool(1, [SR, 16], F32, "Xn")
        NPG = NS * 16
        PF = 4
        gbuf = {}

        def issue(p):
            kpg = kpg_p.next(); vpg = vpg_p.next()
            k.gather(kpg.t[:], ck, idx.t[:, p:p + 1], reads=[idx.r], writes=[kpg.r])
            k.gather(vpg.t[:], cv, idx.t[:, p:p + 1], reads=[idx.r], writes=[vpg.r])
            gbuf[p] = (kpg, vpg)

        for p in range(PF):
            issue(p)

        def page_loop(b):
            kTq = kTq_p.next(); Vq = Vq_p.next(); kms = kms_p.next()
            for j in range(16):
                p = b * 16 + j
                if p + PF < NPG:
                    issue(p + PF)
                kpg, vpg = gbuf.pop(p)
                kb = kb_p.next()
                k.cp("dve", kb.t[:], kpg.t[:], reads=[kpg.r], writes=[kb.r])
                k.cp("act", Vq.t[:, j, :, 0:128], vpg.t[:].rearrange("p (h d) -> p h d", d=128), reads=[vpg.r], writes=[Vq.r],
                     accum=(j > 0))
                pb = 1 + j % 2
                for h in range(4):
                    k.tr(bankb(pb, 128, h * 128, (h + 1) * 128), kb.t[:, h * 128:(h + 1) * 128], identb_t[:],
                         reads=[kb.r, rconst], writes=[rb[pb]], accum=(h > 0))
                kvw = bankb(pb, 128, 0, 512).rearrange("p (h s) -> p h s", s=128)
                k.cp("act", kTq.t[:, :, j * 128:(j + 1) * 128], kvw, reads=[rb[pb]], writes=[kTq.r], accum=(j > 0))
                ksum = ksum_p.next()
                k.op("dve", lambda e, ksum=ksum, kvw=kvw: e.tensor_reduce(ksum.t[:], kvw, axis=AX.X, op=ALU.add), reads=[rb[pb]], writes=[ksum.r])
                if j % 2 == 0:
                    k.cp("pool", kms.t[:, :, j // 2], ksum.t[:], reads=[ksum.r], writes=[kms.r], accum=(j > 0))
                else:
                    k.tt("pool", kms.t[:, :, j // 2], kms.t[:, :, j // 2], ksum.t[:], ALU.add, reads=[ksum.r, kms.r], writes=[kms.r])
            return kTq, Vq, kms

        def chain(b, kTq, Vq, kms):
            Eb = E_p.next()
            prod = prod_p.next()
            qf = qTs_f.t[:, :, 4 * b:4 * b + 4]
            k.tt("dve", prod.t[:], bc(kms.t[:], 3, 4), bc(qf, 2, 8), ALU.mult, reads=[kms.r, qTs_f.r], writes=[prod.r])
            k.mm(bank(3, 128, 0, 128), onesf[:], prod.t[:].rearrange("p h n q -> p (h n q)"), True, True,
                 reads=[rconst, prod.r], writes=[rb[3]])
            gs = gs_p.next(); cmpb = cmp_p.next(); comb = comb_p.next()
            k.cp("act", gs.t[:].rearrange("p (h q) n -> p h q n", q=4),
                 bank(3, 128, 0, 128).rearrange("p (h n q) -> p h q n", h=4, n=8), reads=[rb[3]], writes=[gs.r])
            k.tt("dve", cmpb.t[:], bc(gs.t[:], 2, 8), bc(gs.t[:], 3, 8), ALU.is_gt, reads=[gs.r], writes=[cmpb.r])
            k.op("dve", lambda e, gs=gs, cmpb=cmpb: e.tensor_reduce(gs.t[:], cmpb.t[:], axis=AX.X, op=ALU.add), reads=[cmpb.r], writes=[gs.r])
            k.ts("dve", comb.t[:], gs.t[:], 2.5, NEG, ALU.is_gt, ALU.mult, reads=[gs.r], writes=[comb.r])
            for j in range(16):
                for h in range(4):
                    c0 = j * 16 + h * 4
                    k.mm(bank(0, 128, c0, c0 + 4), kTq.t[:, h, j * 128:(j + 1) * 128], qTs_b.t[:, h, 4 * b:4 * b + 4], True, True,
                         reads=[kTq.r, qTs_b.r], writes=[rb[0]], inc=(j == 15 and h == 3))
            for h in range(4):
                k.mm(bank(0, SR, 256 + h * 4, 260 + h * 4), kTs_b.t[:, h, :], qTs_b.t[:, h, 4 * b:4 * b + 4], True, True,
                     reads=[kTs_b.r, qTs_b.r], writes=[rb[0]], inc=(h == 3))
            pm = pm_p.next(); m16 = m16_p.next()
            k.op("dve", lambda e, pm=pm: e.tensor_reduce(pm.t[:], bank(0, 128, 0, 256).rearrange("p (j c) -> p c j", c=16), axis=AX.X, op=ALU.max),
                 reads=[rb[0]], writes=[pm.r])
            k.tt("dve", pm.t[0:SR, :], pm.t[0:SR, :], bank(0, SR, 256, 272), ALU.max, reads=[pm.r, rb[0]], writes=[pm.r])
            k.tr(bank(3, 16, 128, 256), pm.t[:], ident[:], reads=[pm.r, rconst], writes=[rb[3]])
            k.op("dve", lambda e, m16=m16: e.reduce_max(m16.t[:, 16:17], bank(3, 16, 128, 256), axis=AX.X), reads=[rb[3]], writes=[m16.r])
            k.ts("dve", m16.t[:, 0:16], ident[0:16, 0:16], m16.t[:, 16:17], None, ALU.mult, reads=[m16.r, rconst], writes=[m16.r])
            k.mm(bank(3, 128, 256, 272), onesf[0:16, :], m16.t[:, 0:16], True, True, reads=[rconst, m16.r], writes=[rb[3]])
            mbc = bank(3, 128, 256, 272)
            k.tt("dve", comb.t[:], comb.t[:], bc(mbc, 2, 8), ALU.subtract, reads=[comb.r, rb[3]], writes=[comb.r])
            X = X_p.next(); Xn = Xn_p.next()
            k.tt("dve", X.t[:].rearrange("p (n two) c -> p n two c", two=2),
                 bank(0, 128, 0, 256).rearrange("p (n two c) -> p n two c", two=2, c=16),
                 bc(comb.t[:].rearrange("p c n -> p n c"), 2, 2), ALU.add, reads=[rb[0], comb.r], writes=[X.r])
            k.tt("dve", Xn.t[:], bank(0, SR, 256, 272), nmask[:, b, :], ALU.add, reads=[rb[0], rconst], writes=[Xn.r])
            k.tt("dve", Xn.t[:], Xn.t[:], bank(3, SR, 256, 272), ALU.subtract, reads=[Xn.r, rb[3]], writes=[Xn.r])
            k.act(Eb.t[:, 0:16, :, 4 * b:4 * b + 4], X.t[:].rearrange("p j (h q) -> p j h q", q=4), AF.Exp, reads=[X.r], writes=[Eb.r], scale=SCALE)
            k.act(Eb.t[0:SR, 16, :, 4 * b:4 * b + 4], Xn.t[:].rearrange("p (h q) -> p h q", q=4), AF.Exp, reads=[Xn.r], writes=[Eb.r], scale=SCALE)
            for h in range(4):
                ob = bank(4 + h, SR, 0, 132)
                for j in range(16):
                    k.mm(ob, Eb.t[:, j, h, :], Vq.t[:, j, h, :], (b == 0 and j == 0), False,
                         reads=[Eb.r, Vq.r], writes=[rb[4 + h]], inc=False)
                k.mm(ob, Eb.t[0:SR, 16, h, :], v_s.t[:, h, :], False, (b == NS - 1),
                     reads=[Eb.r, v_s.r], writes=[rb[4 + h]], inc=True)
            k.memset("pool", Eb.t[:, :, :, 4 * b:4 * b + 4], 0.0, writes=[Eb.r])

        prev = None
        for b in range(NS + 1):
            cur = page_loop(b) if b < NS else None
            if prev is not None:
                chain(b - 1, *prev)
            prev = cur
        rin = k.buf([SR, 4], F32, "rin"); oms = k.buf([SR, 512], BF16, "oms")
        for h in range(4):
            c0 = (4 + h) * 512
            k.op("dve", lambda e, h=h, c0=c0: e.reciprocal(rin.t[:, h:h + 1], ps[0:SR, c0 + 128:c0 + 129]),
                 reads=[rb[4 + h]], writes=[rin.r], accum=(h > 0))
        for h in range(4):
            c0 = (4 + h) * 512
            k.act(oms.t[:, h * 128:(h + 1) * 128], ps[0:SR, c0:c0 + 128], AF.Identity, reads=[rb[4 + h], rin.r], writes=[oms.r],
                  accum=(h > 0), scale=rin.t[:, h:h + 1])
        for h in range(4):
            k.tr(bankb(1, 128, h * SR, (h + 1) * SR), oms.t[:, h * 128:(h + 1) * 128], identb_t[0:SR, 0:SR], reads=[oms.r, rconst],
                 writes=[rb[1]], accum=(h > 0))
        k.cp("act", omT.t[:, :, T:T + SR], bankb(1, 128, 0, 4 * SR).rearrange("p (h s) -> p h s", s=SR), reads=[rb[1]], writes=[omT.r])

    def pass_ret(mods_mix, orT):
        k.mark()
        wr = k.buf([128, 8, 2048], BF16, "wr")
        k.mark()
        stg = k.pool(2, [128, 8 * 512], F32, "stg")
        for g in range(4):
            load_w_bf16(wr.t[:, :, g * 512:(g + 1) * 512], wr.r,
                        w_in[:, g * 512:(g + 1) * 512].rearrange("(kc p) n -> p kc n", p=128), stg.next(), first=(g == 0))
        k.release()
        Sst = k.buf([128, 4, 128], F32, "Sst"); Sbf = k.buf([128, 4, 128], BF16, "Sbf")
        k.memset("pool", Sst.t[:], 0.0, writes=[Sst.r]); k.memset("pool", Sbf.t[:], 0.0, writes=[Sbf.r])
        wk = {"hs": k.pool(1, [SR, D], F32, "hs")}
        xt_p = k.pool(2, [128, D], F32, "xt"); hT_p = k.pool(2, [128, 8, 128], BF16, "hT")
        rot_p = k.pool(2, [128, 4, 128], F32, "rot"); qk_p = k.pool(2, [128, 8, 128], F32, "qkrot")
        tmp_p = k.pool(1, [128, 8, 128], F32, "rtmp")
        vb_p = k.pool(2, [128, 512], BF16, "vb"); sg_p = k.pool(2, [128, 512], F32, "sg")
        kd_p = k.pool(2, [128, 4, 128], BF16, "kd")
        qTb_p = k.pool(2, [128, 4, 128], BF16, "qTb"); kTb_p = k.pool(2, [128, 4, 128], BF16, "kTb")
        qdT_p = k.pool(2, [128, 4, 128], BF16, "qdT"); att_p = k.pool(2, [128, 4, 128], BF16, "att")
        ss_p = k.pool(2, [128, 8], F32, "ss"); junk_p = k.pool(1, [128, 128], F32, "junk")
        or_p = k.pool(2, [128, 512], BF16, "or")
        for t in range(NT + 1):
            rows = 128 if t < NT else SR
            if t < NT:
                xt = xt_p.next()
                k.dma("sp", xt.t[:], xp[t * 128:(t + 1) * 128, :], writes=[xt.r])
                xin, xin_reg = xt.t[:], xt.r
            else:
                xin, xin_reg = x_s.t[:], x_s.r
            hT = hT_p.next()
            make_hT(xin, xin_reg, rows, hT.t[:, :, 0:rows], hT.r, 0, 0, mods_s=mods_mix.t, mods_reg=mods_mix.r, wk=wk, pbanks=(0, 1))
            rot = rot_p.next()
            k.dma("sp", rot.t[0:rows], rot_d[t, 0:rows], writes=[rot.r])
            for g in range(4):
                for kc in range(8):
                    k.mm(bank(4 + g, rows), hT.t[:, kc, 0:rows], wr.t[:, kc, g * 512:(g + 1) * 512], kc == 0, kc == 7,
                         reads=[hT.r, wr.r], writes=[rb[4 + g]])
            qk = qk_p.next(); tmp = tmp_p.next()
            zqk = ps[0:rows, 4 * 512:6 * 512].rearrange("p (g d) -> p g d", d=128)
            rotate(zqk, [rb[4], rb[5]], rows, 8, rot.t[0:rows, 0, :], rot.t[0:rows, 1, :], rot.r,
                   qk.t[0:rows], qk.r, tmp.t[0:rows], tmp.r, "pair")
            vb = vb_p.next(); sg = sg_p.next()
            k.cp("act", vb.t[0:rows], bank(6, rows), reads=[rb[6]], writes=[vb.r])
            k.act(sg.t[0:rows], bank(7, rows), AF.Silu, reads=[rb[7]], writes=[sg.r])
            kdc = rkd if t < NT else rkds
            kd = kd_p.next()
            k.tt("dve", kd.t[0:rows], qk.t[0:rows, 4:8, :], bc(kdc[0:rows, :], 2, 128), ALU.mult, reads=[qk.r, rconst], writes=[kd.r])
            for g in range(8):
                b = 2 + g // 4
                k.tr(bank(b, 128, (g % 4) * 128, (g % 4) * 128 + rows), qk.t[0:rows, g, :], ident[0:rows, 0:rows],
                     reads=[qk.r, rconst], writes=[rb[b]], accum=(g % 4 > 0))
            qv = bank(2).rearrange("p (h s) -> p h s", s=128)[:, :, 0:rows]
            kv = bank(3).rearrange("p (h s) -> p h s", s=128)[:, :, 0:rows]
            qTb = qTb_p.next(); kTb = kTb_p.next(); qdT = qdT_p.next()
            k.cp("act", qTb.t[:, :, 0:rows], qv, reads=[rb[2]], writes=[qTb.r])
            k.cp("act", kTb.t[:, :, 0:rows], kv, reads=[rb[3]], writes=[kTb.r])
            qdc = rqd if t < NT else rqds
            k.tt("dve", qdT.t[:, :, 0:rows], qv, qdc[:, :, 0:rows], ALU.mult, reads=[rb[2], rconst], writes=[qdT.r])
            for h in range(4):
                k.mm(bank(0, rows, h * 128, h * 128 + rows), kTb.t[:, h, 0:rows], qTb.t[:, h, 0:rows], True, True,
                     reads=[kTb.r, qTb.r], writes=[rb[0]])
            att = att_p.next()
            dmc = rdm if t < NT else rdms
            k.tt("dve", att.t[0:rows, :, 0:rows], bank(0, rows).rearrange("p (h s) -> p h s", s=128)[:, :, 0:rows], dmc[0:rows, :, 0:rows],
                 ALU.mult, reads=[rb[0], rconst], writes=[att.r])
            if t < NT:
                for h in range(4):
                    ob = bank(1, 128, h * 128, (h + 1) * 128)
                    k.mm(ob, att.t[:, h, :], vb.t[:, h * 128:(h + 1) * 128], True, False, reads=[att.r, vb.r], writes=[rb[1]])
                    k.mm(ob, qdT.t[:, h, :], Sbf.t[:, h, :], False, True, reads=[qdT.r, Sbf.r], writes=[rb[1]])
                for h in range(4):
                    k.mm(bank(2, 128, h * 128, (h + 1) * 128), kd.t[:, h, :], vb.t[:, h * 128:(h + 1) * 128], True, True,
                         reads=[kd.r, vb.r], writes=[rb[2]])
                for h in range(4):
                    k.stt(Sst.t[:, h, :], Sst.t[:, h, :], math.exp(128.0 * LOGG[h]), bank(2, 128, h * 128, (h + 1) * 128), ALU.mult, ALU.add,
                          reads=[Sst.r, rb[2]], writes=[Sst.r])
                k.cp("act", Sbf.t[:], Sst.t[:], reads=[Sst.r], writes=[Sbf.r])
                if t == NT - 1:
                    k.dma("sp", rpo.rearrange("(h d) v -> d h v", d=128), Sst.t[:], reads=[Sst.r], is_output=True)
            else:
                qdm = k.buf([128, NS, 4, SR], BF16, "qdm")
                k.memset("pool", qdm.t[:], 0.0, writes=[qdm.r])
                base = qdm.t[:]
                pstr = base.ap[0][0]
                for h in range(4):
                    dst = bass.AP(qdm.t, base.offset + h * SR, [[pstr, 128], [4 * SR + 4, NS], [1, 4]])
                    k.cp("pool", dst, qdT.t[:, h, 0:SR].rearrange("p (s j) -> p s j", j=4), reads=[qdT.r, qdm.r], writes=[qdm.r])
                for h in range(4):
                    ob = bank(4 + h, SR, 0, 128)
                    k.mm(ob, att.t[0:SR, h, 0:SR], vb.t[0:SR, h * 128:(h + 1) * 128], True, False, reads=[att.r, vb.r], writes=[rb[4 + h]], inc=True)
                s0_p = k.pool(2, [128, 4, 128], F32, "s0"); s0b_p = k.pool(2, [128, 4, 128], BF16, "s0b")
                kdm_p = k.pool(2, [SR, 4, 128], BF16, "kdm"); sn_p = k.pool(2, [128, 4, 128], F32, "sn")
                for sq in range(NS):
                    s0 = s0_p.next(); s0b = s0b_p.next()
                    k.dma("sp", s0.t[:], sret[sq * 512:(sq + 1) * 512, :].rearrange("(h d) v -> d h v", d=128), writes=[s0.r])
                    k.cp("act", s0b.t[:], s0.t[:], reads=[s0.r], writes=[s0b.r])
                    for h in range(4):
                        k.mm(bank(4 + h, SR, 0, 128), qdm.t[:, sq, h, :], s0b.t[:, h, :], False, (sq == NS - 1),
                             reads=[qdm.r, s0b.r], writes=[rb[4 + h]], inc=True)
                    kdm = kdm_p.next()
                    k.ts("dve", kdm.t[:], kd.t[0:SR], blk[:, sq:sq + 1], None, ALU.mult, reads=[kd.r, rconst], writes=[kdm.r])
                    ub = 2 + sq % 2
                    for h in range(4):
                        k.mm(bank(ub, 128, h * 128, (h + 1) * 128), kdm.t[:, h, :], vb.t[0:SR, h * 128:(h + 1) * 128], True, True,
                             reads=[kdm.r, vb.r], writes=[rb[ub]])
                    sn = sn_p.next()
                    for h in range(4):
                        k.stt(sn.t[:, h, :], s0.t[:, h, :], math.exp(4.0 * LOGG[h]), bank(ub, 128, h * 128, (h + 1) * 128), ALU.mult, ALU.add,
                              reads=[s0.r, rb[ub]], writes=[sn.r], accum=(h > 0))
                    k.dma("sp", rso[sq * 512:(sq + 1) * 512, :].rearrange("(h d) v -> d h v", d=128), sn.t[:], reads=[sn.r], is_output=True)
            ss = ss_p.next(); junk = junk_p.next()

            def obank(h):
                return (bank(1, rows, h * 128, (h + 1) * 128), rb[1]) if t < NT else (bank(4 + h, rows, 0, 128), rb[4 + h])

            for h in range(4):
                oap, oreg = obank(h)
                k.act(junk.t[0:rows], oap, AF.Square, reads=[oreg], writes=[junk.r, ss.r],
                      accum_out=ss.t[0:rows, h:h + 1])
            k.ts("dve", ss.t[0:rows, 4:8], ss.t[0:rows, 0:4], 1.0 / 128.0, GN_EPS, ALU.mult, ALU.add, reads=[ss.r], writes=[ss.r])
            k.act(ss.t[0:rows, 4:8], ss.t[0:rows, 4:8], AF.Sqrt, reads=[ss.r], writes=[ss.r])
            k.op("dve", lambda e, ss=ss, rows=rows: e.reciprocal(ss.t[0:rows, 4:8], ss.t[0:rows, 4:8]), reads=[ss.r], writes=[ss.r])
            orr = or_p.next()
            for h in range(4):
                oap, oreg = obank(h)
                k.stt(orr.t[0:rows, h * 128:(h + 1) * 128], oap, ss.t[0:rows, 4 + h:5 + h],
                      sg.t[0:rows, h * 128:(h + 1) * 128], ALU.mult, ALU.mult, reads=[oreg, ss.r, sg.r], writes=[orr.r], accum=(h > 0))
            for h in range(4):
                k.tr(bankb(3, 128, h * 128, h * 128 + rows), orr.t[0:rows, h * 128:(h + 1) * 128], identb_t[0:rows, 0:rows],
                     reads=[orr.r, rconst], writes=[rb[3]], accum=(h > 0))
            k.cp("act", orT.t[:, :, t * 128:t * 128 + rows], bankb(3, 128, 0, 512).rearrange("p (h s) -> p h s", s=128)[:, :, 0:rows],
                 reads=[rb[3]], writes=[orT.r], accum=(t > 0))
        k.release()

    def post_norm(y_ap, y_regs, xt_ap, xt_reg, rows, gate_ap, gate_reg, lng, lnb, gb_reg, wk, bias_ap=None, bias_reg=None):
        rr = wk["rr"].next()
        if bias_ap is not None:
            k.tt("dve", rr.t[0:rows], y_ap, bias_ap[0:rows], ALU.add, reads=list(y_regs) + [bias_reg], writes=[rr.r])
            k.tt("dve", rr.t[0:rows], rr.t[0:rows], gate_ap, ALU.mult, reads=[rr.r, gate_reg], writes=[rr.r])
        else:
            k.tt("dve", rr.t[0:rows], y_ap, gate_ap, ALU.mult, reads=list(y_regs) + [gate_reg], writes=[rr.r])
        k.stt(xt_ap, xt_ap, ALPHA, rr.t[0:rows], ALU.mult, ALU.add, reads=[xt_reg, rr.r], writes=[xt_reg])
        layer_norm(xt_ap, xt_reg, rows, lng, lnb, gb_reg, xt_ap, xt_reg, wk)

    def ln_work(n=2):
        rr = k.pool(n, [128, D], F32, "rr")
        return {"st": k.pool(2, [128, 2, 6], F32, "st"), "mv": k.pool(2, [128, 4], F32, "mv"),
                "xn": k.pool(n, [128, D], F32, "xn"), "rr": rr, "hs": rr}

    def load_ln(i):
        g = k.buf([128, D], F32, "lng"); b = k.buf([128, D], F32, "lnb")
        k.dma("sp", g.t[:], ln_g[i:i + 1, :].partition_broadcast(128), writes=[g.r])
        k.dma("sp", b.t[:], ln_b[i:i + 1, :].partition_broadcast(128), writes=[g.r], anchor=g.r, accum=True)
        return g, b

    def pass_out(mods_mix, g1p, orT, omT):
        k.mark()
        wo = k.buf([128, 8, D], BF16, "wo")
        k.mark()
        stg = k.pool(2, [128, 8 * 512], F32, "stg")
        for g in range(2):
            load_w_bf16(wo.t[:, :, g * 512:(g + 1) * 512], wo.r,
                        w_out[:, g * 512:(g + 1) * 512].rearrange("(kc p) n -> p kc n", p=128), stg.next(), first=(g == 0))
        k.release()
        lng, lnb = load_ln(0)
        wk = ln_work()
        for t in range(NT + 1):
            rows = 128 if t < NT else SR
            if t < NT:
                k.dma("sp", x_all[:, t, :], xp[t * 128:(t + 1) * 128, :], writes=[rx[t]])
                xt_ap, xt_reg = x_all[:, t, :], rx[t]
                gate_ap, gate_reg = g1p.t[:], g1p.r
            else:
                xt_ap, xt_reg = x_s.t[:], x_s.r
                gate_ap, gate_reg = mods_mix.t[:, 2 * D:3 * D], mods_mix.r
            bp = 4 * (t % 2)
            for half in range(2):
                for c in range(8):
                    src = orT if c < 4 else omT
                    k.mm(bank(bp + half, rows), src.t[:, c % 4, t * 128:t * 128 + rows], wo.t[:, c, half * 512:(half + 1) * 512],
                         c == 0, c == 7, reads=[src.r, wo.r], writes=[rb[bp + half]])
            post_norm(ps[0:rows, bp * 512:bp * 512 + D], [rb[bp], rb[bp + 1]], xt_ap, xt_reg, rows, gate_ap, gate_reg,
                      lng.t, lnb.t, lng.r, wk)
        k.release()

    def ffn(l, final):
        k.mark()
        g2s = k.buf([SR, D], F32, "g2s"); g2p = k.buf([128, D], F32, "g2p")
        k.dma("sp", g2s.t[:], sc_mods[l][:, 2 * D:3 * D], reads=[r_scm[l]], writes=[g2s.r])
        k.dma("sp", g2p.t[:], sc_g2p[l].partition_broadcast(128), reads=[r_scg[l]], writes=[g2p.r])
        hT = k.buf([128, 8, T + SR], BF16, "hT_all")
        ffp = k.buf([128, NFC, 4], F32, "ffp"); ust = k.buf([128, NFC, 2 * NS], F32, "ust")
        fo = k.buf([128, NFC, 2], F32, "fo"); fs = k.buf([128, NFC, NS, 2], F32, "fs")
        k.mark()
        mf = k.buf([SR, 2 * D], F32, "mf")
        k.dma("sp", mf.t[:], sc_mods[l][:, 0:2 * D], reads=[r_scm[l]], writes=[mf.r])
        p4 = k.buf([4, DFF], F32, "p4"); p32 = k.buf([2 * NS, DFF], F32, "p32")
        k.dma("sp", p4.t[:], ffp_d[l], writes=[p4.r])
        k.dma("sp", p32.t[:], sffn[l], writes=[p32.r])
        for c0 in range(0, NFC, 4):
            c1 = min(NFC, c0 + 4)
            for c in range(c0, c1):
                k.tr(bank(0, 128, (c - c0) * 4, (c - c0) * 4 + 4), p4.t[:, c * 128:(c + 1) * 128], ident[0:4, 0:4], reads=[p4.r, rconst],
                     writes=[rb[0]], accum=(c > c0))
                k.tr(bank(1, 128, (c - c0) * 32, (c - c0) * 32 + 32), p32.t[:, c * 128:(c + 1) * 128], ident[0:32, 0:32], reads=[p32.r, rconst],
                     writes=[rb[1]], accum=(c > c0))
            k.cp("act", ffp.t[:, c0:c1, :], bank(0, 128, 0, (c1 - c0) * 4).rearrange("p (c j) -> p c j", j=4), reads=[rb[0]], writes=[ffp.r], accum=(c0 > 0))
            k.cp("act", ust.t[:, c0:c1, :], bank(1, 128, 0, (c1 - c0) * 32).rearrange("p (c j) -> p c j", j=32), reads=[rb[1]], writes=[ust.r], accum=(c0 > 0))
        wkh = {"hs": k.pool(1, [SR, D], F32, "hs")}
        for t in range(NT):
            make_hT(x_all[:, t, :], rx[t], 128, hT.t[:, :, t * 128:(t + 1) * 128], hT.r, l, 3, pbanks=(2 + 2 * (t % 2), 3 + 2 * (t % 2)), first=(t == 0))
            k.ts("dve", x_all[:, t, :], x_all[:, t, :], ALPHA, None, ALU.mult, reads=[rx[t]], writes=[rx[t]])
        make_hT(x_s.t[:], x_s.r, SR, hT.t[:, :, T:T + SR], hT.r, l, 3, mods_s=mf.t, mods_reg=mf.r, wk=wkh, pbanks=(2, 3), first=False)
        k.ts("pool", x_s.t[:], x_s.t[:], ALPHA, None, ALU.mult, reads=[x_s.r], writes=[x_s.r])
        k.release()
        k.mark()
        G = 2
        stg = k.pool(2, [128, 2048], F32, "stg")
        wu_p = k.pool(2, [128, 8, G * 128], BF16, "wu"); wv_p = k.pool(2, [128, 8, G * 128], BF16, "wv")
        wds_p = k.pool(2, [128, G, D], BF16, "wds"); wdu_p = k.pool(1, [128, G, D], BF16, "wdu")
        UW = 2 + T + 6 * NS
        u_p = k.pool(2, [128, UW], F32, "u_sb"); a_p = k.pool(1, [128, UW], F32, "acc")
        gT_p = k.pool(1, [128, G, T + SR], BF16, "gT")
        vsb_p = k.pool(1, [128, T + SR], BF16, "vsb")
        nb = [0]

        for g in range(NFC // G):
            f0 = g * G * 128
            wu = wu_p.next(); wv = wv_p.next(); wds = wds_p.next(); wdu = wdu_p.next()
            load_w_bf16(wu.t[:], wu.r, ffn_up[l, :, f0:f0 + G * 128].rearrange("(kc p) n -> p kc n", p=128), stg.next())
            load_w_bf16(wv.t[:], wv.r, ffn_up[l, :, DFF + f0:DFF + f0 + G * 128].rearrange("(kc p) n -> p kc n", p=128), stg.next())
            st = stg.next()
            stv = st.t[:, 0:G * D].rearrange("p (a b) -> p a b", b=D)
            k.dma("sp", stv, ffn_down[l, f0:f0 + G * 128, :].rearrange("(c p) n -> p c n", p=128), writes=[st.r])
            k.cp("act", wdu.t[:], stv, reads=[st.r], writes=[wdu.r])
            k.tt("dve", wds.t[:], stv, bc(g2p.t[:], 1, G), ALU.mult, reads=[st.r, g2p.r], writes=[wds.r])
            gT = gT_p.next()
            for c in range(G):
                fc = g * G + c
                u = u_p.next(); acc = a_p.next()
                w0, w1, w2, bb = (ffp.t[:, fc, j:j + 1] for j in range(4))
                k.memset("pool", u.t[:, 0:2], 0.0, writes=[u.r])
                usv = u.t[:, 2 + T:UW].rearrange("p (s j) -> p s j", j=6)
                asv = acc.t[:, 2 + T:UW].rearrange("p (s j) -> p s j", j=6)
                k.cp("pool", usv[:, :, 0:2], ust.t[:, fc, :].rearrange("p (s j) -> p s j", j=2), reads=[ust.r], writes=[u.r], accum=True)
                vsb = vsb_p.next()
                for n in range(5):
                    ncol = 512 if n < 4 else SR
                    t0 = n * 512
                    nb[0] += 1
                    bu = nb[0] % 2; bv = 2 + nb[0] % 2
                    for kc in range(8):
                        k.mm(bank(bu, 128, 0, ncol), wu.t[:, kc, c * 128:(c + 1) * 128], hT.t[:, kc, t0:t0 + ncol], kc == 0, kc == 7,
                             reads=[wu.r, hT.r], writes=[rb[bu]])
                    for kc in range(8):
                        k.mm(bank(bv, 128, 0, ncol), wv.t[:, kc, c * 128:(c + 1) * 128], hT.t[:, kc, t0:t0 + ncol], kc == 0, kc == 7,
                             reads=[wv.r, hT.r], writes=[rb[bv]])
                    if n < 4:
                        k.cp("act", u.t[:, 2 + t0:2 + t0 + 512], bank(bu), reads=[rb[bu]], writes=[u.r], accum=True)
                    else:
                        k.cp("act", usv[:, :, 2:6], bank(bu, 128, 0, SR).rearrange("p (s j) -> p s j", j=4), reads=[rb[bu]], writes=[u.r], accum=True)
                    k.cp("act", vsb.t[:, t0:t0 + ncol], bank(bv, 128, 0, ncol), reads=[rb[bv]], writes=[vsb.r], accum=(n > 0))
                lo, hi = 2, UW
                k.act(acc.t[:, lo:hi], u.t[:, lo:hi], AF.Identity, reads=[u.r, ffp.r], writes=[acc.r], scale=w2, bias=bb)
                k.stt(acc.t[:, lo:hi], u.t[:, lo - 1:hi - 1], w1, acc.t[:, lo:hi], ALU.mult, ALU.add, reads=[u.r, acc.r, ffp.r], writes=[acc.r])
                k.stt(acc.t[:, lo:hi], u.t[:, lo - 2:hi - 2], w0, acc.t[:, lo:hi], ALU.mult, ALU.add, reads=[u.r, acc.r, ffp.r], writes=[acc.r])
                k.act(acc.t[:, lo:hi], acc.t[:, lo:hi], AF.Gelu, reads=[acc.r], writes=[acc.r])
                k.tt("dve", gT.t[:, c, 0:T], acc.t[:, 2:2 + T], vsb.t[:, 0:T], ALU.mult, reads=[acc.r, vsb.r], writes=[gT.r], accum=(c > 0))
                k.tt("dve", gT.t[:, c, T:T + SR].rearrange("p (s j) -> p s j", j=4), asv[:, :, 2:6],
                     vsb.t[:, T:T + SR].rearrange("p (s j) -> p s j", j=4), ALU.mult, reads=[acc.r, vsb.r], writes=[gT.r], accum=True)
                k.cp("pool", fo.t[:, fc, :], u.t[:, T:T + 2], reads=[u.r], writes=[fo.r], accum=True)
                k.cp("pool", fs.t[:, fc, :, :], usv[:, :, 4:6], reads=[u.r], writes=[fs.r], accum=True)
            for t in range(NT + 1):
                rows = 128 if t < NT else SR
                bp = 4 + 2 * (t % 2)
                wd = wds if t < NT else wdu
                for half in range(2):
                    for c in range(G):
                        k.mm(bank(bp + half, rows), gT.t[:, c, t * 128:t * 128 + rows], wd.t[:, c, half * 512:(half + 1) * 512],
                             c == 0, c == G - 1, reads=[gT.r, wd.r], writes=[rb[bp + half]])
                yv = ps[0:rows, bp * 512:bp * 512 + D]
                if t < NT:
                    k.tt("dve", x_all[:, t, :], x_all[:, t, :], yv, ALU.add, reads=[rx[t], rb[bp], rb[bp + 1]], writes=[rx[t]])
                else:
                    rs_ = a_p.next()
                    k.tt("dve", rs_.t[0:SR, 0:D], yv, g2s.t[:], ALU.mult, reads=[rb[bp], rb[bp + 1], g2s.r], writes=[rs_.r])
                    k.tt("dve", x_s.t[:], x_s.t[:], rs_.t[0:SR, 0:D], ALU.add, reads=[x_s.r, rs_.r], writes=[x_s.r])
        k.release()
        k.mark()
        fo_tok = k.buf([2, DFF], F32, "fo_tok"); fs_tok = k.buf([2 * NS, DFF], F32, "fs_tok")
        for c0 in range(0, NFC, 4):
            c1 = min(NFC, c0 + 4)
            for c in range(c0, c1):
                k.tr(bank(0, 2, (c - c0) * 128, (c - c0 + 1) * 128), fo.t[:, c, :], ident[:], reads=[fo.r, rconst], writes=[rb[0]], accum=(c > c0))
                k.tr(bank(1, 2 * NS, (c - c0) * 128, (c - c0 + 1) * 128), fs.t[:, c, :, :].rearrange("p s j -> p (s j)"), ident[:],
                     reads=[fs.r, rconst], writes=[rb[1]], accum=(c > c0))
            k.cp("act", fo_tok.t[:, c0 * 128:c1 * 128], bank(0, 2, 0, (c1 - c0) * 128), reads=[rb[0]], writes=[fo_tok.r], accum=(c0 > 0))
            k.cp("act", fs_tok.t[:, c0 * 128:c1 * 128], bank(1, 2 * NS, 0, (c1 - c0) * 128), reads=[rb[1]], writes=[fs_tok.r], accum=(c0 > 0))
        k.dma("sp", fpo[l], fo_tok.t[:], reads=[fo_tok.r], is_output=True)
        k.dma("sp", fso[l], fs_tok.t[:], reads=[fs_tok.r], is_output=True)
        lng, lnb = load_ln(2 * l + 1)
        wk = ln_work()
        for t in range(NT + 1):
            rows = 128 if t < NT else SR
            xt_ap, xt_reg = (x_all[:, t, :], rx[t]) if t < NT else (x_s.t[:], x_s.r)
            layer_norm(xt_ap, xt_reg, rows, lng.t, lnb.t, lng.r, xt_ap, xt_reg, wk)
            if final:
                k.dma("sp", yp[t * 128:(t + 1) * 128, :] if t < NT else ys, xt_ap, reads=[xt_reg], is_output=True)
        k.release()
        k.release()

    def conformer(mods_mix, g1p):
        k.mark()
        w1 = k.buf([128, 8, 2048], BF16, "w1"); w2 = k.buf([128, 8, D], BF16, "w2")
        cf = k.buf([128, 8, 36], F32, "cf")
        k.mark()
        stg = k.pool(2, [128, 8 * 512], F32, "stg")
        for g in range(4):
            load_w_bf16(w1.t[:, :, g * 512:(g + 1) * 512], w1.r, pw1[:, g * 512:(g + 1) * 512].rearrange("(kc p) n -> p kc n", p=128),
                        stg.next(), first=(g == 0))
        for g in range(2):
            load_w_bf16(w2.t[:, :, g * 512:(g + 1) * 512], w2.r, pw2[:, g * 512:(g + 1) * 512].rearrange("(kc p) n -> p kc n", p=128),
                        stg.next(), first=(g == 0))
        p36 = k.buf([36, D], F32, "p36")
        k.dma("sp", p36.t[:], cfp_d, writes=[p36.r])
        for c in range(8):
            k.tr(bank(0, 128, c * 36, c * 36 + 36), p36.t[:, c * 128:(c + 1) * 128], ident[0:36, 0:36], reads=[p36.r, rconst],
                 writes=[rb[0]], accum=(c > 0))
        k.cp("act", cf.t[:], bank(0, 128, 0, 288).rearrange("p (c j) -> p c j", j=36), reads=[rb[0]], writes=[cf.r])
        k.release()
        b2 = k.buf([128, D], F32, "b2")
        k.dma("sp", b2.t[:], b_pw2.partition_broadcast(128), writes=[b2.r])
        lng, lnb = load_ln(2)
        wk = ln_work(1)
        BS = 256
        NB = T // BS
        TPB = BS // 128
        GW = 34 * NS
        sconv_p = k.pool(1, [120, 4, 128], F32, "sconv_sb")
        carry = k.buf([128, 8, 30], F32, "carry")
        k.memset("pool", carry.t[:], 0.0, writes=[carry.r])
        hTb = k.buf([128, 8, BS], BF16, "hTb")
        yc = k.buf([128, 8, BS], F32, "yc")
        sT = k.buf([128, 8, BS], BF16, "sT")
        glu_p = k.pool(2, [128, GW], F32, "glu"); sig_p = k.pool(2, [128, BS], F32, "sig")
        glub_p = k.pool(2, [128, GW], BF16, "glub")
        dg_p = k.pool(2, [128, 31, 128], BF16, "dg")
        ycb_p = k.pool(2, [128, BS], BF16, "ycb"); ysq_p = k.pool(2, [128, BS], BF16, "ysq")
        mean = k.buf([128, BS], F32, "mean"); rstd = k.buf([128, BS], F32, "rstd"); xn_p = k.pool(2, [128, BS], F32, "cxn")
        gnew = k.buf([128, 8, SR], F32, "gnew")
        for B in range(NB + 1):
            prompt = B < NB
            ncol = BS if prompt else SR
            if prompt:
                for j in range(TPB):
                    t = TPB * B + j
                    make_hT(x_all[:, t, :], rx[t], 128, hTb.t[:, :, j * 128:(j + 1) * 128], hTb.r, 1, 0, pbanks=(2, 3), first=(j == 0))
            else:
                make_hT(x_s.t[:], x_s.r, SR, hTb.t[:, :, 0:SR], hTb.r, 1, 0, mods_s=mods_mix.t, mods_reg=mods_mix.r, wk=wk, pbanks=(2, 3))
            NO = BS if prompt else GW - 30
            s1b = 6 if prompt else 0
            for c in range(8):
                for kc in range(8):
                    k.mm(bank(4, 128, 0, ncol), w1.t[:, kc, c * 128:(c + 1) * 128], hTb.t[:, kc, 0:ncol], kc == 0, kc == 7,
                         reads=[w1.r, hTb.r], writes=[rb[4]])
                for kc in range(8):
                    k.mm(bank(5, 128, 0, ncol), w1.t[:, kc, D + c * 128:D + (c + 1) * 128], hTb.t[:, kc, 0:ncol], kc == 0, kc == 7,
                         reads=[w1.r, hTb.r], writes=[rb[5]])
                sig = sig_p.next(); glu = glu_p.next()
                k.act(sig.t[:, 0:ncol], bank(5, 128, 0, ncol), AF.Sigmoid, reads=[rb[5], cf.r], writes=[sig.r], bias=cf.t[:, c, 35:36])
                if prompt:
                    k.cp("pool", glu.t[:, 0:30], carry.t[:, c, :], reads=[carry.r], writes=[glu.r])
                    k.stt(glu.t[:, 30:30 + BS], bank(4, 128, 0, BS), cf.t[:, c, 34:35], sig.t[:, 0:BS], ALU.add, ALU.mult,
                          reads=[rb[4], cf.r, sig.r], writes=[glu.r], accum=True)
                    k.cp("pool", carry.t[:, c, :], glu.t[:, BS:BS + 30], reads=[glu.r], writes=[carry.r])
                else:
                    gv = glu.t[:, 0:GW].rearrange("p (s j) -> p s j", j=34)
                    scv = sconv_p.next()
                    for q in range(4):
                        k.dma("sp", scv.t[:, q, :], sconv[q * 120:(q + 1) * 120, c * 128:(c + 1) * 128], writes=[scv.r], accum=(q > 0))
                    for q in range(4):
                        k.tr(bank(6, 128, q * 120, (q + 1) * 120), scv.t[:, q, :], ident[0:120, 0:120],
                             reads=[scv.r, rconst], writes=[rb[6]], accum=(q > 0))
                    k.cp("act", gv[:, :, 0:30], bank(6, 128, 0, 480).rearrange("p (s j) -> p s j", j=30), reads=[rb[6]], writes=[glu.r])
                    k.stt(gv[:, :, 30:34], bank(4, 128, 0, SR).rearrange("p (s j) -> p s j", j=4), cf.t[:, c, 34:35],
                          sig.t[:, 0:SR].rearrange("p (s j) -> p s j", j=4), ALU.add, ALU.mult, reads=[rb[4], cf.r, sig.r], writes=[glu.r], accum=True)
                    k.cp("pool", gnew.t[:, c, :].rearrange("p (s j) -> p s j", j=4), gv[:, :, 30:34], reads=[glu.r], writes=[gnew.r], accum=(c > 0))
                dg = dg_p.next()
                k.tt("dve", dg.t[:], bc(identb_t[:], 1, 31), bc(cf.t[:, c, 0:31], 2, 128), ALU.mult, reads=[rconst, cf.r], writes=[dg.r])
                glub = glub_p.next()
                W = 30 + BS if prompt else GW
                k.cp("act", glub.t[:, 0:W], glu.t[:, 0:W], reads=[glu.r], writes=[glub.r])
                cb = (c % 2) if prompt else 1
                for j in range(31):
                    if prompt:
                        rhs = glub.t[:, j:j + BS]
                    else:
                        rhs = glub.t[:, 0:GW].rearrange("p (s j) -> p s j", j=34)[:, :, j:j + 4]
                    k.mm(bank(cb, 128, 0, ncol), dg.t[:, j, :], rhs, j == 0, j == 30, reads=[dg.r, glub.r], writes=[rb[cb]])
                k.act(yc.t[:, c, 0:ncol], bank(cb, 128, 0, ncol), AF.Identity, reads=[rb[cb], cf.r], writes=[yc.r], accum=(c > 0),
                      bias=cf.t[:, c, 31:32])
                ycb = ycb_p.next(); ysq = ysq_p.next()
                k.cp("dve", ycb.t[:, 0:ncol], yc.t[:, c, 0:ncol], reads=[yc.r], writes=[ycb.r])
                k.act(ysq.t[:, 0:ncol], yc.t[:, c, 0:ncol], AF.Square, reads=[yc.r], writes=[ysq.r])
                k.mm(bank(s1b, 128, 0, ncol), onesb[:], ycb.t[:, 0:ncol], c == 0, c == 7, reads=[rconst, ycb.r], writes=[rb[s1b]], inc=True)
                k.mm(bank(7, 128, 0, ncol), onesb[:], ysq.t[:, 0:ncol], c == 0, c == 7, reads=[rconst, ysq.r], writes=[rb[7]], inc=True)
            k.act(mean.t[:, 0:ncol], bank(s1b, 128, 0, ncol), AF.Identity, reads=[rb[s1b]], writes=[mean.r], scale=1.0 / D)
            k.tt("dve", rstd.t[:, 0:ncol], mean.t[:, 0:ncol], mean.t[:, 0:ncol], ALU.mult, reads=[mean.r], writes=[rstd.r])
            k.stt(rstd.t[:, 0:ncol], bank(7, 128, 0, ncol), 1.0 / D, rstd.t[:, 0:ncol], ALU.mult, ALU.subtract, reads=[rb[7], rstd.r], writes=[rstd.r])
            k.ts("dve", rstd.t[:, 0:ncol], rstd.t[:, 0:ncol], LN_EPS, None, ALU.add, reads=[rstd.r], writes=[rstd.r])
            k.act(rstd.t[:, 0:ncol], rstd.t[:, 0:ncol], AF.Sqrt, reads=[rstd.r], writes=[rstd.r])
            k.op("dve", lambda e, ncol=ncol: e.reciprocal(rstd.t[:, 0:ncol], rstd.t[:, 0:ncol]), reads=[rstd.r], writes=[rstd.r])
            for c in range(8):
                xn = xn_p.next()
                k.tt("dve", xn.t[:, 0:ncol], yc.t[:, c, 0:ncol], mean.t[:, 0:ncol], ALU.subtract, reads=[yc.r, mean.r], writes=[xn.r])
                k.tt("dve", xn.t[:, 0:ncol], xn.t[:, 0:ncol], rstd.t[:, 0:ncol], ALU.mult, reads=[xn.r, rstd.r], writes=[xn.r])
                k.act(sT.t[:, c, 0:ncol], xn.t[:, 0:ncol], AF.Silu, reads=[xn.r, cf.r], writes=[sT.r], accum=(c > 0),
                      scale=cf.t[:, c, 32:33], bias=cf.t[:, c, 33:34])
            for j in range(TPB if prompt else 1):
                rows = 128 if prompt else SR
                t = TPB * B + j
                bp = 4 * (j % 2) if prompt else 2
                for half in range(2):
                    for c in range(8):
                        k.mm(bank(bp + half, rows), sT.t[:, c, j * 128:j * 128 + rows], w2.t[:, c, half * 512:(half + 1) * 512], c == 0, c == 7,
                             reads=[sT.r, w2.r], writes=[rb[bp + half]])
                if prompt:
                    xt_ap, xt_reg, gate_ap, gate_reg = x_all[:, t, :], rx[t], g1p.t[:], g1p.r
                else:
                    xt_ap, xt_reg, gate_ap, gate_reg = x_s.t[:], x_s.r, mods_mix.t[:, 2 * D:3 * D], mods_mix.r
                post_norm(ps[0:rows, bp * 512:bp * 512 + D], [rb[bp], rb[bp + 1]], xt_ap, xt_reg, rows, gate_ap, gate_reg,
                          lng.t, lnb.t, lng.r, wk, bias_ap=b2.t, bias_reg=b2.r)
            if B == NB - 1:
                cp_tok = wk["xn"].next()
                for c in range(8):
                    k.tr(bank(2 + c // 4, 30, (c % 4) * 128, (c % 4 + 1) * 128), carry.t[:, c, :], ident[:], reads=[carry.r, rconst],
                         writes=[rb[2 + c // 4]], accum=(c % 4 > 0))
                k.cp("act", cp_tok.t[0:30, :], ps[0:30, 2 * 512:2 * 512 + D], reads=[rb[2], rb[3]], writes=[cp_tok.r])
                k.dma("sp", cpo, cp_tok.t[0:30, :], reads=[cp_tok.r], is_output=True)
        r_cso = Reg()
        k.dma("sp", cso.rearrange("(s j) f -> s j f", j=30)[:, 0:26, :], sconv.rearrange("(s j) f -> s j f", j=30)[:, 4:30, :],
              reads=[], writes=[r_cso], is_output=True)
        cs_tok = wk["xn"].next()
        for c in range(8):
            k.tr(bank(2 + c // 4, SR, (c % 4) * 128, (c % 4 + 1) * 128), gnew.t[:, c, :], ident[:], reads=[gnew.r, rconst],
                 writes=[rb[2 + c // 4]], accum=(c % 4 > 0))
        k.cp("act", cs_tok.t[0:SR, :], ps[0:SR, 2 * 512:2 * 512 + D], reads=[rb[2], rb[3]], writes=[cs_tok.r])
        for sq in range(NS):
            k.dma("sp", cso[sq * 30 + 26:sq * 30 + 30, :], cs_tok.t[sq * 4:sq * 4 + 4, :], reads=[cs_tok.r], is_output=True)
        k.release()

    k.limit = k.sb_top
    k.mark()
    rdm = cload([128, 4, 128], rdm_d); rqd = cload([128, 4, 128], rqd_d); rkd = cload([128, 4], rkd_d)
    rdms = cload([64, 4, 64], rdms_d); rqds = cload([128, 4, 64], rqds_d); rkds = cload([64, 4], rkds_d)
    blk = cload([64, 16], blk_d); tri = cload([128, 128], tri_d); nmask = cload([64, 16, 16], nmask_d)
    idx = k.buf([128, NS * 16], I32)
    pti = cload([128, NS * 16], ptd.partition_broadcast(128), dt=I32)
    idxf = k.sb([128, NS * 16], F32)
    k.cp("dve", idxf[:], pti[:], reads=[rconst], writes=[idx.r])
    k.stt(idxf[:], idxf[:], 128.0, iota[:].broadcast_to([128, NS * 16]), ALU.mult, ALU.add, reads=[rconst, idx.r], writes=[idx.r])
    k.cp("dve", idx.t[:], idxf[:], reads=[idx.r], writes=[idx.r])
    mods_mix0 = k.buf([SR, 3 * D], F32, "mods_mix")
    g1p0 = k.buf([128, D], F32, "g1p")
    with nc.named_scope("adaln0"):
        adaln(0, mods_mix0, g1p0)
    omT = k.buf([128, 4, T + SR], BF16, "omT")
    orT = k.buf([128, 4, T + SR], BF16, "orT")
    with nc.named_scope("passM"):
        pass_moba(mods_mix0, omT)
    if stage >= 4:
        with nc.named_scope("passR"):
            pass_ret(mods_mix0, orT)
    k.limit = XOFF
    if stage >= 5:
        with nc.named_scope("passO"):
            pass_out(mods_mix0, g1p0, orT, omT)
    k.release()
    if stage >= 6:
        with nc.named_scope("ffn0"):
            ffn(0, final=False)
    if stage >= 7:
        k.mark()
        mods_mix1 = k.buf([SR, 3 * D], F32, "mods_mix")
        g1p1 = k.buf([128, D], F32, "g1p")
        with nc.named_scope("adaln1"):
            adaln(1, mods_mix1, g1p1)
        with nc.named_scope("conformer"):
            conformer(mods_mix1, g1p1)
        k.release()
    if stage >= 8:
        with nc.named_scope("ffn1"):
            ffn(1, final=True)
    k.finish()
    print("instructions:", k.ninst, "sems:", k.nsem, "sbuf_off:", k.sb_off)
    return nc


def _consts():
    f32 = np.float32
    c = {}
    c["ident"] = np.eye(128, dtype=f32)
    c["iota"] = np.arange(128, dtype=f32).reshape(128, 1)
    theta = f32(10000.0)
    inv_m = (theta ** (-np.arange(0, 128, 2, dtype=f32) / f32(128))).astype(f32)
    inv_r = (f32(1.0) / (theta ** np.linspace(0.0, 1.0, 64, dtype=f32))).astype(f32)
    rot = np.zeros((17, 128, 4, 128), f32)
    for t in range(17):
        if t < 16:
            pos = (t * 128 + np.arange(128)).astype(f32)
        else:
            pos = (2048 + (np.arange(128) % 4)).astype(f32)
        am = (pos[:, None] * inv_m[None, :]).astype(f32)
        ar = (pos[:, None] * inv_r[None, :]).astype(f32)
        cm, sm = np.cos(am).astype(f32), np.sin(am).astype(f32)
        cr, sr = np.cos(ar).astype(f32), np.sin(ar).astype(f32)
        rot[t, :, 0, 0::2] = cr; rot[t, :, 0, 1::2] = cr
        rot[t, :, 1, 0::2] = -sr; rot[t, :, 1, 1::2] = sr
        rot[t, :, 2, 0:64] = cm; rot[t, :, 2, 64:128] = cm
        rot[t, :, 3, 0:64] = -sm; rot[t, :, 3, 64:128] = sm
    c["rot"] = rot
    lg = np.array(LOGG, dtype=np.float64)
    i = np.arange(128, dtype=np.float64)
    rdm = np.zeros((128, 4, 128), np.float64)
    for h in range(4):
        diff = i[None, :] - i[:, None]
        rdm[:, h, :] = np.where(diff >= 0, np.exp(np.maximum(diff, 0) * lg[h]), 0.0) * SCALE
    c["rdm"] = rdm.astype(f32)
    c["rqd"] = np.broadcast_to(np.exp((i[None, None, :] + 1.0) * lg[None, :, None]), (128, 4, 128)).astype(f32).copy()
    c["rkd"] = (np.exp((127.0 - i)[:, None] * lg[None, :]) * SCALE).astype(f32)
    r = np.arange(64)
    seq, ii = r // 4, (r % 4).astype(np.float64)
    rdms = np.zeros((64, 4, 64), np.float64)
    for h in range(4):
        diff = ii[None, :] - ii[:, None]
        same = seq[None, :] == seq[:, None]
        rdms[:, h, :] = np.where(same & (diff >= 0), np.exp(np.maximum(diff, 0) * lg[h]), 0.0) * SCALE
    c["rdms"] = rdms.astype(f32)
    c["rqds"] = np.broadcast_to(np.exp((ii[None, None, :] + 1.0) * lg[None, :, None]), (128, 4, 64)).astype(f32).copy()
    c["rkds"] = (np.exp((3.0 - ii)[:, None] * lg[None, :]) * SCALE).astype(f32)
    blk = np.zeros((64, 16), f32)
    blk[r, seq] = 1.0
    c["blk"] = blk
    tri = np.where(np.arange(128)[None, :] <= np.arange(128)[:, None], 0.0, NEG).astype(f32)
    c["tri"] = tri
    nm = np.full((64, 16, 16), NEG, f32)
    for b in range(16):
        for tq in range(4):
            for kk in range(tq + 1):
                nm[b * 4 + kk, b, tq::4] = 0.0
    c["nmask"] = nm
    Ep = np.zeros((18, 128), f32); Ep[0, :] = 1.0; Ep[17, :] = 1.0
    Es = np.zeros((18, 64), f32); Es[17, :] = 1.0
    for s in range(16):
        Es[1 + s, 4 * s:4 * s + 4] = 1.0
    ep = np.zeros((18, 1), f32); ep[0, 0] = 1.0; ep[17, 0] = 1.0
    c["Ep"], c["Es"], c["ep"] = Ep, Es, ep
    return c


def make_in_maps(x_prompt, x_sample, cache_k, cache_v, state_ret, state_conv, state_ffn, page_table, c_prompt, c_sample,
                 ab_w_in, ab_w_out, cf_w_pw1, cf_b_pw1, cf_w_dw, cf_b_dw, cf_ln_g, cf_ln_b, cf_w_pw2, cf_b_pw2,
                 ffn_w_up, ffn_w_dw, ffn_b_dw, ffn_w_down, ada_w, ada_b, ln_g, ln_b):
    A = lambda a: np.ascontiguousarray(np.asarray(a))
    consts = _consts()
    ck = A(cache_k).reshape(-1, 512)
    cv = A(cache_v).reshape(-1, 512)
    cfp = A(np.concatenate([np.asarray(cf_w_dw)[0], np.asarray(cf_b_dw)[0][None], np.asarray(cf_ln_g)[0][None],
                            np.asarray(cf_ln_b)[0][None], np.asarray(cf_b_pw1)[0].reshape(2, D)], axis=0))
    ffp = A(np.concatenate([np.asarray(ffn_w_dw), np.asarray(ffn_b_dw)[:, None, :]], axis=1))
    shared = {
        "ck": ck, "cv": cv, "w_in": A(ab_w_in)[0], "w_out": A(ab_w_out)[0], "pw1": A(cf_w_pw1)[0], "cfp": cfp,
        "pw2": A(cf_w_pw2)[0], "b_pw2": A(cf_b_pw2).reshape(1, D), "ffn_up": A(ffn_w_up), "ffp": ffp,
        "ffn_down": A(ffn_w_down), "ada_w": A(ada_w), "ada_b": A(ada_b), "ln_g": A(ln_g).reshape(4, D),
        "ln_b": A(ln_b).reshape(4, D),
    }
    shared.update(consts)
    maps = []
    for c in range(NCORES):
        s0, s1 = c * NS, (c + 1) * NS
        m = dict(shared)
        m["xp"] = A(x_prompt[c])
        m["xs"] = A(np.asarray(x_sample)[s0:s1].reshape(SR, D))
        m["call"] = A(np.concatenate([np.asarray(c_prompt)[c:c + 1], np.asarray(c_sample)[s0:s1]], axis=0))
        m["pt"] = A(np.asarray(page_table)[s0:s1].reshape(1, NS * 16).astype(np.int32))
        m["sret"] = A(np.asarray(state_ret)[0, s0:s1].reshape(NS * 512, 128))
        m["sconv"] = A(np.asarray(state_conv)[0, s0:s1].reshape(NS * 30, D))
        m["sffn"] = A(np.asarray(state_ffn)[:, s0:s1].reshape(2, NS * 2, DFF))
        maps.append(m)
    return maps


def assemble(results):
    R = results
    cat = lambda name: [r[name] for r in R]
    y_prompt = np.stack(cat("yp"), 0)
    y_sample = np.concatenate(cat("ys"), 0).reshape(128, 4, D)
    k_prompt = np.stack(cat("kp"), 0).reshape(1, 8, T, 4, 128)
    v_prompt = np.stack(cat("vp"), 0).reshape(1, 8, T, 4, 128)
    k_sample = np.concatenate(cat("ks"), 0).reshape(1, 128, 4, 4, 128)
    v_sample = np.concatenate(cat("vs"), 0).reshape(1, 128, 4, 4, 128)
    ret_prompt = np.stack(cat("rpo"), 0).reshape(1, 8, 4, 128, 128)
    ret_sample = np.concatenate(cat("rso"), 0).reshape(1, 128, 4, 128, 128)
    conv_prompt = np.stack(cat("cpo"), 0).reshape(1, 8, 30, D)
    conv_sample = np.concatenate(cat("cso"), 0).reshape(1, 128, 30, D)
    ffn_prompt = np.stack(cat("fpo"), 1).reshape(2, 8, 2, DFF)
    ffn_sample = np.concatenate([r["fso"].reshape(2, NS, 2, DFF) for r in R], 1)
    outs = (y_prompt, y_sample, k_prompt, v_prompt, k_sample, v_sample, ret_prompt, ret_sample,
            conv_prompt, conv_sample, ffn_prompt, ffn_sample)
    return tuple(np.ascontiguousarray(o, dtype=np.float32) for o in outs)


def kernel(**inputs):
    nc = build()
    maps = make_in_maps(**inputs)
    res = run_bass_kernel_spmd(nc, maps, core_ids=list(range(NCORES)))
    return assemble(res.results)
```

```python
import math
import numpy as np
import ml_dtypes
import concourse.bass as bass
import concourse.mybir as mybir
from concourse.bass_utils import run_bass_kernel_spmd

F32 = mybir.dt.float32
BF16 = mybir.dt.bfloat16
I32 = mybir.dt.int32
ALU = mybir.AluOpType
AF = mybir.ActivationFunctionType
AX = mybir.AxisListType

NCORES = 8
D = 1024
T = 2048
NT = 16
NS = 16
SR = 64
DFF = 2816
NFC = 22
ALPHA = 4.0 ** 0.25
LN_EPS = 1e-5
GN_EPS = 1e-6
SCALE = 128.0 ** -0.5
NEG = -1.0e30
LOGG = [math.log1p(-2.0 ** (-5.0 - h)) for h in range(4)]


class Reg:
    __slots__ = ("w", "r", "p", "dsem", "dcnt", "psum")

    def __init__(self, psum=False):
        self.psum = psum
        self.w = {}
        self.r = {}
        self.p = {}
        self.dsem = None
        self.dcnt = 0


class Buf:
    __slots__ = ("t", "r")

    def __init__(self, t):
        self.t = t
        self.r = Reg()


class Pool:
    def __init__(self, bufs):
        self.bufs = bufs
        self.i = 0

    def next(self):
        b = self.bufs[self.i % len(self.bufs)]
        self.i += 1
        return b


def _merge(dst, src):
    for s, v in src.items():
        if dst.get(s, 0) < v:
            dst[s] = v


class KB:
    def __init__(self, nc):
        self.nc = nc
        self.eng = {"pe": nc.tensor, "act": nc.scalar, "dve": nc.vector, "pool": nc.gpsimd, "sp": nc.sync}
        self.esem = {e: nc.alloc_semaphore("es_" + e) for e in ("pe", "act", "dve", "pool")}
        self.ecnt = {e: 0 for e in self.esem}
        self.seen = {e: {} for e in self.eng}
        self.nsem = 4
        self.out_toks = {}
        self.sb_off = (nc.sbuf_base + 63) // 64 * 64
        self.sb_top = nc.sbuf_top
        self.sb_marks = []
        self.uid = 0
        self.ninst = 0
        self.anchors = []
        self.limit = self.sb_top

    def barrier(self):
        for e, E in self.eng.items():
            seen = self.seen[e]
            for e2, s in self.esem.items():
                v = self.ecnt[e2]
                if v > seen.get(s, 0) and not (e == "pe" and e2 == "pe"):
                    E.wait_ge(s, v)
                    seen[s] = v
                    self.ninst += 1
            for a in self.anchors:
                if a.dcnt > seen.get(a.dsem, 0):
                    E.wait_ge(a.dsem, a.dcnt)
                    seen[a.dsem] = a.dcnt
                    self.ninst += 1

    def sb(self, shape, dtype, name=None):
        self.uid += 1
        nm = (name or "t") + "_%d" % self.uid
        esz = 2 if dtype == BF16 else 4
        n = 1
        for s in shape[1:]:
            n *= s
        nbytes = (n * esz + 63) // 64 * 64
        off = self.sb_off
        self.sb_off += nbytes
        assert self.sb_off <= self.limit, "SBUF overflow %d > %d (%s)" % (self.sb_off, self.limit, nm)
        return self.nc.alloc_sbuf_tensor_at(nm, list(shape), dtype, offset=off)

    def buf(self, shape, dtype, name=None):
        return Buf(self.sb(shape, dtype, name))

    def pool(self, n, shape, dtype, name=None):
        return Pool([self.buf(shape, dtype, name) for _ in range(n)])

    def mark(self):
        self.sb_marks.append(self.sb_off)

    def release(self):
        self.sb_off = self.sb_marks.pop()
        self.barrier()

    def _waits(self, eng, reads, writes, accum):
        need = {}
        mysem0 = self.esem.get(eng)
        for r in reads:
            _merge(need, r.w)
            if r.psum:
                for s_, v_ in r.r.items():
                    if s_ is not mysem0 and need.get(s_, 0) < v_:
                        need[s_] = v_
        for w in writes:
            if accum and not w.r:
                _merge(need, w.p)
            else:
                _merge(need, w.r)
                _merge(need, w.w)
        E = self.eng[eng]
        mysem = self.esem.get(eng)
        seen = self.seen[eng]
        for s, v in need.items():
            if eng == "pe" and s is mysem:
                continue
            if seen.get(s, 0) >= v:
                continue
            E.wait_ge(s, v)
            self.ninst += 1
            seen[s] = v

    def _update(self, tok, reads, writes, accum):
        s, v = tok
        for r in reads:
            if r.r.get(s, 0) < v:
                r.r[s] = v
        for w in writes:
            if accum and not w.r:
                if w.w.get(s, 0) < v:
                    w.w[s] = v
            else:
                p = dict(w.w)
                _merge(p, w.r)
                w.p = p
                w.w = {s: v}
                w.r = {}

    def op(self, eng, fn, reads=(), writes=(), accum=False, inc=True):
        self._waits(eng, reads, writes, accum)
        ins = fn(self.eng[eng])
        self.ninst += 1
        if inc:
            self.ecnt[eng] += 1
            ins.then_inc(self.esem[eng], 1)
        tok = (self.esem[eng], self.ecnt[eng] + (0 if inc else 1))
        self._update(tok, reads, writes, accum)
        return ins

    def _dma_any(self, q, mk, reads, writes, anchor, accum, is_output):
        if anchor is None:
            anchor = writes[0] if writes else reads[0]
        if anchor.dsem is None:
            anchor.dsem = self.nc.alloc_semaphore("ds_%d" % self.nsem)
            self.nsem += 1
            self.anchors.append(anchor)
        self._waits(q, reads, writes, accum)
        ins = mk(self.eng[q])
        self.ninst += 1
        anchor.dcnt += 16
        ins.then_inc(anchor.dsem, 16)
        tok = (anchor.dsem, anchor.dcnt)
        self._update(tok, reads, writes, accum)
        if is_output:
            self.out_toks[anchor.dsem] = anchor.dcnt
        return ins

    def dma(self, q, out, in_, reads=(), writes=(), anchor=None, accum=False, is_output=False, **kw):
        return self._dma_any(q, lambda e: e.dma_start(out=out, in_=in_, **kw), reads, writes, anchor, accum, is_output)

    def gather(self, out, table, idx_ap, reads=(), writes=(), accum=False):
        return self._dma_any(
            "pool",
            lambda e: e.indirect_dma_start(out=out, out_offset=None, in_=table,
                                           in_offset=bass.IndirectOffsetOnAxis(ap=idx_ap, axis=0)),
            reads, writes, None, accum, False)

    def finish(self):
        sp = self.eng["sp"]
        for s, v in self.out_toks.items():
            sp.wait_ge(s, v)
        for e, s in self.esem.items():
            if self.ecnt[e] > 0:
                sp.wait_ge(s, self.ecnt[e])

    def mm(self, out, lhsT, rhs, start, stop, reads=(), writes=(), inc=None):
        return self.op("pe", lambda e: e.matmul(out, lhsT, rhs, start=start, stop=stop),
                       reads, writes, accum=not start, inc=(stop if inc is None else inc))

    def tr(self, out, in_, ident, reads=(), writes=(), accum=False):
        return self.op("pe", lambda e: e.transpose(out, in_, ident), reads, writes, accum=accum)

    def act(self, out, in_, func, reads=(), writes=(), accum=False, **kw):
        return self.op("act", lambda e: e.activation(out, in_, func, **kw), reads, writes, accum=accum)

    def tt(self, eng, out, in0, in1, op, reads=(), writes=(), accum=False):
        return self.op(eng, lambda e: e.tensor_tensor(out, in0, in1, op), reads, writes, accum=accum)

    def ts(self, eng, out, in0, s1, s2, op0, op1=None, reads=(), writes=(), accum=False):
        if op1 is None:
            return self.op(eng, lambda e: e.tensor_scalar(out, in0, s1, None, op0), reads, writes, accum=accum)
        return self.op(eng, lambda e: e.tensor_scalar(out, in0, s1, s2, op0, op1), reads, writes, accum=accum)

    def stt(self, out, in0, scalar, in1, op0, op1, reads=(), writes=(), accum=False):
        return self.op("dve", lambda e: e.scalar_tensor_tensor(out, in0, scalar, in1, op0, op1), reads, writes, accum=accum)

    def cp(self, eng, out, in_, reads=(), writes=(), accum=False):
        if eng == "act":
            return self.op("act", lambda e: e.copy(out, in_), reads, writes, accum=accum)
        return self.op(eng, lambda e: e.tensor_copy(out, in_), reads, writes, accum=accum)

    def memset(self, eng, ap, val, writes=(), accum=False):
        return self.op(eng, lambda e: e.memset(ap, val), (), writes, accum=accum)


def bc(ap, axis, n):
    a = ap.unsqueeze(axis)
    shp = list(a.shape)
    shp[axis] = n
    return a.broadcast_to(shp)


def build(stage=99, nphys=2560, skip_ms=False):
    nc = bass.Bass("TRN2", target_bir_lowering=False)
    k = KB(nc)

    def din(name, shape, dt=F32):
        return nc.dram_tensor(name, list(shape), dt, kind="ExternalInput").ap()

    def dout(name, shape):
        return nc.dram_tensor(name, list(shape), F32, kind="ExternalOutput").ap()

    xp = din("xp", [T, D]); xs_d = din("xs", [SR, D]); call = din("call", [17, D])
    ck = din("ck", [nphys * 128, 512]); cv = din("cv", [nphys * 128, 512])
    ptd = din("pt", [1, NS * 16], I32)
    sret = din("sret", [NS * 4 * 128, 128]); sconv = din("sconv", [NS * 30, D]); sffn = din("sffn", [2, NS * 2, DFF])
    w_in = din("w_in", [D, 3584]); w_out = din("w_out", [D, D]); pw1 = din("pw1", [D, 2048])
    cfp_d = din("cfp", [36, D]); pw2 = din("pw2", [D, D]); b_pw2 = din("b_pw2", [1, D])
    ffn_up = din("ffn_up", [2, D, 2 * DFF]); ffp_d = din("ffp", [2, 4, DFF]); ffn_down = din("ffn_down", [2, DFF, D])
    ada_w = din("ada_w", [2, D, 6 * D]); ada_b = din("ada_b", [2, 6 * D])
    ln_g = din("ln_g", [4, D]); ln_b = din("ln_b", [4, D])
    ident_d = din("ident", [128, 128]); iota_d = din("iota", [128, 1])
    rot_d = din("rot", [17, 128, 4, 128])
    rdm_d = din("rdm", [128, 4, 128]); rqd_d = din("rqd", [128, 4, 128]); rkd_d = din("rkd", [128, 4])
    rdms_d = din("rdms", [64, 4, 64]); rqds_d = din("rqds", [128, 4, 64]); rkds_d = din("rkds", [64, 4])
    blk_d = din("blk", [64, 16]); tri_d = din("tri", [128, 128]); nmask_d = din("nmask", [64, 16, 16])
    Ep_d = din("Ep", [18, 128]); Es_d = din("Es", [18, 64]); ep_d = din("ep", [18, 1])

    yp = dout("yp", [T, D]); ys = dout("ys", [SR, D])
    kp = dout("kp", [T, 512]); vp = dout("vp", [T, 512]); ks = dout("ks", [SR, 512]); vs = dout("vs", [SR, 512])
    rpo = dout("rpo", [512, 128]); rso = dout("rso", [NS * 512, 128])
    cpo = dout("cpo", [30, D]); cso = dout("cso", [NS * 30, D])
    fpo = dout("fpo", [2, 2, DFF]); fso = dout("fso", [2, NS * 2, DFF])

    sc_mods = [nc.dram_tensor("sc_mods%d" % l, [SR, 3 * D], F32).ap() for l in range(2)]
    sc_g2p = [nc.dram_tensor("sc_g2p%d" % l, [1, D], F32).ap() for l in range(2)]
    r_scm = [Reg(), Reg()]
    r_scg = [Reg(), Reg()]

    ps = nc.alloc_psum_tensor("ps", [128, 4096], F32)
    psb = ps[:].bitcast(BF16)
    rb = [Reg(psum=True) for _ in range(8)]

    def bank(i, rows=128, c0=0, c1=512):
        return ps[0:rows, i * 512 + c0:i * 512 + c1]

    def bankb(i, rows=128, c0=0, c1=1024):
        return psb[0:rows, i * 1024 + c0:i * 1024 + c1]

    rconst = Reg()

    def cload(shape, src, dt=F32, q="sp"):
        t = k.sb(shape, dt)
        k.dma(q, t[:], src, writes=[rconst], anchor=rconst, accum=True)
        return t

    ident = cload([128, 128], ident_d)
    iota = cload([128, 1], iota_d)
    Ep = cload([18, 128], Ep_d); Es = cload([18, 64], Es_d); ep = cload([18, 1], ep_d)
    identb_t = k.sb([128, 128], BF16)
    onesb = k.sb([128, 128], BF16)
    onesf = k.sb([128, 128], F32)
    k.memset("pool", onesf[:], 1.0, writes=[rconst], accum=True)
    k.cp("dve", identb_t[:], ident[:], reads=[rconst], writes=[rconst], accum=True)
    k.memset("pool", onesb[:], 1.0, writes=[rconst], accum=True)
    XOFF = (k.sb_top - NT * D * 4) // 64 * 64
    x_all = nc.alloc_sbuf_tensor_at("x_all", [128, NT, D], F32, offset=XOFF)
    rx = [Reg() for _ in range(NT)]
    x_s = k.buf([SR, D], F32)
    k.dma("sp", x_s.t[:], xs_d, writes=[x_s.r])
    modT = k.buf([128, 2, 6, 8], F32)
    scT = k.buf([128, 8, 32], BF16)
    k.mark()
    callsb = cload([17, D], call)
    scs = k.buf([17, D], F32)
    k.act(scs.t[:], callsb[:], AF.Silu, reads=[rconst], writes=[scs.r])
    for c in range(8):
        k.tr(bank(0, 128, c * 32, c * 32 + 17), scs.t[0:17, c * 128:(c + 1) * 128], ident[0:17, 0:17],
             reads=[scs.r, rconst], writes=[rb[0]], accum=(c > 0))
    k.cp("dve", scT.t[:, :, 0:17], bank(0, 128, 0, 256).rearrange("p (c j) -> p c j", j=32)[:, :, 0:17],
         reads=[rb[0]], writes=[scT.r])
    k.release()

    cvt_ctr = [0]

    def load_w_bf16(dst_ap, dst_reg, src_ap, stg, eng=None, first=True, mul=None, mul_reg=None):
        shp = list(src_ap.shape)
        if len(shp) == 3:
            st = stg.t[:, 0:shp[1] * shp[2]].rearrange("p (a b) -> p a b", b=shp[2])
        else:
            st = stg.t[:, 0:shp[1]]
        k.dma("sp", st, src_ap, writes=[stg.r])
        if eng is None:
            cvt_ctr[0] += 1
            eng = "act" if cvt_ctr[0] % 2 else "dve"
        if mul is None:
            k.cp(eng, dst_ap, st, reads=[stg.r], writes=[dst_reg], accum=not first)
        else:
            k.tt("dve", dst_ap, st, mul, ALU.mult, reads=[stg.r, mul_reg], writes=[dst_reg], accum=not first)

    def layer_norm(xin, xin_reg, rows, gam, bet, gb_reg, out_ap, out_reg, wk):
        st = wk["st"].next(); mv = wk["mv"].next()
        xv = xin.rearrange("p (c f) -> p c f", f=512)
        for c in range(2):
            k.op("dve", lambda e, c=c: e.bn_stats(st.t[0:rows, c, :], xv[:, c, :]), reads=[xin_reg], writes=[st.r], accum=(c > 0))
        k.op("dve", lambda e: e.bn_aggr(mv.t[0:rows, 0:2], st.t[0:rows, :, :]), reads=[st.r], writes=[mv.r])
        k.ts("dve", mv.t[0:rows, 2:3], mv.t[0:rows, 1:2], LN_EPS, None, ALU.add, reads=[mv.r], writes=[mv.r])
        k.act(mv.t[0:rows, 2:3], mv.t[0:rows, 2:3], AF.Sqrt, reads=[mv.r], writes=[mv.r])
        k.op("dve", lambda e: e.reciprocal(mv.t[0:rows, 2:3], mv.t[0:rows, 2:3]), reads=[mv.r], writes=[mv.r])
        k.stt(mv.t[0:rows, 3:4], mv.t[0:rows, 0:1], -1.0, mv.t[0:rows, 2:3], ALU.mult, ALU.mult, reads=[mv.r], writes=[mv.r])
        xn = wk["xn"].next()
        k.act(xn.t[0:rows, :], xin, AF.Identity, reads=[xin_reg, mv.r], writes=[xn.r], scale=mv.t[0:rows, 2:3], bias=mv.t[0:rows, 3:4])
        k.tt("dve", xn.t[0:rows, :], xn.t[0:rows, :], gam[0:rows, :], ALU.mult, reads=[xn.r, gb_reg], writes=[xn.r])
        k.tt("dve", out_ap, xn.t[0:rows, :], bet[0:rows, :], ALU.add, reads=[xn.r, gb_reg], writes=[out_reg])

    def make_hT(xin, xin_reg, rows, hT_ap, hT_reg, layer, which, mods_s=None, mods_reg=None, wk=None, pbanks=(0, 1), first=True):
        if rows == 128:
            src, src_reg = xin, xin_reg
        else:
            hs = wk["hs"].next()
            k.tt("dve", hs.t[0:rows], xin, mods_s[:, D:2 * D], ALU.mult, reads=[xin_reg, mods_reg], writes=[hs.r])
            k.tt("dve", hs.t[0:rows], hs.t[0:rows], mods_s[:, 0:D], ALU.add, reads=[hs.r, mods_reg], writes=[hs.r])
            src, src_reg = hs.t[0:rows], hs.r
        for c in range(8):
            b = pbanks[c // 4]
            k.tr(bank(b, 128, (c % 4) * 128, (c % 4) * 128 + rows), src[:, c * 128:(c + 1) * 128], ident[0:rows, 0:rows],
                 reads=[src_reg, rconst], writes=[rb[b]], accum=(c % 4 > 0))
        for c in range(8):
            b = pbanks[c // 4]
            pin = bank(b, 128, (c % 4) * 128, (c % 4) * 128 + rows)
            if rows == 128:
                k.act(hT_ap[:, c, :], pin, AF.Identity, reads=[rb[b], modT.r], writes=[hT_reg], accum=not (first and c == 0),
                      scale=modT.t[:, layer, which + 1, c:c + 1], bias=modT.t[:, layer, which, c:c + 1])
            else:
                k.cp("act", hT_ap[:, c, :], pin, reads=[rb[b]], writes=[hT_reg], accum=not (first and c == 0))

    def rotate(xv, xregs, rows, G, Ct, St, tab_reg, out3, out_reg, tmp3, tmp_reg, mode):
        if mode == "half":
            x4 = xv.rearrange("p g (two j) -> p g two j", two=2)
            t4 = tmp3.rearrange("p g (two j) -> p g two j", two=2)
            a0, a1 = x4[:, :, 1, :], x4[:, :, 0, :]
            d0, d1 = t4[:, :, 0, :], t4[:, :, 1, :]
            s0, s1 = St[:, 0:64], St[:, 64:128]
        else:
            x4 = xv.rearrange("p g (j two) -> p g j two", two=2)
            t4 = tmp3.rearrange("p g (j two) -> p g j two", two=2)
            a0, a1 = x4[:, :, :, 1], x4[:, :, :, 0]
            d0, d1 = t4[:, :, :, 0], t4[:, :, :, 1]
            Sv = St.rearrange("p (j two) -> p j two", two=2)
            s0, s1 = Sv[:, :, 0], Sv[:, :, 1]
        k.tt("dve", d0, a0, bc(s0, 1, G), ALU.mult, reads=list(xregs) + [tab_reg], writes=[tmp_reg])
        k.tt("dve", d1, a1, bc(s1, 1, G), ALU.mult, reads=list(xregs) + [tab_reg], writes=[tmp_reg], accum=True)
        k.tt("dve", out3, xv, bc(Ct, 1, G), ALU.mult, reads=list(xregs) + [tab_reg], writes=[out_reg])
        k.tt("dve", out3, out3, tmp3, ALU.add, reads=[out_reg, tmp_reg], writes=[out_reg])

    def adaln(l, mods_mix, g1p):
        k.mark()
        stg = k.pool(2, [128, 8 * 512], F32, "ada_stg")
        wbp = k.pool(2, [128, 8, 512], BF16, "ada_wb")
        m17p = k.pool(2, [18, 512], F32, "m17")
        outp = k.pool(2, [128, 512], F32, "ada_out")
        for j in range(12):
            which, half = divmod(j, 2)
            wb = wbp.next()
            load_w_bf16(wb.t[:], wb.r, ada_w[l, :, j * 512:(j + 1) * 512].rearrange("(kc p) n -> p kc n", p=128), stg.next())
            m17 = m17p.next()
            k.dma("sp", m17.t[17:18, :], ada_b[l:l + 1, j * 512:(j + 1) * 512], writes=[m17.r])
            for kc in range(8):
                k.mm(bank(0, 17), scT.t[:, kc, 0:17], wb.t[:, kc, :], kc == 0, kc == 7, reads=[scT.r, wb.r], writes=[rb[0]])
            k.cp("act", m17.t[0:17, :], bank(0, 17), reads=[rb[0]], writes=[m17.r], accum=True)
            plus1 = 1.0 if which in (1, 2, 4, 5) else 0.0
            k.mm(bank(1, 64), Es[:], m17.t[:], True, True, reads=[rconst, m17.r], writes=[rb[1]])
            if which < 3:
                k.ts("dve", mods_mix.t[:, j * 512:(j + 1) * 512], bank(1, 64), plus1, None, ALU.add,
                     reads=[rb[1]], writes=[mods_mix.r], accum=(j > 0))
            else:
                o = outp.next()
                k.ts("dve", o.t[0:64, :], bank(1, 64), plus1, None, ALU.add, reads=[rb[1]], writes=[o.r])
                k.dma("sp", sc_mods[l][:, (j - 6) * 512:(j - 5) * 512], o.t[0:64, :], reads=[o.r], writes=[r_scm[l]], accum=(j > 6))
            if which in (2, 5):
                k.mm(bank(2), Ep[:], m17.t[:], True, True, reads=[rconst, m17.r], writes=[rb[2]])
                if which == 2:
                    k.ts("dve", g1p.t[:, half * 512:(half + 1) * 512], bank(2), 1.0, None, ALU.add,
                         reads=[rb[2]], writes=[g1p.r], accum=(half > 0))
                else:
                    o = outp.next()
                    k.ts("dve", o.t[:, :], bank(2), 1.0, None, ALU.add, reads=[rb[2]], writes=[o.r])
                    k.dma("sp", sc_g2p[l][:, half * 512:(half + 1) * 512], o.t[0:1, :], reads=[o.r], writes=[r_scg[l]], accum=(half > 0))
            for cc in range(4):
                k.mm(bank(3, 128, cc, cc + 1), m17.t[:, cc * 128:(cc + 1) * 128], ep[:], True, True,
                     reads=[m17.r, rconst], writes=[rb[3]])
            k.ts("dve", modT.t[:, l, which, half * 4:(half + 1) * 4], bank(3, 128, 0, 4), plus1, None, ALU.add,
                 reads=[rb[3]], writes=[modT.r], accum=True)
        k.release()

    def pass_moba(mods_mix, omT):
        k.mark()
        wm = k.buf([128, 8, 1536], BF16, "wm")
        qTs_b = k.buf([128, 4, SR], BF16, "qTs_b"); qTs_f = k.buf([128, 4, SR], F32, "qTs_f")
        kTs_b = k.buf([128, 4, SR], BF16, "kTs_b"); v_s = k.buf([SR, 4, 132], BF16, "v_s")
        k.mark()
        kT_hist = k.buf([128, 4, T], BF16, "kT_hist")
        v_hist = k.buf([128, NT, 512], BF16, "v_hist")
        kmT = k.buf([128, 4, 8], F32, "kmT")
        k.mark()
        stg = k.pool(2, [128, 8 * 512], F32, "stg")
        for g in range(3):
            load_w_bf16(wm.t[:, :, g * 512:(g + 1) * 512], wm.r,
                        w_in[:, 2048 + g * 512:2048 + (g + 1) * 512].rearrange("(kc p) n -> p kc n", p=128),
                        stg.next(), first=(g == 0))
        k.release()
        wk = {"hs": k.pool(1, [SR, D], F32, "hs")}
        xt_p = k.pool(2, [128, D], F32, "xt")
        hT_p = k.pool(2, [128, 8, 128], BF16, "hT")
        rot_p = k.pool(2, [128, 4, 128], F32, "rot")
        qk_p = k.pool(2, [128, 8, 128], F32, "qkrot")
        tmp_p = k.pool(1, [128, 8, 128], F32, "rtmp")
        vf_p = k.pool(2, [128, 512], F32, "vf")
        qTb_p = k.pool(2, [128, 4, 128], BF16, "qTb")
        qTf_p = k.pool(2, [128, 4, 128], F32, "qTf")
        ksum_p = k.pool(2, [128, 4], F32, "ksum")
        gate_p = k.pool(1, [128, 4, 8], F32, "gate")
        cmp_p = k.pool(1, [128, 4, 8, 8], F32, "cmp")
        bias_p = k.pool(2, [128, 4, 8], F32, "bias")
        S_p = k.pool(1, [128, T], F32, "S")
        P_p = k.pool(2, [128, T], BF16, "P")
        PT_p = k.pool(2, [128, NT, 128], BF16, "PT")
        sm_p = k.pool(4, [128, 4], F32, "sm")
        om_p = k.pool(2, [128, 512], BF16, "om")

        for t in range(NT + 1):
            rows = 128 if t < NT else SR
            if t < NT:
                xt = xt_p.next()
                k.dma("sp", xt.t[:], xp[t * 128:(t + 1) * 128, :], writes=[xt.r])
                xin, xin_reg = xt.t[:], xt.r
            else:
                xin, xin_reg = x_s.t[:], x_s.r
            hT = hT_p.next()
            make_hT(xin, xin_reg, rows, hT.t[:, :, 0:rows], hT.r, 0, 0, mods_s=mods_mix.t, mods_reg=mods_mix.r, wk=wk, pbanks=(0, 1))
            rot = rot_p.next()
            k.dma("sp", rot.t[0:rows], rot_d[t, 0:rows], writes=[rot.r])
            for g in range(3):
                for kc in range(8):
                    k.mm(bank(4 + g, rows), hT.t[:, kc, 0:rows], wm.t[:, kc, g * 512:(g + 1) * 512], kc == 0, kc == 7,
                         reads=[hT.r, wm.r], writes=[rb[4 + g]])
            qk = qk_p.next(); tmp = tmp_p.next()
            zqk = ps[0:rows, 4 * 512:6 * 512].rearrange("p (g d) -> p g d", d=128)
            rotate(zqk, [rb[4], rb[5]], rows, 8, rot.t[0:rows, 2, :], rot.t[0:rows, 3, :], rot.r,
                   qk.t[0:rows], qk.r, tmp.t[0:rows], tmp.r, "half")
            vf = vf_p.next()
            k.cp("act", vf.t[0:rows], bank(6, rows), reads=[rb[6]], writes=[vf.r])
            kdst = kp[t * 128:(t + 1) * 128, :] if t < NT else ks
            vdst = vp[t * 128:(t + 1) * 128, :] if t < NT else vs
            k.dma("sp", kdst, qk.t[0:rows, 4:8, :].rearrange("p g d -> p (g d)"), reads=[qk.r], is_output=True)
            k.dma("sp", vdst, vf.t[0:rows], reads=[vf.r], is_output=True)
            if stage < 2:
                continue
            if t < NT:
                k.cp("act", v_hist.t[:, t, :], bank(6), reads=[rb[6]], writes=[v_hist.r], accum=(t > 0))
            else:
                k.memset("pool", v_s.t[:], 1.0, writes=[v_s.r])
                k.cp("pool", v_s.t[:, :, 0:128], vf.t[0:SR].rearrange("p (h d) -> p h d", d=128), reads=[vf.r], writes=[v_s.r])
            for g in range(8):
                b = 2 + g // 4
                k.tr(bank(b, 128, (g % 4) * 128, (g % 4) * 128 + rows), qk.t[0:rows, g, :], ident[0:rows, 0:rows],
                     reads=[qk.r, rconst], writes=[rb[b]], accum=(g % 4 > 0))
            qv = bank(2).rearrange("p (h s) -> p h s", s=128)[:, :, 0:rows]
            kv = bank(3).rearrange("p (h s) -> p h s", s=128)[:, :, 0:rows]
            if t < NT:
                qTb = qTb_p.next(); qTf = qTf_p.next()
                k.cp("act", qTb.t[:], qv, reads=[rb[2]], writes=[qTb.r])
                k.cp("dve", qTf.t[:], qv, reads=[rb[2]], writes=[qTf.r])
                k.cp("act", kT_hist.t[:, :, t * 128:(t + 1) * 128], kv, reads=[rb[3]], writes=[kT_hist.r], accum=(t > 0))
                ksum = ksum_p.next()
                k.op("dve", lambda e, ksum=ksum, kv=kv: e.tensor_reduce(ksum.t[:], kv, axis=AX.X, op=ALU.add), reads=[rb[3]], writes=[ksum.r])
                if t % 2 == 0:
                    k.cp("dve", kmT.t[:, :, t // 2], ksum.t[:], reads=[ksum.r], writes=[kmT.r], accum=True)
                else:
                    k.tt("dve", kmT.t[:, :, t // 2], kmT.t[:, :, t // 2], ksum.t[:], ALU.add, reads=[ksum.r, kmT.r], writes=[kmT.r])
            else:
                k.cp("act", qTs_b.t[:], qv, reads=[rb[2]], writes=[qTs_b.r])
                k.cp("dve", qTs_f.t[:], qv, reads=[rb[2]], writes=[qTs_f.r])
                k.cp("act", kTs_b.t[:], kv, reads=[rb[3]], writes=[kTs_b.r])
                continue
            own = t // 2
            bias = None
            if own >= 4:
                for h in range(4):
                    k.mm(bank(7, 128, 448 + h * 8, 448 + h * 8 + own), qTf.t[:, h, :], kmT.t[:, h, 0:own], True, True,
                         reads=[qTf.r, kmT.r], writes=[rb[7]])
                gate = gate_p.next(); cmpb = cmp_p.next(); bias = bias_p.next()
                gpv = bank(7, 128, 448, 480).rearrange("p (h n) -> p h n", n=8)[:, :, 0:own]
                k.cp("act", gate.t[:, :, 0:own], gpv, reads=[rb[7]], writes=[gate.r])
                gv = gate.t[:, :, 0:own]
                k.tt("dve", cmpb.t[:, :, 0:own, 0:own], bc(gv, 2, own), bc(gv, 3, own), ALU.is_gt, reads=[gate.r], writes=[cmpb.r])
                k.op("dve", lambda e, gate=gate, cmpb=cmpb, own=own: e.tensor_reduce(gate.t[:, :, 0:own], cmpb.t[:, :, 0:own, 0:own], axis=AX.X, op=ALU.add),
                     reads=[cmpb.r], writes=[gate.r])
                k.ts("dve", bias.t[:, :, 0:own], gate.t[:, :, 0:own], 2.5, NEG, ALU.is_gt, ALU.mult, reads=[gate.r], writes=[bias.r])
            nk = (t + 1) * 128
            om = om_p.next()
            for h in range(4):
                for c0 in range(0, nk, 512):
                    c1 = min(nk, c0 + 512)
                    b = c0 // 512
                    k.mm(bank(b, 128, 0, c1 - c0), qTb.t[:, h, :], kT_hist.t[:, h, c0:c1], True, True,
                         reads=[qTb.r, kT_hist.r], writes=[rb[b]])
                nb_used = (nk + 511) // 512
                sregs = [rb[i] for i in range(nb_used)]
                S = S_p.next()
                npast = own * 256
                first = True
                if npast > 0:
                    if bias is not None:
                        k.tt("dve", S.t[:, 0:npast].rearrange("p (n s) -> p n s", s=256),
                             ps[:, 0:npast].rearrange("p (n s) -> p n s", s=256), bc(bias.t[:, h, 0:own], 2, 256), ALU.add,
                             reads=sregs + [bias.r], writes=[S.r])
                    else:
                        k.cp("act", S.t[:, 0:npast], ps[:, 0:npast], reads=sregs, writes=[S.r])
                    first = False
                if t % 2 == 1:
                    k.cp("act", S.t[:, npast:npast + 128], ps[:, npast:npast + 128], reads=sregs, writes=[S.r], accum=not first)
                    first = False
                k.tt("dve", S.t[:, nk - 128:nk], ps[:, nk - 128:nk], tri[:], ALU.add, reads=sregs + [rconst], writes=[S.r], accum=not first)
                sm = sm_p.next()
                k.op("dve", lambda e, sm=sm, S=S, nk=nk: e.reduce_max(sm.t[:, 0:1], S.t[:, 0:nk], axis=AX.X), reads=[S.r], writes=[sm.r])
                k.ts("dve", sm.t[:, 1:2], sm.t[:, 0:1], -SCALE, None, ALU.mult, reads=[sm.r], writes=[sm.r])
                P = P_p.next()
                k.act(P.t[:, 0:nk], S.t[:, 0:nk], AF.Exp, reads=[S.r, sm.r], writes=[P.r, sm.r], scale=SCALE, bias=sm.t[:, 1:2],
                      accum_out=sm.t[:, 2:3])
                PT = PT_p.next()
                for j0 in range(0, t + 1, 8):
                    j1 = min(t + 1, j0 + 8)
                    for j in range(j0, j1):
                        k.tr(bankb(6, 128, (j - j0) * 128, (j - j0 + 1) * 128), P.t[:, j * 128:(j + 1) * 128], identb_t[:],
                             reads=[P.r, rconst], writes=[rb[6]], accum=(j > j0))
                    k.cp("act" if (j0 // 8) % 2 == 0 else "dve", PT.t[:, j0:j1, :],
                         bankb(6, 128, 0, (j1 - j0) * 128).rearrange("p (j s) -> p j s", s=128), reads=[rb[6]], writes=[PT.r], accum=(j0 > 0))
                for j in range(t + 1):
                    k.mm(bank(7, 128, 0, 128), PT.t[:, j, :], v_hist.t[:, j, h * 128:(h + 1) * 128],
                         j == 0, j == t, reads=[PT.r, v_hist.r], writes=[rb[7]])
                k.op("dve", lambda e, sm=sm: e.reciprocal(sm.t[:, 3:4], sm.t[:, 2:3]), reads=[sm.r], writes=[sm.r])
                k.act(om.t[:, h * 128:(h + 1) * 128], bank(7, 128, 0, 128), AF.Identity, reads=[rb[7], sm.r], writes=[om.r], accum=(h > 0),
                      scale=sm.t[:, 3:4])
            for h in range(4):
                k.tr(bankb(6, 128, h * 128, (h + 1) * 128), om.t[:, h * 128:(h + 1) * 128], identb_t[:], reads=[om.r, rconst],
                     writes=[rb[6]], accum=(h > 0))
            k.cp("act", omT.t[:, :, t * 128:(t + 1) * 128], bankb(6, 128, 0, 512).rearrange("p (h s) -> p h s", s=128),
                 reads=[rb[6]], writes=[omT.r], accum=(t > 0))
        k.release()
        if stage >= 3 and not skip_ms:
            moba_sample(qTs_b, qTs_f, kTs_b, v_s, omT)
        k.release()

    def moba_sample(qTs_b, qTs_f, kTs_b, v_s, omT):
        kpg_p = k.pool(5, [128, 512], F32, "kpg")
        vpg_p = k.pool(5, [128, 512], F32, "vpg")
        kb_p = k.pool(2, [128, 512], BF16, "kb")
        kTq_p = k.pool(2, [128, 4, T], BF16, "kTq")
        Vq_p = k.pool(2, [128, 16, 4, 132], BF16, "Vq")
        E_p = k.pool(2, [128, 17, 4, SR], BF16, "Eb")
        for b_ in Vq_p.bufs:
            k.memset("pool", b_.t[:], 1.0, writes=[b_.r])
        for b_ in E_p.bufs:
            k.memset("pool", b_.t[:], 0.0, writes=[b_.r])
        kms_p = k.pool(2, [128, 4, 8], F32, "kms")
        ksum_p = k.pool(2, [128, 4], F32, "ksum2")
        prod_p = k.pool(1, [128, 4, 8, 4], F32, "prod")
        gs_p = k.pool(1, [128, 16, 8], F32, "gs")
        cmp_p = k.pool(1, [128, 16, 8, 8], F32, "cmp2")
        comb_p = k.pool(1, [128, 16, 8], F32, "comb")
        pm_p = k.pool(1, [128, 16], F32, "pm")
        m16_p = k.pool(1, [16, 20], F32, "m16")
        X_p = k.pool(1, [128, 16, 16], F32, "X")
        Xn_p = k.pool(1, [SR, 16], F32, "Xn")
        NPG = NS * 16
        PF = 4
        gbuf = {}

        def issue(p):
            kpg = kpg_p.next(); vpg = vpg_p.next()
            k.gather(kpg.t[:], ck, idx.t[:, p:p + 1], reads=[idx.r], writes=[kpg.r])
            k.gather(vpg.t[:], cv, idx.t[:, p:p + 1], reads=[idx.r], writes=[vpg.r])
            gbuf[p] = (kpg, vpg)

        for p in range(PF):
            issue(p)

        def page_loop(b):
            kTq = kTq_p.next(); Vq = Vq_p.next(); kms = kms_p.next()
            for j in range(16):
                p = b * 16 + j
                if p + PF < NPG:
                    issue(p + PF)
                kpg, vpg = gbuf.pop(p)
                kb = kb_p.next()
                k.cp("dve", kb.t[:], kpg.t[:], reads=[kpg.r], writes=[kb.r])
                k.cp("act", Vq.t[:, j, :, 0:128], vpg.t[:].rearrange("p (h d) -> p h d", d=128), reads=[vpg.r], writes=[Vq.r],
                     accum=(j > 0))
                pb = 1 + j % 2
                for h in range(4):
                    k.tr(bankb(pb, 128, h * 128, (h + 1) * 128), kb.t[:, h * 128:(h + 1) * 128], identb_t[:],
                         reads=[kb.r, rconst], writes=[rb[pb]], accum=(h > 0))
                kvw = bankb(pb, 128, 0, 512).rearrange("p (h s) -> p h s", s=128)
                k.cp("act", kTq.t[:, :, j * 128:(j + 1) * 128], kvw, reads=[rb[pb]], writes=[kTq.r], accum=(j > 0))
                ksum = ksum_p.next()
                k.op("dve", lambda e, ksum=ksum, kvw=kvw: e.tensor_reduce(ksum.t[:], kvw, axis=AX.X, op=ALU.add), reads=[rb[pb]], writes=[ksum.r])
                if j % 2 == 0:
                    k.cp("dve", kms.t[:, :, j // 2], ksum.t[:], reads=[ksum.r], writes=[kms.r], accum=(j > 0))
                else:
                    k.tt("dve", kms.t[:, :, j // 2], kms.t[:, :, j // 2], ksum.t[:], ALU.add, reads=[ksum.r, kms.r], writes=[kms.r])
            return kTq, Vq, kms

        def chain(b, kTq, Vq, kms):
            Eb = E_p.next()
            prod = prod_p.next()
            qf = qTs_f.t[:, :, 4 * b:4 * b + 4]
            k.tt("dve", prod.t[:], bc(kms.t[:], 3, 4), bc(qf, 2, 8), ALU.mult, reads=[kms.r, qTs_f.r], writes=[prod.r])
            k.mm(bank(3, 128, 0, 128), onesf[:], prod.t[:].rearrange("p h n q -> p (h n q)"), True, True,
                 reads=[rconst, prod.r], writes=[rb[3]])
            gs = gs_p.next(); cmpb = cmp_p.next(); comb = comb_p.next()
            k.cp("act", gs.t[:].rearrange("p (h q) n -> p h q n", q=4),
                 bank(3, 128, 0, 128).rearrange("p (h n q) -> p h q n", h=4, n=8), reads=[rb[3]], writes=[gs.r])
            k.tt("dve", cmpb.t[:], bc(gs.t[:], 2, 8), bc(gs.t[:], 3, 8), ALU.is_gt, reads=[gs.r], writes=[cmpb.r])
            k.op("dve", lambda e, gs=gs, cmpb=cmpb: e.tensor_reduce(gs.t[:], cmpb.t[:], axis=AX.X, op=ALU.add), reads=[cmpb.r], writes=[gs.r])
            k.ts("dve", comb.t[:], gs.t[:], 2.5, NEG, ALU.is_gt, ALU.mult, reads=[gs.r], writes=[comb.r])
            for j in range(16):
                for h in range(4):
                    c0 = j * 16 + h * 4
                    k.mm(bank(0, 128, c0, c0 + 4), kTq.t[:, h, j * 128:(j + 1) * 128], qTs_b.t[:, h, 4 * b:4 * b + 4], True, True,
                         reads=[kTq.r, qTs_b.r], writes=[rb[0]], inc=(j == 15 and h == 3))
            for h in range(4):
                k.mm(bank(0, SR, 256 + h * 4, 260 + h * 4), kTs_b.t[:, h, :], qTs_b.t[:, h, 4 * b:4 * b + 4], True, True,
                     reads=[kTs_b.r, qTs_b.r], writes=[rb[0]], inc=(h == 3))
            pm = pm_p.next(); m16 = m16_p.next()
            k.op("dve", lambda e, pm=pm: e.tensor_reduce(pm.t[:], bank(0, 128, 0, 256).rearrange("p (j c) -> p c j", c=16), axis=AX.X, op=ALU.max),
                 reads=[rb[0]], writes=[pm.r])
            k.tt("dve", pm.t[0:SR, :], pm.t[0:SR, :], bank(0, SR, 256, 272), ALU.max, reads=[pm.r, rb[0]], writes=[pm.r])
            k.tr(bank(3, 16, 128, 256), pm.t[:], ident[:], reads=[pm.r, rconst], writes=[rb[3]])
            k.op("dve", lambda e, m16=m16: e.reduce_max(m16.t[:, 16:17], bank(3, 16, 128, 256), axis=AX.X), reads=[rb[3]], writes=[m16.r])
            k.ts("dve", m16.t[:, 0:16], ident[0:16, 0:16], m16.t[:, 16:17], None, ALU.mult, reads=[m16.r, rconst], writes=[m16.r])
            k.mm(bank(3, 128, 256, 272), onesf[0:16, :], m16.t[:, 0:16], True, True, reads=[rconst, m16.r], writes=[rb[3]])
            mbc = bank(3, 128, 256, 272)
            k.tt("dve", comb.t[:], comb.t[:], bc(mbc, 2, 8), ALU.subtract, reads=[comb.r, rb[3]], writes=[comb.r])
            X = X_p.next(); Xn = Xn_p.next()
            k.tt("dve", X.t[:].rearrange("p (n two) c -> p n two c", two=2),
                 bank(0, 128, 0, 256).rearrange("p (n two c) -> p n two c", two=2, c=16),
                 bc(comb.t[:].rearrange("p c n -> p n c"), 2, 2), ALU.add, reads=[rb[0], comb.r], writes=[X.r])
            k.tt("dve", Xn.t[:], bank(0, SR, 256, 272), nmask[:, b, :], ALU.add, reads=[rb[0], rconst], writes=[Xn.r])
            k.tt("dve", Xn.t[:], Xn.t[:], bank(3, SR, 256, 272), ALU.subtract, reads=[Xn.r, rb[3]], writes=[Xn.r])
            k.act(Eb.t[:, 0:16, :, 4 * b:4 * b + 4], X.t[:].rearrange("p j (h q) -> p j h q", q=4), AF.Exp, reads=[X.r], writes=[Eb.r], scale=SCALE)
            k.act(Eb.t[0:SR, 16, :, 4 * b:4 * b + 4], Xn.t[:].rearrange("p (h q) -> p h q", q=4), AF.Exp, reads=[Xn.r], writes=[Eb.r], scale=SCALE)
            for h in range(4):
                ob = bank(4 + h, SR, 0, 132)
                for j in range(16):
                    k.mm(ob, Eb.t[:, j, h, :], Vq.t[:, j, h, :], (b == 0 and j == 0), False,
                         reads=[Eb.r, Vq.r], writes=[rb[4 + h]], inc=False)
                k.mm(ob, Eb.t[0:SR, 16, h, :], v_s.t[:, h, :], False, (b == NS - 1),
                     reads=[Eb.r, v_s.r], writes=[rb[4 + h]], inc=True)
            k.memset("dve", Eb.t[:, :, :, 4 * b:4 * b + 4], 0.0, writes=[Eb.r])

        prev = None
        for b in range(NS + 1):
            cur = page_loop(b) if b < NS else None
            if prev is not None:
                chain(b - 1, *prev)
            prev = cur
        rin = k.buf([SR, 4], F32, "rin"); oms = k.buf([SR, 512], BF16, "oms")
        for h in range(4):
            c0 = (4 + h) * 512
            k.op("dve", lambda e, h=h, c0=c0: e.reciprocal(rin.t[:, h:h + 1], ps[0:SR, c0 + 128:c0 + 129]),
                 reads=[rb[4 + h]], writes=[rin.r], accum=(h > 0))
        for h in range(4):
            c0 = (4 + h) * 512
            k.act(oms.t[:, h * 128:(h + 1) * 128], ps[0:SR, c0:c0 + 128], AF.Identity, reads=[rb[4 + h], rin.r], writes=[oms.r],
                  accum=(h > 0), scale=rin.t[:, h:h + 1])
        for h in range(4):
            k.tr(bankb(1, 128, h * SR, (h + 1) * SR), oms.t[:, h * 128:(h + 1) * 128], identb_t[0:SR, 0:SR], reads=[oms.r, rconst],
                 writes=[rb[1]], accum=(h > 0))
        k.cp("act", omT.t[:, :, T:T + SR], bankb(1, 128, 0, 4 * SR).rearrange("p (h s) -> p h s", s=SR), reads=[rb[1]], writes=[omT.r])

    def pass_ret(mods_mix, orT):
        k.mark()
        wr = k.buf([128, 8, 2048], BF16, "wr")
        k.mark()
        stg = k.pool(2, [128, 8 * 512], F32, "stg")
        for g in range(4):
            load_w_bf16(wr.t[:, :, g * 512:(g + 1) * 512], wr.r,
                        w_in[:, g * 512:(g + 1) * 512].rearrange("(kc p) n -> p kc n", p=128), stg.next(), first=(g == 0))
        k.release()
        Sst = k.buf([128, 4, 128], F32, "Sst"); Sbf = k.buf([128, 4, 128], BF16, "Sbf")
        k.memset("pool", Sst.t[:], 0.0, writes=[Sst.r]); k.memset("pool", Sbf.t[:], 0.0, writes=[Sbf.r])
        wk = {"hs": k.pool(1, [SR, D], F32, "hs")}
        xt_p = k.pool(2, [128, D], F32, "xt"); hT_p = k.pool(2, [128, 8, 128], BF16, "hT")
        rot_p = k.pool(2, [128, 4, 128], F32, "rot"); qk_p = k.pool(2, [128, 8, 128], F32, "qkrot")
        tmp_p = k.pool(1, [128, 8, 128], F32, "rtmp")
        vb_p = k.pool(2, [128, 512], BF16, "vb"); sg_p = k.pool(2, [128, 512], F32, "sg")
        kd_p = k.pool(2, [128, 4, 128], BF16, "kd")
        qTb_p = k.pool(2, [128, 4, 128], BF16, "qTb"); kTb_p = k.pool(2, [128, 4, 128], BF16, "kTb")
        qdT_p = k.pool(2, [128, 4, 128], BF16, "qdT"); att_p = k.pool(2, [128, 4, 128], BF16, "att")
        ss_p = k.pool(2, [128, 8], F32, "ss"); junk_p = k.pool(1, [128, 128], F32, "junk")
        or_p = k.pool(2, [128, 512], BF16, "or")
        for t in range(NT + 1):
            rows = 128 if t < NT else SR
            if t < NT:
                xt = xt_p.next()
                k.dma("sp", xt.t[:], xp[t * 128:(t + 1) * 128, :], writes=[xt.r])
                xin, xin_reg = xt.t[:], xt.r
            else:
                xin, xin_reg = x_s.t[:], x_s.r
            hT = hT_p.next()
            make_hT(xin, xin_reg, rows, hT.t[:, :, 0:rows], hT.r, 0, 0, mods_s=mods_mix.t, mods_reg=mods_mix.r, wk=wk, pbanks=(0, 1))
            rot = rot_p.next()
            k.dma("sp", rot.t[0:rows], rot_d[t, 0:rows], writes=[rot.r])
            for g in range(4):
                for kc in range(8):
                    k.mm(bank(4 + g, rows), hT.t[:, kc, 0:rows], wr.t[:, kc, g * 512:(g + 1) * 512], kc == 0, kc == 7,
                         reads=[hT.r, wr.r], writes=[rb[4 + g]])
            qk = qk_p.next(); tmp = tmp_p.next()
            zqk = ps[0:rows, 4 * 512:6 * 512].rearrange("p (g d) -> p g d", d=128)
            rotate(zqk, [rb[4], rb[5]], rows, 8, rot.t[0:rows, 0, :], rot.t[0:rows, 1, :], rot.r,
                   qk.t[0:rows], qk.r, tmp.t[0:rows], tmp.r, "pair")
            vb = vb_p.next(); sg = sg_p.next()
            k.cp("act", vb.t[0:rows], bank(6, rows), reads=[rb[6]], writes=[vb.r])
            k.act(sg.t[0:rows], bank(7, rows), AF.Silu, reads=[rb[7]], writes=[sg.r])
            kdc = rkd if t < NT else rkds
            kd = kd_p.next()
            k.tt("dve", kd.t[0:rows], qk.t[0:rows, 4:8, :], bc(kdc[0:rows, :], 2, 128), ALU.mult, reads=[qk.r, rconst], writes=[kd.r])
            for g in range(8):
                b = 2 + g // 4
                k.tr(bank(b, 128, (g % 4) * 128, (g % 4) * 128 + rows), qk.t[0:rows, g, :], ident[0:rows, 0:rows],
                     reads=[qk.r, rconst], writes=[rb[b]], accum=(g % 4 > 0))
            qv = bank(2).rearrange("p (h s) -> p h s", s=128)[:, :, 0:rows]
            kv = bank(3).rearrange("p (h s) -> p h s", s=128)[:, :, 0:rows]
            qTb = qTb_p.next(); kTb = kTb_p.next(); qdT = qdT_p.next()
            k.cp("act", qTb.t[:, :, 0:rows], qv, reads=[rb[2]], writes=[qTb.r])
            k.cp("act", kTb.t[:, :, 0:rows], kv, reads=[rb[3]], writes=[kTb.r])
            qdc = rqd if t < NT else rqds
            k.tt("dve", qdT.t[:, :, 0:rows], qv, qdc[:, :, 0:rows], ALU.mult, reads=[rb[2], rconst], writes=[qdT.r])
            for h in range(4):
                k.mm(bank(0, rows, h * 128, h * 128 + rows), kTb.t[:, h, 0:rows], qTb.t[:, h, 0:rows], True, True,
                     reads=[kTb.r, qTb.r], writes=[rb[0]])
            att = att_p.next()
            dmc = rdm if t < NT else rdms
            k.tt("dve", att.t[0:rows, :, 0:rows], bank(0, rows).rearrange("p (h s) -> p h s", s=128)[:, :, 0:rows], dmc[0:rows, :, 0:rows],
                 ALU.mult, reads=[rb[0], rconst], writes=[att.r])
            if t < NT:
                for h in range(4):
                    ob = bank(1, 128, h * 128, (h + 1) * 128)
                    k.mm(ob, att.t[:, h, :], vb.t[:, h * 128:(h + 1) * 128], True, False, reads=[att.r, vb.r], writes=[rb[1]])
                    k.mm(ob, qdT.t[:, h, :], Sbf.t[:, h, :], False, True, reads=[qdT.r, Sbf.r], writes=[rb[1]])
                for h in range(4):
                    k.mm(bank(2, 128, h * 128, (h + 1) * 128), kd.t[:, h, :], vb.t[:, h * 128:(h + 1) * 128], True, True,
                         reads=[kd.r, vb.r], writes=[rb[2]])
                for h in range(4):
                    k.stt(Sst.t[:, h, :], Sst.t[:, h, :], math.exp(128.0 * LOGG[h]), bank(2, 128, h * 128, (h + 1) * 128), ALU.mult, ALU.add,
                          reads=[Sst.r, rb[2]], writes=[Sst.r])
                k.cp("act", Sbf.t[:], Sst.t[:], reads=[Sst.r], writes=[Sbf.r])
                if t == NT - 1:
                    k.dma("sp", rpo.rearrange("(h d) v -> d h v", d=128), Sst.t[:], reads=[Sst.r], is_output=True)
            else:
                qdm = k.buf([128, NS, 4, SR], BF16, "qdm")
                k.memset("pool", qdm.t[:], 0.0, writes=[qdm.r])
                base = qdm.t[:]
                pstr = base.ap[0][0]
                for h in range(4):
                    dst = bass.AP(qdm.t, base.offset + h * SR, [[pstr, 128], [4 * SR + 4, NS], [1, 4]])
                    k.cp("pool", dst, qdT.t[:, h, 0:SR].rearrange("p (s j) -> p s j", j=4), reads=[qdT.r, qdm.r], writes=[qdm.r])
                for h in range(4):
                    ob = bank(4 + h, SR, 0, 128)
                    k.mm(ob, att.t[0:SR, h, 0:SR], vb.t[0:SR, h * 128:(h + 1) * 128], True, False, reads=[att.r, vb.r], writes=[rb[4 + h]], inc=True)
                s0_p = k.pool(2, [128, 4, 128], F32, "s0"); s0b_p = k.pool(2, [128, 4, 128], BF16, "s0b")
                kdm_p = k.pool(2, [SR, 4, 128], BF16, "kdm"); sn_p = k.pool(2, [128, 4, 128], F32, "sn")
                for sq in range(NS):
                    s0 = s0_p.next(); s0b = s0b_p.next()
                    k.dma("sp", s0.t[:], sret[sq * 512:(sq + 1) * 512, :].rearrange("(h d) v -> d h v", d=128), writes=[s0.r])
                    k.cp("act", s0b.t[:], s0.t[:], reads=[s0.r], writes=[s0b.r])
                    for h in range(4):
                        k.mm(bank(4 + h, SR, 0, 128), qdm.t[:, sq, h, :], s0b.t[:, h, :], False, (sq == NS - 1),
                             reads=[qdm.r, s0b.r], writes=[rb[4 + h]], inc=True)
                    kdm = kdm_p.next()
                    k.ts("dve", kdm.t[:], kd.t[0:SR], blk[:, sq:sq + 1], None, ALU.mult, reads=[kd.r, rconst], writes=[kdm.r])
                    ub = 2 + sq % 2
                    for h in range(4):
                        k.mm(bank(ub, 128, h * 128, (h + 1) * 128), kdm.t[:, h, :], vb.t[0:SR, h * 128:(h + 1) * 128], True, True,
                             reads=[kdm.r, vb.r], writes=[rb[ub]])
                    sn = sn_p.next()
                    for h in range(4):
                        k.stt(sn.t[:, h, :], s0.t[:, h, :], math.exp(4.0 * LOGG[h]), bank(ub, 128, h * 128, (h + 1) * 128), ALU.mult, ALU.add,
                              reads=[s0.r, rb[ub]], writes=[sn.r], accum=(h > 0))
                    k.dma("sp", rso[sq * 512:(sq + 1) * 512, :].rearrange("(h d) v -> d h v", d=128), sn.t[:], reads=[sn.r], is_output=True)
            ss = ss_p.next(); junk = junk_p.next()

            def obank(h):
                return (bank(1, rows, h * 128, (h + 1) * 128), rb[1]) if t < NT else (bank(4 + h, rows, 0, 128), rb[4 + h])

            for h in range(4):
                oap, oreg = obank(h)
                k.act(junk.t[0:rows], oap, AF.Square, reads=[oreg], writes=[junk.r, ss.r],
                      accum_out=ss.t[0:rows, h:h + 1])
            k.ts("dve", ss.t[0:rows, 4:8], ss.t[0:rows, 0:4], 1.0 / 128.0, GN_EPS, ALU.mult, ALU.add, reads=[ss.r], writes=[ss.r])
            k.act(ss.t[0:rows, 4:8], ss.t[0:rows, 4:8], AF.Sqrt, reads=[ss.r], writes=[ss.r])
            k.op("dve", lambda e, ss=ss, rows=rows: e.reciprocal(ss.t[0:rows, 4:8], ss.t[0:rows, 4:8]), reads=[ss.r], writes=[ss.r])
            orr = or_p.next()
            for h in range(4):
                oap, oreg = obank(h)
                k.stt(orr.t[0:rows, h * 128:(h + 1) * 128], oap, ss.t[0:rows, 4 + h:5 + h],
                      sg.t[0:rows, h * 128:(h + 1) * 128], ALU.mult, ALU.mult, reads=[oreg, ss.r, sg.r], writes=[orr.r], accum=(h > 0))
            for h in range(4):
                k.tr(bankb(3, 128, h * 128, h * 128 + rows), orr.t[0:rows, h * 128:(h + 1) * 128], identb_t[0:rows, 0:rows],
                     reads=[orr.r, rconst], writes=[rb[3]], accum=(h > 0))
            k.cp("act", orT.t[:, :, t * 128:t * 128 + rows], bankb(3, 128, 0, 512).rearrange("p (h s) -> p h s", s=128)[:, :, 0:rows],
                 reads=[rb[3]], writes=[orT.r], accum=(t > 0))
        k.release()

    def post_norm(y_ap, y_regs, xt_ap, xt_reg, rows, gate_ap, gate_reg, lng, lnb, gb_reg, wk, bias_ap=None, bias_reg=None):
        rr = wk["rr"].next()
        if bias_ap is not None:
            k.tt("dve", rr.t[0:rows], y_ap, bias_ap[0:rows], ALU.add, reads=list(y_regs) + [bias_reg], writes=[rr.r])
            k.tt("dve", rr.t[0:rows], rr.t[0:rows], gate_ap, ALU.mult, reads=[rr.r, gate_reg], writes=[rr.r])
        else:
            k.tt("dve", rr.t[0:rows], y_ap, gate_ap, ALU.mult, reads=list(y_regs) + [gate_reg], writes=[rr.r])
        k.stt(xt_ap, xt_ap, ALPHA, rr.t[0:rows], ALU.mult, ALU.add, reads=[xt_reg, rr.r], writes=[xt_reg])
        layer_norm(xt_ap, xt_reg, rows, lng, lnb, gb_reg, xt_ap, xt_reg, wk)

    def ln_work(n=2):
        rr = k.pool(n, [128, D], F32, "rr")
        return {"st": k.pool(2, [128, 2, 6], F32, "st"), "mv": k.pool(2, [128, 4], F32, "mv"),
                "xn": k.pool(n, [128, D], F32, "xn"), "rr": rr, "hs": rr}

    def load_ln(i):
        g = k.buf([128, D], F32, "lng"); b = k.buf([128, D], F32, "lnb")
        k.dma("sp", g.t[:], ln_g[i:i + 1, :].partition_broadcast(128), writes=[g.r])
        k.dma("sp", b.t[:], ln_b[i:i + 1, :].partition_broadcast(128), writes=[g.r], anchor=g.r, accum=True)
        return g, b

    def pass_out(mods_mix, g1p, orT, omT):
        k.mark()
        wo = k.buf([128, 8, D], BF16, "wo")
        k.mark()
        stg = k.pool(2, [128, 8 * 512], F32, "stg")
        for g in range(2):
            load_w_bf16(wo.t[:, :, g * 512:(g + 1) * 512], wo.r,
                        w_out[:, g * 512:(g + 1) * 512].rearrange("(kc p) n -> p kc n", p=128), stg.next(), first=(g == 0))
        k.release()
        lng, lnb = load_ln(0)
        wk = ln_work()
        for t in range(NT + 1):
            rows = 128 if t < NT else SR
            if t < NT:
                k.dma("sp", x_all[:, t, :], xp[t * 128:(t + 1) * 128, :], writes=[rx[t]])
                xt_ap, xt_reg = x_all[:, t, :], rx[t]
                gate_ap, gate_reg = g1p.t[:], g1p.r
            else:
                xt_ap, xt_reg = x_s.t[:], x_s.r
                gate_ap, gate_reg = mods_mix.t[:, 2 * D:3 * D], mods_mix.r
            bp = 4 * (t % 2)
            for half in range(2):
                for c in range(8):
                    src = orT if c < 4 else omT
                    k.mm(bank(bp + half, rows), src.t[:, c % 4, t * 128:t * 128 + rows], wo.t[:, c, half * 512:(half + 1) * 512],
                         c == 0, c == 7, reads=[src.r, wo.r], writes=[rb[bp + half]])
            post_norm(ps[0:rows, bp * 512:bp * 512 + D], [rb[bp], rb[bp + 1]], xt_ap, xt_reg, rows, gate_ap, gate_reg,
                      lng.t, lnb.t, lng.r, wk)
        k.release()

    def ffn(l, final):
        k.mark()
        g2s = k.buf([SR, D], F32, "g2s"); g2p = k.buf([128, D], F32, "g2p")
        k.dma("sp", g2s.t[:], sc_mods[l][:, 2 * D:3 * D], reads=[r_scm[l]], writes=[g2s.r])
        k.dma("sp", g2p.t[:], sc_g2p[l].partition_broadcast(128), reads=[r_scg[l]], writes=[g2p.r])
        hT = k.buf([128, 8, T + SR], BF16, "hT_all")
        ffp = k.buf([128, NFC, 4], F32, "ffp"); ust = k.buf([128, NFC, 2 * NS], F32, "ust")
        fo = k.buf([128, NFC, 2], F32, "fo"); fs = k.buf([128, NFC, NS, 2], F32, "fs")
        k.mark()
        mf = k.buf([SR, 2 * D], F32, "mf")
        k.dma("sp", mf.t[:], sc_mods[l][:, 0:2 * D], reads=[r_scm[l]], writes=[mf.r])
        p4 = k.buf([4, DFF], F32, "p4"); p32 = k.buf([2 * NS, DFF], F32, "p32")
        k.dma("sp", p4.t[:], ffp_d[l], writes=[p4.r])
        k.dma("sp", p32.t[:], sffn[l], writes=[p32.r])
        for c0 in range(0, NFC, 4):
            c1 = min(NFC, c0 + 4)
            for c in range(c0, c1):
                k.tr(bank(0, 128, (c - c0) * 4, (c - c0) * 4 + 4), p4.t[:, c * 128:(c + 1) * 128], ident[0:4, 0:4], reads=[p4.r, rconst],
                     writes=[rb[0]], accum=(c > c0))
                k.tr(bank(1, 128, (c - c0) * 32, (c - c0) * 32 + 32), p32.t[:, c * 128:(c + 1) * 128], ident[0:32, 0:32], reads=[p32.r, rconst],
                     writes=[rb[1]], accum=(c > c0))
            k.cp("act", ffp.t[:, c0:c1, :], bank(0, 128, 0, (c1 - c0) * 4).rearrange("p (c j) -> p c j", j=4), reads=[rb[0]], writes=[ffp.r], accum=(c0 > 0))
            k.cp("act", ust.t[:, c0:c1, :], bank(1, 128, 0, (c1 - c0) * 32).rearrange("p (c j) -> p c j", j=32), reads=[rb[1]], writes=[ust.r], accum=(c0 > 0))
        wkh = {"hs": k.pool(1, [SR, D], F32, "hs")}
        for t in range(NT):
            make_hT(x_all[:, t, :], rx[t], 128, hT.t[:, :, t * 128:(t + 1) * 128], hT.r, l, 3, pbanks=(2 + 2 * (t % 2), 3 + 2 * (t % 2)), first=(t == 0))
            k.ts("dve", x_all[:, t, :], x_all[:, t, :], ALPHA, None, ALU.mult, reads=[rx[t]], writes=[rx[t]])
        make_hT(x_s.t[:], x_s.r, SR, hT.t[:, :, T:T + SR], hT.r, l, 3, mods_s=mf.t, mods_reg=mf.r, wk=wkh, pbanks=(2, 3), first=False)
        k.ts("pool", x_s.t[:], x_s.t[:], ALPHA, None, ALU.mult, reads=[x_s.r], writes=[x_s.r])
        k.release()
        k.mark()
        G = 2
        stg = k.pool(2, [128, 2048], F32, "stg")
        wu_p = k.pool(2, [128, 8, G * 128], BF16, "wu"); wv_p = k.pool(2, [128, 8, G * 128], BF16, "wv")
        wds_p = k.pool(2, [128, G, D], BF16, "wds"); wdu_p = k.pool(1, [128, G, D], BF16, "wdu")
        UW = 2 + T + 6 * NS
        u_p = k.pool(2, [128, UW], F32, "u_sb"); a_p = k.pool(1, [128, UW], F32, "acc")
        gT_p = k.pool(1, [128, G, T + SR], BF16, "gT")
        vsb_p = k.pool(1, [128, T + SR], BF16, "vsb")
        nb = [0]

        for g in range(NFC // G):
            f0 = g * G * 128
            wu = wu_p.next(); wv = wv_p.next(); wds = wds_p.next(); wdu = wdu_p.next()
            load_w_bf16(wu.t[:], wu.r, ffn_up[l, :, f0:f0 + G * 128].rearrange("(kc p) n -> p kc n", p=128), stg.next())
            load_w_bf16(wv.t[:], wv.r, ffn_up[l, :, DFF + f0:DFF + f0 + G * 128].rearrange("(kc p) n -> p kc n", p=128), stg.next())
            st = stg.next()
            stv = st.t[:, 0:G * D].rearrange("p (a b) -> p a b", b=D)
            k.dma("sp", stv, ffn_down[l, f0:f0 + G * 128, :].rearrange("(c p) n -> p c n", p=128), writes=[st.r])
            k.cp("act", wdu.t[:], stv, reads=[st.r], writes=[wdu.r])
            k.tt("dve", wds.t[:], stv, bc(g2p.t[:], 1, G), ALU.mult, reads=[st.r, g2p.r], writes=[wds.r])
            gT = gT_p.next()
            for c in range(G):
                fc = g * G + c
                u = u_p.next(); acc = a_p.next()
                w0, w1, w2, bb = (ffp.t[:, fc, j:j + 1] for j in range(4))
                k.memset("pool", u.t[:, 0:2], 0.0, writes=[u.r])
                usv = u.t[:, 2 + T:UW].rearrange("p (s j) -> p s j", j=6)
                asv = acc.t[:, 2 + T:UW].rearrange("p (s j) -> p s j", j=6)
                k.cp("pool", usv[:, :, 0:2], ust.t[:, fc, :].rearrange("p (s j) -> p s j", j=2), reads=[ust.r], writes=[u.r], accum=True)
                vsb = vsb_p.next()
                for n in range(5):
                    ncol = 512 if n < 4 else SR
                    t0 = n * 512
                    nb[0] += 1
                    bu = nb[0] % 2; bv = 2 + nb[0] % 2
                    for kc in range(8):
                        k.mm(bank(bu, 128, 0, ncol), wu.t[:, kc, c * 128:(c + 1) * 128], hT.t[:, kc, t0:t0 + ncol], kc == 0, kc == 7,
                             reads=[wu.r, hT.r], writes=[rb[bu]])
                    for kc in range(8):
                        k.mm(bank(bv, 128, 0, ncol), wv.t[:, kc, c * 128:(c + 1) * 128], hT.t[:, kc, t0:t0 + ncol], kc == 0, kc == 7,
                             reads=[wv.r, hT.r], writes=[rb[bv]])
                    if n < 4:
                        k.cp("act", u.t[:, 2 + t0:2 + t0 + 512], bank(bu), reads=[rb[bu]], writes=[u.r], accum=True)
                    else:
                        k.cp("act", usv[:, :, 2:6], bank(bu, 128, 0, SR).rearrange("p (s j) -> p s j", j=4), reads=[rb[bu]], writes=[u.r], accum=True)
                    k.cp("act", vsb.t[:, t0:t0 + ncol], bank(bv, 128, 0, ncol), reads=[rb[bv]], writes=[vsb.r], accum=(n > 0))
                lo, hi = 2, UW
                k.act(acc.t[:, lo:hi], u.t[:, lo:hi], AF.Identity, reads=[u.r, ffp.r], writes=[acc.r], scale=w2, bias=bb)
                k.stt(acc.t[:, lo:hi], u.t[:, lo - 1:hi - 1], w1, acc.t[:, lo:hi], ALU.mult, ALU.add, reads=[u.r, acc.r, ffp.r], writes=[acc.r])
                k.stt(acc.t[:, lo:hi], u.t[:, lo - 2:hi - 2], w0, acc.t[:, lo:hi], ALU.mult, ALU.add, reads=[u.r, acc.r, ffp.r], writes=[acc.r])
                k.act(acc.t[:, lo:hi], acc.t[:, lo:hi], AF.Gelu, reads=[acc.r], writes=[acc.r])
                k.tt("dve", gT.t[:, c, 0:T], acc.t[:, 2:2 + T], vsb.t[:, 0:T], ALU.mult, reads=[acc.r, vsb.r], writes=[gT.r], accum=(c > 0))
                k.tt("dve", gT.t[:, c, T:T + SR].rearrange("p (s j) -> p s j", j=4), asv[:, :, 2:6],
                     vsb.t[:, T:T + SR].rearrange("p (s j) -> p s j", j=4), ALU.mult, reads=[acc.r, vsb.r], writes=[gT.r], accum=True)
                k.cp("pool", fo.t[:, fc, :], u.t[:, T:T + 2], reads=[u.r], writes=[fo.r], accum=True)
                k.cp("pool", fs.t[:, fc, :, :], usv[:, :, 4:6], reads=[u.r], writes=[fs.r], accum=True)
            for t in range(NT + 1):
                rows = 128 if t < NT else SR
                bp = 4 + 2 * (t % 2)
                wd = wds if t < NT else wdu
                for half in range(2):
                    for c in range(G):
                        k.mm(bank(bp + half, rows), gT.t[:, c, t * 128:t * 128 + rows], wd.t[:, c, half * 512:(half + 1) * 512],
                             c == 0, c == G - 1, reads=[gT.r, wd.r], writes=[rb[bp + half]])
                yv = ps[0:rows, bp * 512:bp * 512 + D]
                if t < NT:
                    k.tt("dve", x_all[:, t, :], x_all[:, t, :], yv, ALU.add, reads=[rx[t], rb[bp], rb[bp + 1]], writes=[rx[t]])
                else:
                    rs_ = a_p.next()
                    k.tt("dve", rs_.t[0:SR, 0:D], yv, g2s.t[:], ALU.mult, reads=[rb[bp], rb[bp + 1], g2s.r], writes=[rs_.r])
                    k.tt("dve", x_s.t[:], x_s.t[:], rs_.t[0:SR, 0:D], ALU.add, reads=[x_s.r, rs_.r], writes=[x_s.r])
        k.release()
        k.mark()
        fo_tok = k.buf([2, DFF], F32, "fo_tok"); fs_tok = k.buf([2 * NS, DFF], F32, "fs_tok")
        for c0 in range(0, NFC, 4):
            c1 = min(NFC, c0 + 4)
            for c in range(c0, c1):
                k.tr(bank(0, 2, (c - c0) * 128, (c - c0 + 1) * 128), fo.t[:, c, :], ident[:], reads=[fo.r, rconst], writes=[rb[0]], accum=(c > c0))
                k.tr(bank(1, 2 * NS, (c - c0) * 128, (c - c0 + 1) * 128), fs.t[:, c, :, :].rearrange("p s j -> p (s j)"), ident[:],
                     reads=[fs.r, rconst], writes=[rb[1]], accum=(c > c0))
            k.cp("act", fo_tok.t[:, c0 * 128:c1 * 128], bank(0, 2, 0, (c1 - c0) * 128), reads=[rb[0]], writes=[fo_tok.r], accum=(c0 > 0))
            k.cp("act", fs_tok.t[:, c0 * 128:c1 * 128], bank(1, 2 * NS, 0, (c1 - c0) * 128), reads=[rb[1]], writes=[fs_tok.r], accum=(c0 > 0))
        k.dma("sp", fpo[l], fo_tok.t[:], reads=[fo_tok.r], is_output=True)
        k.dma("sp", fso[l], fs_tok.t[:], reads=[fs_tok.r], is_output=True)
        lng, lnb = load_ln(2 * l + 1)
        stt_ = k.sb([128, NT + 1, 2, 6], F32, "ln_st")
        mvt = k.sb([128, NT + 1, 4], F32, "ln_mv")
        r_st = [Reg() for _ in range(NT + 1)]
        tiles = []
        for t in range(NT + 1):
            rows = 128 if t < NT else SR
            xt_ap, xt_reg = (x_all[:, t, :], rx[t]) if t < NT else (x_s.t[:], x_s.r)
            tiles.append((t, rows, xt_ap, xt_reg))
        for t, rows, xt_ap, xt_reg in tiles:
            xv = xt_ap.rearrange("p (c f) -> p c f", f=512)
            for c in range(2):
                k.op("dve", lambda e, t=t, c=c, rows=rows, xv=xv: e.bn_stats(stt_[0:rows, t, c, :], xv[:, c, :]), reads=[xt_reg], writes=[r_st[t]], accum=(c > 0))
            k.op("dve", lambda e, t=t, rows=rows: e.bn_aggr(mvt[0:rows, t, 0:2], stt_[0:rows, t, :, :]), reads=[r_st[t]], writes=[r_st[t]])
            k.ts("dve", mvt[0:rows, t, 2:3], mvt[0:rows, t, 1:2], LN_EPS, None, ALU.add, reads=[r_st[t]], writes=[r_st[t]])
        for t, rows, xt_ap, xt_reg in tiles:
            k.act(mvt[0:rows, t, 2:3], mvt[0:rows, t, 2:3], AF.Sqrt, reads=[r_st[t]], writes=[r_st[t]])
        for t, rows, xt_ap, xt_reg in tiles:
            k.op("dve", lambda e, t=t, rows=rows: e.reciprocal(mvt[0:rows, t, 2:3], mvt[0:rows, t, 2:3]), reads=[r_st[t]], writes=[r_st[t]])
            k.stt(mvt[0:rows, t, 3:4], mvt[0:rows, t, 0:1], -1.0, mvt[0:rows, t, 2:3], ALU.mult, ALU.mult, reads=[r_st[t]], writes=[r_st[t]])
        for t, rows, xt_ap, xt_reg in tiles:
            k.act(xt_ap, xt_ap, AF.Identity, reads=[xt_reg, r_st[t]], writes=[xt_reg], scale=mvt[0:rows, t, 2:3], bias=mvt[0:rows, t, 3:4])
        for t, rows, xt_ap, xt_reg in tiles:
            k.tt("dve", xt_ap, xt_ap, lng.t[0:rows, :], ALU.mult, reads=[xt_reg, lng.r], writes=[xt_reg])
            k.tt("dve", xt_ap, xt_ap, lnb.t[0:rows, :], ALU.add, reads=[xt_reg, lng.r], writes=[xt_reg])
            if final:
                k.dma("sp", yp[t * 128:(t + 1) * 128, :] if t < NT else ys, xt_ap, reads=[xt_reg], is_output=True)
        k.release()
        k.release()

    def conformer(mods_mix, g1p):
        k.mark()
        w1 = k.buf([128, 8, 2048], BF16, "w1"); w2 = k.buf([128, 8, D], BF16, "w2")
        cf = k.buf([128, 8, 36], F32, "cf")
        k.mark()
        stg = k.pool(2, [128, 8 * 512], F32, "stg")
        for g in range(4):
            load_w_bf16(w1.t[:, :, g * 512:(g + 1) * 512], w1.r, pw1[:, g * 512:(g + 1) * 512].rearrange("(kc p) n -> p kc n", p=128),
                        stg.next(), first=(g == 0))
        for g in range(2):
            load_w_bf16(w2.t[:, :, g * 512:(g + 1) * 512], w2.r, pw2[:, g * 512:(g + 1) * 512].rearrange("(kc p) n -> p kc n", p=128),
                        stg.next(), first=(g == 0))
        p36 = k.buf([36, D], F32, "p36")
        k.dma("sp", p36.t[:], cfp_d, writes=[p36.r])
        for c in range(8):
            k.tr(bank(0, 128, c * 36, c * 36 + 36), p36.t[:, c * 128:(c + 1) * 128], ident[0:36, 0:36], reads=[p36.r, rconst],
                 writes=[rb[0]], accum=(c > 0))
        k.cp("act", cf.t[:], bank(0, 128, 0, 288).rearrange("p (c j) -> p c j", j=36), reads=[rb[0]], writes=[cf.r])
        k.release()
        b2 = k.buf([128, D], F32, "b2")
        k.dma("sp", b2.t[:], b_pw2.partition_broadcast(128), writes=[b2.r])
        lng, lnb = load_ln(2)
        wk = ln_work(1)
        BS = 256
        NB = T // BS
        TPB = BS // 128
        GW = 34 * NS
        sconv_p = k.pool(1, [120, 4, 128], F32, "sconv_sb")
        carry = k.buf([128, 8, 30], F32, "carry")
        k.memset("pool", carry.t[:], 0.0, writes=[carry.r])
        hTb = k.buf([128, 8, BS], BF16, "hTb")
        yc = k.buf([128, 8, BS], F32, "yc")
        sT = k.buf([128, 8, BS], BF16, "sT")
        glu_p = k.pool(2, [128, GW], F32, "glu"); sig_p = k.pool(2, [128, BS], F32, "sig")
        glub_p = k.pool(2, [128, GW], BF16, "glub")
        dg_p = k.pool(2, [128, 31, 128], BF16, "dg")
        ycb_p = k.pool(2, [128, BS], BF16, "ycb"); ysq_p = k.pool(2, [128, BS], BF16, "ysq")
        mean = k.buf([128, BS], F32, "mean"); rstd = k.buf([128, BS], F32, "rstd"); xn_p = k.pool(2, [128, BS], F32, "cxn")
        gnew = k.buf([128, 8, SR], F32, "gnew")
        for B in range(NB + 1):
            prompt = B < NB
            ncol = BS if prompt else SR
            if prompt:
                for j in range(TPB):
                    t = TPB * B + j
                    make_hT(x_all[:, t, :], rx[t], 128, hTb.t[:, :, j * 128:(j + 1) * 128], hTb.r, 1, 0, pbanks=(2, 3), first=(j == 0))
            else:
                make_hT(x_s.t[:], x_s.r, SR, hTb.t[:, :, 0:SR], hTb.r, 1, 0, mods_s=mods_mix.t, mods_reg=mods_mix.r, wk=wk, pbanks=(2, 3))
            NO = BS if prompt else GW - 30
            s1b = 6 if prompt else 0
            for c in range(8):
                for kc in range(8):
                    k.mm(bank(4, 128, 0, ncol), w1.t[:, kc, c * 128:(c + 1) * 128], hTb.t[:, kc, 0:ncol], kc == 0, kc == 7,
                         reads=[w1.r, hTb.r], writes=[rb[4]])
                for kc in range(8):
                    k.mm(bank(5, 128, 0, ncol), w1.t[:, kc, D + c * 128:D + (c + 1) * 128], hTb.t[:, kc, 0:ncol], kc == 0, kc == 7,
                         reads=[w1.r, hTb.r], writes=[rb[5]])
                sig = sig_p.next(); glu = glu_p.next()
                k.act(sig.t[:, 0:ncol], bank(5, 128, 0, ncol), AF.Sigmoid, reads=[rb[5], cf.r], writes=[sig.r], bias=cf.t[:, c, 35:36])
                if prompt:
                    k.cp("pool", glu.t[:, 0:30], carry.t[:, c, :], reads=[carry.r], writes=[glu.r])
                    k.stt(glu.t[:, 30:30 + BS], bank(4, 128, 0, BS), cf.t[:, c, 34:35], sig.t[:, 0:BS], ALU.add, ALU.mult,
                          reads=[rb[4], cf.r, sig.r], writes=[glu.r], accum=True)
                    k.cp("pool", carry.t[:, c, :], glu.t[:, BS:BS + 30], reads=[glu.r], writes=[carry.r])
                else:
                    gv = glu.t[:, 0:GW].rearrange("p (s j) -> p s j", j=34)
                    scv = sconv_p.next()
                    for q in range(4):
                        k.dma("sp", scv.t[:, q, :], sconv[q * 120:(q + 1) * 120, c * 128:(c + 1) * 128], writes=[scv.r], accum=(q > 0))
                    for q in range(4):
                        k.tr(bank(6, 128, q * 120, (q + 1) * 120), scv.t[:, q, :], ident[0:120, 0:120],
                             reads=[scv.r, rconst], writes=[rb[6]], accum=(q > 0))
                    k.cp("act", gv[:, :, 0:30], bank(6, 128, 0, 480).rearrange("p (s j) -> p s j", j=30), reads=[rb[6]], writes=[glu.r])
                    k.stt(gv[:, :, 30:34], bank(4, 128, 0, SR).rearrange("p (s j) -> p s j", j=4), cf.t[:, c, 34:35],
                          sig.t[:, 0:SR].rearrange("p (s j) -> p s j", j=4), ALU.add, ALU.mult, reads=[rb[4], cf.r, sig.r], writes=[glu.r], accum=True)
                    k.cp("pool", gnew.t[:, c, :].rearrange("p (s j) -> p s j", j=4), gv[:, :, 30:34], reads=[glu.r], writes=[gnew.r], accum=(c > 0))
                dg = dg_p.next()
                k.tt("dve", dg.t[:], bc(identb_t[:], 1, 31), bc(cf.t[:, c, 0:31], 2, 128), ALU.mult, reads=[rconst, cf.r], writes=[dg.r])
                glub = glub_p.next()
                W = 30 + BS if prompt else GW
                k.cp("act", glub.t[:, 0:W], glu.t[:, 0:W], reads=[glu.r], writes=[glub.r])
                cb = (c % 2) if prompt else 1
                for j in range(31):
                    if prompt:
                        rhs = glub.t[:, j:j + BS]
                    else:
                        rhs = glub.t[:, 0:GW].rearrange("p (s j) -> p s j", j=34)[:, :, j:j + 4]
                    k.mm(bank(cb, 128, 0, ncol), dg.t[:, j, :], rhs, j == 0, j == 30, reads=[dg.r, glub.r], writes=[rb[cb]])
                k.act(yc.t[:, c, 0:ncol], bank(cb, 128, 0, ncol), AF.Identity, reads=[rb[cb], cf.r], writes=[yc.r], accum=(c > 0),
                      bias=cf.t[:, c, 31:32])
                ycb = ycb_p.next(); ysq = ysq_p.next()
                k.cp("dve", ycb.t[:, 0:ncol], yc.t[:, c, 0:ncol], reads=[yc.r], writes=[ycb.r])
                k.act(ysq.t[:, 0:ncol], yc.t[:, c, 0:ncol], AF.Square, reads=[yc.r], writes=[ysq.r])
                k.mm(bank(s1b, 128, 0, ncol), onesb[:], ycb.t[:, 0:ncol], c == 0, c == 7, reads=[rconst, ycb.r], writes=[rb[s1b]], inc=True)
                k.mm(bank(7, 128, 0, ncol), onesb[:], ysq.t[:, 0:ncol], c == 0, c == 7, reads=[rconst, ysq.r], writes=[rb[7]], inc=True)
            k.act(mean.t[:, 0:ncol], bank(s1b, 128, 0, ncol), AF.Identity, reads=[rb[s1b]], writes=[mean.r], scale=1.0 / D)
            k.tt("dve", rstd.t[:, 0:ncol], mean.t[:, 0:ncol], mean.t[:, 0:ncol], ALU.mult, reads=[mean.r], writes=[rstd.r])
            k.stt(rstd.t[:, 0:ncol], bank(7, 128, 0, ncol), 1.0 / D, rstd.t[:, 0:ncol], ALU.mult, ALU.subtract, reads=[rb[7], rstd.r], writes=[rstd.r])
            k.ts("dve", rstd.t[:, 0:ncol], rstd.t[:, 0:ncol], LN_EPS, None, ALU.add, reads=[rstd.r], writes=[rstd.r])
            k.act(rstd.t[:, 0:ncol], rstd.t[:, 0:ncol], AF.Sqrt, reads=[rstd.r], writes=[rstd.r])
            k.op("dve", lambda e, ncol=ncol: e.reciprocal(rstd.t[:, 0:ncol], rstd.t[:, 0:ncol]), reads=[rstd.r], writes=[rstd.r])
            for c in range(8):
                xn = xn_p.next()
                k.tt("dve", xn.t[:, 0:ncol], yc.t[:, c, 0:ncol], mean.t[:, 0:ncol], ALU.subtract, reads=[yc.r, mean.r], writes=[xn.r])
                k.tt("dve", xn.t[:, 0:ncol], xn.t[:, 0:ncol], rstd.t[:, 0:ncol], ALU.mult, reads=[xn.r, rstd.r], writes=[xn.r])
                k.act(sT.t[:, c, 0:ncol], xn.t[:, 0:ncol], AF.Silu, reads=[xn.r, cf.r], writes=[sT.r], accum=(c > 0),
                      scale=cf.t[:, c, 32:33], bias=cf.t[:, c, 33:34])
            for j in range(TPB if prompt else 1):
                rows = 128 if prompt else SR
                t = TPB * B + j
                bp = 4 * (j % 2) if prompt else 2
                for half in range(2):
                    for c in range(8):
                        k.mm(bank(bp + half, rows), sT.t[:, c, j * 128:j * 128 + rows], w2.t[:, c, half * 512:(half + 1) * 512], c == 0, c == 7,
                             reads=[sT.r, w2.r], writes=[rb[bp + half]])
                if prompt:
                    xt_ap, xt_reg, gate_ap, gate_reg = x_all[:, t, :], rx[t], g1p.t[:], g1p.r
                else:
                    xt_ap, xt_reg, gate_ap, gate_reg = x_s.t[:], x_s.r, mods_mix.t[:, 2 * D:3 * D], mods_mix.r
                post_norm(ps[0:rows, bp * 512:bp * 512 + D], [rb[bp], rb[bp + 1]], xt_ap, xt_reg, rows, gate_ap, gate_reg,
                          lng.t, lnb.t, lng.r, wk, bias_ap=b2.t, bias_reg=b2.r)
            if B == NB - 1:
                cp_tok = wk["xn"].next()
                for c in range(8):
                    k.tr(bank(2 + c // 4, 30, (c % 4) * 128, (c % 4 + 1) * 128), carry.t[:, c, :], ident[:], reads=[carry.r, rconst],
                         writes=[rb[2 + c // 4]], accum=(c % 4 > 0))
                k.cp("act", cp_tok.t[0:30, :], ps[0:30, 2 * 512:2 * 512 + D], reads=[rb[2], rb[3]], writes=[cp_tok.r])
                k.dma("sp", cpo, cp_tok.t[0:30, :], reads=[cp_tok.r], is_output=True)
        r_cso = Reg()
        k.dma("sp", cso.rearrange("(s j) f -> s j f", j=30)[:, 0:26, :], sconv.rearrange("(s j) f -> s j f", j=30)[:, 4:30, :],
              reads=[], writes=[r_cso], is_output=True)
        cs_tok = wk["xn"].next()
        for c in range(8):
            k.tr(bank(2 + c // 4, SR, (c % 4) * 128, (c % 4 + 1) * 128), gnew.t[:, c, :], ident[:], reads=[gnew.r, rconst],
                 writes=[rb[2 + c // 4]], accum=(c % 4 > 0))
        k.cp("act", cs_tok.t[0:SR, :], ps[0:SR, 2 * 512:2 * 512 + D], reads=[rb[2], rb[3]], writes=[cs_tok.r])
        for sq in range(NS):
            k.dma("sp", cso[sq * 30 + 26:sq * 30 + 30, :], cs_tok.t[sq * 4:sq * 4 + 4, :], reads=[cs_tok.r], is_output=True)
        k.release()

    k.limit = k.sb_top
    k.mark()
    rdm = cload([128, 4, 128], rdm_d); rqd = cload([128, 4, 128], rqd_d); rkd = cload([128, 4], rkd_d)
    rdms = cload([64, 4, 64], rdms_d); rqds = cload([128, 4, 64], rqds_d); rkds = cload([64, 4], rkds_d)
    blk = cload([64, 16], blk_d); tri = cload([128, 128], tri_d); nmask = cload([64, 16, 16], nmask_d)
    idx = k.buf([128, NS * 16], I32)
    pti = cload([128, NS * 16], ptd.partition_broadcast(128), dt=I32)
    idxf = k.sb([128, NS * 16], F32)
    k.cp("dve", idxf[:], pti[:], reads=[rconst], writes=[idx.r])
    k.stt(idxf[:], idxf[:], 128.0, iota[:].broadcast_to([128, NS * 16]), ALU.mult, ALU.add, reads=[rconst, idx.r], writes=[idx.r])
    k.cp("dve", idx.t[:], idxf[:], reads=[idx.r], writes=[idx.r])
    mods_mix0 = k.buf([SR, 3 * D], F32, "mods_mix")
    g1p0 = k.buf([128, D], F32, "g1p")
    with nc.named_scope("adaln0"):
        adaln(0, mods_mix0, g1p0)
    omT = k.buf([128, 4, T + SR], BF16, "omT")
    orT = k.buf([128, 4, T + SR], BF16, "orT")
    with nc.named_scope("passM"):
        pass_moba(mods_mix0, omT)
    if stage >= 4:
        with nc.named_scope("passR"):
            pass_ret(mods_mix0, orT)
    k.limit = XOFF
    if stage >= 5:
        with nc.named_scope("passO"):
            pass_out(mods_mix0, g1p0, orT, omT)
    k.release()
    if stage >= 6:
        with nc.named_scope("ffn0"):
            ffn(0, final=False)
    if stage >= 7:
        k.mark()
        mods_mix1 = k.buf([SR, 3 * D], F32, "mods_mix")
        g1p1 = k.buf([128, D], F32, "g1p")
        with nc.named_scope("adaln1"):
            adaln(1, mods_mix1, g1p1)
        with nc.named_scope("conformer"):
            conformer(mods_mix1, g1p1)
        k.release()
    if stage >= 8:
        with nc.named_scope("ffn1"):
            ffn(1, final=True)
    k.finish()
    print("instructions:", k.ninst, "sems:", k.nsem, "sbuf_off:", k.sb_off)
    return nc


def _consts():
    f32 = np.float32
    c = {}
    c["ident"] = np.eye(128, dtype=f32)
    c["iota"] = np.arange(128, dtype=f32).reshape(128, 1)
    theta = f32(10000.0)
    inv_m = (theta ** (-np.arange(0, 128, 2, dtype=f32) / f32(128))).astype(f32)
    inv_r = (f32(1.0) / (theta ** np.linspace(0.0, 1.0, 64, dtype=f32))).astype(f32)
    rot = np.zeros((17, 128, 4, 128), f32)
    for t in range(17):
        if t < 16:
            pos = (t * 128 + np.arange(128)).astype(f32)
        else:
            pos = (2048 + (np.arange(128) % 4)).astype(f32)
        am = (pos[:, None] * inv_m[None, :]).astype(f32)
        ar = (pos[:, None] * inv_r[None, :]).astype(f32)
        cm, sm = np.cos(am).astype(f32), np.sin(am).astype(f32)
        cr, sr = np.cos(ar).astype(f32), np.sin(ar).astype(f32)
        rot[t, :, 0, 0::2] = cr; rot[t, :, 0, 1::2] = cr
        rot[t, :, 1, 0::2] = -sr; rot[t, :, 1, 1::2] = sr
        rot[t, :, 2, 0:64] = cm; rot[t, :, 2, 64:128] = cm
        rot[t, :, 3, 0:64] = -sm; rot[t, :, 3, 64:128] = sm
    c["rot"] = rot
    lg = np.array(LOGG, dtype=np.float64)
    i = np.arange(128, dtype=np.float64)
    rdm = np.zeros((128, 4, 128), np.float64)
    for h in range(4):
        diff = i[None, :] - i[:, None]
        rdm[:, h, :] = np.where(diff >= 0, np.exp(np.maximum(diff, 0) * lg[h]), 0.0) * SCALE
    c["rdm"] = rdm.astype(f32)
    c["rqd"] = np.broadcast_to(np.exp((i[None, None, :] + 1.0) * lg[None, :, None]), (128, 4, 128)).astype(f32).copy()
    c["rkd"] = (np.exp((127.0 - i)[:, None] * lg[None, :]) * SCALE).astype(f32)
    r = np.arange(64)
    seq, ii = r // 4, (r % 4).astype(np.float64)
    rdms = np.zeros((64, 4, 64), np.float64)
    for h in range(4):
        diff = ii[None, :] - ii[:, None]
        same = seq[None, :] == seq[:, None]
        rdms[:, h, :] = np.where(same & (diff >= 0), np.exp(np.maximum(diff, 0) * lg[h]), 0.0) * SCALE
    c["rdms"] = rdms.astype(f32)
    c["rqds"] = np.broadcast_to(np.exp((ii[None, None, :] + 1.0) * lg[None, :, None]), (128, 4, 64)).astype(f32).copy()
    c["rkds"] = (np.exp((3.0 - ii)[:, None] * lg[None, :]) * SCALE).astype(f32)
    blk = np.zeros((64, 16), f32)
    blk[r, seq] = 1.0
    c["blk"] = blk
    tri = np.where(np.arange(128)[None, :] <= np.arange(128)[:, None], 0.0, NEG).astype(f32)
    c["tri"] = tri
    nm = np.full((64, 16, 16), NEG, f32)
    for b in range(16):
        for tq in range(4):
            for kk in range(tq + 1):
                nm[b * 4 + kk, b, tq::4] = 0.0
    c["nmask"] = nm
    Ep = np.zeros((18, 128), f32); Ep[0, :] = 1.0; Ep[17, :] = 1.0
    Es = np.zeros((18, 64), f32); Es[17, :] = 1.0
    for s in range(16):
        Es[1 + s, 4 * s:4 * s + 4] = 1.0
    ep = np.zeros((18, 1), f32); ep[0, 0] = 1.0; ep[17, 0] = 1.0
    c["Ep"], c["Es"], c["ep"] = Ep, Es, ep
    return c


def make_in_maps(x_prompt, x_sample, cache_k, cache_v, state_ret, state_conv, state_ffn, page_table, c_prompt, c_sample,
                 ab_w_in, ab_w_out, cf_w_pw1, cf_b_pw1, cf_w_dw, cf_b_dw, cf_ln_g, cf_ln_b, cf_w_pw2, cf_b_pw2,
                 ffn_w_up, ffn_w_dw, ffn_b_dw, ffn_w_down, ada_w, ada_b, ln_g, ln_b):
    A = lambda a: np.ascontiguousarray(np.asarray(a))
    consts = _consts()
    ck = A(cache_k).reshape(-1, 512)
    cv = A(cache_v).reshape(-1, 512)
    cfp = A(np.concatenate([np.asarray(cf_w_dw)[0], np.asarray(cf_b_dw)[0][None], np.asarray(cf_ln_g)[0][None],
                            np.asarray(cf_ln_b)[0][None], np.asarray(cf_b_pw1)[0].reshape(2, D)], axis=0))
    ffp = A(np.concatenate([np.asarray(ffn_w_dw), np.asarray(ffn_b_dw)[:, None, :]], axis=1))
    shared = {
        "ck": ck, "cv": cv, "w_in": A(ab_w_in)[0], "w_out": A(ab_w_out)[0], "pw1": A(cf_w_pw1)[0], "cfp": cfp,
        "pw2": A(cf_w_pw2)[0], "b_pw2": A(cf_b_pw2).reshape(1, D), "ffn_up": A(ffn_w_up), "ffp": ffp,
        "ffn_down": A(ffn_w_down), "ada_w": A(ada_w), "ada_b": A(ada_b), "ln_g": A(ln_g).reshape(4, D),
        "ln_b": A(ln_b).reshape(4, D),
    }
    shared.update(consts)
    maps = []
    for c in range(NCORES):
        s0, s1 = c * NS, (c + 1) * NS
        m = dict(shared)
        m["xp"] = A(x_prompt[c])
        m["xs"] = A(np.asarray(x_sample)[s0:s1].reshape(SR, D))
        m["call"] = A(np.concatenate([np.asarray(c_prompt)[c:c + 1], np.asarray(c_sample)[s0:s1]], axis=0))
        m["pt"] = A(np.asarray(page_table)[s0:s1].reshape(1, NS * 16).astype(np.int32))
        m["sret"] = A(np.asarray(state_ret)[0, s0:s1].reshape(NS * 512, 128))
        m["sconv"] = A(np.asarray(state_conv)[0, s0:s1].reshape(NS * 30, D))
        m["sffn"] = A(np.asarray(state_ffn)[:, s0:s1].reshape(2, NS * 2, DFF))
        maps.append(m)
    return maps


def assemble(results):
    R = results
    cat = lambda name: [r[name] for r in R]
    y_prompt = np.stack(cat("yp"), 0)
    y_sample = np.concatenate(cat("ys"), 0).reshape(128, 4, D)
    k_prompt = np.stack(cat("kp"), 0).reshape(1, 8, T, 4, 128)
    v_prompt = np.stack(cat("vp"), 0).reshape(1, 8, T, 4, 128)
    k_sample = np.concatenate(cat("ks"), 0).reshape(1, 128, 4, 4, 128)
    v_sample = np.concatenate(cat("vs"), 0).reshape(1, 128, 4, 4, 128)
    ret_prompt = np.stack(cat("rpo"), 0).reshape(1, 8, 4, 128, 128)
    ret_sample = np.concatenate(cat("rso"), 0).reshape(1, 128, 4, 128, 128)
    conv_prompt = np.stack(cat("cpo"), 0).reshape(1, 8, 30, D)
    conv_sample = np.concatenate(cat("cso"), 0).reshape(1, 128, 30, D)
    ffn_prompt = np.stack(cat("fpo"), 1).reshape(2, 8, 2, DFF)
    ffn_sample = np.concatenate([r["fso"].reshape(2, NS, 2, DFF) for r in R], 1)
    outs = (y_prompt, y_sample, k_prompt, v_prompt, k_sample, v_sample, ret_prompt, ret_sample,
            conv_prompt, conv_sample, ffn_prompt, ffn_sample)
    return tuple(np.ascontiguousarray(o, dtype=np.float32) for o in outs)


def kernel(**inputs):
    nc = build()
    maps = make_in_maps(**inputs)
    res = run_bass_kernel_spmd(nc, maps, core_ids=list(range(NCORES)))
    return assemble(res.results)
```

```python
import math
import numpy as np
import ml_dtypes
import concourse.bass as bass
import concourse.mybir as mybir
from concourse.bass_utils import run_bass_kernel_spmd

F32 = mybir.dt.float32
BF16 = mybir.dt.bfloat16
I32 = mybir.dt.int32
ALU = mybir.AluOpType
AF = mybir.ActivationFunctionType
AX = mybir.AxisListType

NCORES = 8
D = 1024
T = 2048
NT = 16
NS = 16
SR = 64
DFF = 2816
NFC = 22
ALPHA = 4.0 ** 0.25
LN_EPS = 1e-5
GN_EPS = 1e-6
SCALE = 128.0 ** -0.5
NEG = -1.0e30
LOGG = [math.log1p(-2.0 ** (-5.0 - h)) for h in range(4)]


class Reg:
    __slots__ = ("w", "r", "p", "dsem", "dcnt", "psum")

    def __init__(self, psum=False):
        self.psum = psum
        self.w = {}
        self.r = {}
        self.p = {}
        self.dsem = None
        self.dcnt = 0


class Buf:
    __slots__ = ("t", "r")

    def __init__(self, t):
        self.t = t
        self.r = Reg()


class Pool:
    def __init__(self, bufs):
        self.bufs = bufs
        self.i = 0

    def next(self):
        b = self.bufs[self.i % len(self.bufs)]
        self.i += 1
        return b


def _merge(dst, src):
    for s, v in src.items():
        if dst.get(s, 0) < v:
            dst[s] = v


class KB:
    def __init__(self, nc):
        self.nc = nc
        self.eng = {"pe": nc.tensor, "act": nc.scalar, "dve": nc.vector, "pool": nc.gpsimd, "sp": nc.sync}
        self.esem = {e: nc.alloc_semaphore("es_" + e) for e in ("pe", "act", "dve", "pool")}
        self.ecnt = {e: 0 for e in self.esem}
        self.seen = {e: {} for e in self.eng}
        self.nsem = 4
        self.out_toks = {}
        self.sb_off = (nc.sbuf_base + 63) // 64 * 64
        self.sb_top = nc.sbuf_top
        self.sb_marks = []
        self.uid = 0
        self.ninst = 0
        self.anchors = []
        self.limit = self.sb_top

    def barrier(self):
        for e, E in self.eng.items():
            seen = self.seen[e]
            for e2, s in self.esem.items():
                v = self.ecnt[e2]
                if v > seen.get(s, 0) and not (e == "pe" and e2 == "pe"):
                    E.wait_ge(s, v)
                    seen[s] = v
                    self.ninst += 1
            for a in self.anchors:
                if a.dcnt > seen.get(a.dsem, 0):
                    E.wait_ge(a.dsem, a.dcnt)
                    seen[a.dsem] = a.dcnt
                    self.ninst += 1

    def sb(self, shape, dtype, name=None):
        self.uid += 1
        nm = (name or "t") + "_%d" % self.uid
        esz = 2 if dtype == BF16 else 4
        n = 1
        for s in shape[1:]:
            n *= s
        nbytes = (n * esz + 63) // 64 * 64
        off = self.sb_off
        self.sb_off += nbytes
        assert self.sb_off <= self.limit, "SBUF overflow %d > %d (%s)" % (self.sb_off, self.limit, nm)
        return self.nc.alloc_sbuf_tensor_at(nm, list(shape), dtype, offset=off)

    def buf(self, shape, dtype, name=None):
        return Buf(self.sb(shape, dtype, name))

    def pool(self, n, shape, dtype, name=None):
        return Pool([self.buf(shape, dtype, name) for _ in range(n)])

    def mark(self):
        self.sb_marks.append(self.sb_off)

    def release(self):
        self.sb_off = self.sb_marks.pop()
        self.barrier()

    def _waits(self, eng, reads, writes, accum):
        need = {}
        mysem0 = self.esem.get(eng)
        for r in reads:
            _merge(need, r.w)
            if r.psum:
                for s_, v_ in r.r.items():
                    if s_ is not mysem0 and need.get(s_, 0) < v_:
                        need[s_] = v_
        for w in writes:
            if accum and not w.r:
                _merge(need, w.p)
            else:
                _merge(need, w.r)
                _merge(need, w.w)
        E = self.eng[eng]
        mysem = self.esem.get(eng)
        seen = self.seen[eng]
        for s, v in need.items():
            if eng == "pe" and s is mysem:
                continue
            if seen.get(s, 0) >= v:
                continue
            E.wait_ge(s, v)
            self.ninst += 1
            seen[s] = v

    def _update(self, tok, reads, writes, accum):
        s, v = tok
        for r in reads:
            if r.r.get(s, 0) < v:
                r.r[s] = v
        for w in writes:
            if accum and not w.r:
                if w.w.get(s, 0) < v:
                    w.w[s] = v
            else:
                p = dict(w.w)
                _merge(p, w.r)
                w.p = p
                w.w = {s: v}
                w.r = {}

    def op(self, eng, fn, reads=(), writes=(), accum=False, inc=True):
        self._waits(eng, reads, writes, accum)
        ins = fn(self.eng[eng])
        self.ninst += 1
        if inc:
            self.ecnt[eng] += 1
            ins.then_inc(self.esem[eng], 1)
        tok = (self.esem[eng], self.ecnt[eng] + (0 if inc else 1))
        self._update(tok, reads, writes, accum)
        return ins

    def _dma_any(self, q, mk, reads, writes, anchor, accum, is_output):
        if anchor is None:
            anchor = writes[0] if writes else reads[0]
        if anchor.dsem is None:
            anchor.dsem = self.nc.alloc_semaphore("ds_%d" % self.nsem)
            self.nsem += 1
            self.anchors.append(anchor)
        self._waits(q, reads, writes, accum)
        ins = mk(self.eng[q])
        self.ninst += 1
        anchor.dcnt += 16
        ins.then_inc(anchor.dsem, 16)
        tok = (anchor.dsem, anchor.dcnt)
        self._update(tok, reads, writes, accum)
        if is_output:
            self.out_toks[anchor.dsem] = anchor.dcnt
        return ins

    def dma(self, q, out, in_, reads=(), writes=(), anchor=None, accum=False, is_output=False, **kw):
        return self._dma_any(q, lambda e: e.dma_start(out=out, in_=in_, **kw), reads, writes, anchor, accum, is_output)

    def gather(self, out, table, idx_ap, reads=(), writes=(), accum=False):
        return self._dma_any(
            "pool",
            lambda e: e.indirect_dma_start(out=out, out_offset=None, in_=table,
                                           in_offset=bass.IndirectOffsetOnAxis(ap=idx_ap, axis=0)),
            reads, writes, None, accum, False)

    def finish(self):
        sp = self.eng["sp"]
        for s, v in self.out_toks.items():
            sp.wait_ge(s, v)
        for e, s in self.esem.items():
            if self.ecnt[e] > 0:
                sp.wait_ge(s, self.ecnt[e])

    def mm(self, out, lhsT, rhs, start, stop, reads=(), writes=(), inc=None):
        return self.op("pe", lambda e: e.matmul(out, lhsT, rhs, start=start, stop=stop),
                       reads, writes, accum=not start, inc=(stop if inc is None else inc))

    def tr(self, out, in_, ident, reads=(), writes=(), accum=False):
        return self.op("pe", lambda e: e.transpose(out, in_, ident), reads, writes, accum=accum)

    def act(self, out, in_, func, reads=(), writes=(), accum=False, **kw):
        return self.op("act", lambda e: e.activation(out, in_, func, **kw), reads, writes, accum=accum)

    def tt(self, eng, out, in0, in1, op, reads=(), writes=(), accum=False):
        return self.op(eng, lambda e: e.tensor_tensor(out, in0, in1, op), reads, writes, accum=accum)

    def ts(self, eng, out, in0, s1, s2, op0, op1=None, reads=(), writes=(), accum=False):
        if op1 is None:
            return self.op(eng, lambda e: e.tensor_scalar(out, in0, s1, None, op0), reads, writes, accum=accum)
        return self.op(eng, lambda e: e.tensor_scalar(out, in0, s1, s2, op0, op1), reads, writes, accum=accum)

    def stt(self, out, in0, scalar, in1, op0, op1, reads=(), writes=(), accum=False):
        return self.op("dve", lambda e: e.scalar_tensor_tensor(out, in0, scalar, in1, op0, op1), reads, writes, accum=accum)

    def cp(self, eng, out, in_, reads=(), writes=(), accum=False):
        if eng == "act":
            return self.op("act", lambda e: e.copy(out, in_), reads, writes, accum=accum)
        return self.op(eng, lambda e: e.tensor_copy(out, in_), reads, writes, accum=accum)

    def memset(self, eng, ap, val, writes=(), accum=False):
        return self.op(eng, lambda e: e.memset(ap, val), (), writes, accum=accum)


def bc(ap, axis, n):
    a = ap.unsqueeze(axis)
    shp = list(a.shape)
    shp[axis] = n
    return a.broadcast_to(shp)


def build(stage=99, nphys=2560, skip_ms=False):
    nc = bass.Bass("TRN2", target_bir_lowering=False)
    k = KB(nc)

    def din(name, shape, dt=F32):
        return nc.dram_tensor(name, list(shape), dt, kind="ExternalInput").ap()

    def dout(name, shape):
        return nc.dram_tensor(name, list(shape), F32, kind="ExternalOutput").ap()

    xp = din("xp", [T, D]); xs_d = din("xs", [SR, D]); call = din("call", [17, D])
    ck = din("ck", [nphys * 128, 512]); cv = din("cv", [nphys * 128, 512])
    ptd = din("pt", [1, NS * 16], I32)
    sret = din("sret", [NS * 4 * 128, 128]); sconv = din("sconv", [NS * 30, D]); sffn = din("sffn", [2, NS * 2, DFF])
    w_in = din("w_in", [D, 3584]); w_out = din("w_out", [D, D]); pw1 = din("pw1", [D, 2048])
    cfp_d = din("cfp", [36, D]); pw2 = din("pw2", [D, D]); b_pw2 = din("b_pw2", [1, D])
    ffn_up = din("ffn_up", [2, D, 2 * DFF]); ffp_d = din("ffp", [2, 4, DFF]); ffn_down = din("ffn_down", [2, DFF, D])
    ada_w = din("ada_w", [2, D, 6 * D]); ada_b = din("ada_b", [2, 6 * D])
    ln_g = din("ln_g", [4, D]); ln_b = din("ln_b", [4, D])
    ident_d = din("ident", [128, 128]); iota_d = din("iota", [128, 1])
    rot_d = din("rot", [17, 128, 4, 128])
    rdm_d = din("rdm", [128, 4, 128]); rqd_d = din("rqd", [128, 4, 128]); rkd_d = din("rkd", [128, 4])
    rdms_d = din("rdms", [64, 4, 64]); rqds_d = din("rqds", [128, 4, 64]); rkds_d = din("rkds", [64, 4])
    blk_d = din("blk", [64, 16]); tri_d = din("tri", [128, 128]); nmask_d = din("nmask", [64, 16, 16])
    Ep_d = din("Ep", [18, 128]); Es_d = din("Es", [18, 64]); ep_d = din("ep", [18, 1])

    yp = dout("yp", [T, D]); ys = dout("ys", [SR, D])
    kp = dout("kp", [T, 512]); vp = dout("vp", [T, 512]); ks = dout("ks", [SR, 512]); vs = dout("vs", [SR, 512])
    rpo = dout("rpo", [512, 128]); rso = dout("rso", [NS * 512, 128])
    cpo = dout("cpo", [30, D]); cso = dout("cso", [NS * 30, D])
    fpo = dout("fpo", [2, 2, DFF]); fso = dout("fso", [2, NS * 2, DFF])

    sc_mods = [nc.dram_tensor("sc_mods%d" % l, [SR, 3 * D], F32).ap() for l in range(2)]
    sc_g2p = [nc.dram_tensor("sc_g2p%d" % l, [1, D], F32).ap() for l in range(2)]
    r_scm = [Reg(), Reg()]
    r_scg = [Reg(), Reg()]

    ps = nc.alloc_psum_tensor("ps", [128, 4096], F32)
    psb = ps[:].bitcast(BF16)
    rb = [Reg(psum=True) for _ in range(8)]

    def bank(i, rows=128, c0=0, c1=512):
        return ps[0:rows, i * 512 + c0:i * 512 + c1]

    def bankb(i, rows=128, c0=0, c1=1024):
        return psb[0:rows, i * 1024 + c0:i * 1024 + c1]

    rconst = Reg()

    def cload(shape, src, dt=F32, q="sp"):
        t = k.sb(shape, dt)
        k.dma(q, t[:], src, writes=[rconst], anchor=rconst, accum=True)
        return t

    ident = cload([128, 128], ident_d)
    iota = cload([128, 1], iota_d)
    Ep = cload([18, 128], Ep_d); Es = cload([18, 64], Es_d); ep = cload([18, 1], ep_d)
    identb_t = k.sb([128, 128], BF16)
    onesb = k.sb([128, 128], BF16)
    onesf = k.sb([128, 128], F32)
    k.memset("pool", onesf[:], 1.0, writes=[rconst], accum=True)
    k.cp("dve", identb_t[:], ident[:], reads=[rconst], writes=[rconst], accum=True)
    k.memset("pool", onesb[:], 1.0, writes=[rconst], accum=True)
    XOFF = (k.sb_top - NT * D * 4) // 64 * 64
    x_all = nc.alloc_sbuf_tensor_at("x_all", [128, NT, D], F32, offset=XOFF)
    rx = [Reg() for _ in range(NT)]
    x_s = k.buf([SR, D], F32)
    k.dma("sp", x_s.t[:], xs_d, writes=[x_s.r])
    modT = k.buf([128, 2, 6, 8], F32)
    scT = k.buf([128, 8, 32], BF16)
    k.mark()
    callsb = cload([17, D], call)
    scs = k.buf([17, D], F32)
    k.act(scs.t[:], callsb[:], AF.Silu, reads=[rconst], writes=[scs.r])
    for c in range(8):
        k.tr(bank(0, 128, c * 32, c * 32 + 17), scs.t[0:17, c * 128:(c + 1) * 128], ident[0:17, 0:17],
             reads=[scs.r, rconst], writes=[rb[0]], accum=(c > 0))
    k.cp("dve", scT.t[:, :, 0:17], bank(0, 128, 0, 256).rearrange("p (c j) -> p c j", j=32)[:, :, 0:17],
         reads=[rb[0]], writes=[scT.r])
    k.release()

    cvt_ctr = [0]

    def load_w_bf16(dst_ap, dst_reg, src_ap, stg, eng=None, first=True, mul=None, mul_reg=None):
        shp = list(src_ap.shape)
        if len(shp) == 3:
            st = stg.t[:, 0:shp[1] * shp[2]].rearrange("p (a b) -> p a b", b=shp[2])
        else:
            st = stg.t[:, 0:shp[1]]
        k.dma("sp", st, src_ap, writes=[stg.r])
        if eng is None:
            cvt_ctr[0] += 1
            eng = "act" if cvt_ctr[0] % 2 else "dve"
        if mul is None:
            k.cp(eng, dst_ap, st, reads=[stg.r], writes=[dst_reg], accum=not first)
        else:
            k.tt("dve", dst_ap, st, mul, ALU.mult, reads=[stg.r, mul_reg], writes=[dst_reg], accum=not first)

    def layer_norm(xin, xin_reg, rows, gam, bet, gb_reg, out_ap, out_reg, wk):
        st = wk["st"].next(); mv = wk["mv"].next()
        xv = xin.rearrange("p (c f) -> p c f", f=512)
        for c in range(2):
            k.op("dve", lambda e, c=c: e.bn_stats(st.t[0:rows, c, :], xv[:, c, :]), reads=[xin_reg], writes=[st.r], accum=(c > 0))
        k.op("dve", lambda e: e.bn_aggr(mv.t[0:rows, 0:2], st.t[0:rows, :, :]), reads=[st.r], writes=[mv.r])
        k.ts("dve", mv.t[0:rows, 2:3], mv.t[0:rows, 1:2], LN_EPS, None, ALU.add, reads=[mv.r], writes=[mv.r])
        k.act(mv.t[0:rows, 2:3], mv.t[0:rows, 2:3], AF.Sqrt, reads=[mv.r], writes=[mv.r])
        k.op("dve", lambda e: e.reciprocal(mv.t[0:rows, 2:3], mv.t[0:rows, 2:3]), reads=[mv.r], writes=[mv.r])
        k.stt(mv.t[0:rows, 3:4], mv.t[0:rows, 0:1], -1.0, mv.t[0:rows, 2:3], ALU.mult, ALU.mult, reads=[mv.r], writes=[mv.r])
        xn = wk["xn"].next()
        k.act(xn.t[0:rows, :], xin, AF.Identity, reads=[xin_reg, mv.r], writes=[xn.r], scale=mv.t[0:rows, 2:3], bias=mv.t[0:rows, 3:4])
        k.tt("dve", xn.t[0:rows, :], xn.t[0:rows, :], gam[0:rows, :], ALU.mult, reads=[xn.r, gb_reg], writes=[xn.r])
        k.tt("dve", out_ap, xn.t[0:rows, :], bet[0:rows, :], ALU.add, reads=[xn.r, gb_reg], writes=[out_reg])

    def make_hT(xin, xin_reg, rows, hT_ap, hT_reg, layer, which, mods_s=None, mods_reg=None, wk=None, pbanks=(0, 1), first=True):
        if rows == 128:
            src, src_reg = xin, xin_reg
        else:
            hs = wk["hs"].next()
            k.tt("dve", hs.t[0:rows], xin, mods_s[:, D:2 * D], ALU.mult, reads=[xin_reg, mods_reg], writes=[hs.r])
            k.tt("dve", hs.t[0:rows], hs.t[0:rows], mods_s[:, 0:D], ALU.add, reads=[hs.r, mods_reg], writes=[hs.r])
            src, src_reg = hs.t[0:rows], hs.r
        for c in range(8):
            b = pbanks[c // 4]
            k.tr(bank(b, 128, (c % 4) * 128, (c % 4) * 128 + rows), src[:, c * 128:(c + 1) * 128], ident[0:rows, 0:rows],
                 reads=[src_reg, rconst], writes=[rb[b]], accum=(c % 4 > 0))
        for c in range(8):
            b = pbanks[c // 4]
            pin = bank(b, 128, (c % 4) * 128, (c % 4) * 128 + rows)
            if rows == 128:
                k.act(hT_ap[:, c, :], pin, AF.Identity, reads=[rb[b], modT.r], writes=[hT_reg], accum=not (first and c == 0),
                      scale=modT.t[:, layer, which + 1, c:c + 1], bias=modT.t[:, layer, which, c:c + 1])
            else:
                k.cp("act", hT_ap[:, c, :], pin, reads=[rb[b]], writes=[hT_reg], accum=not (first and c == 0))

    def rotate(xv, xregs, rows, G, Ct, St, tab_reg, out3, out_reg, tmp3, tmp_reg, mode):
        if mode == "half":
            x4 = xv.rearrange("p g (two j) -> p g two j", two=2)
            t4 = tmp3.rearrange("p g (two j) -> p g two j", two=2)
            a0, a1 = x4[:, :, 1, :], x4[:, :, 0, :]
            d0, d1 = t4[:, :, 0, :], t4[:, :, 1, :]
            s0, s1 = St[:, 0:64], St[:, 64:128]
        else:
            x4 = xv.rearrange("p g (j two) -> p g j two", two=2)
            t4 = tmp3.rearrange("p g (j two) -> p g j two", two=2)
            a0, a1 = x4[:, :, :, 1], x4[:, :, :, 0]
            d0, d1 = t4[:, :, :, 0], t4[:, :, :, 1]
            Sv = St.rearrange("p (j two) -> p j two", two=2)
            s0, s1 = Sv[:, :, 0], Sv[:, :, 1]
        k.tt("dve", d0, a0, bc(s0, 1, G), ALU.mult, reads=list(xregs) + [tab_reg], writes=[tmp_reg])
        k.tt("dve", d1, a1, bc(s1, 1, G), ALU.mult, reads=list(xregs) + [tab_reg], writes=[tmp_reg], accum=True)
        k.tt("dve", out3, xv, bc(Ct, 1, G), ALU.mult, reads=list(xregs) + [tab_reg], writes=[out_reg])
        k.tt("dve", out3, out3, tmp3, ALU.add, reads=[out_reg, tmp_reg], writes=[out_reg])

    def adaln(l, mods_mix, g1p):
        k.mark()
        stg = k.pool(2, [128, 8 * 512], F32, "ada_stg")
        wbp = k.pool(2, [128, 8, 512], BF16, "ada_wb")
        m17p = k.pool(2, [18, 512], F32, "m17")
        outp = k.pool(2, [128, 512], F32, "ada_out")
        for j in range(12):
            which, half = divmod(j, 2)
            wb = wbp.next()
            load_w_bf16(wb.t[:], wb.r, ada_w[l, :, j * 512:(j + 1) * 512].rearrange("(kc p) n -> p kc n", p=128), stg.next())
            m17 = m17p.next()
            k.dma("sp", m17.t[17:18, :], ada_b[l:l + 1, j * 512:(j + 1) * 512], writes=[m17.r])
            for kc in range(8):
                k.mm(bank(0, 17), scT.t[:, kc, 0:17], wb.t[:, kc, :], kc == 0, kc == 7, reads=[scT.r, wb.r], writes=[rb[0]])
            k.cp("act", m17.t[0:17, :], bank(0, 17), reads=[rb[0]], writes=[m17.r], accum=True)
            plus1 = 1.0 if which in (1, 2, 4, 5) else 0.0
            k.mm(bank(1, 64), Es[:], m17.t[:], True, True, reads=[rconst, m17.r], writes=[rb[1]])
            if which < 3:
                k.ts("dve", mods_mix.t[:, j * 512:(j + 1) * 512], bank(1, 64), plus1, None, ALU.add,
                     reads=[rb[1]], writes=[mods_mix.r], accum=(j > 0))
            else:
                o = outp.next()
                k.ts("dve", o.t[0:64, :], bank(1, 64), plus1, None, ALU.add, reads=[rb[1]], writes=[o.r])
                k.dma("sp", sc_mods[l][:, (j - 6) * 512:(j - 5) * 512], o.t[0:64, :], reads=[o.r], writes=[r_scm[l]], accum=(j > 6))
            if which in (2, 5):
                k.mm(bank(2), Ep[:], m17.t[:], True, True, reads=[rconst, m17.r], writes=[rb[2]])
                if which == 2:
                    k.ts("dve", g1p.t[:, half * 512:(half + 1) * 512], bank(2), 1.0, None, ALU.add,
                         reads=[rb[2]], writes=[g1p.r], accum=(half > 0))
                else:
                    o = outp.next()
                    k.ts("dve", o.t[:, :], bank(2), 1.0, None, ALU.add, reads=[rb[2]], writes=[o.r])
                    k.dma("sp", sc_g2p[l][:, half * 512:(half + 1) * 512], o.t[0:1, :], reads=[o.r], writes=[r_scg[l]], accum=(half > 0))
            for cc in range(4):
                k.mm(bank(3, 128, cc, cc + 1), m17.t[:, cc * 128:(cc + 1) * 128], ep[:], True, True,
                     reads=[m17.r, rconst], writes=[rb[3]])
            k.ts("dve", modT.t[:, l, which, half * 4:(half + 1) * 4], bank(3, 128, 0, 4), plus1, None, ALU.add,
                 reads=[rb[3]], writes=[modT.r], accum=True)
        k.release()

    def pass_moba(mods_mix, omT):
        k.mark()
        wm = k.buf([128, 8, 1536], BF16, "wm")
        qTs_b = k.buf([128, 4, SR], BF16, "qTs_b"); qTs_f = k.buf([128, 4, SR], F32, "qTs_f")
        kTs_b = k.buf([128, 4, SR], BF16, "kTs_b"); v_s = k.buf([SR, 4, 132], BF16, "v_s")
        k.mark()
        kT_hist = k.buf([128, 4, T], BF16, "kT_hist")
        v_hist = k.buf([128, NT, 512], BF16, "v_hist")
        kmT = k.buf([128, 4, 8], F32, "kmT")
        k.mark()
        stg = k.pool(2, [128, 8 * 512], F32, "stg")
        for g in range(3):
            load_w_bf16(wm.t[:, :, g * 512:(g + 1) * 512], wm.r,
                        w_in[:, 2048 + g * 512:2048 + (g + 1) * 512].rearrange("(kc p) n -> p kc n", p=128),
                        stg.next(), first=(g == 0))
        k.release()
        wk = {"hs": k.pool(1, [SR, D], F32, "hs")}
        xt_p = k.pool(2, [128, D], F32, "xt")
        hT_p = k.pool(2, [128, 8, 128], BF16, "hT")
        rot_p = k.pool(2, [128, 4, 128], F32, "rot")
        qk_p = k.pool(2, [128, 8, 128], F32, "qkrot")
        tmp_p = k.pool(1, [128, 8, 128], F32, "rtmp")
        vf_p = k.pool(2, [128, 512], F32, "vf")
        qTb_p = k.pool(2, [128, 4, 128], BF16, "qTb")
        qTf_p = k.pool(2, [128, 4, 128], F32, "qTf")
        ksum_p = k.pool(2, [128, 4], F32, "ksum")
        gate_p = k.pool(1, [128, 4, 8], F32, "gate")
        cmp_p = k.pool(1, [128, 4, 8, 8], F32, "cmp")
        bias_p = k.pool(2, [128, 4, 8], F32, "bias")
        S_p = k.pool(1, [128, T], F32, "S")
        P_p = k.pool(2, [128, T], BF16, "P")
        PT_p = k.pool(2, [128, NT, 128], BF16, "PT")
        sm_p = k.pool(4, [128, 4], F32, "sm")
        om_p = k.pool(2, [128, 512], BF16, "om")

        for t in range(NT + 1):
            rows = 128 if t < NT else SR
            if t < NT:
                xt = xt_p.next()
                k.dma("sp", xt.t[:], xp[t * 128:(t + 1) * 128, :], writes=[xt.r])
                xin, xin_reg = xt.t[:], xt.r
            else:
                xin, xin_reg = x_s.t[:], x_s.r
            hT = hT_p.next()
            make_hT(xin, xin_reg, rows, hT.t[:, :, 0:rows], hT.r, 0, 0, mods_s=mods_mix.t, mods_reg=mods_mix.r, wk=wk, pbanks=(0, 1))
            rot = rot_p.next()
            k.dma("sp", rot.t[0:rows], rot_d[t, 0:rows], writes=[rot.r])
            for g in range(3):
                for kc in range(8):
                    k.mm(bank(4 + g, rows), hT.t[:, kc, 0:rows], wm.t[:, kc, g * 512:(g + 1) * 512], kc == 0, kc == 7,
                         reads=[hT.r, wm.r], writes=[rb[4 + g]])
            qk = qk_p.next(); tmp = tmp_p.next()
            zqk = ps[0:rows, 4 * 512:6 * 512].rearrange("p (g d) -> p g d", d=128)
            rotate(zqk, [rb[4], rb[5]], rows, 8, rot.t[0:rows, 2, :], rot.t[0:rows, 3, :], rot.r,
                   qk.t[0:rows], qk.r, tmp.t[0:rows], tmp.r, "half")
            vf = vf_p.next()
            k.cp("act", vf.t[0:rows], bank(6, rows), reads=[rb[6]], writes=[vf.r])
            kdst = kp[t * 128:(t + 1) * 128, :] if t < NT else ks
            vdst = vp[t * 128:(t + 1) * 128, :] if t < NT else vs
            k.dma("sp", kdst, qk.t[0:rows, 4:8, :].rearrange("p g d -> p (g d)"), reads=[qk.r], is_output=True)
            k.dma("sp", vdst, vf.t[0:rows], reads=[vf.r], is_output=True)
            if stage < 2:
                continue
            if t < NT:
                k.cp("act", v_hist.t[:, t, :], bank(6), reads=[rb[6]], writes=[v_hist.r], accum=(t > 0))
            else:
                k.memset("pool", v_s.t[:], 1.0, writes=[v_s.r])
                k.cp("pool", v_s.t[:, :, 0:128], vf.t[0:SR].rearrange("p (h d) -> p h d", d=128), reads=[vf.r], writes=[v_s.r])
            for g in range(8):
                b = 2 + g // 4
                k.tr(bank(b, 128, (g % 4) * 128, (g % 4) * 128 + rows), qk.t[0:rows, g, :], ident[0:rows, 0:rows],
                     reads=[qk.r, rconst], writes=[rb[b]], accum=(g % 4 > 0))
            qv = bank(2).rearrange("p (h s) -> p h s", s=128)[:, :, 0:rows]
            kv = bank(3).rearrange("p (h s) -> p h s", s=128)[:, :, 0:rows]
            if t < NT:
                qTb = qTb_p.next(); qTf = qTf_p.next()
                k.cp("act", qTb.t[:], qv, reads=[rb[2]], writes=[qTb.r])
                k.cp("dve", qTf.t[:], qv, reads=[rb[2]], writes=[qTf.r])
                k.cp("act", kT_hist.t[:, :, t * 128:(t + 1) * 128], kv, reads=[rb[3]], writes=[kT_hist.r], accum=(t > 0))
                ksum = ksum_p.next()
                k.op("dve", lambda e, ksum=ksum, kv=kv: e.tensor_reduce(ksum.t[:], kv, axis=AX.X, op=ALU.add), reads=[rb[3]], writes=[ksum.r])
                if t % 2 == 0:
                    k.cp("dve", kmT.t[:, :, t // 2], ksum.t[:], reads=[ksum.r], writes=[kmT.r], accum=True)
                else:
                    k.tt("dve", kmT.t[:, :, t // 2], kmT.t[:, :, t // 2], ksum.t[:], ALU.add, reads=[ksum.r, kmT.r], writes=[kmT.r])
            else:
                k.cp("act", qTs_b.t[:], qv, reads=[rb[2]], writes=[qTs_b.r])
                k.cp("dve", qTs_f.t[:], qv, reads=[rb[2]], writes=[qTs_f.r])
                k.cp("act", kTs_b.t[:], kv, reads=[rb[3]], writes=[kTs_b.r])
                continue
            own = t // 2
            bias = None
            if own >= 4:
                for h in range(4):
                    k.mm(bank(7, 128, 448 + h * 8, 448 + h * 8 + own), qTf.t[:, h, :], kmT.t[:, h, 0:own], True, True,
                         reads=[qTf.r, kmT.r], writes=[rb[7]])
                gate = gate_p.next(); cmpb = cmp_p.next(); bias = bias_p.next()
                gpv = bank(7, 128, 448, 480).rearrange("p (h n) -> p h n", n=8)[:, :, 0:own]
                k.cp("act", gate.t[:, :, 0:own], gpv, reads=[rb[7]], writes=[gate.r])
                gv = gate.t[:, :, 0:own]
                k.tt("dve", cmpb.t[:, :, 0:own, 0:own], bc(gv, 2, own), bc(gv, 3, own), ALU.is_gt, reads=[gate.r], writes=[cmpb.r])
                k.op("dve", lambda e, gate=gate, cmpb=cmpb, own=own: e.tensor_reduce(gate.t[:, :, 0:own], cmpb.t[:, :, 0:own, 0:own], axis=AX.X, op=ALU.add),
                     reads=[cmpb.r], writes=[gate.r])
                k.ts("dve", bias.t[:, :, 0:own], gate.t[:, :, 0:own], 2.5, NEG, ALU.is_gt, ALU.mult, reads=[gate.r], writes=[bias.r])
            nk = (t + 1) * 128
            om = om_p.next()
            for h in range(4):
                for c0 in range(0, nk, 512):
                    c1 = min(nk, c0 + 512)
                    b = c0 // 512
                    k.mm(bank(b, 128, 0, c1 - c0), qTb.t[:, h, :], kT_hist.t[:, h, c0:c1], True, True,
                         reads=[qTb.r, kT_hist.r], writes=[rb[b]])
                nb_used = (nk + 511) // 512
                sregs = [rb[i] for i in range(nb_used)]
                S = S_p.next()
                npast = own * 256
                first = True
                if npast > 0:
                    if bias is not None:
                        k.tt("dve", S.t[:, 0:npast].rearrange("p (n s) -> p n s", s=256),
                             ps[:, 0:npast].rearrange("p (n s) -> p n s", s=256), bc(bias.t[:, h, 0:own], 2, 256), ALU.add,
                             reads=sregs + [bias.r], writes=[S.r])
                    else:
                        k.cp("act", S.t[:, 0:npast], ps[:, 0:npast], reads=sregs, writes=[S.r])
                    first = False
                if t % 2 == 1:
                    k.cp("act", S.t[:, npast:npast + 128], ps[:, npast:npast + 128], reads=sregs, writes=[S.r], accum=not first)
                    first = False
                k.tt("dve", S.t[:, nk - 128:nk], ps[:, nk - 128:nk], tri[:], ALU.add, reads=sregs + [rconst], writes=[S.r], accum=not first)
                sm = sm_p.next()
                k.op("dve", lambda e, sm=sm, S=S, nk=nk: e.reduce_max(sm.t[:, 0:1], S.t[:, 0:nk], axis=AX.X), reads=[S.r], writes=[sm.r])
                k.ts("dve", sm.t[:, 1:2], sm.t[:, 0:1], -SCALE, None, ALU.mult, reads=[sm.r], writes=[sm.r])
                P = P_p.next()
                k.act(P.t[:, 0:nk], S.t[:, 0:nk], AF.Exp, reads=[S.r, sm.r], writes=[P.r, sm.r], scale=SCALE, bias=sm.t[:, 1:2],
                      accum_out=sm.t[:, 2:3])
                PT = PT_p.next()
                for j0 in range(0, t + 1, 8):
                    j1 = min(t + 1, j0 + 8)
                    for j in range(j0, j1):
                        k.tr(bankb(6, 128, (j - j0) * 128, (j - j0 + 1) * 128), P.t[:, j * 128:(j + 1) * 128], identb_t[:],
                             reads=[P.r, rconst], writes=[rb[6]], accum=(j > j0))
                    k.cp("act" if (j0 // 8) % 2 == 0 else "dve", PT.t[:, j0:j1, :],
                         bankb(6, 128, 0, (j1 - j0) * 128).rearrange("p (j s) -> p j s", s=128), reads=[rb[6]], writes=[PT.r], accum=(j0 > 0))
                for j in range(t + 1):
                    k.mm(bank(7, 128, 0, 128), PT.t[:, j, :], v_hist.t[:, j, h * 128:(h + 1) * 128],
                         j == 0, j == t, reads=[PT.r, v_hist.r], writes=[rb[7]])
                k.op("dve", lambda e, sm=sm: e.reciprocal(sm.t[:, 3:4], sm.t[:, 2:3]), reads=[sm.r], writes=[sm.r])
                k.act(om.t[:, h * 128:(h + 1) * 128], bank(7, 128, 0, 128), AF.Identity, reads=[rb[7], sm.r], writes=[om.r], accum=(h > 0),
                      scale=sm.t[:, 3:4])
            for h in range(4):
                k.tr(bankb(6, 128, h * 128, (h + 1) * 128), om.t[:, h * 128:(h + 1) * 128], identb_t[:], reads=[om.r, rconst],
                     writes=[rb[6]], accum=(h > 0))
            k.cp("act", omT.t[:, :, t * 128:(t + 1) * 128], bankb(6, 128, 0, 512).rearrange("p (h s) -> p h s", s=128),
                 reads=[rb[6]], writes=[omT.r], accum=(t > 0))
        k.release()
        if stage >= 3 and not skip_ms:
            moba_sample(qTs_b, qTs_f, kTs_b, v_s, omT)
        k.release()

    def moba_sample(qTs_b, qTs_f, kTs_b, v_s, omT):
        kpg_p = k.pool(5, [128, 512], F32, "kpg")
        vpg_p = k.pool(5, [128, 512], F32, "vpg")
        kb_p = k.pool(2, [128, 512], BF16, "kb")
        kTq_p = k.pool(2, [128, 4, T], BF16, "kTq")
        Vq_p = k.pool(2, [128, 16, 4, 132], BF16, "Vq")
        E_p = k.pool(2, [128, 17, 4, SR], BF16, "Eb")
        for b_ in Vq_p.bufs:
            k.memset("pool", b_.t[:], 1.0, writes=[b_.r])
        for b_ in E_p.bufs:
            k.memset("pool", b_.t[:], 0.0, writes=[b_.r])
        kms_p = k.pool(2, [128, 4, 8], F32, "kms")
        ksum_p = k.pool(2, [128, 4], F32, "ksum2")
        prod_p = k.pool(1, [128, 4, 8, 4], F32, "prod")
        gs_p = k.pool(1, [128, 16, 8], F32, "gs")
        cmp_p = k.pool(1, [128, 16, 8, 8], F32, "cmp2")
        comb_p = k.pool(1, [128, 16, 8], F32, "comb")
        pm_p = k.pool(1, [128, 16], F32, "pm")
        m16_p = k.pool(1, [16, 20], F32, "m16")
        X_p = k.pool(1, [128, 16, 16], F32, "X")
        Xn_p = k.pool(1, [SR, 16], F32, "Xn")
        NPG = NS * 16
        PF = 4
        gbuf = {}

        def issue(p):
            kpg = kpg_p.next(); vpg = vpg_p.next()
            k.gather(kpg.t[:], ck, idx.t[:, p:p + 1], reads=[idx.r], writes=[kpg.r])
            k.gather(vpg.t[:], cv, idx.t[:, p:p + 1], reads=[idx.r], writes=[vpg.r])
            gbuf[p] = (kpg, vpg)

        for p in range(PF):
            issue(p)

        def page_loop(b):
            kTq = kTq_p.next(); Vq = Vq_p.next(); kms = kms_p.next()
            for j in range(16):
                p = b * 16 + j
                if p + PF < NPG:
                    issue(p + PF)
                kpg, vpg = gbuf.pop(p)
                kb = kb_p.next()
                k.cp("dve", kb.t[:], kpg.t[:], reads=[kpg.r], writes=[kb.r])
                k.cp("act", Vq.t[:, j, :, 0:128], vpg.t[:].rearrange("p (h d) -> p h d", d=128), reads=[vpg.r], writes=[Vq.r],
                     accum=(j > 0))
                pb = 1 + j % 2
                for h in range(4):
                    k.tr(bankb(pb, 128, h * 128, (h + 1) * 128), kb.t[:, h * 128:(h + 1) * 128], identb_t[:],
                         reads=[kb.r, rconst], writes=[rb[pb]], accum=(h > 0))
                kvw = bankb(pb, 128, 0, 512).rearrange("p (h s) -> p h s", s=128)
                k.cp("act", kTq.t[:, :, j * 128:(j + 1) * 128], kvw, reads=[rb[pb]], writes=[kTq.r], accum=(j > 0))
                ksum = ksum_p.next()
                k.op("dve", lambda e, ksum=ksum, kvw=kvw: e.tensor_reduce(ksum.t[:], kvw, axis=AX.X, op=ALU.add), reads=[rb[pb]], writes=[ksum.r])
                if j % 2 == 0:
                    k.cp("dve", kms.t[:, :, j // 2], ksum.t[:], reads=[ksum.r], writes=[kms.r], accum=(j > 0))
                else:
                    k.tt("dve", kms.t[:, :, j // 2], kms.t[:, :, j // 2], ksum.t[:], ALU.add, reads=[ksum.r, kms.r], writes=[kms.r])
            return kTq, Vq, kms

        def chain(b, kTq, Vq, kms):
            Eb = E_p.next()
            prod = prod_p.next()
            qf = qTs_f.t[:, :, 4 * b:4 * b + 4]
            k.tt("dve", prod.t[:], bc(kms.t[:], 3, 4), bc(qf, 2, 8), ALU.mult, reads=[kms.r, qTs_f.r], writes=[prod.r])
            k.mm(bank(3, 128, 0, 128), onesf[:], prod.t[:].rearrange("p h n q -> p (h n q)"), True, True,
                 reads=[rconst, prod.r], writes=[rb[3]])
            gs = gs_p.next(); cmpb = cmp_p.next(); comb = comb_p.next()
            k.cp("act", gs.t[:].rearrange("p (h q) n -> p h q n", q=4),
                 bank(3, 128, 0, 128).rearrange("p (h n q) -> p h q n", h=4, n=8), reads=[rb[3]], writes=[gs.r])
            k.tt("dve", cmpb.t[:], bc(gs.t[:], 2, 8), bc(gs.t[:], 3, 8), ALU.is_gt, reads=[gs.r], writes=[cmpb.r])
            k.op("dve", lambda e, gs=gs, cmpb=cmpb: e.tensor_reduce(gs.t[:], cmpb.t[:], axis=AX.X, op=ALU.add), reads=[cmpb.r], writes=[gs.r])
            k.ts("dve", comb.t[:], gs.t[:], 2.5, NEG, ALU.is_gt, ALU.mult, reads=[gs.r], writes=[comb.r])
            for j in range(16):
                for h in range(4):
                    c0 = j * 16 + h * 4
                    k.mm(bank(0, 128, c0, c0 + 4), kTq.t[:, h, j * 128:(j + 1) * 128], qTs_b.t[:, h, 4 * b:4 * b + 4], True, True,
                         reads=[kTq.r, qTs_b.r], writes=[rb[0]], inc=(j == 15 and h == 3))
            for h in range(4):
                k.mm(bank(0, SR, 256 + h * 4, 260 + h * 4), kTs_b.t[:, h, :], qTs_b.t[:, h, 4 * b:4 * b + 4], True, True,
                     reads=[kTs_b.r, qTs_b.r], writes=[rb[0]], inc=(h == 3))
            pm = pm_p.next(); m16 = m16_p.next()
            k.op("dve", lambda e, pm=pm: e.tensor_reduce(pm.t[:], bank(0, 128, 0, 256).rearrange("p (j c) -> p c j", c=16), axis=AX.X, op=ALU.max),
                 reads=[rb[0]], writes=[pm.r])
            k.tt("dve", pm.t[0:SR, :], pm.t[0:SR, :], bank(0, SR, 256, 272), ALU.max, reads=[pm.r, rb[0]], writes=[pm.r])
            k.tr(bank(3, 16, 128, 256), pm.t[:], ident[:], reads=[pm.r, rconst], writes=[rb[3]])
            k.op("dve", lambda e, m16=m16: e.reduce_max(m16.t[:, 16:17], bank(3, 16, 128, 256), axis=AX.X), reads=[rb[3]], writes=[m16.r])
            k.ts("dve", m16.t[:, 0:16], ident[0:16, 0:16], m16.t[:, 16:17], None, ALU.mult, reads=[m16.r, rconst], writes=[m16.r])
            k.mm(bank(3, 128, 256, 272), onesf[0:16, :], m16.t[:, 0:16], True, True, reads=[rconst, m16.r], writes=[rb[3]])
            mbc = bank(3, 128, 256, 272)
            k.tt("dve", comb.t[:], comb.t[:], bc(mbc, 2, 8), ALU.subtract, reads=[comb.r, rb[3]], writes=[comb.r])
            X = X_p.next(); Xn = Xn_p.next()
            k.tt("dve", X.t[:].rearrange("p (n two) c -> p n two c", two=2),
                 bank(0, 128, 0, 256).rearrange("p (n two c) -> p n two c", two=2, c=16),
                 bc(comb.t[:].rearrange("p c n -> p n c"), 2, 2), ALU.add, reads=[rb[0], comb.r], writes=[X.r])
            k.tt("dve", Xn.t[:], bank(0, SR, 256, 272), nmask[:, b, :], ALU.add, reads=[rb[0], rconst], writes=[Xn.r])
            k.tt("dve", Xn.t[:], Xn.t[:], bank(3, SR, 256, 272), ALU.subtract, reads=[Xn.r, rb[3]], writes=[Xn.r])
            k.act(Eb.t[:, 0:16, :, 4 * b:4 * b + 4], X.t[:].rearrange("p j (h q) -> p j h q", q=4), AF.Exp, reads=[X.r], writes=[Eb.r], scale=SCALE)
            k.act(Eb.t[0:SR, 16, :, 4 * b:4 * b + 4], Xn.t[:].rearrange("p (h q) -> p h q", q=4), AF.Exp, reads=[Xn.r], writes=[Eb.r], scale=SCALE)
            for h in range(4):
                ob = bank(4 + h, SR, 0, 132)
                for j in range(16):
                    k.mm(ob, Eb.t[:, j, h, :], Vq.t[:, j, h, :], (b == 0 and j == 0), False,
                         reads=[Eb.r, Vq.r], writes=[rb[4 + h]], inc=False)
                k.mm(ob, Eb.t[0:SR, 16, h, :], v_s.t[:, h, :], False, (b == NS - 1),
                     reads=[Eb.r, v_s.r], writes=[rb[4 + h]], inc=True)
            k.memset("dve", Eb.t[:, :, :, 4 * b:4 * b + 4], 0.0, writes=[Eb.r])

        prev = None
        for b in range(NS + 1):
            cur = page_loop(b) if b < NS else None
            if prev is not None:
                chain(b - 1, *prev)
            prev = cur
        rin = k.buf([SR, 4], F32, "rin"); oms = k.buf([SR, 512], BF16, "oms")
        for h in range(4):
            c0 = (4 + h) * 512
            k.op("dve", lambda e, h=h, c0=c0: e.reciprocal(rin.t[:, h:h + 1], ps[0:SR, c0 + 128:c0 + 129]),
                 reads=[rb[4 + h]], writes=[rin.r], accum=(h > 0))
        for h in range(4):
            c0 = (4 + h) * 512
            k.act(oms.t[:, h * 128:(h + 1) * 128], ps[0:SR, c0:c0 + 128], AF.Identity, reads=[rb[4 + h], rin.r], writes=[oms.r],
                  accum=(h > 0), scale=rin.t[:, h:h + 1])
        for h in range(4):
            k.tr(bankb(1, 128, h * SR, (h + 1) * SR), oms.t[:, h * 128:(h + 1) * 128], identb_t[0:SR, 0:SR], reads=[oms.r, rconst],
                 writes=[rb[1]], accum=(h > 0))
        k.cp("act", omT.t[:, :, T:T + SR], bankb(1, 128, 0, 4 * SR).rearrange("p (h s) -> p h s", s=SR), reads=[rb[1]], writes=[omT.r])

    def pass_ret(mods_mix, orT):
        k.mark()
        wr = k.buf([128, 8, 2048], BF16, "wr")
        k.mark()
        stg = k.pool(2, [128, 8 * 512], F32, "stg")
        for g in range(4):
            load_w_bf16(wr.t[:, :, g * 512:(g + 1) * 512], wr.r,
                        w_in[:, g * 512:(g + 1) * 512].rearrange("(kc p) n -> p kc n", p=128), stg.next(), first=(g == 0))
        k.release()
        Sst = k.buf([128, 4, 128], F32, "Sst"); Sbf = k.buf([128, 4, 128], BF16, "Sbf")
        k.memset("pool", Sst.t[:], 0.0, writes=[Sst.r]); k.memset("pool", Sbf.t[:], 0.0, writes=[Sbf.r])
        wk = {"hs": k.pool(1, [SR, D], F32, "hs")}
        xt_p = k.pool(2, [128, D], F32, "xt"); hT_p = k.pool(2, [128, 8, 128], BF16, "hT")
        rot_p = k.pool(2, [128, 4, 128], F32, "rot"); qk_p = k.pool(2, [128, 8, 128], F32, "qkrot")
        tmp_p = k.pool(1, [128, 8, 128], F32, "rtmp")
        vb_p = k.pool(2, [128, 512], BF16, "vb"); sg_p = k.pool(2, [128, 512], F32, "sg")
        kd_p = k.pool(2, [128, 4, 128], BF16, "kd")
        qTb_p = k.pool(2, [128, 4, 128], BF16, "qTb"); kTb_p = k.pool(2, [128, 4, 128], BF16, "kTb")
        qdT_p = k.pool(2, [128, 4, 128], BF16, "qdT"); att_p = k.pool(2, [128, 4, 128], BF16, "att")
        ss_p = k.pool(2, [128, 8], F32, "ss"); junk_p = k.pool(1, [128, 128], F32, "junk")
        or_p = k.pool(2, [128, 512], BF16, "or")
        for t in range(NT + 1):
            rows = 128 if t < NT else SR
            if t < NT:
                xt = xt_p.next()
                k.dma("sp", xt.t[:], xp[t * 128:(t + 1) * 128, :], writes=[xt.r])
                xin, xin_reg = xt.t[:], xt.r
            else:
                xin, xin_reg = x_s.t[:], x_s.r
            hT = hT_p.next()
            make_hT(xin, xin_reg, rows, hT.t[:, :, 0:rows], hT.r, 0, 0, mods_s=mods_mix.t, mods_reg=mods_mix.r, wk=wk, pbanks=(0, 1))
            rot = rot_p.next()
            k.dma("sp", rot.t[0:rows], rot_d[t, 0:rows], writes=[rot.r])
            for g in range(4):
                for kc in range(8):
                    k.mm(bank(4 + g, rows), hT.t[:, kc, 0:rows], wr.t[:, kc, g * 512:(g + 1) * 512], kc == 0, kc == 7,
                         reads=[hT.r, wr.r], writes=[rb[4 + g]])
            qk = qk_p.next(); tmp = tmp_p.next()
            zqk = ps[0:rows, 4 * 512:6 * 512].rearrange("p (g d) -> p g d", d=128)
            rotate(zqk, [rb[4], rb[5]], rows, 8, rot.t[0:rows, 0, :], rot.t[0:rows, 1, :], rot.r,
                   qk.t[0:rows], qk.r, tmp.t[0:rows], tmp.r, "pair")
            vb = vb_p.next(); sg = sg_p.next()
            k.cp("act", vb.t[0:rows], bank(6, rows), reads=[rb[6]], writes=[vb.r])
            k.act(sg.t[0:rows], bank(7, rows), AF.Silu, reads=[rb[7]], writes=[sg.r])
            kdc = rkd if t < NT else rkds
            kd = kd_p.next()
            k.tt("dve", kd.t[0:rows], qk.t[0:rows, 4:8, :], bc(kdc[0:rows, :], 2, 128), ALU.mult, reads=[qk.r, rconst], writes=[kd.r])
            for g in range(8):
                b = 2 + g // 4
                k.tr(bank(b, 128, (g % 4) * 128, (g % 4) * 128 + rows), qk.t[0:rows, g, :], ident[0:rows, 0:rows],
                     reads=[qk.r, rconst], writes=[rb[b]], accum=(g % 4 > 0))
            qv = bank(2).rearrange("p (h s) -> p h s", s=128)[:, :, 0:rows]
            kv = bank(3).rearrange("p (h s) -> p h s", s=128)[:, :, 0:rows]
            qTb = qTb_p.next(); kTb = kTb_p.next(); qdT = qdT_p.next()
            k.cp("act", qTb.t[:, :, 0:rows], qv, reads=[rb[2]], writes=[qTb.r])
            k.cp("act", kTb.t[:, :, 0:rows], kv, reads=[rb[3]], writes=[kTb.r])
            qdc = rqd if t < NT else rqds
            k.tt("dve", qdT.t[:, :, 0:rows], qv, qdc[:, :, 0:rows], ALU.mult, reads=[rb[2], rconst], writes=[qdT.r])
            for h in range(4):
                k.mm(bank(0, rows, h * 128, h * 128 + rows), kTb.t[:, h, 0:rows], qTb.t[:, h, 0:rows], True, True,
                     reads=[kTb.r, qTb.r], writes=[rb[0]])
            att = att_p.next()
            dmc = rdm if t < NT else rdms
            k.tt("dve", att.t[0:rows, :, 0:rows], bank(0, rows).rearrange("p (h s) -> p h s", s=128)[:, :, 0:rows], dmc[0:rows, :, 0:rows],
                 ALU.mult, reads=[rb[0], rconst], writes=[att.r])
            if t < NT:
                for h in range(4):
                    ob = bank(1, 128, h * 128, (h + 1) * 128)
                    k.mm(ob, att.t[:, h, :], vb.t[:, h * 128:(h + 1) * 128], True, False, reads=[att.r, vb.r], writes=[rb[1]])
                    k.mm(ob, qdT.t[:, h, :], Sbf.t[:, h, :], False, True, reads=[qdT.r, Sbf.r], writes=[rb[1]])
                for h in range(4):
                    k.mm(bank(2, 128, h * 128, (h + 1) * 128), kd.t[:, h, :], vb.t[:, h * 128:(h + 1) * 128], True, True,
                         reads=[kd.r, vb.r], writes=[rb[2]])
                for h in range(4):
                    k.stt(Sst.t[:, h, :], Sst.t[:, h, :], math.exp(128.0 * LOGG[h]), bank(2, 128, h * 128, (h + 1) * 128), ALU.mult, ALU.add,
                          reads=[Sst.r, rb[2]], writes=[Sst.r])
                k.cp("act", Sbf.t[:], Sst.t[:], reads=[Sst.r], writes=[Sbf.r])
                if t == NT - 1:
                    k.dma("sp", rpo.rearrange("(h d) v -> d h v", d=128), Sst.t[:], reads=[Sst.r], is_output=True)
            else:
                qdm = k.buf([128, NS, 4, SR], BF16, "qdm")
                k.memset("pool", qdm.t[:], 0.0, writes=[qdm.r])
                base = qdm.t[:]
                pstr = base.ap[0][0]
                for h in range(4):
                    dst = bass.AP(qdm.t, base.offset + h * SR, [[pstr, 128], [4 * SR + 4, NS], [1, 4]])
                    k.cp("pool", dst, qdT.t[:, h, 0:SR].rearrange("p (s j) -> p s j", j=4), reads=[qdT.r, qdm.r], writes=[qdm.r])
                for h in range(4):
                    ob = bank(4 + h, SR, 0, 128)
                    k.mm(ob, att.t[0:SR, h, 0:SR], vb.t[0:SR, h * 128:(h + 1) * 128], True, False, reads=[att.r, vb.r], writes=[rb[4 + h]], inc=True)
                s0_p = k.pool(2, [128, 4, 128], F32, "s0"); s0b_p = k.pool(2, [128, 4, 128], BF16, "s0b")
                kdm_p = k.pool(2, [SR, 4, 128], BF16, "kdm"); sn_p = k.pool(2, [128, 4, 128], F32, "sn")
                for sq in range(NS):
                    s0 = s0_p.next(); s0b = s0b_p.next()
                    k.dma("sp", s0.t[:], sret[sq * 512:(sq + 1) * 512, :].rearrange("(h d) v -> d h v", d=128), writes=[s0.r])
                    k.cp("act", s0b.t[:], s0.t[:], reads=[s0.r], writes=[s0b.r])
                    for h in range(4):
                        k.mm(bank(4 + h, SR, 0, 128), qdm.t[:, sq, h, :], s0b.t[:, h, :], False, (sq == NS - 1),
                             reads=[qdm.r, s0b.r], writes=[rb[4 + h]], inc=True)
                    kdm = kdm_p.next()
                    k.ts("dve", kdm.t[:], kd.t[0:SR], blk[:, sq:sq + 1], None, ALU.mult, reads=[kd.r, rconst], writes=[kdm.r])
                    ub = 2 + sq % 2
                    for h in range(4):
                        k.mm(bank(ub, 128, h * 128, (h + 1) * 128), kdm.t[:, h, :], vb.t[0:SR, h * 128:(h + 1) * 128], True, True,
                             reads=[kdm.r, vb.r], writes=[rb[ub]])
                    sn = sn_p.next()
                    for h in range(4):
                        k.stt(sn.t[:, h, :], s0.t[:, h, :], math.exp(4.0 * LOGG[h]), bank(ub, 128, h * 128, (h + 1) * 128), ALU.mult, ALU.add,
                              reads=[s0.r, rb[ub]], writes=[sn.r], accum=(h > 0))
                    k.dma("sp", rso[sq * 512:(sq + 1) * 512, :].rearrange("(h d) v -> d h v", d=128), sn.t[:], reads=[sn.r], is_output=True)
            ss = ss_p.next(); junk = junk_p.next()

            def obank(h):
                return (bank(1, rows, h * 128, (h + 1) * 128), rb[1]) if t < NT else (bank(4 + h, rows, 0, 128), rb[4 + h])

            for h in range(4):
                oap, oreg = obank(h)
                k.act(junk.t[0:rows], oap, AF.Square, reads=[oreg], writes=[junk.r, ss.r],
                      accum_out=ss.t[0:rows, h:h + 1])
            k.ts("dve", ss.t[0:rows, 4:8], ss.t[0:rows, 0:4], 1.0 / 128.0, GN_EPS, ALU.mult, ALU.add, reads=[ss.r], writes=[ss.r])
            k.act(ss.t[0:rows, 4:8], ss.t[0:rows, 4:8], AF.Sqrt, reads=[ss.r], writes=[ss.r])
            k.op("dve", lambda e, ss=ss, rows=rows: e.reciprocal(ss.t[0:rows, 4:8], ss.t[0:rows, 4:8]), reads=[ss.r], writes=[ss.r])
            orr = or_p.next()
            for h in range(4):
                oap, oreg = obank(h)
                k.stt(orr.t[0:rows, h * 128:(h + 1) * 128], oap, ss.t[0:rows, 4 + h:5 + h],
                      sg.t[0:rows, h * 128:(h + 1) * 128], ALU.mult, ALU.mult, reads=[oreg, ss.r, sg.r], writes=[orr.r], accum=(h > 0))
            for h in range(4):
                k.tr(bankb(3, 128, h * 128, h * 128 + rows), orr.t[0:rows, h * 128:(h + 1) * 128], identb_t[0:rows, 0:rows],
                     reads=[orr.r, rconst], writes=[rb[3]], accum=(h > 0))
            k.cp("act", orT.t[:, :, t * 128:t * 128 + rows], bankb(3, 128, 0, 512).rearrange("p (h s) -> p h s", s=128)[:, :, 0:rows],
                 reads=[rb[3]], writes=[orT.r], accum=(t > 0))
        k.release()

    def ln_all_tiles(lng, lnb, final):
        stt_ = k.sb([128, NT + 1, 2, 6], F32, "ln_st")
        mvt = k.sb([128, NT + 1, 4], F32, "ln_mv")
        r_st = [Reg() for _ in range(NT + 1)]
        tiles = []
        for t in range(NT + 1):
            rows = 128 if t < NT else SR
            xt_ap, xt_reg = (x_all[:, t, :], rx[t]) if t < NT else (x_s.t[:], x_s.r)
            tiles.append((t, rows, xt_ap, xt_reg))
        for t, rows, xt_ap, xt_reg in tiles:
            xv = xt_ap.rearrange("p (c f) -> p c f", f=512)
            for c in range(2):
                k.op("dve", lambda e, t=t, c=c, rows=rows, xv=xv: e.bn_stats(stt_[0:rows, t, c, :], xv[:, c, :]), reads=[xt_reg], writes=[r_st[t]], accum=(c > 0))
            k.op("dve", lambda e, t=t, rows=rows: e.bn_aggr(mvt[0:rows, t, 0:2], stt_[0:rows, t, :, :]), reads=[r_st[t]], writes=[r_st[t]])
            k.ts("dve", mvt[0:rows, t, 2:3], mvt[0:rows, t, 1:2], LN_EPS, None, ALU.add, reads=[r_st[t]], writes=[r_st[t]])
        for t, rows, xt_ap, xt_reg in tiles:
            k.act(mvt[0:rows, t, 2:3], mvt[0:rows, t, 2:3], AF.Sqrt, reads=[r_st[t]], writes=[r_st[t]])
        for t, rows, xt_ap, xt_reg in tiles:
            k.op("dve", lambda e, t=t, rows=rows: e.reciprocal(mvt[0:rows, t, 2:3], mvt[0:rows, t, 2:3]), reads=[r_st[t]], writes=[r_st[t]])
            k.stt(mvt[0:rows, t, 3:4], mvt[0:rows, t, 0:1], -1.0, mvt[0:rows, t, 2:3], ALU.mult, ALU.mult, reads=[r_st[t]], writes=[r_st[t]])
        for t, rows, xt_ap, xt_reg in tiles:
            k.act(xt_ap, xt_ap, AF.Identity, reads=[xt_reg, r_st[t]], writes=[xt_reg], scale=mvt[0:rows, t, 2:3], bias=mvt[0:rows, t, 3:4])
        for t, rows, xt_ap, xt_reg in tiles:
            k.tt("dve", xt_ap, xt_ap, lng.t[0:rows, :], ALU.mult, reads=[xt_reg, lng.r], writes=[xt_reg])
            k.tt("dve", xt_ap, xt_ap, lnb.t[0:rows, :], ALU.add, reads=[xt_reg, lng.r], writes=[xt_reg])
            if final:
                k.dma("sp", yp[t * 128:(t + 1) * 128, :] if t < NT else ys, xt_ap, reads=[xt_reg], is_output=True)

    def post_norm(y_ap, y_regs, xt_ap, xt_reg, rows, gate_ap, gate_reg, lng, lnb, gb_reg, wk, bias_ap=None, bias_reg=None, defer_ln=False):
        rr = wk["rr"].next()
        if bias_ap is not None:
            k.tt("dve", rr.t[0:rows], y_ap, bias_ap[0:rows], ALU.add, reads=list(y_regs) + [bias_reg], writes=[rr.r])
            k.tt("dve", rr.t[0:rows], rr.t[0:rows], gate_ap, ALU.mult, reads=[rr.r, gate_reg], writes=[rr.r])
        else:
            k.tt("dve", rr.t[0:rows], y_ap, gate_ap, ALU.mult, reads=list(y_regs) + [gate_reg], writes=[rr.r])
        k.stt(xt_ap, xt_ap, ALPHA, rr.t[0:rows], ALU.mult, ALU.add, reads=[xt_reg, rr.r], writes=[xt_reg])
        if not defer_ln:
            layer_norm(xt_ap, xt_reg, rows, lng, lnb, gb_reg, xt_ap, xt_reg, wk)

    def ln_work(n=2):
        rr = k.pool(n, [128, D], F32, "rr")
        return {"st": k.pool(2, [128, 2, 6], F32, "st"), "mv": k.pool(2, [128, 4], F32, "mv"),
                "xn": k.pool(n, [128, D], F32, "xn"), "rr": rr, "hs": rr}

    def load_ln(i):
        g = k.buf([128, D], F32, "lng"); b = k.buf([128, D], F32, "lnb")
        k.dma("sp", g.t[:], ln_g[i:i + 1, :].partition_broadcast(128), writes=[g.r])
        k.dma("sp", b.t[:], ln_b[i:i + 1, :].partition_broadcast(128), writes=[g.r], anchor=g.r, accum=True)
        return g, b

    def pass_out(mods_mix, g1p, orT, omT):
        k.mark()
        wo = k.buf([128, 8, D], BF16, "wo")
        k.mark()
        stg = k.pool(2, [128, 8 * 512], F32, "stg")
        for g in range(2):
            load_w_bf16(wo.t[:, :, g * 512:(g + 1) * 512], wo.r,
                        w_out[:, g * 512:(g + 1) * 512].rearrange("(kc p) n -> p kc n", p=128), stg.next(), first=(g == 0))
        k.release()
        lng, lnb = load_ln(0)
        wk = ln_work()
        for t in range(NT + 1):
            rows = 128 if t < NT else SR
            if t < NT:
                k.dma("sp", x_all[:, t, :], xp[t * 128:(t + 1) * 128, :], writes=[rx[t]])
                xt_ap, xt_reg = x_all[:, t, :], rx[t]
                gate_ap, gate_reg = g1p.t[:], g1p.r
            else:
                xt_ap, xt_reg = x_s.t[:], x_s.r
                gate_ap, gate_reg = mods_mix.t[:, 2 * D:3 * D], mods_mix.r
            bp = 4 * (t % 2)
            for half in range(2):
                for c in range(8):
                    src = orT if c < 4 else omT
                    k.mm(bank(bp + half, rows), src.t[:, c % 4, t * 128:t * 128 + rows], wo.t[:, c, half * 512:(half + 1) * 512],
                         c == 0, c == 7, reads=[src.r, wo.r], writes=[rb[bp + half]])
            post_norm(ps[0:rows, bp * 512:bp * 512 + D], [rb[bp], rb[bp + 1]], xt_ap, xt_reg, rows, gate_ap, gate_reg,
                      lng.t, lnb.t, lng.r, wk, defer_ln=True)
        ln_all_tiles(lng, lnb, False)
        k.release()

    def ffn(l, final):
        k.mark()
        g2s = k.buf([SR, D], F32, "g2s"); g2p = k.buf([128, D], F32, "g2p")
        k.dma("sp", g2s.t[:], sc_mods[l][:, 2 * D:3 * D], reads=[r_scm[l]], writes=[g2s.r])
        k.dma("sp", g2p.t[:], sc_g2p[l].partition_broadcast(128), reads=[r_scg[l]], writes=[g2p.r])
        hT = k.buf([128, 8, T + SR], BF16, "hT_all")
        ffp = k.buf([128, NFC, 4], F32, "ffp"); ust = k.buf([128, NFC, 2 * NS], F32, "ust")
        fo = k.buf([128, NFC, 2], F32, "fo"); fs = k.buf([128, NFC, NS, 2], F32, "fs")
        k.mark()
        mf = k.buf([SR, 2 * D], F32, "mf")
        k.dma("sp", mf.t[:], sc_mods[l][:, 0:2 * D], reads=[r_scm[l]], writes=[mf.r])
        p4 = k.buf([4, DFF], F32, "p4"); p32 = k.buf([2 * NS, DFF], F32, "p32")
        k.dma("sp", p4.t[:], ffp_d[l], writes=[p4.r])
        k.dma("sp", p32.t[:], sffn[l], writes=[p32.r])
        for c0 in range(0, NFC, 4):
            c1 = min(NFC, c0 + 4)
            for c in range(c0, c1):
                k.tr(bank(0, 128, (c - c0) * 4, (c - c0) * 4 + 4), p4.t[:, c * 128:(c + 1) * 128], ident[0:4, 0:4], reads=[p4.r, rconst],
                     writes=[rb[0]], accum=(c > c0))
                k.tr(bank(1, 128, (c - c0) * 32, (c - c0) * 32 + 32), p32.t[:, c * 128:(c + 1) * 128], ident[0:32, 0:32], reads=[p32.r, rconst],
                     writes=[rb[1]], accum=(c > c0))
            k.cp("act", ffp.t[:, c0:c1, :], bank(0, 128, 0, (c1 - c0) * 4).rearrange("p (c j) -> p c j", j=4), reads=[rb[0]], writes=[ffp.r], accum=(c0 > 0))
            k.cp("act", ust.t[:, c0:c1, :], bank(1, 128, 0, (c1 - c0) * 32).rearrange("p (c j) -> p c j", j=32), reads=[rb[1]], writes=[ust.r], accum=(c0 > 0))
        wkh = {"hs": k.pool(1, [SR, D], F32, "hs")}
        for t in range(NT):
            make_hT(x_all[:, t, :], rx[t], 128, hT.t[:, :, t * 128:(t + 1) * 128], hT.r, l, 3, pbanks=(2 + 2 * (t % 2), 3 + 2 * (t % 2)), first=(t == 0))
            k.ts("dve", x_all[:, t, :], x_all[:, t, :], ALPHA, None, ALU.mult, reads=[rx[t]], writes=[rx[t]])
        make_hT(x_s.t[:], x_s.r, SR, hT.t[:, :, T:T + SR], hT.r, l, 3, mods_s=mf.t, mods_reg=mf.r, wk=wkh, pbanks=(2, 3), first=False)
        k.ts("pool", x_s.t[:], x_s.t[:], ALPHA, None, ALU.mult, reads=[x_s.r], writes=[x_s.r])
        k.release()
        k.mark()
        G = 2
        stg = k.pool(2, [128, 2048], F32, "stg")
        wu_p = k.pool(2, [128, 8, G * 128], BF16, "wu"); wv_p = k.pool(2, [128, 8, G * 128], BF16, "wv")
        wds_p = k.pool(2, [128, G, D], BF16, "wds"); wdu_p = k.pool(1, [128, G, D], BF16, "wdu")
        UW = 2 + T + 6 * NS
        u_p = k.pool(2, [128, UW], F32, "u_sb"); a_p = k.pool(1, [128, UW], F32, "acc")
        gT_p = k.pool(1, [128, G, T + SR], BF16, "gT")
        vsb_p = k.pool(1, [128, T + SR], BF16, "vsb")
        nb = [0]

        for g in range(NFC // G):
            f0 = g * G * 128
            wu = wu_p.next(); wv = wv_p.next(); wds = wds_p.next(); wdu = wdu_p.next()
            load_w_bf16(wu.t[:], wu.r, ffn_up[l, :, f0:f0 + G * 128].rearrange("(kc p) n -> p kc n", p=128), stg.next())
            load_w_bf16(wv.t[:], wv.r, ffn_up[l, :, DFF + f0:DFF + f0 + G * 128].rearrange("(kc p) n -> p kc n", p=128), stg.next())
            st = stg.next()
            stv = st.t[:, 0:G * D].rearrange("p (a b) -> p a b", b=D)
            k.dma("sp", stv, ffn_down[l, f0:f0 + G * 128, :].rearrange("(c p) n -> p c n", p=128), writes=[st.r])
            k.cp("act", wdu.t[:], stv, reads=[st.r], writes=[wdu.r])
            k.tt("dve", wds.t[:], stv, bc(g2p.t[:], 1, G), ALU.mult, reads=[st.r, g2p.r], writes=[wds.r])
            gT = gT_p.next()
            for c in range(G):
                fc = g * G + c
                u = u_p.next(); acc = a_p.next()
                w0, w1, w2, bb = (ffp.t[:, fc, j:j + 1] for j in range(4))
                k.memset("pool", u.t[:, 0:2], 0.0, writes=[u.r])
                usv = u.t[:, 2 + T:UW].rearrange("p (s j) -> p s j", j=6)
                asv = acc.t[:, 2 + T:UW].rearrange("p (s j) -> p s j", j=6)
                k.cp("pool", usv[:, :, 0:2], ust.t[:, fc, :].rearrange("p (s j) -> p s j", j=2), reads=[ust.r], writes=[u.r], accum=True)
                vsb = vsb_p.next()
                for n in range(5):
                    ncol = 512 if n < 4 else SR
                    t0 = n * 512
                    nb[0] += 1
                    bu = nb[0] % 2; bv = 2 + nb[0] % 2
                    for kc in range(8):
                        k.mm(bank(bu, 128, 0, ncol), wu.t[:, kc, c * 128:(c + 1) * 128], hT.t[:, kc, t0:t0 + ncol], kc == 0, kc == 7,
                             reads=[wu.r, hT.r], writes=[rb[bu]])
                    for kc in range(8):
                        k.mm(bank(bv, 128, 0, ncol), wv.t[:, kc, c * 128:(c + 1) * 128], hT.t[:, kc, t0:t0 + ncol], kc == 0, kc == 7,
                             reads=[wv.r, hT.r], writes=[rb[bv]])
                    if n < 4:
                        k.cp("act", u.t[:, 2 + t0:2 + t0 + 512], bank(bu), reads=[rb[bu]], writes=[u.r], accum=True)
                    else:
                        k.cp("act", usv[:, :, 2:6], bank(bu, 128, 0, SR).rearrange("p (s j) -> p s j", j=4), reads=[rb[bu]], writes=[u.r], accum=True)
                    k.cp("act", vsb.t[:, t0:t0 + ncol], bank(bv, 128, 0, ncol), reads=[rb[bv]], writes=[vsb.r], accum=(n > 0))
                lo, hi = 2, UW
                k.act(acc.t[:, lo:hi], u.t[:, lo:hi], AF.Identity, reads=[u.r, ffp.r], writes=[acc.r], scale=w2, bias=bb)
                k.stt(acc.t[:, lo:hi], u.t[:, lo - 1:hi - 1], w1, acc.t[:, lo:hi], ALU.mult, ALU.add, reads=[u.r, acc.r, ffp.r], writes=[acc.r])
                k.stt(acc.t[:, lo:hi], u.t[:, lo - 2:hi - 2], w0, acc.t[:, lo:hi], ALU.mult, ALU.add, reads=[u.r, acc.r, ffp.r], writes=[acc.r])
                k.act(acc.t[:, lo:hi], acc.t[:, lo:hi], AF.Gelu, reads=[acc.r], writes=[acc.r])
                k.tt("dve", gT.t[:, c, 0:T], acc.t[:, 2:2 + T], vsb.t[:, 0:T], ALU.mult, reads=[acc.r, vsb.r], writes=[gT.r], accum=(c > 0))
                k.tt("dve", gT.t[:, c, T:T + SR].rearrange("p (s j) -> p s j", j=4), asv[:, :, 2:6],
                     vsb.t[:, T:T + SR].rearrange("p (s j) -> p s j", j=4), ALU.mult, reads=[acc.r, vsb.r], writes=[gT.r], accum=True)
                k.cp("pool", fo.t[:, fc, :], u.t[:, T:T + 2], reads=[u.r], writes=[fo.r], accum=True)
                k.cp("pool", fs.t[:, fc, :, :], usv[:, :, 4:6], reads=[u.r], writes=[fs.r], accum=True)
            for t in range(NT + 1):
                rows = 128 if t < NT else SR
                bp = 4 + 2 * (t % 2)
                wd = wds if t < NT else wdu
                for half in range(2):
                    for c in range(G):
                        k.mm(bank(bp + half, rows), gT.t[:, c, t * 128:t * 128 + rows], wd.t[:, c, half * 512:(half + 1) * 512],
                             c == 0, c == G - 1, reads=[gT.r, wd.r], writes=[rb[bp + half]])
                yv = ps[0:rows, bp * 512:bp * 512 + D]
                if t < NT:
                    k.tt("dve", x_all[:, t, :], x_all[:, t, :], yv, ALU.add, reads=[rx[t], rb[bp], rb[bp + 1]], writes=[rx[t]])
                else:
                    rs_ = a_p.next()
                    k.tt("dve", rs_.t[0:SR, 0:D], yv, g2s.t[:], ALU.mult, reads=[rb[bp], rb[bp + 1], g2s.r], writes=[rs_.r])
                    k.tt("dve", x_s.t[:], x_s.t[:], rs_.t[0:SR, 0:D], ALU.add, reads=[x_s.r, rs_.r], writes=[x_s.r])
        k.release()
        k.mark()
        fo_tok = k.buf([2, DFF], F32, "fo_tok"); fs_tok = k.buf([2 * NS, DFF], F32, "fs_tok")
        for c0 in range(0, NFC, 4):
            c1 = min(NFC, c0 + 4)
            for c in range(c0, c1):
                k.tr(bank(0, 2, (c - c0) * 128, (c - c0 + 1) * 128), fo.t[:, c, :], ident[:], reads=[fo.r, rconst], writes=[rb[0]], accum=(c > c0))
                k.tr(bank(1, 2 * NS, (c - c0) * 128, (c - c0 + 1) * 128), fs.t[:, c, :, :].rearrange("p s j -> p (s j)"), ident[:],
                     reads=[fs.r, rconst], writes=[rb[1]], accum=(c > c0))
            k.cp("act", fo_tok.t[:, c0 * 128:c1 * 128], bank(0, 2, 0, (c1 - c0) * 128), reads=[rb[0]], writes=[fo_tok.r], accum=(c0 > 0))
            k.cp("act", fs_tok.t[:, c0 * 128:c1 * 128], bank(1, 2 * NS, 0, (c1 - c0) * 128), reads=[rb[1]], writes=[fs_tok.r], accum=(c0 > 0))
        k.dma("sp", fpo[l], fo_tok.t[:], reads=[fo_tok.r], is_output=True)
        k.dma("sp", fso[l], fs_tok.t[:], reads=[fs_tok.r], is_output=True)
        lng, lnb = load_ln(2 * l + 1)
        ln_all_tiles(lng, lnb, final)
        k.release()
        k.release()

    def conformer(mods_mix, g1p):
        k.mark()
        w1 = k.buf([128, 8, 2048], BF16, "w1"); w2 = k.buf([128, 8, D], BF16, "w2")
        cf = k.buf([128, 8, 36], F32, "cf")
        k.mark()
        stg = k.pool(2, [128, 8 * 512], F32, "stg")
        for g in range(4):
            load_w_bf16(w1.t[:, :, g * 512:(g + 1) * 512], w1.r, pw1[:, g * 512:(g + 1) * 512].rearrange("(kc p) n -> p kc n", p=128),
                        stg.next(), first=(g == 0))
        for g in range(2):
            load_w_bf16(w2.t[:, :, g * 512:(g + 1) * 512], w2.r, pw2[:, g * 512:(g + 1) * 512].rearrange("(kc p) n -> p kc n", p=128),
                        stg.next(), first=(g == 0))
        p36 = k.buf([36, D], F32, "p36")
        k.dma("sp", p36.t[:], cfp_d, writes=[p36.r])
        for c in range(8):
            k.tr(bank(0, 128, c * 36, c * 36 + 36), p36.t[:, c * 128:(c + 1) * 128], ident[0:36, 0:36], reads=[p36.r, rconst],
                 writes=[rb[0]], accum=(c > 0))
        k.cp("act", cf.t[:], bank(0, 128, 0, 288).rearrange("p (c j) -> p c j", j=36), reads=[rb[0]], writes=[cf.r])
        k.release()
        b2 = k.buf([128, D], F32, "b2")
        k.dma("sp", b2.t[:], b_pw2.partition_broadcast(128), writes=[b2.r])
        lng, lnb = load_ln(2)
        wk = ln_work(1)
        BS = 256
        NB = T // BS
        TPB = BS // 128
        GW = 34 * NS
        sconv_p = k.pool(1, [120, 4, 128], F32, "sconv_sb")
        carry = k.buf([128, 8, 30], F32, "carry")
        k.memset("pool", carry.t[:], 0.0, writes=[carry.r])
        hTb = k.buf([128, 8, BS], BF16, "hTb")
        yc = k.buf([128, 8, BS], F32, "yc")
        sT = k.buf([128, 8, BS], BF16, "sT")
        glu_p = k.pool(2, [128, GW], F32, "glu"); sig_p = k.pool(2, [128, BS], F32, "sig")
        glub_p = k.pool(2, [128, GW], BF16, "glub")
        dg_p = k.pool(2, [128, 31, 128], BF16, "dg")
        ycb_p = k.pool(2, [128, BS], BF16, "ycb"); ysq_p = k.pool(2, [128, BS], BF16, "ysq")
        mean = k.buf([128, BS], F32, "mean"); rstd = k.buf([128, BS], F32, "rstd"); xn_p = k.pool(1, [128, BS], F32, "cxn")
        gnew = k.buf([128, 8, SR], F32, "gnew")
        for B in range(NB + 1):
            prompt = B < NB
            ncol = BS if prompt else SR
            if prompt:
                for j in range(TPB):
                    t = TPB * B + j
                    make_hT(x_all[:, t, :], rx[t], 128, hTb.t[:, :, j * 128:(j + 1) * 128], hTb.r, 1, 0, pbanks=(2, 3), first=(j == 0))
            else:
                make_hT(x_s.t[:], x_s.r, SR, hTb.t[:, :, 0:SR], hTb.r, 1, 0, mods_s=mods_mix.t, mods_reg=mods_mix.r, wk=wk, pbanks=(2, 3))
            NO = BS if prompt else GW - 30
            s1b = 6 if prompt else 0
            for c in range(8):
                for kc in range(8):
                    k.mm(bank(4, 128, 0, ncol), w1.t[:, kc, c * 128:(c + 1) * 128], hTb.t[:, kc, 0:ncol], kc == 0, kc == 7,
                         reads=[w1.r, hTb.r], writes=[rb[4]])
                for kc in range(8):
                    k.mm(bank(5, 128, 0, ncol), w1.t[:, kc, D + c * 128:D + (c + 1) * 128], hTb.t[:, kc, 0:ncol], kc == 0, kc == 7,
                         reads=[w1.r, hTb.r], writes=[rb[5]])
                sig = sig_p.next(); glu = glu_p.next()
                k.act(sig.t[:, 0:ncol], bank(5, 128, 0, ncol), AF.Sigmoid, reads=[rb[5], cf.r], writes=[sig.r], bias=cf.t[:, c, 35:36])
                if prompt:
                    k.cp("pool", glu.t[:, 0:30], carry.t[:, c, :], reads=[carry.r], writes=[glu.r])
                    k.stt(glu.t[:, 30:30 + BS], bank(4, 128, 0, BS), cf.t[:, c, 34:35], sig.t[:, 0:BS], ALU.add, ALU.mult,
                          reads=[rb[4], cf.r, sig.r], writes=[glu.r], accum=True)
                    k.cp("pool", carry.t[:, c, :], glu.t[:, BS:BS + 30], reads=[glu.r], writes=[carry.r])
                else:
                    gv = glu.t[:, 0:GW].rearrange("p (s j) -> p s j", j=34)
                    scv = sconv_p.next()
                    for q in range(4):
                        k.dma("sp", scv.t[:, q, :], sconv[q * 120:(q + 1) * 120, c * 128:(c + 1) * 128], writes=[scv.r], accum=(q > 0))
                    for q in range(4):
                        k.tr(bank(6, 128, q * 120, (q + 1) * 120), scv.t[:, q, :], ident[0:120, 0:120],
                             reads=[scv.r, rconst], writes=[rb[6]], accum=(q > 0))
                    k.cp("act", gv[:, :, 0:30], bank(6, 128, 0, 480).rearrange("p (s j) -> p s j", j=30), reads=[rb[6]], writes=[glu.r])
                    k.stt(gv[:, :, 30:34], bank(4, 128, 0, SR).rearrange("p (s j) -> p s j", j=4), cf.t[:, c, 34:35],
                          sig.t[:, 0:SR].rearrange("p (s j) -> p s j", j=4), ALU.add, ALU.mult, reads=[rb[4], cf.r, sig.r], writes=[glu.r], accum=True)
                    k.cp("pool", gnew.t[:, c, :].rearrange("p (s j) -> p s j", j=4), gv[:, :, 30:34], reads=[glu.r], writes=[gnew.r], accum=(c > 0))
                dg = dg_p.next()
                k.tt("dve", dg.t[:], bc(identb_t[:], 1, 31), bc(cf.t[:, c, 0:31], 2, 128), ALU.mult, reads=[rconst, cf.r], writes=[dg.r])
                glub = glub_p.next()
                W = 30 + BS if prompt else GW
                k.cp("act", glub.t[:, 0:W], glu.t[:, 0:W], reads=[glu.r], writes=[glub.r])
                cb = (c % 2) if prompt else 1
                for j in range(31):
                    if prompt:
                        rhs = glub.t[:, j:j + BS]
                    else:
                        rhs = glub.t[:, 0:GW].rearrange("p (s j) -> p s j", j=34)[:, :, j:j + 4]
                    k.mm(bank(cb, 128, 0, ncol), dg.t[:, j, :], rhs, j == 0, j == 30, reads=[dg.r, glub.r], writes=[rb[cb]])
                k.act(yc.t[:, c, 0:ncol], bank(cb, 128, 0, ncol), AF.Identity, reads=[rb[cb], cf.r], writes=[yc.r], accum=(c > 0),
                      bias=cf.t[:, c, 31:32])
                ycb = ycb_p.next(); ysq = ysq_p.next()
                k.cp("dve", ycb.t[:, 0:ncol], yc.t[:, c, 0:ncol], reads=[yc.r], writes=[ycb.r])
                k.act(ysq.t[:, 0:ncol], yc.t[:, c, 0:ncol], AF.Square, reads=[yc.r], writes=[ysq.r])
                k.mm(bank(s1b, 128, 0, ncol), onesb[:], ycb.t[:, 0:ncol], c == 0, c == 7, reads=[rconst, ycb.r], writes=[rb[s1b]], inc=True)
                k.mm(bank(7, 128, 0, ncol), onesb[:], ysq.t[:, 0:ncol], c == 0, c == 7, reads=[rconst, ysq.r], writes=[rb[7]], inc=True)
            k.act(mean.t[:, 0:ncol], bank(s1b, 128, 0, ncol), AF.Identity, reads=[rb[s1b]], writes=[mean.r], scale=1.0 / D)
            k.tt("dve", rstd.t[:, 0:ncol], mean.t[:, 0:ncol], mean.t[:, 0:ncol], ALU.mult, reads=[mean.r], writes=[rstd.r])
            k.stt(rstd.t[:, 0:ncol], bank(7, 128, 0, ncol), 1.0 / D, rstd.t[:, 0:ncol], ALU.mult, ALU.subtract, reads=[rb[7], rstd.r], writes=[rstd.r])
            k.ts("dve", rstd.t[:, 0:ncol], rstd.t[:, 0:ncol], LN_EPS, None, ALU.add, reads=[rstd.r], writes=[rstd.r])
            k.act(rstd.t[:, 0:ncol], rstd.t[:, 0:ncol], AF.Sqrt, reads=[rstd.r], writes=[rstd.r])
            k.op("dve", lambda e, ncol=ncol: e.reciprocal(rstd.t[:, 0:ncol], rstd.t[:, 0:ncol]), reads=[rstd.r], writes=[rstd.r])
            for c in range(8):
                xn = xn_p.next()
                k.tt("dve", xn.t[:, 0:ncol], yc.t[:, c, 0:ncol], mean.t[:, 0:ncol], ALU.subtract, reads=[yc.r, mean.r], writes=[xn.r])
                k.tt("dve", xn.t[:, 0:ncol], xn.t[:, 0:ncol], rstd.t[:, 0:ncol], ALU.mult, reads=[xn.r, rstd.r], writes=[xn.r])
                k.act(sT.t[:, c, 0:ncol], xn.t[:, 0:ncol], AF.Silu, reads=[xn.r, cf.r], writes=[sT.r], accum=(c > 0),
                      scale=cf.t[:, c, 32:33], bias=cf.t[:, c, 33:34])
            for j in range(TPB if prompt else 1):
                rows = 128 if prompt else SR
                t = TPB * B + j
                bp = 4 * (j % 2) if prompt else 2
                for half in range(2):
                    for c in range(8):
                        k.mm(bank(bp + half, rows), sT.t[:, c, j * 128:j * 128 + rows], w2.t[:, c, half * 512:(half + 1) * 512], c == 0, c == 7,
                             reads=[sT.r, w2.r], writes=[rb[bp + half]])
                if prompt:
                    xt_ap, xt_reg, gate_ap, gate_reg = x_all[:, t, :], rx[t], g1p.t[:], g1p.r
                else:
                    xt_ap, xt_reg, gate_ap, gate_reg = x_s.t[:], x_s.r, mods_mix.t[:, 2 * D:3 * D], mods_mix.r
                post_norm(ps[0:rows, bp * 512:bp * 512 + D], [rb[bp], rb[bp + 1]], xt_ap, xt_reg, rows, gate_ap, gate_reg,
                          lng.t, lnb.t, lng.r, wk, bias_ap=b2.t, bias_reg=b2.r, defer_ln=True)
            if B == NB - 1:
                cp_tok = wk["xn"].next()
                for c in range(8):
                    k.tr(bank(2 + c // 4, 30, (c % 4) * 128, (c % 4 + 1) * 128), carry.t[:, c, :], ident[:], reads=[carry.r, rconst],
                         writes=[rb[2 + c // 4]], accum=(c % 4 > 0))
                k.cp("act", cp_tok.t[0:30, :], ps[0:30, 2 * 512:2 * 512 + D], reads=[rb[2], rb[3]], writes=[cp_tok.r])
                k.dma("sp", cpo, cp_tok.t[0:30, :], reads=[cp_tok.r], is_output=True)
        ln_all_tiles(lng, lnb, False)
        r_cso = Reg()
        k.dma("sp", cso.rearrange("(s j) f -> s j f", j=30)[:, 0:26, :], sconv.rearrange("(s j) f -> s j f", j=30)[:, 4:30, :],
              reads=[], writes=[r_cso], is_output=True)
        cs_tok = wk["xn"].next()
        for c in range(8):
            k.tr(bank(2 + c // 4, SR, (c % 4) * 128, (c % 4 + 1) * 128), gnew.t[:, c, :], ident[:], reads=[gnew.r, rconst],
                 writes=[rb[2 + c // 4]], accum=(c % 4 > 0))
        k.cp("act", cs_tok.t[0:SR, :], ps[0:SR, 2 * 512:2 * 512 + D], reads=[rb[2], rb[3]], writes=[cs_tok.r])
        for sq in range(NS):
            k.dma("sp", cso[sq * 30 + 26:sq * 30 + 30, :], cs_tok.t[sq * 4:sq * 4 + 4, :], reads=[cs_tok.r], is_output=True)
        k.release()

    k.limit = k.sb_top
    k.mark()
    rdm = cload([128, 4, 128], rdm_d); rqd = cload([128, 4, 128], rqd_d); rkd = cload([128, 4], rkd_d)
    rdms = cload([64, 4, 64], rdms_d); rqds = cload([128, 4, 64], rqds_d); rkds = cload([64, 4], rkds_d)
    blk = cload([64, 16], blk_d); tri = cload([128, 128], tri_d); nmask = cload([64, 16, 16], nmask_d)
    idx = k.buf([128, NS * 16], I32)
    pti = cload([128, NS * 16], ptd.partition_broadcast(128), dt=I32)
    idxf = k.sb([128, NS * 16], F32)
    k.cp("dve", idxf[:], pti[:], reads=[rconst], writes=[idx.r])
    k.stt(idxf[:], idxf[:], 128.0, iota[:].broadcast_to([128, NS * 16]), ALU.mult, ALU.add, reads=[rconst, idx.r], writes=[idx.r])
    k.cp("dve", idx.t[:], idxf[:], reads=[idx.r], writes=[idx.r])
    mods_mix0 = k.buf([SR, 3 * D], F32, "mods_mix")
    g1p0 = k.buf([128, D], F32, "g1p")
    with nc.named_scope("adaln0"):
        adaln(0, mods_mix0, g1p0)
    omT = k.buf([128, 4, T + SR], BF16, "omT")
    orT = k.buf([128, 4, T + SR], BF16, "orT")
    with nc.named_scope("passM"):
        pass_moba(mods_mix0, omT)
    if stage >= 4:
        with nc.named_scope("passR"):
            pass_ret(mods_mix0, orT)
    k.limit = XOFF
    if stage >= 5:
        with nc.named_scope("passO"):
            pass_out(mods_mix0, g1p0, orT, omT)
    k.release()
    if stage >= 6:
        with nc.named_scope("ffn0"):
            ffn(0, final=False)
    if stage >= 7:
        k.mark()
        mods_mix1 = k.buf([SR, 3 * D], F32, "mods_mix")
        g1p1 = k.buf([128, D], F32, "g1p")
        with nc.named_scope("adaln1"):
            adaln(1, mods_mix1, g1p1)
        with nc.named_scope("conformer"):
            conformer(mods_mix1, g1p1)
        k.release()
    if stage >= 8:
        with nc.named_scope("ffn1"):
            ffn(1, final=True)
    k.finish()
    print("instructions:", k.ninst, "sems:", k.nsem, "sbuf_off:", k.sb_off)
    return nc


def _consts():
    f32 = np.float32
    c = {}
    c["ident"] = np.eye(128, dtype=f32)
    c["iota"] = np.arange(128, dtype=f32).reshape(128, 1)
    theta = f32(10000.0)
    inv_m = (theta ** (-np.arange(0, 128, 2, dtype=f32) / f32(128))).astype(f32)
    inv_r = (f32(1.0) / (theta ** np.linspace(0.0, 1.0, 64, dtype=f32))).astype(f32)
    rot = np.zeros((17, 128, 4, 128), f32)
    for t in range(17):
        if t < 16:
            pos = (t * 128 + np.arange(128)).astype(f32)
        else:
            pos = (2048 + (np.arange(128) % 4)).astype(f32)
        am = (pos[:, None] * inv_m[None, :]).astype(f32)
        ar = (pos[:, None] * inv_r[None, :]).astype(f32)
        cm, sm = np.cos(am).astype(f32), np.sin(am).astype(f32)
        cr, sr = np.cos(ar).astype(f32), np.sin(ar).astype(f32)
        rot[t, :, 0, 0::2] = cr; rot[t, :, 0, 1::2] = cr
        rot[t, :, 1, 0::2] = -sr; rot[t, :, 1, 1::2] = sr
        rot[t, :, 2, 0:64] = cm; rot[t, :, 2, 64:128] = cm
        rot[t, :, 3, 0:64] = -sm; rot[t, :, 3, 64:128] = sm
    c["rot"] = rot
    lg = np.array(LOGG, dtype=np.float64)
    i = np.arange(128, dtype=np.float64)
    rdm = np.zeros((128, 4, 128), np.float64)
    for h in range(4):
        diff = i[None, :] - i[:, None]
        rdm[:, h, :] = np.where(diff >= 0, np.exp(np.maximum(diff, 0) * lg[h]), 0.0) * SCALE
    c["rdm"] = rdm.astype(f32)
    c["rqd"] = np.broadcast_to(np.exp((i[None, None, :] + 1.0) * lg[None, :, None]), (128, 4, 128)).astype(f32).copy()
    c["rkd"] = (np.exp((127.0 - i)[:, None] * lg[None, :]) * SCALE).astype(f32)
    r = np.arange(64)
    seq, ii = r // 4, (r % 4).astype(np.float64)
    rdms = np.zeros((64, 4, 64), np.float64)
    for h in range(4):
        diff = ii[None, :] - ii[:, None]
        same = seq[None, :] == seq[:, None]
        rdms[:, h, :] = np.where(same & (diff >= 0), np.exp(np.maximum(diff, 0) * lg[h]), 0.0) * SCALE
    c["rdms"] = rdms.astype(f32)
    c["rqds"] = np.broadcast_to(np.exp((ii[None, None, :] + 1.0) * lg[None, :, None]), (128, 4, 64)).astype(f32).copy()
    c["rkds"] = (np.exp((3.0 - ii)[:, None] * lg[None, :]) * SCALE).astype(f32)
    blk = np.zeros((64, 16), f32)
    blk[r, seq] = 1.0
    c["blk"] = blk
    tri = np.where(np.arange(128)[None, :] <= np.arange(128)[:, None], 0.0, NEG).astype(f32)
    c["tri"] = tri
    nm = np.full((64, 16, 16), NEG, f32)
    for b in range(16):
        for tq in range(4):
            for kk in range(tq + 1):
                nm[b * 4 + kk, b, tq::4] = 0.0
    c["nmask"] = nm
    Ep = np.zeros((18, 128), f32); Ep[0, :] = 1.0; Ep[17, :] = 1.0
    Es = np.zeros((18, 64), f32); Es[17, :] = 1.0
    for s in range(16):
        Es[1 + s, 4 * s:4 * s + 4] = 1.0
    ep = np.zeros((18, 1), f32); ep[0, 0] = 1.0; ep[17, 0] = 1.0
    c["Ep"], c["Es"], c["ep"] = Ep, Es, ep
    return c


def make_in_maps(x_prompt, x_sample, cache_k, cache_v, state_ret, state_conv, state_ffn, page_table, c_prompt, c_sample,
                 ab_w_in, ab_w_out, cf_w_pw1, cf_b_pw1, cf_w_dw, cf_b_dw, cf_ln_g, cf_ln_b, cf_w_pw2, cf_b_pw2,
                 ffn_w_up, ffn_w_dw, ffn_b_dw, ffn_w_down, ada_w, ada_b, ln_g, ln_b):
    A = lambda a: np.ascontiguousarray(np.asarray(a))
    consts = _consts()
    ck = A(cache_k).reshape(-1, 512)
    cv = A(cache_v).reshape(-1, 512)
    cfp = A(np.concatenate([np.asarray(cf_w_dw)[0], np.asarray(cf_b_dw)[0][None], np.asarray(cf_ln_g)[0][None],
                            np.asarray(cf_ln_b)[0][None], np.asarray(cf_b_pw1)[0].reshape(2, D)], axis=0))
    ffp = A(np.concatenate([np.asarray(ffn_w_dw), np.asarray(ffn_b_dw)[:, None, :]], axis=1))
    shared = {
        "ck": ck, "cv": cv, "w_in": A(ab_w_in)[0], "w_out": A(ab_w_out)[0], "pw1": A(cf_w_pw1)[0], "cfp": cfp,
        "pw2": A(cf_w_pw2)[0], "b_pw2": A(cf_b_pw2).reshape(1, D), "ffn_up": A(ffn_w_up), "ffp": ffp,
        "ffn_down": A(ffn_w_down), "ada_w": A(ada_w), "ada_b": A(ada_b), "ln_g": A(ln_g).reshape(4, D),
        "ln_b": A(ln_b).reshape(4, D),
    }
    shared.update(consts)
    maps = []
    for c in range(NCORES):
        s0, s1 = c * NS, (c + 1) * NS
        m = dict(shared)
        m["xp"] = A(x_prompt[c])
        m["xs"] = A(np.asarray(x_sample)[s0:s1].reshape(SR, D))
        m["call"] = A(np.concatenate([np.asarray(c_prompt)[c:c + 1], np.asarray(c_sample)[s0:s1]], axis=0))
        m["pt"] = A(np.asarray(page_table)[s0:s1].reshape(1, NS * 16).astype(np.int32))
        m["sret"] = A(np.asarray(state_ret)[0, s0:s1].reshape(NS * 512, 128))
        m["sconv"] = A(np.asarray(state_conv)[0, s0:s1].reshape(NS * 30, D))
        m["sffn"] = A(np.asarray(state_ffn)[:, s0:s1].reshape(2, NS * 2, DFF))
        maps.append(m)
    return maps


def assemble(results):
    R = results
    cat = lambda name: [r[name] for r in R]
    y_prompt = np.stack(cat("yp"), 0)
    y_sample = np.concatenate(cat("ys"), 0).reshape(128, 4, D)
    k_prompt = np.stack(cat("kp"), 0).reshape(1, 8, T, 4, 128)
    v_prompt = np.stack(cat("vp"), 0).reshape(1, 8, T, 4, 128)
    k_sample = np.concatenate(cat("ks"), 0).reshape(1, 128, 4, 4, 128)
    v_sample = np.concatenate(cat("vs"), 0).reshape(1, 128, 4, 4, 128)
    ret_prompt = np.stack(cat("rpo"), 0).reshape(1, 8, 4, 128, 128)
    ret_sample = np.concatenate(cat("rso"), 0).reshape(1, 128, 4, 128, 128)
    conv_prompt = np.stack(cat("cpo"), 0).reshape(1, 8, 30, D)
    conv_sample = np.concatenate(cat("cso"), 0).reshape(1, 128, 30, D)
    ffn_prompt = np.stack(cat("fpo"), 1).reshape(2, 8, 2, DFF)
    ffn_sample = np.concatenate([r["fso"].reshape(2, NS, 2, DFF) for r in R], 1)
    outs = (y_prompt, y_sample, k_prompt, v_prompt, k_sample, v_sample, ret_prompt, ret_sample,
            conv_prompt, conv_sample, ffn_prompt, ffn_sample)
    return tuple(np.ascontiguousarray(o, dtype=np.float32) for o in outs)


def kernel(**inputs):
    nc = build()
    maps = make_in_maps(**inputs)
    res = run_bass_kernel_spmd(nc, maps, core_ids=list(range(NCORES)))
    return assemble(res.results)
```
